# Optimizing a Trainium2 kernel written in Bass

```python
import math
import jax, jax.numpy as jnp
from jax import lax
import numpy as np

D_MODEL = 1024
BATCH = 16
SEQ = 4096
DEPTH = 2

N_MIXERS = 2
N_CONV_LAYERS = (DEPTH + 1) // 2
N_ATTN_LAYERS = DEPTH // 2
CONV_WIDTH = 3
DILATED_GROUPS = ((128, 1), (512, 4), (2048, 16))
N_DIL_GROUPS = len(DILATED_GROUPS)
ATTN_HEADS = 8
HEAD_DIM = 64
ATTN_OUT_DIM = ATTN_HEADS * HEAD_DIM
ATTN_IN_DIM = N_DIL_GROUPS * 3 * ATTN_OUT_DIM
ROT_DIM = HEAD_DIM // 4
ROPE_THETA = 500000.0
N_GROUPS = 4
EXPERTS_PER_GROUP = 8
N_EXPERTS = N_GROUPS * EXPERTS_PER_GROUP
TOP_K_EXPERTS = 2
D_EXPERT = 512
MOE_BLOCK = 512
NORM_EPS = 1e-6

kernel_name = "hybrid_shortconv_dilatedattn_hmoe_adaln"


def rms_norm(x, g):
    xf = x.astype(jnp.float32)
    y = xf * lax.rsqrt(jnp.mean(xf * xf, axis=-1, keepdims=True) + NORM_EPS)
    return (y * g.astype(jnp.float32)).astype(x.dtype)


def modulate(h, shift, scale):
    return h * (1.0 + scale[:, None, :]) + shift[:, None, :]


def short_conv_mixer(h, w_in, w_conv, w_out):
    d = h.shape[-1]
    b_gate, c_gate, u = jnp.split(h @ w_in, 3, axis=-1)
    z = c_gate * u
    zc = lax.conv_general_dilated(
        z, w_conv[:, None, :], window_strides=(1,), padding=[(CONV_WIDTH - 1, 0)],
        dimension_numbers=("NWC", "WIO", "NWC"), feature_group_count=d)
    return (b_gate * zc) @ w_out


def rotary_tables(seq):
    pos = jnp.arange(seq, dtype=jnp.float32)
    inv_freq = jnp.power(jnp.float32(ROPE_THETA),
                         -jnp.arange(0, ROT_DIM, 2, dtype=jnp.float32) / ROT_DIM)
    ang = pos[:, None] * inv_freq[None, :]
    return jnp.cos(ang)[None, :, None, :], jnp.sin(ang)[None, :, None, :]


def apply_partial_rope(t, cos, sin):
    half = ROT_DIM // 2
    rot = t[..., :ROT_DIM].astype(jnp.float32)
    x1, x2 = rot[..., :half], rot[..., half:]
    r = jnp.concatenate([x1 * cos - x2 * sin, x2 * cos + x1 * sin], axis=-1)
    return jnp.concatenate([r.astype(t.dtype), t[..., ROT_DIM:]], axis=-1)


def dilated_window_attention(q, k, v, dil, span):
    b, s, h, e = q.shape
    L = s // dil
    blk = span
    nb = -(-L // blk)
    pad = nb * blk - L

    def split(t):
        t = t.reshape(b, L, dil, h, e)
        t = jnp.pad(t, ((0, 0), (0, pad), (0, 0), (0, 0), (0, 0)))
        return t.reshape(b, nb, blk, dil, h, e)

    def banded(t):
        t_ext = jnp.pad(t, ((0, 0), (1, 0), (0, 0), (0, 0), (0, 0), (0, 0)))
        return jnp.concatenate([t_ext[:, :-1], t_ext[:, 1:]], axis=2)

    qb = split(q)
    kw = banded(split(k))
    vw = banded(split(v))
    scores = jnp.einsum("bnqrhe,bnkrhe->bnrhqk", qb, kw).astype(jnp.float32) * (HEAD_DIM ** -0.5)
    qi = jnp.arange(blk)[:, None]
    kj = jnp.arange(2 * blk)[None, :]
    dist = qi + blk - kj
    band = (dist >= 0) & (dist <= span)
    valid_start = (jnp.arange(nb)[:, None, None] > 0) | (kj[None] >= blk)
    mask = (band[None] & valid_start)[None, :, None, None]
    scores = jnp.where(mask, scores, -jnp.inf)
    m = jnp.max(scores, axis=-1, keepdims=True)
    p = jnp.exp(scores - m)
    den = jnp.sum(p, axis=-1, keepdims=True)
    lse = (m + jnp.log(den))[..., 0]
    out = jnp.einsum("bnrhqk,bnkrhe->bnqrhe", p / den, vw.astype(jnp.float32))
    out = out.reshape(b, nb * blk, dil, h, e)[:, :L].reshape(b, s, h, e)
    lse = jnp.transpose(lse, (0, 1, 4, 2, 3)).reshape(b, nb * blk, dil, h)[:, :L].reshape(b, s, h)
    return out, lse


def dilated_attention_mixer(h, w_in, w_out):
    b, s, _ = h.shape
    qkv = (h @ w_in).reshape(b, s, N_DIL_GROUPS, 3, ATTN_HEADS, HEAD_DIM)
    cos, sin = rotary_tables(s)
    outs, lses = [], []
    for g, (window, dil) in enumerate(DILATED_GROUPS):
        q = apply_partial_rope(qkv[:, :, g, 0], cos, sin)
        k = apply_partial_rope(qkv[:, :, g, 1], cos, sin)
        o, lse = dilated_window_attention(q, k, qkv[:, :, g, 2], dil, window // dil)
        outs.append(o)
        lses.append(lse)
    wts = jax.nn.softmax(jnp.stack(lses, axis=0), axis=0)
    o = jnp.einsum("gbshe,gbsh->bshe", jnp.stack(outs, axis=0), wts)
    return o.astype(h.dtype).reshape(b, s, ATTN_OUT_DIM) @ w_out


def expert_dispatch(hf, eid, wts, w_gate, w_up, w_down):
    t, d = hf.shape
    k = eid.shape[1]
    tk = t * k
    n_blocks = -(-tk // MOE_BLOCK) + N_EXPERTS
    rows = n_blocks * MOE_BLOCK
    flat_e = eid.reshape(-1)
    flat_tok = jnp.repeat(jnp.arange(t, dtype=jnp.int32), k)
    flat_w = wts.reshape(-1)
    order = jnp.argsort(flat_e)
    se, stok, sw = flat_e[order], flat_tok[order], flat_w[order]
    counts = jnp.bincount(flat_e, length=N_EXPERTS)
    starts = jnp.cumsum(counts) - counts
    pcounts = (counts + MOE_BLOCK - 1) // MOE_BLOCK * MOE_BLOCK
    pends = jnp.cumsum(pcounts)
    pstarts = pends - pcounts
    dest = pstarts[se] + (jnp.arange(tk) - starts[se])
    row_tok = jnp.full((rows,), t, dtype=jnp.int32).at[dest].set(stok)
    row_w = jnp.zeros((rows,), jnp.float32).at[dest].set(sw)
    block_e = jnp.clip(jnp.searchsorted(pends, jnp.arange(n_blocks) * MOE_BLOCK, side="right"),
                       0, N_EXPERTS - 1)
    hf_pad = jnp.concatenate([hf, jnp.zeros((1, d), hf.dtype)], axis=0)
    xs = hf_pad[row_tok].reshape(n_blocks, MOE_BLOCK, d)

    def expert_block(args):
        xb, e = args
        return (jax.nn.silu(xb @ w_gate[e]) * (xb @ w_up[e])) @ w_down[e]

    ys = lax.map(expert_block, (xs, block_e)).reshape(rows, d)
    out = jnp.zeros((t + 1, d), ys.dtype).at[row_tok].add(row_w[:, None].astype(ys.dtype) * ys)
    return out[:t]


def hierarchical_moe(h, w_grp, b_grp, w_exp_r, b_exp_r, w_gate, w_up, w_down):
    b, s, d = h.shape
    hf = h.reshape(b * s, d)
    hf32 = hf.astype(jnp.float32)
    grp_prob = jax.nn.softmax(hf32 @ w_grp.astype(jnp.float32) + b_grp.astype(jnp.float32), axis=-1)
    grp_p, grp_idx = lax.top_k(grp_prob, 1)
    exp_logits = jnp.einsum("td,gde->tge", hf32, w_exp_r.astype(jnp.float32)) \
        + b_exp_r.astype(jnp.float32)[None]
    sel = jnp.take_along_axis(exp_logits, grp_idx[:, :, None], axis=1)[:, 0]
    top_p, top_i = lax.top_k(jax.nn.softmax(sel, axis=-1), TOP_K_EXPERTS)
    wts = grp_p * top_p / jnp.sum(top_p, axis=-1, keepdims=True)
    eid = grp_idx * EXPERTS_PER_GROUP + top_i
    return expert_dispatch(hf, eid, wts, w_gate, w_up, w_down).reshape(b, s, d)


def setup_inputs(seed: int = 0) -> dict:
    key = jax.random.key(seed)
    ks = jax.random.split(key, 20)
    d = D_MODEL
    nrm = lambda k, shape, scale: jax.random.normal(k, shape, jnp.float32) * scale
    return {
        "x": nrm(ks[0], (BATCH, SEQ, d), 1.0),
        "c": nrm(ks[1], (BATCH, d), 1.0),
        "norm_mix_g": 1.0 + nrm(ks[2], (DEPTH, d), 0.02),
        "norm_ffn_g": 1.0 + nrm(ks[3], (DEPTH, d), 0.02),
        "ada_w": nrm(ks[4], (DEPTH, d, 6 * d), 0.5 * d ** -0.5),
        "ada_b": nrm(ks[5], (DEPTH, 6 * d), 0.02),
        "conv_in_w": nrm(ks[6], (N_CONV_LAYERS, d, 3 * d), d ** -0.5),
        "conv_w": nrm(ks[7], (N_CONV_LAYERS, CONV_WIDTH, d), CONV_WIDTH ** -0.5),
        "conv_out_w": nrm(ks[8], (N_CONV_LAYERS, d, d), d ** -0.5),
        "attn_in_w": nrm(ks[9], (N_ATTN_LAYERS, d, ATTN_IN_DIM), d ** -0.5),
        "attn_out_w": nrm(ks[10], (N_ATTN_LAYERS, ATTN_OUT_DIM, d), ATTN_OUT_DIM ** -0.5),
        "router_grp_w": nrm(ks[11], (DEPTH, d, N_GROUPS), d ** -0.5),
        "router_grp_b": nrm(ks[12], (DEPTH, N_GROUPS), 0.01),
        "router_exp_w": nrm(ks[13], (DEPTH, N_GROUPS, d, EXPERTS_PER_GROUP), d ** -0.5),
        "router_exp_b": nrm(ks[14], (DEPTH, N_GROUPS, EXPERTS_PER_GROUP), 0.01),
        "exp_gate_w": nrm(ks[15], (DEPTH, N_EXPERTS, d, D_EXPERT), d ** -0.5),
        "exp_up_w": nrm(ks[16], (DEPTH, N_EXPERTS, d, D_EXPERT), d ** -0.5),
        "exp_down_w": nrm(ks[17], (DEPTH, N_EXPERTS, D_EXPERT, d), D_EXPERT ** -0.5),
        "final_norm_g": 1.0 + nrm(ks[18], (d,), 0.02),
    }


def reference(x, c, norm_mix_g, norm_ffn_g, ada_w, ada_b, conv_in_w, conv_w, conv_out_w,
              attn_in_w, attn_out_w, router_grp_w, router_grp_b, router_exp_w, router_exp_b,
              exp_gate_w, exp_up_w, exp_down_w, final_norm_g):
    c_act = jax.nn.silu(c)
    for i in range(DEPTH):
        mod = c_act @ ada_w[i] + ada_b[i]
        sh1, sc1, g1, sh2, sc2, g2 = jnp.split(mod, 6, axis=-1)
        h = modulate(rms_norm(x, norm_mix_g[i]), sh1, sc1)
        j = i // N_MIXERS
        if i % N_MIXERS == 0:
            y = short_conv_mixer(h, conv_in_w[j], conv_w[j], conv_out_w[j])
        else:
            y = dilated_attention_mixer(h, attn_in_w[j], attn_out_w[j])
        x = x + g1[:, None, :] * y
        h = modulate(rms_norm(x, norm_ffn_g[i]), sh2, sc2)
        x = x + g2[:, None, :] * hierarchical_moe(
            h, router_grp_w[i], router_grp_b[i], router_exp_w[i], router_exp_b[i],
            exp_gate_w[i], exp_up_w[i], exp_down_w[i])
    return rms_norm(x, final_norm_g)
```

```python
import contextlib
import os
import numpy as np
import concourse.bass as bass
import concourse.mybir as mybir
from concourse.bass_utils import run_bass_kernel_spmd

F32 = mybir.dt.float32
BF16 = mybir.dt.bfloat16
I32 = mybir.dt.int32
AF = mybir.ActivationFunctionType
ALU = mybir.AluOpType
AX = mybir.AxisListType

D = 1024
NCORES = 8
ENGS = ("pe", "act", "dve", "pool", "sp")
SAME_ENG_SYNC = True
DYN_SKIP = False


class Buf:
    __slots__ = ("name", "lw", "rd")

    def __init__(self, name=""):
        self.name = name
        self.lw = None
        self.rd = {}


class DSem:
    def __init__(self, h):
        self.h = h
        self.groups = []


class Ins:
    __slots__ = ("eng", "fn", "dsem", "signal", "sig", "target", "deps", "gk", "scope", "guard")


class Sched:
    def __init__(self):
        self.L = {e: [] for e in ENGS}
        self.n = 0
        self.dsems = []
        self.scope = None
        self.trace_scopes = False
        self.guard = None


    def dsem(self, h):
        d = DSem(h)
        self.dsems.append(d)
        return d

    def add(self, eng, fn, reads=(), writes=(), dsem=None, grp=None):
        ins = Ins()
        ins.eng, ins.fn, ins.dsem = eng, fn, dsem
        ins.signal, ins.sig, ins.target = False, 0, 0
        ins.gk = (id(dsem), grp) if (dsem is not None and grp is not None) else None
        ins.scope = self.scope
        ins.guard = self.guard
        deps = {}

        def need(d, kind):
            if d.dsem is not None:
                return not (ins.gk is not None and d.gk == ins.gk)
            if d.eng == eng:
                if dsem is not None:
                    return True
                if eng == "pe":
                    return False
                return kind == "raw" and SAME_ENG_SYNC
            return True

        for b in reads:
            if b.lw is not None and need(b.lw, "raw"):
                deps[id(b.lw)] = b.lw
        for b in writes:
            if b.lw is not None and need(b.lw, "waw"):
                deps[id(b.lw)] = b.lw
            for r in b.rd.values():
                if need(r, "war"):
                    deps[id(r)] = r
        if dsem is not None:
            g = dsem.groups
            if g and grp is not None and g[-1][0] == grp:
                g[-1][1].append(ins)
            else:
                if g:
                    prev = g[-1][1][-1]
                    deps[id(prev)] = prev
                g.append((grp if grp is not None else object(), [ins]))
        ins.deps = list(deps.values())
        for d in ins.deps:
            d.signal = True
        for b in reads:
            key = eng if dsem is None else ("dma", self.n)
            b.rd[key] = ins
        for b in writes:
            b.lw = ins
            b.rd = {}
        self.L[eng].append(ins)
        self.n += 1
        return ins

    def barrier(self):
        lasts = []
        for e in ENGS:
            for ins in reversed(self.L[e]):
                if ins.fn is not None and ins.dsem is None:
                    lasts.append(ins)
                    break
        for ds in self.dsems:
            if ds.groups:
                lasts.append(ds.groups[-1][1][-1])
        for e in ENGS:
            ins = Ins()
            ins.eng, ins.fn, ins.dsem = e, None, None
            ins.signal, ins.sig, ins.target = False, 0, 0
            ins.gk = None
            ins.scope = self.scope
            ins.guard = None
            ins.deps = [d for d in lasts if not (d.dsem is None and d.eng == e)]
            for d in ins.deps:
                d.signal = True
            self.L[e].append(ins)

    def simulate(self):
        for e in ENGS:
            c = 0
            for ins in self.L[e]:
                if ins.dsem is None and ins.signal:
                    c += 1
                    ins.sig = c
        for ds in self.dsems:
            tot = 0
            for _, lst in ds.groups:
                tot += 16 * len(lst)
                for i in lst:
                    i.target = tot
        pc = {e: 0 for e in ENGS}
        ev = {e: 0 for e in ENGS}
        dv = {id(ds): 0 for ds in self.dsems}
        prog = True
        while prog:
            prog = False
            for e in ENGS:
                while pc[e] < len(self.L[e]):
                    ins = self.L[e][pc[e]]
                    ok = True
                    for d in ins.deps:
                        if d.dsem is not None:
                            if dv[id(d.dsem)] < d.target:
                                ok = False
                        elif ev[d.eng] < d.sig:
                            ok = False
                    if not ok:
                        break
                    if ins.fn is not None:
                        if ins.dsem is not None:
                            dv[id(ins.dsem)] += 16
                        elif ins.signal:
                            ev[e] += 1
                    pc[e] += 1
                    prog = True
        stuck = {e: (pc[e], len(self.L[e])) for e in ENGS if pc[e] < len(self.L[e])}
        return stuck

    def emit(self, nc, esem):
        for e in ENGS:
            c = 0
            for ins in self.L[e]:
                if ins.dsem is None and ins.signal:
                    c += 1
                    ins.sig = c
        for ds in self.dsems:
            tot = 0
            for _, lst in ds.groups:
                tot += 16 * len(lst)
                for i in lst:
                    i.target = tot

        def emit_one(e, eo, ins, seen):
            for d in ins.deps:
                if d.dsem is not None:
                    key, val, h = ("d", id(d.dsem)), d.target, d.dsem.h
                else:
                    key, val, h = ("e", d.eng), d.sig, esem[d.eng]
                if seen.get(key, 0) < val:
                    eo.wait_ge(h, val)
                    seen[key] = val
            if ins.fn is None:
                return
            r = ins.fn(eo)
            if ins.dsem is not None:
                r.then_inc(ins.dsem.h, 16)
            elif ins.signal:
                r.then_inc(esem[e], 1)

        def run(e, eo):
            seen = {}
            cur = [None, None]
            L_ = self.L[e]
            i = 0
            while i < len(L_):
                ins = L_[i]
                if self.trace_scopes and ins.scope != cur[0]:
                    if cur[1] is not None:
                        cur[1].__exit__(None, None, None)
                    cur[0] = ins.scope
                    cur[1] = nc.named_scope(f"{ins.scope}") if ins.scope else None
                    if cur[1] is not None:
                        cur[1].__enter__()
                if ins.guard is None:
                    emit_one(e, eo, ins, seen)
                    i += 1
                    continue
                g = ins.guard
                j = i
                while j < len(L_) and L_[j].guard is g and L_[j].scope == ins.scope:
                    j += 1
                region = L_[i:j]
                nsig = sum(1 for x in region if x.dsem is None and x.signal and x.fn is not None)
                dcount = {}
                for x in region:
                    if x.dsem is not None:
                        dcount[id(x.dsem)] = (x.dsem, dcount.get(id(x.dsem), (x.dsem, 0))[1] + 16)
                regs, thresh = g
                snap = dict(seen)
                with eo.If_lt(regs[e], thresh + 1):
                    for _ in range(nsig):
                        eo.nop(nofuse=True).then_inc(esem[e], 1)
                    for ds, n in dcount.values():
                        for _ in range(n // 16):
                            eo.nop(nofuse=True).then_inc(ds.h, 16)
                with eo.Else():
                    for x in region:
                        emit_one(e, eo, x, seen)
                seen = snap
                i = j
            if cur[1] is not None:
                cur[1].__exit__(None, None, None)

        with nc.Block() as block:
            @block.sync
            def _(eo):
                run("sp", eo)

            @block.tensor
            def _(eo):
                run("pe", eo)

            @block.scalar
            def _(eo):
                run("act", eo)

            @block.vector
            def _(eo):
                run("dve", eo)

            @block.gpsimd
            def _(eo):
                run("pool", eo)


class K:
    def __init__(self, NB, S, dbg=None):
        self.NB, self.S, self.T = NB, S, NB * S
        self.dbg = dbg
        self.nc = bass.Bass("TRN2", target_bir_lowering=False)
        self.ctx = contextlib.ExitStack()
        self.s = Sched()
        self.pctx = contextlib.ExitStack()
        self.nsem = 0
        self.dpool = []
        self.dcur = 0
        self._regs = None

    def din(self, name, shape, dt=F32):
        return self.nc.dram_tensor(name, list(shape), dt, kind="ExternalInput").ap()

    def dout(self, name, shape, dt=F32):
        return self.nc.dram_tensor(name, list(shape), dt, kind="ExternalOutput").ap()

    def dscr(self, name, shape, dt=F32):
        return self.nc.dram_tensor(name, list(shape), dt, kind="Internal").ap()

    def phase(self):
        self.s.barrier()
        self.pctx.close()
        self.pctx = contextlib.ExitStack()
        self.dcur = 0

    def sbp(self, name, shape, dt=F32):
        return self.pctx.enter_context(self.nc.sbuf_tensor("s_" + name, list(shape), dt))

    def psp(self, name, shape, dt=F32):
        return self.pctx.enter_context(self.nc.psum_tensor("p_" + name, list(shape), dt))

    def sb(self, name, shape, dt=F32):
        return self.ctx.enter_context(self.nc.sbuf_tensor("s_" + name, list(shape), dt))

    def ps(self, name, shape, dt=F32):
        return self.ctx.enter_context(self.nc.psum_tensor("p_" + name, list(shape), dt))

    def sem(self, name):
        self.nsem += 1
        return self.ctx.enter_context(self.nc.semaphore(name))

    def nused_regs(self):
        if self._regs is None:
            nc = self.nc
            eng = {"pe": nc.tensor, "act": nc.scalar, "dve": nc.vector, "pool": nc.gpsimd, "sp": nc.sync}
            self._regs = {e: self.ctx.enter_context(eng[e].register("nused_" + e)) for e in ENGS}
        return self._regs

    def dsem(self, name):
        if self.dcur < len(self.dpool):
            d = self.dpool[self.dcur]
        else:
            d = self.s.dsem(self.sem(f"dma{len(self.dpool)}"))
            self.dpool.append(d)
        self.dcur += 1
        return d

    def dma(self, eng, out, in_, dsem, reads=(), writes=(), grp=None, **kw):
        return self.s.add(eng, lambda e: e.dma_start(out=out, in_=in_, **kw), reads, writes, dsem=dsem, grp=grp)

    def mm(self, out, lhsT, rhs, start, stop, reads=(), writes=()):
        return self.s.add("pe", lambda e: e.matmul(out, lhsT=lhsT, rhs=rhs, start=start, stop=stop), reads, writes)

    def tr(self, out, in_, ident, reads=(), writes=()):
        return self.s.add("pe", lambda e: e.transpose(out=out, in_=in_, identity=ident), reads, writes)

    def op(self, eng, fn, reads=(), writes=()):
        return self.s.add(eng, fn, reads, writes)


def build(NB, S, stage="all", trace_scopes=False):
    k = K(NB, S)
    k.s.trace_scopes = trace_scopes
    nc, s = k.nc, k.s
    T = NB * S
    NT = T // 128
    NG = T // 512
    GPB = S // 512

    x_in = k.din("x", [T, D])
    cT_in = k.din("cT", [128, 8 * NB])
    ada_w = k.din("ada_w", [2, D, 6 * D])
    ada_b = k.din("ada_b", [2, 6 * D])
    gmixF = k.din("gmixF", [2, 128, 8])
    gffn = k.din("gffn", [2, D])
    conv_in_w = k.din("conv_in_w", [D, 3 * D])
    convwF = k.din("convwF", [128, 24])
    conv_out_w = k.din("conv_out_w", [D, D])
    ident_in = k.din("ident", [128, 128])
    tri_in = k.din("tri", [128, 128])
    Wr_in = k.din("Wr", [2, D, 36])
    br_in = k.din("br", [2, 36])
    exp_gate_w = k.din("exp_gate_w", [2 * 32 * 128, 8 * 512])
    exp_up_w = k.din("exp_up_w", [2 * 32 * 128, 8 * 512])
    exp_down_w = k.din("exp_down_w", [2 * 32 * 128, 4 * D])
    pcol_in = k.din("pcol", [128, 1])
    attn_in_w = k.din("attn_in_w", [D, 4608])
    attn_out_w = k.din("attn_out_w", [512, D])
    ropeC_in = k.din("ropeC", [128, S])
    ropeS_in = k.din("ropeS", [128, S])
    aconst_in = k.din("aconst", [128, 640])
    fing_in = k.din("fing", [1, D])
    out_ap = k.dout("out", [T, D])
    NBLK = (2 * T) // 512 + 32
    hs = k.dscr("hs", [T, D], BF16)
    xs = k.dscr("xs", [NBLK * 512, D], BF16)
    ys = k.dscr("ys", [NBLK * 512, D], F32)
    xr = k.dout("xr", [T, D])
    modrow = k.dout("modrow", [2, NB, 6 * D])

    ident_f = k.sb("ident_f", [128, 128], F32)
    ident_b = k.sb("ident_b", [128, 128], BF16)
    cT = k.sb("cT", [128, 8 * NB], F32)
    B_ident_f, B_ident_b, B_cT = Buf(), Buf(), Buf()
    B_modrow = [Buf() for _ in range(2)]

    ld0 = k.dsem("ld0")
    k.dma("sp", ident_f[:], ident_in[:, :], ld0, writes=[B_ident_f], grp="init")
    k.dma("sp", cT[:], cT_in[:, :], ld0, writes=[B_cT], grp="init")
    k.op("dve", lambda e: e.tensor_copy(out=ident_b[:], in_=ident_f[:]), [B_ident_f], [B_ident_b])
    k.op("act", lambda e: e.activation(out=cT[:], in_=cT[:], func=AF.Silu), [B_cT], [B_cT])

    s.scope = "A_mod"
    NSA = 3
    aw = [k.sbp(f"aw{i}", [128, 8, 512], F32) for i in range(NSA)]
    adab = [k.sbp(f"adab{i}", [NB, 512], F32) for i in range(NSA)]
    modc = [k.sbp(f"modc{i}", [NB, 512], F32) for i in range(NSA)]
    B_aw, B_adab, B_modc = [Buf() for _ in range(NSA)], [Buf() for _ in range(NSA)], [Buf() for _ in range(NSA)]
    aw_sem = [k.dsem(f"aw{i}") for i in range(NSA)]
    adab_sem = [k.dsem(f"adab{i}") for i in range(NSA)]
    mod_st = [k.dsem(f"mod_st{i}") for i in range(NSA)]
    ps_mod = [k.psp(f"ps_mod{i}", [128, 512], F32) for i in range(2)]
    B_psmod = [Buf(), Buf()]
    chunks = [(l, cc) for l in range(2) for cc in range(12)]

    def a_load(ci):
        l, cc = chunks[ci]
        sl = ci % NSA
        src_b = ada_b[l:l + 1, cc * 512:(cc + 1) * 512]
        k.dma("sp", adab[sl][:], src_b.partition_broadcast(NB) if NB > 1 else src_b, adab_sem[sl],
              writes=[B_adab[sl]])
        k.dma("sp", aw[sl][:], ada_w[l, :, cc * 512:(cc + 1) * 512].rearrange("(kc p) f -> p kc f", p=128),
              aw_sem[sl], writes=[B_aw[sl]])
    for ci in range(NSA - 1):
        a_load(ci)
    for ci, (l, cc) in enumerate(chunks):
        sl = ci % NSA
        pq = ci % 2
        if ci + NSA - 1 < len(chunks):
            a_load(ci + NSA - 1)
        for kc in range(8):
            k.mm(ps_mod[pq][0:NB, :], cT[:, kc * NB:(kc + 1) * NB], aw[sl][:, kc, :], kc == 0, kc == 7,
                 reads=[B_cT, B_aw[sl]], writes=[B_psmod[pq]])
        k.op("dve", lambda e, sl=sl, pq=pq: e.tensor_tensor(out=modc[sl][:], in0=ps_mod[pq][0:NB, :], in1=adab[sl][:],
                                                           op=ALU.add),
             [B_psmod[pq], B_adab[sl]], [B_modc[sl]])
        k.dma("pool", modrow[l, :, cc * 512:(cc + 1) * 512], modc[sl][:], mod_st[sl], reads=[B_modc[sl]],
              writes=[B_modrow[l]])
    k.phase()
    if stage == "A":
        return finish(k, [B_modrow[0], B_modrow[1]])

    s.scope = "L0_conv"
    L = 0
    w_in = k.sbp("w_in", [128, 8, 3 * D], BF16)
    w_stage = [k.sbp(f"w_stage{i}", [128, D], F32) for i in range(2)]
    w_out_b = [k.sbp(f"w_out_b{b}", [128, 8, D], BF16) for b in range(NB)]
    g1bc = [k.sbp(f"g1bc{b}", [128, D], F32) for b in range(NB)]
    A1 = [k.sbp(f"A1_{b}", [128, 8], F32) for b in range(NB)]
    sh1 = [k.sbp(f"sh1_{b}", [128, 8], F32) for b in range(NB)]
    sc1t = k.sbp("sc1t", [128, 8], F32)
    gmix = k.sbp("gmix", [128, 8], F32)
    convw = k.sbp("convw", [128, 24], F32)
    B_w_in, B_sc1t, B_gmix, B_convw = Buf(), Buf(), Buf(), Buf()
    B_w_stage = [Buf(), Buf()]
    B_g1bc = [Buf() for _ in range(NB)]
    B_w_out_b = [Buf() for _ in range(NB)]
    B_A1 = [Buf() for _ in range(NB)]
    B_sh1 = [Buf() for _ in range(NB)]
    wl = k.dsem("wl")
    wst = [k.dsem("wst0"), k.dsem("wst1")]
    vl = k.dsem("vl")
    for kc in range(8):
        k.dma("pool", w_in[:, kc, :], conv_in_w[kc * 128:(kc + 1) * 128, :], wl, writes=[B_w_in], grp="w_in",
              max_dma_last_dim=4096)
    k.dma("sp", gmix[:], gmixF[L], vl, writes=[B_gmix], grp="v0")
    k.dma("sp", convw[:], convwF[:, :], vl, writes=[B_convw], grp="v0")
    for b in range(NB):
        k.dma("sp", sh1[b][:], modrow[L, b, 0:D].rearrange("(c p) -> p c", p=128), vl,
              reads=[B_modrow[L]], writes=[B_sh1[b]], allow_slow_non_contiguous=True)
        k.dma("sp", sc1t[:], modrow[L, b, D:2 * D].rearrange("(c p) -> p c", p=128), vl,
              reads=[B_modrow[L]], writes=[B_sc1t], allow_slow_non_contiguous=True)
        k.op("dve", lambda e, b=b: e.scalar_tensor_tensor(out=A1[b][:], in0=sc1t[:], scalar=1.0, in1=gmix[:],
                                                         op0=ALU.add, op1=ALU.mult),
             [B_sc1t, B_gmix], [B_A1[b]])
        k.dma("sp", g1bc[b][:], modrow[L, b:b + 1, 2 * D:3 * D].partition_broadcast(128), vl,
              reads=[B_modrow[L]], writes=[B_g1bc[b]])
    for kc in range(8):
        sl = kc % 2
        k.dma("sp", w_stage[sl][:], conv_out_w[kc * 128:(kc + 1) * 128, :], wst[sl], writes=[B_w_stage[sl]])
        for b in range(NB):
            k.op("dve", lambda e, b=b, kc=kc, sl=sl: e.tensor_tensor(out=w_out_b[b][:, kc, :], in0=w_stage[sl][:],
                                                                    in1=g1bc[b][:], op=ALU.mult),
                 [B_w_stage[sl], B_g1bc[b]], [B_w_out_b[b]])

    if stage == "L0setup":
        return finish(k, [B_modrow[0], B_modrow[1]] + B_w_out_b + B_A1 + B_sh1 + [B_w_in, B_convw])
    NXS = 3
    xt = [k.sbp(f"xt{i}", [128, 4, D], F32) for i in range(NXS)]
    B_xt = [Buf() for _ in range(NXS)]
    xt_ld = [k.dsem(f"xt_ld{i}") for i in range(NXS)]
    xt_st = [k.dsem(f"xt_st{i}") for i in range(NXS)]
    junk = k.sbp("junk", [128, D], BF16)
    B_junk = Buf()
    ss = k.sbp("ss", [128, 4], F32)
    rstd = k.sbp("rstd", [128, 4], F32)
    B_ss, B_rstd = Buf(), Buf()
    xn = [k.sbp(f"xn{i}", [128, 4, D], BF16) for i in range(2)]
    B_xn = [[Buf() for _ in range(4)] for _ in range(2)]
    pT = k.psp("pT", [128, 512], BF16)
    B_pT = Buf()
    hT = [k.sbp(f"hT{i}", [128, 8, 512], BF16) for i in range(2)]
    B_hT = [[Buf() for _ in range(8)] for _ in range(2)]
    psBCU = [[k.psp(f"ps{n}{i}", [128, 512]) for n in "BCU"] for i in range(2)]
    B_psBCU = [[Buf() for _ in range(3)] for _ in range(2)]
    Csb = [k.sbp(f"Csb{i}", [128, 512], F32) for i in range(2)]
    B_Csb = [Buf(), Buf()]
    zb = [k.sbp(f"zb{i}", [128, 514], F32) for i in range(2)]
    B_zb = [Buf(), Buf()]
    zh = k.sbp("zh", [128, 8, 2], F32)
    B_zh = [Buf() for _ in range(8)]
    zc = [k.sbp(f"zc{i}", [128, 512], F32) for i in range(2)]
    B_zc = [Buf(), Buf()]
    gT = [k.sbp(f"gT{i}", [128, 8, 512], BF16) for i in range(2)]
    B_gT = [[Buf() for _ in range(8)] for _ in range(2)]
    psY = k.psp("psY", [128, 512])
    B_psY = Buf()
    B_xr = [Buf() for _ in range(NG)]

    def load_x(G):
        sl = G % NXS
        k.dma("sp", xt[sl][:], x_in[G * 512:(G + 1) * 512, :].rearrange("(j p) d -> p j d", p=128),
              xt_ld[sl], writes=[B_xt[sl]])

    def norm_pre(G):
        sl = G % 2
        xt_t, B_x = xt[G % NXS], B_xt[G % NXS]
        for j in range(4):
            k.op("act", lambda e, j=j: e.activation(out=junk[:], in_=xt_t[:, j, :], func=AF.Square,
                                                    accum_out=ss[:, j:j + 1]),
                 [B_x], [B_junk, B_ss])
        k.op("dve", lambda e: e.tensor_scalar(out=rstd[:], in0=ss[:], scalar1=1.0 / D, scalar2=1e-6,
                                              op0=ALU.mult, op1=ALU.add), [B_ss], [B_rstd])
        k.op("act", lambda e: e.activation(out=rstd[:], in_=rstd[:], func=AF.Sqrt), [B_rstd], [B_rstd])
        k.op("dve", lambda e: e.reciprocal(out=rstd[:], in_=rstd[:]), [B_rstd], [B_rstd])
        for j in range(4):
            k.op("pool", lambda e, j=j: e.tensor_scalar(out=xn[sl][:, j, :], in0=xt_t[:, j, :],
                                                        scalar1=rstd[:, j:j + 1], scalar2=1.0,
                                                        op0=ALU.mult, op1=ALU.mult),
                 [B_x, B_rstd], [B_xn[sl][j]])

    def norm_tr(G, c):
        b = G // GPB
        sl = G % 2
        A, B_A, sh, B_sh = A1[b], B_A1[b], sh1[b], B_sh1[b]
        for j in range(4):
            k.tr(pT[:, j * 128:(j + 1) * 128], xn[sl][:, j, c * 128:(c + 1) * 128], ident_b[:],
                 reads=[B_xn[sl][j], B_ident_b], writes=[B_pT])
        k.op("act", lambda e, c=c: e.activation(out=hT[sl][:, c, :], in_=pT[:], func=AF.Identity,
                                                scale=A[:, c:c + 1], bias=sh[:, c:c + 1]),
             [B_pT, B_A, B_sh], [B_hT[sl][c]])

    def inproj(G, fcs):
        sl = G % 2
        first = (G % GPB == 0)
        for fc in fcs:
            q = fc % 2
            (psB, psC, psU), (B_psB, B_psC, B_psU) = psBCU[q], B_psBCU[q]
            for (pst, B_p, off) in ((psB, B_psB, 0), (psC, B_psC, D), (psU, B_psU, 2 * D)):
                for kc in range(8):
                    k.mm(pst[:], w_in[:, kc, off + fc * 128: off + (fc + 1) * 128], hT[sl][:, kc, :], kc == 0, kc == 7,
                         reads=[B_w_in, B_hT[sl][kc]], writes=[B_p])
            zt, Bz, zcb, Bzc, Cs, BCs = zb[q], B_zb[q], zc[q], B_zc[q], Csb[q], B_Csb[q]
            if first:
                k.op("pool", lambda e, zt=zt: e.memset(zt[:, 0:2], 0.0), [], [Bz])
            else:
                k.op("pool", lambda e, zt=zt, fc=fc: e.tensor_copy(out=zt[:, 0:2], in_=zh[:, fc, :]),
                     [B_zh[fc]], [Bz])
            k.op("act", lambda e, Cs=Cs, psC=psC: e.copy(out=Cs[:], in_=psC[:]), [B_psC], [BCs])
            k.op("dve", lambda e, zt=zt, Cs=Cs, psU=psU: e.tensor_tensor(out=zt[:, 2:514], in0=Cs[:], in1=psU[:],
                                                                        op=ALU.mult),
                 [BCs, B_psU, Bz], [Bz])
            k.op("pool", lambda e, zt=zt, fc=fc: e.tensor_copy(out=zh[:, fc, :], in_=zt[:, 512:514]),
                 [Bz], [B_zh[fc]])
            k.op("pool", lambda e, fc=fc, zt=zt, zcb=zcb: e.tensor_scalar(
                out=zcb[:], in0=zt[:, 2:514], scalar1=convw[:, fc * 3 + 2: fc * 3 + 3], scalar2=1.0,
                op0=ALU.mult, op1=ALU.mult), [Bz, B_convw], [Bzc])
            k.op("dve", lambda e, fc=fc, zt=zt, zcb=zcb: e.scalar_tensor_tensor(
                out=zcb[:], in0=zt[:, 1:513], scalar=convw[:, fc * 3 + 1: fc * 3 + 2], in1=zcb[:],
                op0=ALU.mult, op1=ALU.add), [Bz, B_convw, Bzc], [Bzc])
            k.op("dve", lambda e, fc=fc, zt=zt, zcb=zcb: e.scalar_tensor_tensor(
                out=zcb[:], in0=zt[:, 0:512], scalar=convw[:, fc * 3: fc * 3 + 1], in1=zcb[:],
                op0=ALU.mult, op1=ALU.add), [Bz, B_convw, Bzc], [Bzc])
            k.op("dve", lambda e, fc=fc, zcb=zcb, psB=psB, sl=sl: e.tensor_tensor(out=gT[sl][:, fc, :], in0=zcb[:],
                                                                                 in1=psB[:], op=ALU.mult),
                 [Bzc, B_psB], [B_gT[sl][fc]])

    def outproj_unit(G, u):
        b = G // GPB
        sl = G % 2
        xs_ = G % NXS
        j, dh = u // 2, u % 2
        for kc in range(8):
            k.mm(psY[:], gT[sl][:, kc, j * 128:(j + 1) * 128], w_out_b[b][:, kc, dh * 512:(dh + 1) * 512],
                 kc == 0, kc == 7, reads=[B_gT[sl][kc], B_w_out_b[b]], writes=[B_psY])
        k.op("dve", lambda e: e.tensor_tensor(
            out=xt[xs_][:, j, dh * 512:(dh + 1) * 512], in0=psY[:], in1=xt[xs_][:, j, dh * 512:(dh + 1) * 512],
            op=ALU.add), [B_psY, B_xt[xs_]], [B_xt[xs_]])
        if u == 7:
            k.dma("sp", xr[G * 512:(G + 1) * 512, :].rearrange("(j p) d -> p j d", p=128), xt[xs_][:], xt_st[xs_],
                  reads=[B_xt[xs_]], writes=[B_xr[G]])

    load_x(0)
    if NG > 1:
        load_x(1)
    norm_pre(0)
    for c in range(8):
        norm_tr(0, c)
    for G in range(NG):
        if G + 1 < NG:
            norm_pre(G + 1)
        for fc in range(8):
            inproj(G, [fc])
            if G + 1 < NG:
                norm_tr(G + 1, fc)
            if G > 0:
                outproj_unit(G - 1, fc)
        if G + 2 < NG:
            load_x(G + 2)
    for u in range(8):
        outproj_unit(NG - 1, u)

    if stage == "mix0":
        return finish(k, [B_xr[G] for G in range(NG)])
    k.phase()
    dbg = k.dout("dbg", [128, 4096]) if stage.startswith("M") else None
    r = moe_layer(k, 0, dict(stage=stage, dbg=dbg, xr=xr, B_xr=B_xr, modrow=modrow, B_modrow=B_modrow, gffn=gffn, Wr_in=Wr_in, br_in=br_in,
                         tri_in=tri_in, ident_f=ident_f, B_ident_f=B_ident_f, ident_b=ident_b, B_ident_b=B_ident_b,
                         hs=hs, xs=xs, ys=ys, gw=exp_gate_w, uw=exp_up_w, dw=exp_down_w, NBLK=NBLK, pcol_in=pcol_in))
    if r is not None:
        return finish(k, [B_xr[G] for G in range(NG)] + r)
    if stage == "ffn0":
        return finish(k, [B_xr[G] for G in range(NG)])
    k.phase()
    attn_layer(k, dict(xr=xr, B_xr=B_xr, modrow=modrow, B_modrow=B_modrow, gmixF=gmixF, ident_b=ident_b,
                       B_ident_b=B_ident_b, w_in=attn_in_w, w_out=attn_out_w, ropeC=ropeC_in, ropeS=ropeS_in,
                       aconst=aconst_in, pcol_in=pcol_in))
    if stage == "mix1":
        return finish(k, [B_xr[G] for G in range(NG)])
    k.phase()
    B_out = [Buf() for _ in range(NG)]
    moe_layer(k, 1, dict(stage=stage, dbg=None, final=dict(fing=fing_in, out=out_ap, B_out=B_out), xr=xr, B_xr=B_xr, modrow=modrow, B_modrow=B_modrow, gffn=gffn,
                         Wr_in=Wr_in, br_in=br_in, tri_in=tri_in, ident_f=ident_f, B_ident_f=B_ident_f,
                         ident_b=ident_b, B_ident_b=B_ident_b, hs=hs, xs=xs, ys=ys, gw=exp_gate_w, uw=exp_up_w,
                         dw=exp_down_w, NBLK=NBLK, pcol_in=pcol_in))
    return finish(k, [B_out[G] for G in range(NG)])


DIL = (1, 4, 16)
import os
SKIP = os.environ.get("ATT_SKIP", "").split(",")


def attn_layer(k, a):
    L = 1
    NB, S, T = k.NB, k.S, k.T
    GPB = S // 512
    xr, B_xr, modrow, B_modrow = a["xr"], a["B_xr"], a["modrow"], a["B_modrow"]
    ident_b, B_ident_b = a["ident_b"], a["B_ident_b"]
    w_in, w_out = a["w_in"], a["w_out"]
    hT = k.sbp("a_hT", [128, 8, S], BF16)
    oT = k.sbp("a_oT", [128, 4, S], BF16)
    B_hT = [[Buf() for _ in range(GPB)] for _ in range(8)]
    B_oT = [Buf() for _ in range(4)]
    vl = k.dsem("a_vl")
    for b in range(NB):
        k.s.scope = f"att{b}_1_hT"
        with contextlib.ExitStack() as c1:
            def sb1(name, shape, dt=F32):
                return c1.enter_context(k.nc.sbuf_tensor(f"s_a1_{b}_{name}", list(shape), dt))
            A1, sh1, sc1t, gmix = sb1("A1", [128, 8]), sb1("sh1", [128, 8]), sb1("sc1t", [128, 8]), sb1("gmix", [128, 8])
            B_A1, B_sh1, B_sc1t, B_gmix = Buf(), Buf(), Buf(), Buf()
            k.dma("sp", gmix[:], a["gmixF"][L], vl, writes=[B_gmix])
            k.dma("sp", sh1[:], modrow[L, b, 0:D].rearrange("(c p) -> p c", p=128), vl,
                  reads=[B_modrow[L]], writes=[B_sh1], allow_slow_non_contiguous=True)
            k.dma("sp", sc1t[:], modrow[L, b, D:2 * D].rearrange("(c p) -> p c", p=128), vl,
                  reads=[B_modrow[L]], writes=[B_sc1t], allow_slow_non_contiguous=True)
            k.op("dve", lambda e: e.scalar_tensor_tensor(out=A1[:], in0=sc1t[:], scalar=1.0, in1=gmix[:],
                                                         op0=ALU.add, op1=ALU.mult), [B_sc1t, B_gmix], [B_A1])
            xt = [sb1(f"xt{i}", [128, 4, D]) for i in range(2)]
            B_xt = [Buf(), Buf()]
            xt_ld = [k.dsem(f"a1_{b}_xt_ld0"), k.dsem(f"a1_{b}_xt_ld1")]
            junk = sb1("junk", [128, D], BF16)
            ss, rstd = sb1("ss", [128, 4]), sb1("rstd", [128, 4])
            xn = sb1("xn", [128, 4, D], BF16)
            B_junk, B_ss, B_rstd = Buf(), Buf(), Buf()
            B_xn = [Buf() for _ in range(4)]
            pT = [c1.enter_context(k.nc.psum_tensor(f"p_a1_{b}_pT{i}", [128, 512], BF16)) for i in range(2)]
            B_pT = [Buf(), Buf()]
            def a1_load(gi):
                G = b * GPB + gi
                sl = gi % 2
                k.dma("sp", xt[sl][:], xr[G * 512:(G + 1) * 512, :].rearrange("(j p) d -> p j d", p=128), xt_ld[sl],
                      reads=[B_xr[G]], writes=[B_xt[sl]])
            a1_load(0)
            for gi in range(GPB):
                G = b * GPB + gi
                sl = gi % 2
                if gi + 1 < GPB:
                    a1_load(gi + 1)
                xt_t, B_x = xt[sl], B_xt[sl]
                for j in range(4):
                    k.op("act", lambda e, j=j, xt_t=xt_t: e.activation(out=junk[:], in_=xt_t[:, j, :], func=AF.Square,
                                                                      accum_out=ss[:, j:j + 1]), [B_x], [B_junk, B_ss])
                k.op("dve", lambda e: e.tensor_scalar(out=rstd[:], in0=ss[:], scalar1=1.0 / D, scalar2=1e-6,
                                                      op0=ALU.mult, op1=ALU.add), [B_ss], [B_rstd])
                k.op("act", lambda e: e.activation(out=rstd[:], in_=rstd[:], func=AF.Sqrt), [B_rstd], [B_rstd])
                k.op("dve", lambda e: e.reciprocal(out=rstd[:], in_=rstd[:]), [B_rstd], [B_rstd])
                for j in range(4):
                    k.op("pool", lambda e, j=j, xt_t=xt_t: e.tensor_scalar(out=xn[:, j, :], in0=xt_t[:, j, :],
                                                                          scalar1=rstd[:, j:j + 1], scalar2=1.0,
                                                                          op0=ALU.mult, op1=ALU.mult),
                         [B_x, B_rstd], [B_xn[j]])
                for c in range(8):
                    p = c % 2
                    for j in range(4):
                        k.tr(pT[p][:, j * 128:(j + 1) * 128], xn[:, j, c * 128:(c + 1) * 128], ident_b[:],
                             reads=[B_xn[j], B_ident_b], writes=[B_pT[p]])
                    k.op("act", lambda e, c=c, p=p, gi=gi: e.activation(
                        out=hT[:, c, gi * 512:(gi + 1) * 512], in_=pT[p][:], func=AF.Identity,
                        scale=A1[:, c:c + 1], bias=sh1[:, c:c + 1]), [B_pT[p], B_A1, B_sh1], [B_hT[c][gi]])
            k.s.barrier()
        k.s.scope = f"att{b}_2_core"
        with contextlib.ExitStack() as c2:
            def sb2(name, shape, dt=F32):
                return c2.enter_context(k.nc.sbuf_tensor(f"s_a2_{b}_{name}", list(shape), dt))

            def ps2(name, shape, dt=F32):
                return c2.enter_context(k.nc.psum_tensor(f"p_a2_{b}_{name}", list(shape), dt))
            Ct, St = sb2("Ct", [128, S], BF16), sb2("St", [128, S], BF16)
            acb = sb2("acb", [128, 640], BF16)
            B_Ct, B_St, B_acb = Buf(), Buf(), Buf()
            k.dma("pool", Ct[:], a["ropeC"][:, :], vl, writes=[B_Ct], max_dma_last_dim=4096)
            k.dma("pool", St[:], a["ropeS"][:, :], vl, writes=[B_St], max_dma_last_dim=4096)
            k.dma("pool", acb[:], a["aconst"][:, :], vl, writes=[B_acb])
            Rm, Mprev, Mcur = acb[:, 0:128], acb[:, 128:256], acb[:, 256:384]
            onesp = [acb[:, 384:512], acb[:, 512:640]]
            qz = [sb2(f"qz{h}", [128, S], BF16) for h in range(2)]
            kT = sb2("kT", [128, S], BF16)
            B_qz = [[Buf() for _ in range(GPB)] for _ in range(2)]
            B_kT = [Buf() for _ in range(GPB)]
            pc = sb2("pc", [128, 1])
            hm = sb2("hm", [128, 2])
            hmb = sb2("hmb", [128, 2], BF16)
            B_pc, B_hm = Buf(), Buf()
            k.dma("sp", pc[:], a["pcol_in"][:, :], vl, writes=[B_pc])
            k.op("dve", lambda e: e.tensor_scalar(out=hm[:, 0:1], in0=pc[:], scalar1=64.0, scalar2=None, op0=ALU.is_lt),
                 [B_pc], [B_hm])
            k.op("dve", lambda e: e.tensor_scalar(out=hm[:, 1:2], in0=pc[:], scalar1=64.0, scalar2=None, op0=ALU.is_ge),
                 [B_pc], [B_hm])
            k.op("dve", lambda e: e.tensor_copy(out=hmb[:], in_=hm[:]), [B_hm], [B_hm])
            NBK = S // 128
            Vp = sb2("Vp", [128, NBK, 2, 128], BF16)
            B_Vp = [Buf() for _ in range(NBK // 4)]
            k.op("pool", lambda e: e.memset(Vp[:].rearrange("p a h f -> p (a h f)"), 0.0), [], B_Vp)
            Nacc, Dacc = sb2("Nacc", [128, S]), sb2("Dacc", [128, S])
            B_Nacc, B_Dacc = Buf(), Buf()
            wq = [sb2(f"wq{i}", [128, 8, 384], BF16) for i in range(2)]
            B_wq = [Buf(), Buf()]
            wq_sem = [k.dsem(f"a2_{b}_wq0"), k.dsem(f"a2_{b}_wq1")]
            qsb_ = [sb2(f"qsb{i}", [128, 512], BF16) for i in range(2)]
            t1s, t2s = sb2("t1s", [128, 512]), sb2("t2s", [128, 512])
            t1_, t2_ = [t1s, t1s], [t2s, t2s]
            Bt1, Bt2 = Buf(), Buf()
            B_qsb_, B_t1_, B_t2_ = [Buf(), Buf()], [Bt1, Bt1], [Bt2, Bt2]
            PT = [sb2(f"PT{i}", [128, 2, 2, 128], BF16) for i in range(2)]
            B_PT = [Buf(), Buf()]
            psQ_ = [ps2(f"psQ{i}", [128, 512]) for i in range(2)]
            psR0 = ps2("psR0", [128, 512])
            psR_ = [psR0, psR0]
            psV = ps2("psV", [128, 4, 128])
            psS = [ps2(f"psS{i}", [128, 2, 2, 128]) for i in range(2)]
            psND = [ps2(f"psND{i}", [128, 2, 128]) for i in range(2)]
            BpsR = Buf()
            B_psQ_, B_psR_, B_psV = [Buf(), Buf()], [BpsR, BpsR], Buf()
            B_psS, B_psND = [Buf(), Buf()], [Buf(), Buf()]
            pit = 0
            allhT = [B_hT[c][gi] for c in range(8) for gi in range(GPB)]
            it = 0

            def wq_load(hp, g):
                wsl = (hp * 3 + g) % 2
                for qi in range(3):
                    col = g * 1536 + qi * 512 + hp * 128
                    for kc in range(8):
                        k.dma("pool", wq[wsl][:, kc, qi * 128:(qi + 1) * 128],
                              w_in[kc * 128:(kc + 1) * 128, col:col + 128], wq_sem[wsl], writes=[B_wq[wsl]],
                              grp=("wq", hp, g))
            wq_load(0, 0)
            for hp in range(4):
                for g in range(3):
                    dil = DIL[g]
                    nb = S // dil // 128
                    wsl = (hp * 3 + g) % 2
                    nxt = hp * 3 + g + 1
                    if nxt < 12:
                        wq_load(nxt // 3, nxt % 3)
                    for qi in range(2 if "qk" not in SKIP else 0):
                        for tg in range(GPB):
                            pi = pit % 2
                            pit += 1
                            psQ, psR, qsb, t1, t2 = psQ_[pi], psR_[pi], qsb_[pi], t1_[pi], t2_[pi]
                            B_psQ, B_psR, B_qsb, B_t1, B_t2 = B_psQ_[pi], B_psR_[pi], B_qsb_[pi], B_t1_[pi], B_t2_[pi]
                            for kc in range(8):
                                k.mm(psQ[:], wq[wsl][:, kc, qi * 128:(qi + 1) * 128], hT[:, kc, tg * 512:(tg + 1) * 512],
                                     kc == 0, kc == 7, reads=[B_wq[wsl], B_hT[kc][tg]], writes=[B_psQ])
                            k.op("act", lambda e, qsb=qsb, psQ=psQ: e.copy(out=qsb[:], in_=psQ[:]), [B_psQ], [B_qsb])
                            k.mm(psR[:], Rm, qsb[:], True, True, reads=[B_acb, B_qsb], writes=[B_psR])
                            k.op("dve", lambda e, tg=tg, t1=t1, psQ=psQ: e.tensor_tensor(
                                out=t1[:], in0=psQ[:], in1=Ct[:, tg * 512:(tg + 1) * 512], op=ALU.mult),
                                 [B_psQ, B_Ct, B_qsb], [B_t1])
                            k.op("dve", lambda e, tg=tg, t2=t2, psR=psR: e.tensor_tensor(
                                out=t2[:], in0=psR[:], in1=St[:, tg * 512:(tg + 1) * 512], op=ALU.mult),
                                 [B_psR, B_St], [B_t2])
                            if qi == 0:
                                k.op("pool", lambda e, t1=t1, t2=t2, qsb=qsb: e.tensor_tensor(
                                    out=qsb[:], in0=t1[:], in1=t2[:], op=ALU.add), [B_t1, B_t2, B_qsb], [B_qsb])
                                for h in range(2):
                                    k.op("dve", lambda e, h=h, tg=tg, qsb=qsb: e.tensor_scalar(
                                        out=qz[h][:, tg * 512:(tg + 1) * 512], in0=qsb[:], scalar1=hmb[:, h:h + 1],
                                        scalar2=None, op0=ALU.mult), [B_qsb, B_hm], [B_qz[h][tg]])
                            else:
                                k.op("pool", lambda e, tg=tg, t1=t1, t2=t2: e.tensor_tensor(
                                    out=kT[:, tg * 512:(tg + 1) * 512], in0=t1[:], in1=t2[:], op=ALU.add),
                                     [B_t1, B_t2], [B_kT[tg]])
                    def tok(r, n):
                        st = r + n * 128 * dil
                        return slice(st, st + 127 * dil + 1, dil)
                    blks = [(r, n) for r in range(dil) for n in range(nb)]
                    for bi, (r, n) in enumerate(blks if "vproj" not in SKIP else []):
                        for kc in range(8):
                            k.mm(psV[:, bi % 4, :], hT[:, kc, tok(r, n)], wq[wsl][:, kc, 256:384], kc == 0, kc == 7,
                                 reads=[B_wq[wsl]] + [B_hT[kc][gi] for gi in range(GPB)], writes=[B_psV])
                        if bi % 4 == 3:
                            b4 = bi // 4
                            k.op("dve", lambda e, b4=b4: e.tensor_copy(out=Vp[:, b4 * 4:(b4 + 1) * 4, 0, 0:64],
                                                                      in_=psV[:, :, 0:64]), [B_psV], [B_Vp[b4]])
                            k.op("dve", lambda e, b4=b4: e.tensor_copy(out=Vp[:, b4 * 4:(b4 + 1) * 4, 1, 64:128],
                                                                      in_=psV[:, :, 64:128]), [B_psV], [B_Vp[b4]])
                    ablks = blks if "blocks" not in SKIP else []

                    def chunks_of(bi):
                        r, n = ablks[bi]
                        ch = [(1, tok(r, n), Mcur, bi)]
                        if n > 0:
                            ch.append((0, tok(r, n - 1), Mprev, bi - 1))
                        return ch

                    def emit_scores(bi, si):
                        r, n = ablks[bi]
                        cur = tok(r, n)
                        for h in range(2):
                            for (ci, ktok, Mk, vb) in chunks_of(bi):
                                k.mm(psS[si][:, h, ci, :], kT[:, ktok], qz[h][:, cur], True, False,
                                     reads=B_kT + B_qz[h], writes=[B_psS[si]])
                                k.mm(psS[si][:, h, ci, :], ident_b[:], Mk, False, True,
                                     reads=[B_ident_b, B_acb], writes=[B_psS[si]])

                    if ablks:
                        emit_scores(0, it % 2)
                    for bi, (r, n) in enumerate(ablks):
                        si = it % 2
                        it += 1
                        cur = tok(r, n)
                        chunks = chunks_of(bi)
                        if bi + 1 < len(ablks):
                            emit_scores(bi + 1, it % 2)
                        k.op("act", lambda e, si=si: e.activation(
                            out=PT[si][:].rearrange("p h c q -> p (h c q)"),
                            in_=psS[si][:].rearrange("p h c q -> p (h c q)"), func=AF.Exp, scale=0.125),
                            [B_psS[si]], [B_PT[si]])
                        nmm = 2 * len(chunks)
                        for which in range(2):
                            ii = 0
                            for h in range(2):
                                for (ci, ktok, Mk, vb) in chunks:
                                    lhs = Vp[:, vb, h, :] if which == 0 else onesp[h]
                                    k.mm(psND[si][:, which, :], lhs, PT[si][:, h, ci, :], ii == 0, ii == nmm - 1,
                                         reads=[B_Vp[vb // 4], B_PT[si], B_acb], writes=[B_psND[si]])
                                    ii += 1
                        if g == 0:
                            k.op("dve", lambda e, si=si, cur=cur: e.tensor_copy(out=Nacc[:, cur], in_=psND[si][:, 0, :]),
                                 [B_psND[si]], [B_Nacc])
                            k.op("dve", lambda e, si=si, cur=cur: e.tensor_copy(out=Dacc[:, cur], in_=psND[si][:, 1, :]),
                                 [B_psND[si]], [B_Dacc])
                        else:
                            k.op("dve", lambda e, si=si, cur=cur: e.tensor_tensor(out=Nacc[:, cur], in0=psND[si][:, 0, :],
                                                                                 in1=Nacc[:, cur], op=ALU.add),
                                 [B_psND[si], B_Nacc], [B_Nacc])
                            k.op("dve", lambda e, si=si, cur=cur: e.tensor_tensor(out=Dacc[:, cur], in0=psND[si][:, 1, :],
                                                                                 in1=Dacc[:, cur], op=ALU.add),
                                 [B_psND[si], B_Dacc], [B_Dacc])
                if "norm" in SKIP:
                    continue
                k.op("act", lambda e: e.activation(out=Dacc[:], in_=Dacc[:], func=AF.Ln), [B_Dacc], [B_Dacc])
                k.op("act", lambda e: e.activation(out=Dacc[:], in_=Dacc[:], func=AF.Exp, scale=-1.0),
                     [B_Dacc], [B_Dacc])
                k.op("dve", lambda e, hp=hp: e.tensor_tensor(out=oT[:, hp, :], in0=Nacc[:], in1=Dacc[:], op=ALU.mult),
                     [B_Nacc, B_Dacc], [B_oT[hp]])
            k.s.barrier()
        k.s.scope = f"att{b}_3_out"
        with contextlib.ExitStack() as c3:
            def sb3(name, shape, dt=F32):
                return c3.enter_context(k.nc.sbuf_tensor(f"s_a3_{b}_{name}", list(shape), dt))
            wof = sb3("wof", [128, 4, D])
            wob = sb3("wob", [128, 4, D], BF16)
            g1bc = sb3("g1bc", [128, D])
            B_wof, B_wob, B_g1bc = Buf(), Buf(), Buf()
            k.dma("sp", wof[:], w_out.rearrange("(c p) d -> p c d", p=128), vl, writes=[B_wof])
            k.dma("sp", g1bc[:], modrow[L, b:b + 1, 2 * D:3 * D].partition_broadcast(128), vl,
                  reads=[B_modrow[L]], writes=[B_g1bc])
            for c in range(4):
                k.op("dve", lambda e, c=c: e.tensor_tensor(out=wob[:, c, :], in0=wof[:, c, :], in1=g1bc[:], op=ALU.mult),
                     [B_wof, B_g1bc], [B_wob])
            xt = [sb3(f"xt{i}", [128, 4, D]) for i in range(3)]
            B_xt = [Buf(), Buf(), Buf()]
            xt_ld = [k.dsem(f"a3_{b}_xt_ld{i}") for i in range(3)]
            xt_st = [k.dsem(f"a3_{b}_xt_st{i}") for i in range(3)]
            psY = [c3.enter_context(k.nc.psum_tensor(f"p_a3_{b}_psY{i}", [128, 512], F32)) for i in range(2)]
            B_psY = [Buf(), Buf()]
            def a3_load(gi):
                G = b * GPB + gi
                sl = gi % 3
                k.dma("sp", xt[sl][:], xr[G * 512:(G + 1) * 512, :].rearrange("(j p) d -> p j d", p=128), xt_ld[sl],
                      reads=[B_xr[G]], writes=[B_xt[sl]])
            a3_load(0)
            for gi in range(GPB if "p3" not in SKIP else 0):
                G = b * GPB + gi
                sl = gi % 3
                if gi + 1 < GPB:
                    a3_load(gi + 1)
                for j in range(4):
                    for dh in range(2):
                        yi = (j * 2 + dh) % 2
                        t0 = gi * 512 + j * 128
                        for c in range(4):
                            k.mm(psY[yi][:], oT[:, c, t0:t0 + 128], wob[:, c, dh * 512:(dh + 1) * 512], c == 0, c == 3,
                                 reads=[B_oT[c], B_wob], writes=[B_psY[yi]])
                        k.op("dve", lambda e, j=j, dh=dh, yi=yi, sl=sl: e.tensor_tensor(
                            out=xt[sl][:, j, dh * 512:(dh + 1) * 512], in0=psY[yi][:],
                            in1=xt[sl][:, j, dh * 512:(dh + 1) * 512], op=ALU.add), [B_psY[yi], B_xt[sl]], [B_xt[sl]])
                k.dma("sp", xr[G * 512:(G + 1) * 512, :].rearrange("(j p) d -> p j d", p=128), xt[sl][:], xt_st[sl],
                      reads=[B_xt[sl]], writes=[B_xr[G]])
            k.s.barrier()


def moe_layer(k, L, a):
    NB, S, T = k.NB, k.S, k.T
    NT, NG, GPB = T // 128, T // 512, S // 512
    NBLK = a["NBLK"]
    xr, B_xr, modrow, B_modrow = a["xr"], a["B_xr"], a["modrow"], a["B_modrow"]
    ident_f, B_ident_f, ident_b, B_ident_b = a["ident_f"], a["B_ident_f"], a["ident_b"], a["B_ident_b"]
    hs, xs, ys = a["hs"], a["xs"], a["ys"]
    B_hs = [Buf() for _ in range(NG)]
    pfx = f"m{L}_"

    E1all = k.sbp(pfx + "E1all", [128, NT, 32], F32)
    E2all = k.sbp(pfx + "E2all", [128, NT, 32], F32)
    Oall = k.sbp(pfx + "Oall", [128, NT, 32], BF16)
    Wall = k.sbp(pfx + "Wall", [128, NT, 2], F32)
    B_E1 = [Buf() for _ in range(NT)]
    B_E2 = [Buf() for _ in range(NT)]
    B_O = [Buf() for _ in range(NT)]
    B_W = [Buf() for _ in range(NT)]
    g2bc = [k.sbp(pfx + f"g2bc{b}", [128, D], F32) for b in range(NB)]
    B_g2bc = [Buf() for _ in range(NB)]
    d1i = k.sbp(pfx + "d1i", [128, NT], I32)
    d2i = k.sbp(pfx + "d2i", [128, NT], I32)
    bei = k.sbp(pfx + "bei", [128, 2, NBLK], I32)
    B_d1i, B_d2i, B_bei = Buf(), Buf(), Buf()
    nusedi = k.sbp(pfx + "nusedi", [128, 1], I32)
    B_nusedi = Buf()
    vl = k.dsem(pfx + "vl")
    for b in range(NB):
        k.dma("sp", g2bc[b][:], modrow[L, b:b + 1, 5 * D:6 * D].partition_broadcast(128), vl,
              reads=[B_modrow[L]], writes=[B_g2bc[b]], grp="g2")

    k.s.scope = pfx + "M1_router"
    with contextlib.ExitStack() as c1:
        def sb1(name, shape, dt=F32):
            return c1.enter_context(k.nc.sbuf_tensor("s_" + pfx + name, list(shape), dt))

        def ps1(name, shape, dt=F32):
            return c1.enter_context(k.nc.psum_tensor("p_" + pfx + name, list(shape), dt))
        A2bc = [sb1(f"A2bc{b}", [128, D]) for b in range(NB)]
        sh2bc = [sb1(f"sh2bc{b}", [128, D]) for b in range(NB)]
        gfbc = sb1("gfbc", [128, D])
        B_A2bc = [Buf() for _ in range(NB)]
        B_sh2bc = [Buf() for _ in range(NB)]
        B_gfbc = Buf()
        Wr = sb1("Wr", [128, 8, 36])
        brbc = sb1("brbc", [128, 36])
        B_Wr, B_brbc = Buf(), Buf()
        k.dma("sp", gfbc[:], a["gffn"][L:L + 1, :].partition_broadcast(128), vl, writes=[B_gfbc], grp="g2")
        k.dma("sp", Wr[:], a["Wr_in"][L].rearrange("(kc p) f -> p kc f", p=128), vl, writes=[B_Wr], grp="g2")
        k.dma("sp", brbc[:], a["br_in"][L:L + 1, :].partition_broadcast(128), vl, writes=[B_brbc], grp="g2")
        for b in range(NB):
            k.dma("sp", sh2bc[b][:], modrow[L, b:b + 1, 3 * D:4 * D].partition_broadcast(128), vl,
                  reads=[B_modrow[L]], writes=[B_sh2bc[b]], grp="g2")
            k.dma("sp", A2bc[b][:], modrow[L, b:b + 1, 4 * D:5 * D].partition_broadcast(128), vl,
                  reads=[B_modrow[L]], writes=[B_A2bc[b]], grp="g2")
            k.op("dve", lambda e, b=b: e.scalar_tensor_tensor(out=A2bc[b][:], in0=A2bc[b][:], scalar=1.0, in1=gfbc[:],
                                                             op0=ALU.add, op1=ALU.mult),
                 [B_A2bc[b], B_gfbc], [B_A2bc[b]])
        xt = [sb1(f"xt{i}", [128, 4, D]) for i in range(2)]
        B_xt = [Buf(), Buf()]
        xt_ld = [k.dsem(pfx + "xt_ld0"), k.dsem(pfx + "xt_ld1")]
        junk = sb1("junk", [128, D], BF16)
        B_junk = Buf()
        ss, rstd = sb1("ss", [128, 4]), sb1("rstd", [128, 4])
        B_ss, B_rstd = Buf(), Buf()
        h2_ = [sb1(f"h2_{i}", [128, 4, D]) for i in range(2)]
        B_h2_ = [[Buf() for _ in range(4)] for _ in range(2)]
        h2b = [sb1(f"h2b{i}", [128, 4, D], BF16) for i in range(2)]
        B_h2b = [Buf(), Buf()]
        h2b_st = [k.dsem(pfx + "h2b_st0"), k.dsem(pfx + "h2b_st1")]
        pTf = [ps1(f"pTf{i}", [128, 512]) for i in range(2)]
        B_pTf = [Buf(), Buf()]
        hT2 = sb1("hT2", [128, 8, 512])
        B_hT2 = [Buf() for _ in range(8)]
        pslg = ps1("pslg", [128, 4, 36])
        B_pslg = Buf()
        lg4 = sb1("lg4", [128, 4, 36])
        gmax, gsum = sb1("gmax", [128, 4]), sb1("gsum", [128, 4])
        gsh = sb1("gsh", [128, 4, 4])
        ohg4 = sb1("ohg4", [128, 4, 4])
        tmp4 = sb1("tmp4", [128, 4, 4, 8])
        sel4, sel4b = sb1("sel4", [128, 4, 8]), sb1("sel4b", [128, 4, 8])
        oh1_4, oh2_4 = sb1("oh1_4", [128, 4, 8]), sb1("oh2_4", [128, 4, 8])
        m1, m2, dlt = sb1("m1", [128, 4]), sb1("m2", [128, 4]), sb1("dlt", [128, 4])
        B_lg, B_sm, B_sm2, B_ohg, B_ge, B_sel, B_selb, B_oh1, B_oh2, B_tmp4, B_m1, B_m2, B_dlt = (Buf() for _ in range(13))

        def load_x(G):
            sl = G % 2
            k.dma("sp", xt[sl][:], xr[G * 512:(G + 1) * 512, :].rearrange("(j p) d -> p j d", p=128),
                  xt_ld[sl], reads=[B_xr[G]], writes=[B_xt[sl]])
        def stage1(G):
            b = G // GPB
            sl = G % 2
            xt_t, B_x = xt[sl], B_xt[sl]
            h2, B_h2 = h2_[sl], B_h2_[sl]
            for j in range(4):
                k.op("act", lambda e, j=j, xt_t=xt_t: e.activation(out=junk[:], in_=xt_t[:, j, :], func=AF.Square,
                                                                  accum_out=ss[:, j:j + 1]), [B_x], [B_junk, B_ss])
            k.op("dve", lambda e: e.tensor_scalar(out=rstd[:], in0=ss[:], scalar1=1.0 / D, scalar2=1e-6,
                                                  op0=ALU.mult, op1=ALU.add), [B_ss], [B_rstd])
            k.op("act", lambda e: e.activation(out=rstd[:], in_=rstd[:], func=AF.Sqrt), [B_rstd], [B_rstd])
            k.op("dve", lambda e: e.reciprocal(out=rstd[:], in_=rstd[:]), [B_rstd], [B_rstd])
            for j in range(4):
                k.op("dve", lambda e, j=j, xt_t=xt_t, b=b, h2=h2: e.scalar_tensor_tensor(
                    out=h2[:, j, :], in0=xt_t[:, j, :], scalar=rstd[:, j:j + 1], in1=A2bc[b][:],
                    op0=ALU.mult, op1=ALU.mult), [B_x, B_rstd, B_A2bc[b]], [B_h2[j]])
                k.op("pool" if j % 2 else "dve", lambda e, j=j, b=b, h2=h2: e.tensor_tensor(
                    out=h2[:, j, :], in0=h2[:, j, :], in1=sh2bc[b][:], op=ALU.add),
                     [B_h2[j], B_sh2bc[b]], [B_h2[j]])

        def stage1b(G):
            sl = G % 2
            h2, B_h2 = h2_[sl], B_h2_[sl]
            for j in range(4):
                k.op("act", lambda e, j=j, sl=sl, h2=h2: e.copy(out=h2b[sl][:, j, :], in_=h2[:, j, :]),
                     [B_h2[j]], [B_h2b[sl]])
            k.dma("sp", hs[G * 512:(G + 1) * 512, :].rearrange("(j p) d -> p j d", p=128), h2b[sl][:], h2b_st[sl],
                  reads=[B_h2b[sl]], writes=[B_hs[G]])

        load_x(0)
        if NG > 1:
            load_x(1)
        stage1(0)
        stage1b(0)
        for G in range(NG):
            b = G // GPB
            sl = G % 2
            if G + 1 < NG:
                stage1(G + 1)
            if G + 2 < NG:
                load_x(G + 2)
            h2, B_h2 = h2_[sl], B_h2_[sl]
            for c in range(8):
                p = c % 2
                for j in range(4):
                    k.tr(pTf[p][:, j * 128:(j + 1) * 128], h2[:, j, c * 128:(c + 1) * 128], ident_f[:],
                         reads=[B_h2[j], B_ident_f], writes=[B_pTf[p]])
                if c % 2 == 0:
                    k.op("act", lambda e, c=c, p=p: e.copy(out=hT2[:, c, :], in_=pTf[p][:]), [B_pTf[p]], [B_hT2[c]])
                else:
                    k.op("dve", lambda e, c=c, p=p: e.tensor_copy(out=hT2[:, c, :], in_=pTf[p][:]),
                         [B_pTf[p]], [B_hT2[c]])
            for j in range(4):
                for kc in range(8):
                    k.mm(pslg[:, j, :], hT2[:, kc, j * 128:(j + 1) * 128], Wr[:, kc, :], kc == 0, kc == 7,
                         reads=[B_hT2[kc], B_Wr], writes=[B_pslg])
            V = "dve"
            i0_ = G * 4
            J = 4

            def bc(ap, shape):
                return ap.to_broadcast(list(shape))
            k.op(V, lambda e: e.tensor_tensor(out=lg4[:], in0=pslg[:], in1=bc(brbc[:].unsqueeze(1), [128, J, 36]),
                                              op=ALU.add), [B_pslg, B_brbc], [B_lg])
            k.op(V, lambda e: e.tensor_reduce(out=gmax[:], in_=lg4[:, :, 0:4], axis=AX.X, op=ALU.max), [B_lg], [B_sm])
            k.op(V, lambda e: e.tensor_tensor(out=gsh[:], in0=lg4[:, :, 0:4], in1=bc(gmax[:].unsqueeze(2), [128, J, 4]),
                                              op=ALU.subtract), [B_lg, B_sm], [B_ge])
            k.op(V, lambda e: e.tensor_tensor(out=ohg4[:], in0=lg4[:, :, 0:4], in1=bc(gmax[:].unsqueeze(2), [128, J, 4]),
                                              op=ALU.is_equal), [B_lg, B_sm], [B_ohg])
            k.op("act", lambda e: e.activation(out=gsh[:], in_=gsh[:], func=AF.Exp), [B_ge], [B_ge])
            k.op(V, lambda e: e.tensor_reduce(out=gsum[:], in_=gsh[:], axis=AX.X, op=ALU.add), [B_ge], [B_sm2])
            k.op(V, lambda e: e.reciprocal(out=gsum[:], in_=gsum[:]), [B_sm2], [B_sm2])
            k.op(V, lambda e: e.tensor_tensor(out=tmp4[:], in0=lg4[:, :, 4:36].rearrange("p j (g e) -> p j g e", g=4),
                                              in1=bc(ohg4[:].unsqueeze(3), [128, J, 4, 8]), op=ALU.mult),
                 [B_lg, B_ohg], [B_tmp4])
            k.op(V, lambda e: e.tensor_reduce(out=sel4[:], in_=tmp4[:].rearrange("p j g e -> p j e g"), axis=AX.X,
                                              op=ALU.add), [B_tmp4], [B_sel])
            k.op(V, lambda e: e.tensor_reduce(out=m1[:], in_=sel4[:], axis=AX.X, op=ALU.max), [B_sel], [B_m1])
            k.op(V, lambda e: e.tensor_tensor(out=oh1_4[:], in0=sel4[:], in1=bc(m1[:].unsqueeze(2), [128, J, 8]),
                                              op=ALU.is_equal), [B_sel, B_m1], [B_oh1])
            k.op(V, lambda e: e.scalar_tensor_tensor(out=sel4b[:], in0=oh1_4[:], scalar=-1.0e30, in1=sel4[:],
                                                     op0=ALU.mult, op1=ALU.add), [B_oh1, B_sel], [B_selb])
            k.op(V, lambda e: e.tensor_reduce(out=m2[:], in_=sel4b[:], axis=AX.X, op=ALU.max), [B_selb], [B_m2])
            k.op(V, lambda e: e.tensor_tensor(out=oh2_4[:], in0=sel4b[:], in1=bc(m2[:].unsqueeze(2), [128, J, 8]),
                                              op=ALU.is_equal), [B_selb, B_m2], [B_oh2])
            k.op(V, lambda e: e.tensor_tensor(out=dlt[:], in0=m2[:], in1=m1[:], op=ALU.subtract), [B_m1, B_m2], [B_dlt])
            k.op("act", lambda e: e.activation(out=dlt[:], in_=dlt[:], func=AF.Exp), [B_dlt], [B_dlt])
            k.op(V, lambda e: e.tensor_scalar(out=dlt[:], in0=dlt[:], scalar1=1.0, scalar2=None, op0=ALU.add),
                 [B_dlt], [B_dlt])
            k.op(V, lambda e: e.reciprocal(out=dlt[:], in_=dlt[:]), [B_dlt], [B_dlt])
            Bws = B_W[i0_:i0_ + J]
            k.op(V, lambda e, i0_=i0_: e.tensor_tensor(out=Wall[:, i0_:i0_ + J, 0], in0=gsum[:], in1=dlt[:], op=ALU.mult),
                 [B_sm2, B_dlt], Bws)
            k.op(V, lambda e, i0_=i0_: e.tensor_tensor(out=Wall[:, i0_:i0_ + J, 1], in0=gsum[:],
                                                      in1=Wall[:, i0_:i0_ + J, 0], op=ALU.subtract),
                 [B_sm2] + Bws, Bws)
            for (Eall, oh, B_oh, B_E) in ((E1all, oh1_4, B_oh1, B_E1), (E2all, oh2_4, B_oh2, B_E2)):
                k.op(V, lambda e, Eall=Eall, oh=oh, i0_=i0_: e.tensor_tensor(
                    out=Eall[:, i0_:i0_ + J, :].rearrange("p j (g e) -> p j g e", g=4),
                    in0=bc(ohg4[:].unsqueeze(3), [128, J, 4, 8]), in1=bc(oh[:].unsqueeze(2), [128, J, 4, 8]),
                    op=ALU.mult), [B_ohg, B_oh], B_E[i0_:i0_ + J])
            k.op("pool", lambda e, i0_=i0_: e.tensor_tensor(out=Oall[:, i0_:i0_ + J, :], in0=E1all[:, i0_:i0_ + J, :],
                                                           in1=E2all[:, i0_:i0_ + J, :], op=ALU.add),
                 B_E1[i0_:i0_ + J] + B_E2[i0_:i0_ + J], B_O[i0_:i0_ + J])
            if G + 1 < NG:
                stage1b(G + 1)
        k.s.barrier()
    dbgsem = k.dsem(pfx + "dbg")
    B_dbg = Buf()
    if a.get("stage") == "M1":
        dbg = a["dbg"]
        k.dma("sp", dbg[:, 0:NT * 2], Wall[:].rearrange("p i w -> p (i w)"), dbgsem, reads=B_W, writes=[B_dbg])
        k.dma("sp", dbg[:, 1024:1024 + NT * 32], E1all[:].rearrange("p i e -> p (i e)"), dbgsem, reads=B_E1, writes=[B_dbg])
        k.dma("sp", dbg[:, 2048:2048 + NT * 32], E2all[:].rearrange("p i e -> p (i e)"), dbgsem, reads=B_E2, writes=[B_dbg])
        return [B_dbg]

    k.s.scope = pfx + "M2_index"
    with contextlib.ExitStack() as c2:
        def sb2(name, shape, dt=F32):
            return c2.enter_context(k.nc.sbuf_tensor("s_" + pfx + name, list(shape), dt))

        def ps2(name, shape, dt=F32):
            return c2.enter_context(k.nc.psum_tensor("p_" + pfx + name, list(shape), dt))
        trif, trib, onesb = sb2("trif", [128, 128]), sb2("trib", [128, 128], BF16), sb2("onesb", [128, 128], BF16)
        B_trif, B_trib, B_onesb = Buf(), Buf(), Buf()
        k.dma("sp", trif[:], a["tri_in"][:, :], vl, writes=[B_trif])
        k.op("dve", lambda e: e.tensor_copy(out=trib[:], in_=trif[:]), [B_trif], [B_trib])
        k.op("dve", lambda e: e.memset(onesb[:], 1.0), [], [B_onesb])
        base = sb2("base", [128, NT, 32])
        tot = sb2("tot", [128, NT, 32])
        B_base, B_tot = Buf(), Buf()
        psw = [ps2(f"psw{i}", [128, 512]) for i in range(2)]
        B_psw = [Buf(), Buf()]
        TPC = 16
        nch = (NT + TPC - 1) // TPC
        for ci in range(nch):
            t0, t1 = ci * TPC, min(NT, (ci + 1) * TPC)
            w = (t1 - t0) * 32
            for which, (lhs, B_l, dst, B_d) in enumerate(((trib, B_trib, base, B_base), (onesb, B_onesb, tot, B_tot))):
                k.mm(psw[which][:, 0:w], lhs[:], Oall[:, t0:t1, :], True, True,
                     reads=[B_l] + B_O[t0:t1], writes=[B_psw[which]])
                k.op("dve", lambda e, which=which, dst=dst, t0=t0, t1=t1, w=w: e.tensor_copy(
                    out=dst[:, t0:t1, :], in_=psw[which][:, 0:w]), [B_psw[which]], [B_d])
        cnt = sb2("cnt", [128, 32])
        nblk = sb2("nblk", [128, 32])
        cum = sb2("cum", [128, 32])
        carry = sb2("carry", [128, 32])
        tmp32 = sb2("tmp32", [128, 32])
        B_cnt, B_nblk, B_cum, B_carry, B_tmp32 = Buf(), Buf(), Buf(), Buf(), Buf()
        V = "dve"
        k.op(V, lambda e: e.tensor_reduce(out=cnt[:], in_=tot[:].rearrange("p i e -> p e i"), axis=AX.X, op=ALU.add),
             [B_tot], [B_cnt])
        k.op(V, lambda e: e.tensor_scalar(out=nblk[:], in0=cnt[:], scalar1=0.0, scalar2=None, op0=ALU.is_gt),
             [B_cnt], [B_nblk])
        for j in range(1, (2 * T) // 512 + 1):
            k.op(V, lambda e, j=j: e.scalar_tensor_tensor(out=nblk[:], in0=cnt[:], scalar=512.0 * j, in1=nblk[:],
                                                         op0=ALU.is_gt, op1=ALU.add), [B_cnt, B_nblk], [B_nblk])
        k.op(V, lambda e: e.tensor_copy(out=cum[:], in_=nblk[:]), [B_nblk], [B_cum])
        for ee in range(1, 32):
            k.op(V, lambda e, ee=ee: e.tensor_tensor(out=cum[:, ee:ee + 1], in0=cum[:, ee - 1:ee], in1=nblk[:, ee:ee + 1],
                                                    op=ALU.add), [B_cum, B_nblk], [B_cum])
        k.op(V, lambda e: e.tensor_tensor(out=carry[:], in0=cum[:], in1=nblk[:], op=ALU.subtract),
             [B_cum, B_nblk], [B_carry])
        k.op(V, lambda e: e.tensor_scalar(out=carry[:], in0=carry[:], scalar1=512.0, scalar2=None, op0=ALU.mult),
             [B_carry], [B_carry])
        for i in range(NT):
            k.op(V, lambda e, i=i: e.tensor_tensor(out=base[:, i, :], in0=base[:, i, :], in1=carry[:], op=ALU.add),
                 [B_base, B_carry], [B_base])
            if i + 1 < NT:
                k.op(V, lambda e, i=i: e.tensor_tensor(out=carry[:], in0=carry[:], in1=tot[:, i, :], op=ALU.add),
                     [B_carry, B_tot], [B_carry])
        dtmp = sb2("dtmp", [128, NT, 32])
        dfl = sb2("dfl", [128, NT])
        B_dtmp, B_dfl = Buf(), Buf()
        for (Eall, B_E, di, B_di) in ((E1all, B_E1, d1i, B_d1i), (E2all, B_E2, d2i, B_d2i)):
            k.op(V, lambda e, Eall=Eall: e.tensor_tensor(out=dtmp[:], in0=Eall[:], in1=base[:], op=ALU.mult),
                 list(B_E) + [B_base], [B_dtmp])
            k.op(V, lambda e: e.tensor_reduce(out=dfl[:], in_=dtmp[:], axis=AX.X, op=ALU.add), [B_dtmp], [B_dfl])
            k.op(V, lambda e, di=di: e.tensor_copy(out=di[:], in_=dfl[:]), [B_dfl], [B_di])
        bef = sb2("bef", [128, NBLK])
        B_bef = Buf()
        for j in range(NBLK):
            k.op(V, lambda e, j=j: e.tensor_scalar(out=tmp32[:], in0=cum[:], scalar1=float(j), scalar2=None,
                                                  op0=ALU.is_le, op1=ALU.add, accum_out=bef[:, j:j + 1]),
                 [B_cum], [B_tmp32, B_bef])
        k.op(V, lambda e: e.tensor_scalar(out=bef[:], in0=bef[:], scalar1=31.0, scalar2=None, op0=ALU.min),
             [B_bef], [B_bef])
        k.op(V, lambda e: e.tensor_copy(out=nusedi[:], in_=cum[:, 31:32]), [B_cum], [B_nusedi])
        pcol = sb2("pcol", [128, 1])
        B_pcol = Buf()
        k.dma("sp", pcol[:], a["pcol_in"][:, :], vl, writes=[B_pcol])
        k.op(V, lambda e: e.tensor_scalar(out=bef[:], in0=bef[:], scalar1=float(L * 32), scalar2=128.0,
                                          op0=ALU.add, op1=ALU.mult), [B_bef], [B_bef])
        k.op(V, lambda e: e.tensor_scalar(out=bef[:], in0=bef[:], scalar1=pcol[:, 0:1], scalar2=None, op0=ALU.add),
             [B_bef, B_pcol], [B_bef])
        k.op(V, lambda e: e.tensor_scalar(out=bef[:], in0=bef[:], scalar1=2.0, scalar2=None, op0=ALU.mult),
             [B_bef], [B_bef])
        k.op(V, lambda e: e.tensor_copy(out=bei[:, 0, :], in_=bef[:]), [B_bef], [B_bei])
        k.op(V, lambda e: e.tensor_scalar(out=bef[:], in0=bef[:], scalar1=1.0, scalar2=None, op0=ALU.add),
             [B_bef], [B_bef])
        k.op(V, lambda e: e.tensor_copy(out=bei[:, 1, :], in_=bef[:]), [B_bef], [B_bei])
        k.s.barrier()
        if a.get("stage") == "M2":
            dbg = a["dbg"].bitcast(I32)
            k.dma("sp", dbg[:, 0:NT], d1i[:], dbgsem, reads=[B_d1i], writes=[B_dbg])
            k.dma("sp", dbg[:, 1024:1024 + NT], d2i[:], dbgsem, reads=[B_d2i], writes=[B_dbg])
            k.dma("sp", dbg[:, 2048:2048 + NBLK], bei[:, 0, :], dbgsem, reads=[B_bei], writes=[B_dbg])
            k.s.barrier()
    if a.get("stage") == "M2":
        return [B_dbg]

    k.s.scope = pfx + "M3_scatter"
    with contextlib.ExitStack() as c3:
        NS3 = 6
        hsb = [c3.enter_context(k.nc.sbuf_tensor(f"s_{pfx}hsb{i}", [128, D], BF16)) for i in range(NS3)]
        B_hsb = [Buf() for _ in range(NS3)]
        hsb_ld = [k.dsem(pfx + f"hsb_ld{i}") for i in range(NS3)]
        sc_sem = [k.dsem(pfx + f"sc{i}") for i in range(NS3)]
        B_xs = Buf()

        def m3_load(i):
            sl = i % NS3
            k.dma("sp", hsb[sl][:], hs[i * 128:(i + 1) * 128, :], hsb_ld[sl], reads=[B_hs[i // 4]], writes=[B_hsb[sl]])
        for i in range(min(NS3 - 1, NT)):
            m3_load(i)
        for i in range(NT):
            sl = i % NS3
            if i + NS3 - 1 < NT:
                m3_load(i + NS3 - 1)
            for (di, B_di) in ((d1i, B_d1i), (d2i, B_d2i)):
                k.s.add("pool", lambda e, sl=sl, di=di, i=i: e.indirect_dma_start(
                    out=xs[:, :], out_offset=bass.IndirectOffsetOnAxis(ap=di[:, i:i + 1], axis=0),
                    in_=hsb[sl][:], in_offset=None), [B_hsb[sl], B_di], [B_xs], dsem=sc_sem[sl], grp=("sc", i))
        k.s.barrier()
    if a.get("stage") == "M3":
        return []

    k.s.scope = pfx + "M4_experts"
    with contextlib.ExitStack() as c4:
        def sb4(name, shape, dt=F32):
            return c4.enter_context(k.nc.sbuf_tensor("s_" + pfx + name, list(shape), dt))

        def ps4(name, shape, dt=F32):
            return c4.enter_context(k.nc.psum_tensor("p_" + pfx + name, list(shape), dt))
        wg = [sb4(f"wg{i}", [128, 8, 512], BF16) for i in range(2)]
        wu = [sb4(f"wu{i}", [128, 8, 512], BF16) for i in range(2)]
        wd = [sb4(f"wd{i}", [128, 4, D], BF16) for i in range(2)]
        B_wg, B_wu, B_wd = [Buf(), Buf()], [Buf(), Buf()], [Buf(), Buf()]
        wsem = [[k.dsem(pfx + f"w{n}{i}") for i in range(2)] for n in "gud"]
        xb = [sb4(f"xb{i}", [128, 4, D], BF16) for i in range(2)]
        B_xb = [Buf(), Buf()]
        xb_ld = [k.dsem(pfx + "xb_ld0"), k.dsem(pfx + "xb_ld1")]
        xsT_ = [sb4(f"xsT{i}", [128, 8, 512], BF16) for i in range(2)]
        B_xsT_ = [[Buf() for _ in range(8)] for _ in range(2)]
        pTx = [ps4(f"pTx{i}", [128, 512], BF16) for i in range(2)]
        B_pTx = [Buf(), Buf()]
        psG = [ps4(f"psG{i}", [128, 512]) for i in range(2)]
        psU = [ps4(f"psU{i}", [128, 512]) for i in range(2)]
        B_psG, B_psU = [Buf(), Buf()], [Buf(), Buf()]
        sg = [sb4(f"sg{i}", [128, 512]) for i in range(2)]
        B_sg = [Buf(), Buf()]
        hTe = sb4("hTe", [128, 4, 512], BF16)
        B_hTe = [Buf() for _ in range(4)]
        psY = [ps4(f"psY{i}", [128, 512]) for i in range(2)]
        B_psY = [Buf(), Buf()]
        yb = [sb4(f"yb{i}", [128, 4, D]) for i in range(2)]
        B_yb = [Buf(), Buf()]
        yb_st = [k.dsem(pfx + "yb_st0"), k.dsem(pfx + "yb_st1")]
        B_ys = Buf()
        gw, uw, dw = a["gw"], a["uw"], a["dw"]
        gw2 = gw.rearrange("r (h f) -> (r h) f", h=2)
        uw2 = uw.rearrange("r (h f) -> (r h) f", h=2)
        dw2 = dw.rearrange("r (h f) -> (r h) f", h=2)

        def wload(j, sl):
            for (dst, B_d, tab, ws, nh) in ((wg[sl], B_wg[sl], gw2, wsem[0][sl], 4), (wu[sl], B_wu[sl], uw2, wsem[1][sl], 4),
                                            (wd[sl], B_wd[sl], dw2, wsem[2][sl], 2)):
                for h in range(2):
                    k.s.add("pool", lambda e, dst=dst, tab=tab, j=j, h=h, nh=nh: e.indirect_dma_start(
                        out=dst[:, h * nh:(h + 1) * nh, :].rearrange("p a f -> p (a f)"), out_offset=None, in_=tab,
                        in_offset=bass.IndirectOffsetOnAxis(ap=bei[:, h, j:j + 1], axis=0)), [B_bei], [B_d], dsem=ws,
                        grp=("w", j))

        def xload(j, sl):
            k.dma("sp", xb[sl][:], xs[j * 512:(j + 1) * 512, :].rearrange("(t p) d -> p t d", p=128), xb_ld[sl],
                  reads=[B_xs], writes=[B_xb[sl]])
        def xpose(j, cs):
            sl = j % 2
            for c in cs:
                p = c % 2
                for t in range(4):
                    k.tr(pTx[p][:, t * 128:(t + 1) * 128], xb[sl][:, t, c * 128:(c + 1) * 128], ident_b[:],
                         reads=[B_xb[sl], B_ident_b], writes=[B_pTx[p]])
                if c % 2 == 0:
                    k.op("act", lambda e, c=c, p=p, sl=sl: e.copy(out=xsT_[sl][:, c, :], in_=pTx[p][:]),
                         [B_pTx[p]], [B_xsT_[sl][c]])
                else:
                    k.op("dve", lambda e, c=c, p=p, sl=sl: e.tensor_copy(out=xsT_[sl][:, c, :], in_=pTx[p][:]),
                         [B_pTx[p]], [B_xsT_[sl][c]])
        regs = k.nused_regs()
        for eng_ in ENGS:
            k.s.add(eng_, lambda e, eng_=eng_: e.reg_load(regs[eng_], nusedi[0:1, 0:1]), [B_nusedi], [])
        JS = (2 * T) // 512
        wload(0, 0)
        xload(0, 0)
        if NBLK > 1:
            wload(1, 1)
            xload(1, 1)
        xpose(0, range(8))
        for j in range(NBLK):
            sl = j % 2
            k.s.guard = (regs, (j if not os.environ.get("DYN_NEVER") else -1)) if (j >= max(JS, int(os.environ.get("DYN_FROM", "0"))) and DYN_SKIP) else None
            xsT, B_xsT = xsT_[sl], B_xsT_[sl]
            for fc in range(4):
                q = fc % 2
                for kc in range(8):
                    k.mm(psG[q][:], wg[sl][:, kc, fc * 128:(fc + 1) * 128], xsT[:, kc, :], kc == 0, kc == 7,
                         reads=[B_wg[sl], B_xsT[kc]], writes=[B_psG[q]])
                for kc in range(8):
                    k.mm(psU[q][:], wu[sl][:, kc, fc * 128:(fc + 1) * 128], xsT[:, kc, :], kc == 0, kc == 7,
                         reads=[B_wu[sl], B_xsT[kc]], writes=[B_psU[q]])
                k.op("act", lambda e, q=q: e.activation(out=sg[q][:], in_=psG[q][:], func=AF.Silu),
                     [B_psG[q]], [B_sg[q]])
                k.op("dve", lambda e, q=q, fc=fc: e.tensor_tensor(out=hTe[:, fc, :], in0=sg[q][:], in1=psU[q][:],
                                                                 op=ALU.mult), [B_sg[q], B_psU[q]], [B_hTe[fc]])
                if j + 1 < NBLK:
                    xpose(j + 1, [2 * fc, 2 * fc + 1])
            for t in range(4):
                for dh in range(2):
                    yi = (t * 2 + dh) % 2
                    for fc in range(4):
                        k.mm(psY[yi][:], hTe[:, fc, t * 128:(t + 1) * 128], wd[sl][:, fc, dh * 512:(dh + 1) * 512],
                             fc == 0, fc == 3, reads=[B_hTe[fc], B_wd[sl]], writes=[B_psY[yi]])
                    if yi == 0:
                        k.op("act", lambda e, t=t, dh=dh, sl=sl: e.copy(out=yb[sl][:, t, dh * 512:(dh + 1) * 512],
                                                                       in_=psY[0][:]), [B_psY[0]], [B_yb[sl]])
                    else:
                        k.op("dve", lambda e, t=t, dh=dh, sl=sl: e.tensor_copy(
                            out=yb[sl][:, t, dh * 512:(dh + 1) * 512], in_=psY[1][:]), [B_psY[1]], [B_yb[sl]])
            if j + 2 < NBLK:
                wload(j + 2, sl)
                xload(j + 2, sl)
            k.dma("sp", ys[j * 512:(j + 1) * 512, :].rearrange("(t p) d -> p t d", p=128), yb[sl][:], yb_st[sl],
                  reads=[B_yb[sl]], writes=[B_ys])
        k.s.guard = None
        k.s.barrier()
    if a.get("stage") == "M4":
        return []

    k.s.scope = pfx + "M5_combine"
    fin = a.get("final")
    with contextlib.ExitStack() as c5:
        def sb5(name, shape, dt=F32):
            return c5.enter_context(k.nc.sbuf_tensor("s_" + pfx + name, list(shape), dt))
        NS = 6
        y1 = [sb5(f"y1_{i}", [128, D]) for i in range(NS)]
        y2 = [sb5(f"y2_{i}", [128, D]) for i in range(NS)]
        xc = [sb5(f"xc{i}", [128, D]) for i in range(NS)]
        B_y1, B_y2, B_xc = [Buf() for _ in range(NS)], [Buf() for _ in range(NS)], [Buf() for _ in range(NS)]
        g_sem = [[k.dsem(pfx + f"g{n}{i}") for i in range(NS)] for n in "12"]
        xc_ld = [k.dsem(pfx + f"xc_ld{i}") for i in range(NS)]
        xc_st = [k.dsem(pfx + f"xc_st{i}") for i in range(NS)]
        if fin is not None:
            fg = sb5("fg", [128, D])
            fjunk = sb5("fjunk", [128, D], BF16)
            fss = [sb5(f"fss{i}", [128, 1]) for i in range(NS)]
            B_fg, B_fjunk = Buf(), Buf()
            B_fss = [Buf() for _ in range(NS)]
            k.dma("sp", fg[:], fin["fing"][0:1, :].partition_broadcast(128), vl, writes=[B_fg])
        def m5_load(i):
            sl = i % NS
            G = i // 4
            k.s.add("pool", lambda e, sl=sl, i=i: e.indirect_dma_start(
                out=y1[sl][:], out_offset=None, in_=ys[:, :],
                in_offset=bass.IndirectOffsetOnAxis(ap=d1i[:, i:i + 1], axis=0)), [B_ys, B_d1i], [B_y1[sl]],
                dsem=g_sem[0][sl])
            k.s.add("pool", lambda e, sl=sl, i=i: e.indirect_dma_start(
                out=y2[sl][:], out_offset=None, in_=ys[:, :],
                in_offset=bass.IndirectOffsetOnAxis(ap=d2i[:, i:i + 1], axis=0)), [B_ys, B_d2i], [B_y2[sl]],
                dsem=g_sem[1][sl])
            k.dma("sp", xc[sl][:], xr[i * 128:(i + 1) * 128, :], xc_ld[sl], reads=[B_xr[G]], writes=[B_xc[sl]])
        for i in range(min(NS - 1, NT)):
            m5_load(i)
        for i in range(NT):
            sl = i % NS
            b = (i * 128) // S
            G = i // 4
            if i + NS - 1 < NT:
                m5_load(i + NS - 1)
            k.op("act", lambda e, sl=sl, i=i: e.activation(out=y1[sl][:], in_=y1[sl][:], func=AF.Copy,
                                                          scale=Wall[:, i, 0:1]), [B_y1[sl], B_W[i]], [B_y1[sl]])
            k.op("dve", lambda e, sl=sl, i=i: e.scalar_tensor_tensor(out=y1[sl][:], in0=y2[sl][:],
                                                                    scalar=Wall[:, i, 1:2], in1=y1[sl][:],
                                                                    op0=ALU.mult, op1=ALU.add),
                 [B_y1[sl], B_y2[sl], B_W[i]], [B_y1[sl]])
            k.op("dve", lambda e, sl=sl, b=b: e.tensor_tensor(out=y1[sl][:], in0=y1[sl][:], in1=g2bc[b][:],
                                                             op=ALU.mult), [B_y1[sl], B_g2bc[b]], [B_y1[sl]])
            k.op("dve", lambda e, sl=sl: e.tensor_tensor(out=xc[sl][:], in0=xc[sl][:], in1=y1[sl][:], op=ALU.add),
                 [B_xc[sl], B_y1[sl]], [B_xc[sl]])
            if fin is None:
                k.dma("sp", xr[i * 128:(i + 1) * 128, :], xc[sl][:], xc_st[sl], reads=[B_xc[sl]], writes=[B_xr[G]])
            else:
                k.op("act", lambda e, sl=sl: e.activation(out=fjunk[:], in_=xc[sl][:], func=AF.Square,
                                                          accum_out=fss[sl][:, 0:1]), [B_xc[sl]], [B_fjunk, B_fss[sl]])
                k.op("dve", lambda e, sl=sl: e.tensor_scalar(out=fss[sl][:], in0=fss[sl][:], scalar1=1.0 / D,
                                                             scalar2=1e-6, op0=ALU.mult, op1=ALU.add),
                     [B_fss[sl]], [B_fss[sl]])
                k.op("act", lambda e, sl=sl: e.activation(out=fss[sl][:], in_=fss[sl][:], func=AF.Sqrt),
                     [B_fss[sl]], [B_fss[sl]])
                k.op("dve", lambda e, sl=sl: e.reciprocal(out=fss[sl][:], in_=fss[sl][:]), [B_fss[sl]], [B_fss[sl]])
                k.op("dve", lambda e, sl=sl: e.scalar_tensor_tensor(out=xc[sl][:], in0=xc[sl][:], scalar=fss[sl][:, 0:1],
                                                                    in1=fg[:], op0=ALU.mult, op1=ALU.mult),
                     [B_xc[sl], B_fss[sl], B_fg], [B_xc[sl]])
                k.dma("sp", fin["out"][i * 128:(i + 1) * 128, :], xc[sl][:], xc_st[sl], reads=[B_xc[sl]],
                      writes=[fin["B_out"][G]])
        k.s.barrier()


def finish(k, out_bufs):
    k.s.add("sp", None, reads=out_bufs)
    esem = {e: k.sem("e_" + e) for e in ENGS}
    stuck = k.s.simulate()
    assert not stuck, f"schedule deadlock: {stuck}"
    k.s.emit(k.nc, esem)
    k.pctx.close()
    k.ctx.close()
    return k.nc


_RL = {}


def _relayout(w, key, nch):
    if key not in _RL or _RL[key][0] is not w:
        Lr, E, R, N = w.shape
        _RL[key] = (w, np.ascontiguousarray(w.reshape(Lr, E, nch, 128, N).transpose(0, 1, 3, 2, 4)).reshape(
            Lr * E * 128, nch * N))
    return _RL[key][1]


def host_inputs(inp, core, NB, S):
    b0 = core * NB
    f = np.float32
    m = {}
    m["x"] = np.ascontiguousarray(inp["x"][b0:b0 + NB, :S].reshape(NB * S, D))
    c = inp["c"][b0:b0 + NB]
    m["cT"] = np.ascontiguousarray(c.reshape(NB, 8, 128).transpose(2, 1, 0).reshape(128, 8 * NB))
    m["ada_w"] = inp["ada_w"]
    m["ada_b"] = inp["ada_b"]
    m["gmixF"] = np.ascontiguousarray(inp["norm_mix_g"].reshape(2, 8, 128).transpose(0, 2, 1))
    m["gffn"] = inp["norm_ffn_g"]
    m["conv_in_w"] = inp["conv_in_w"][0]
    m["convwF"] = np.ascontiguousarray(inp["conv_w"][0].reshape(3, 8, 128).transpose(2, 1, 0).reshape(128, 24))
    m["conv_out_w"] = inp["conv_out_w"][0]
    m["ident"] = np.eye(128, dtype=f)
    m["tri"] = np.triu(np.ones((128, 128), dtype=f), 1)
    m["Wr"] = np.ascontiguousarray(np.concatenate(
        [inp["router_grp_w"], inp["router_exp_w"].transpose(0, 2, 1, 3).reshape(2, D, 32)], axis=2))
    m["br"] = np.ascontiguousarray(np.concatenate([inp["router_grp_b"], inp["router_exp_b"].reshape(2, 32)], axis=1))
    m["exp_gate_w"] = _relayout(inp["exp_gate_w"], "g", 8)
    m["exp_up_w"] = _relayout(inp["exp_up_w"], "u", 8)
    m["exp_down_w"] = _relayout(inp["exp_down_w"], "d", 4)
    m["pcol"] = np.arange(128, dtype=f).reshape(128, 1)
    m["attn_in_w"] = inp["attn_in_w"][0]
    m["attn_out_w"] = inp["attn_out_w"][0]
    pos = np.arange(S, dtype=f)
    inv = np.power(f(500000.0), -np.arange(0, 16, 2, dtype=f) / f(16)).astype(f)
    ang = pos[None, :] * inv[:, None]
    C = np.ones((128, S), f)
    Sg = np.zeros((128, S), f)
    Rm = np.zeros((128, 128), f)
    for h in range(2):
        for e in range(16):
            C[h * 64 + e] = np.cos(ang[e % 8])
            Sg[h * 64 + e] = (-np.sin(ang[e % 8])) if e < 8 else np.sin(ang[e % 8])
            Rm[h * 64 + (e + 8 if e < 8 else e - 8), h * 64 + e] = 1.0
    m["ropeC"], m["ropeS"] = C, Sg
    kk = np.arange(128)[:, None]
    qq = np.arange(128)[None, :]
    NEG = f(-30000.0)
    Mprev = np.where(kk >= qq, f(0), NEG).astype(f)
    Mcur = np.where(kk <= qq, f(0), NEG).astype(f)
    o0 = np.zeros((128, 128), f)
    o0[:, :64] = 1
    o1 = np.zeros((128, 128), f)
    o1[:, 64:] = 1
    m["aconst"] = np.ascontiguousarray(np.concatenate([Rm, Mprev, Mcur, o0, o1], axis=1))
    m["fing"] = inp["final_norm_g"].reshape(1, D)
    return m


def kernel(**inputs):
    NB, S = 2, 4096
    nc = build(NB, S)
    in_maps = [host_inputs(inputs, c, NB, S) for c in range(NCORES)]
    res = run_bass_kernel_spmd(nc, in_maps, core_ids=list(range(NCORES)))
    out = np.stack([r["out"].reshape(NB, S, D) for r in res.results]).reshape(NCORES * NB, S, D)
    return out.astype(np.float32)
```

```python
import contextlib
import os
import numpy as np
import concourse.bass as bass
import concourse.mybir as mybir
from concourse.bass_utils import run_bass_kernel_spmd

F32 = mybir.dt.float32
BF16 = mybir.dt.bfloat16
I32 = mybir.dt.int32
AF = mybir.ActivationFunctionType
ALU = mybir.AluOpType
AX = mybir.AxisListType

D = 1024
NCORES = 8
ENGS = ("pe", "act", "dve", "pool", "sp")
SAME_ENG_SYNC = True
DYN_SKIP = False


class Buf:
    __slots__ = ("name", "lw", "rd")

    def __init__(self, name=""):
        self.name = name
        self.lw = None
        self.rd = {}


class DSem:
    def __init__(self, h):
        self.h = h
        self.groups = []


class Ins:
    __slots__ = ("eng", "fn", "dsem", "signal", "sig", "target", "deps", "gk", "scope", "guard")


class Sched:
    def __init__(self):
        self.L = {e: [] for e in ENGS}
        self.n = 0
        self.dsems = []
        self.scope = None
        self.trace_scopes = False
        self.guard = None


    def dsem(self, h):
        d = DSem(h)
        self.dsems.append(d)
        return d

    def add(self, eng, fn, reads=(), writes=(), dsem=None, grp=None):
        ins = Ins()
        ins.eng, ins.fn, ins.dsem = eng, fn, dsem
        ins.signal, ins.sig, ins.target = False, 0, 0
        ins.gk = (id(dsem), grp) if (dsem is not None and grp is not None) else None
        ins.scope = self.scope
        ins.guard = self.guard
        deps = {}

        def need(d, kind):
            if d.dsem is not None:
                return not (ins.gk is not None and d.gk == ins.gk)
            if d.eng == eng:
                if dsem is not None:
                    return True
                if eng == "pe":
                    return False
                return kind == "raw" and SAME_ENG_SYNC
            return True

        for b in reads:
            if b.lw is not None and need(b.lw, "raw"):
                deps[id(b.lw)] = b.lw
        for b in writes:
            if b.lw is not None and need(b.lw, "waw"):
                deps[id(b.lw)] = b.lw
            for r in b.rd.values():
                if need(r, "war"):
                    deps[id(r)] = r
        if dsem is not None:
            assert getattr(dsem, "sw", False) == (eng == "pool"), f"DMA semaphore class mismatch on {eng}"
            g = dsem.groups
            if g and grp is not None and g[-1][0] == grp:
                g[-1][1].append(ins)
            else:
                if g:
                    prev = g[-1][1][-1]
                    deps[id(prev)] = prev
                g.append((grp if grp is not None else object(), [ins]))
        ins.deps = list(deps.values())
        for d in ins.deps:
            d.signal = True
        for b in reads:
            key = eng if dsem is None else ("dma", self.n)
            b.rd[key] = ins
        for b in writes:
            b.lw = ins
            b.rd = {}
        self.L[eng].append(ins)
        self.n += 1
        return ins

    def barrier(self):
        lasts = []
        for e in ENGS:
            for ins in reversed(self.L[e]):
                if ins.fn is not None and ins.dsem is None:
                    lasts.append(ins)
                    break
        for ds in self.dsems:
            if ds.groups:
                lasts.append(ds.groups[-1][1][-1])
        for e in ENGS:
            ins = Ins()
            ins.eng, ins.fn, ins.dsem = e, None, None
            ins.signal, ins.sig, ins.target = False, 0, 0
            ins.gk = None
            ins.scope = self.scope
            ins.guard = None
            ins.deps = [d for d in lasts if not (d.dsem is None and d.eng == e)]
            for d in ins.deps:
                d.signal = True
            self.L[e].append(ins)

    def simulate(self):
        for e in ENGS:
            c = 0
            for ins in self.L[e]:
                if ins.dsem is None and ins.signal:
                    c += 1
                    ins.sig = c
        for ds in self.dsems:
            tot = 0
            for _, lst in ds.groups:
                tot += 16 * len(lst)
                for i in lst:
                    i.target = tot
        pc = {e: 0 for e in ENGS}
        ev = {e: 0 for e in ENGS}
        dv = {id(ds): 0 for ds in self.dsems}
        prog = True
        while prog:
            prog = False
            for e in ENGS:
                while pc[e] < len(self.L[e]):
                    ins = self.L[e][pc[e]]
                    ok = True
                    for d in ins.deps:
                        if d.dsem is not None:
                            if dv[id(d.dsem)] < d.target:
                                ok = False
                        elif ev[d.eng] < d.sig:
                            ok = False
                    if not ok:
                        break
                    if ins.fn is not None:
                        if ins.dsem is not None:
                            dv[id(ins.dsem)] += 16
                        elif ins.signal:
                            ev[e] += 1
                    pc[e] += 1
                    prog = True
        stuck = {e: (pc[e], len(self.L[e])) for e in ENGS if pc[e] < len(self.L[e])}
        return stuck

    def emit(self, nc, esem):
        for e in ENGS:
            c = 0
            for ins in self.L[e]:
                if ins.dsem is None and ins.signal:
                    c += 1
                    ins.sig = c
        for ds in self.dsems:
            tot = 0
            for _, lst in ds.groups:
                tot += 16 * len(lst)
                for i in lst:
                    i.target = tot

        def emit_one(e, eo, ins, seen):
            for d in ins.deps:
                if d.dsem is not None:
                    key, val, h = ("d", id(d.dsem)), d.target, d.dsem.h
                else:
                    key, val, h = ("e", d.eng), d.sig, esem[d.eng]
                if seen.get(key, 0) < val:
                    eo.wait_ge(h, val)
                    seen[key] = val
            if ins.fn is None:
                return
            r = ins.fn(eo)
            if ins.dsem is not None:
                r.then_inc(ins.dsem.h, 16)
            elif ins.signal:
                r.then_inc(esem[e], 1)

        def run(e, eo):
            seen = {}
            cur = [None, None]
            L_ = self.L[e]
            i = 0
            while i < len(L_):
                ins = L_[i]
                if self.trace_scopes and ins.scope != cur[0]:
                    if cur[1] is not None:
                        cur[1].__exit__(None, None, None)
                    cur[0] = ins.scope
                    cur[1] = nc.named_scope(f"{ins.scope}") if ins.scope else None
                    if cur[1] is not None:
                        cur[1].__enter__()
                if ins.guard is None:
                    emit_one(e, eo, ins, seen)
                    i += 1
                    continue
                g = ins.guard
                j = i
                while j < len(L_) and L_[j].guard is g and L_[j].scope == ins.scope:
                    j += 1
                region = L_[i:j]
                nsig = sum(1 for x in region if x.dsem is None and x.signal and x.fn is not None)
                dcount = {}
                for x in region:
                    if x.dsem is not None:
                        dcount[id(x.dsem)] = (x.dsem, dcount.get(id(x.dsem), (x.dsem, 0))[1] + 16)
                regs, thresh = g
                snap = dict(seen)
                with eo.If_lt(regs[e], thresh + 1):
                    for _ in range(nsig):
                        eo.nop(nofuse=True).then_inc(esem[e], 1)
                    for ds, n in dcount.values():
                        for _ in range(n // 16):
                            eo.nop(nofuse=True).then_inc(ds.h, 16)
                with eo.Else():
                    for x in region:
                        emit_one(e, eo, x, seen)
                seen = snap
                i = j
            if cur[1] is not None:
                cur[1].__exit__(None, None, None)

        with nc.Block() as block:
            @block.sync
            def _(eo):
                run("sp", eo)

            @block.tensor
            def _(eo):
                run("pe", eo)

            @block.scalar
            def _(eo):
                run("act", eo)

            @block.vector
            def _(eo):
                run("dve", eo)

            @block.gpsimd
            def _(eo):
                run("pool", eo)


class K:
    def __init__(self, NB, S, dbg=None):
        self.NB, self.S, self.T = NB, S, NB * S
        self.dbg = dbg
        self.nc = bass.Bass("TRN2", target_bir_lowering=False)
        self.ctx = contextlib.ExitStack()
        self.s = Sched()
        self.pctx = contextlib.ExitStack()
        self.nsem = 0
        self.dpool = []
        self.dcur = 0
        self.dpool_sw = []
        self.dcur_sw = 0
        self._regs = None

    def din(self, name, shape, dt=F32):
        return self.nc.dram_tensor(name, list(shape), dt, kind="ExternalInput").ap()

    def dout(self, name, shape, dt=F32):
        return self.nc.dram_tensor(name, list(shape), dt, kind="ExternalOutput").ap()

    def dscr(self, name, shape, dt=F32):
        return self.nc.dram_tensor(name, list(shape), dt, kind="Internal").ap()

    def phase(self):
        self.s.barrier()
        self.pctx.close()
        self.pctx = contextlib.ExitStack()
        self.dcur = 0
        self.dcur_sw = 0

    def sbp(self, name, shape, dt=F32):
        return self.pctx.enter_context(self.nc.sbuf_tensor("s_" + name, list(shape), dt))

    def psp(self, name, shape, dt=F32):
        return self.pctx.enter_context(self.nc.psum_tensor("p_" + name, list(shape), dt))

    def sb(self, name, shape, dt=F32):
        return self.ctx.enter_context(self.nc.sbuf_tensor("s_" + name, list(shape), dt))

    def ps(self, name, shape, dt=F32):
        return self.ctx.enter_context(self.nc.psum_tensor("p_" + name, list(shape), dt))

    def sem(self, name):
        self.nsem += 1
        return self.ctx.enter_context(self.nc.semaphore(name))

    def nused_regs(self):
        if self._regs is None:
            nc = self.nc
            eng = {"pe": nc.tensor, "act": nc.scalar, "dve": nc.vector, "pool": nc.gpsimd, "sp": nc.sync}
            self._regs = {e: self.ctx.enter_context(eng[e].register("nused_" + e)) for e in ENGS}
        return self._regs

    def dsem(self, name, sw=False):
        pool, cur = (self.dpool_sw, self.dcur_sw) if sw else (self.dpool, self.dcur)
        if cur < len(pool):
            d = pool[cur]
        else:
            d = self.s.dsem(self.sem(f"dma{'s' if sw else 'h'}{len(pool)}"))
            d.sw = sw
            pool.append(d)
        if sw:
            self.dcur_sw += 1
        else:
            self.dcur += 1
        return d

    def dma(self, eng, out, in_, dsem, reads=(), writes=(), grp=None, **kw):
        return self.s.add(eng, lambda e: e.dma_start(out=out, in_=in_, **kw), reads, writes, dsem=dsem, grp=grp)

    def mm(self, out, lhsT, rhs, start, stop, reads=(), writes=()):
        return self.s.add("pe", lambda e: e.matmul(out, lhsT=lhsT, rhs=rhs, start=start, stop=stop), reads, writes)

    def tr(self, out, in_, ident, reads=(), writes=()):
        return self.s.add("pe", lambda e: e.transpose(out=out, in_=in_, identity=ident), reads, writes)

    def op(self, eng, fn, reads=(), writes=()):
        return self.s.add(eng, fn, reads, writes)


def build(NB, S, stage="all", trace_scopes=False):
    k = K(NB, S)
    k.s.trace_scopes = trace_scopes
    nc, s = k.nc, k.s
    T = NB * S
    NT = T // 128
    NG = T // 512
    GPB = S // 512

    x_in = k.din("x", [T, D])
    cT_in = k.din("cT", [128, 8 * NB])
    ada_w = k.din("ada_w", [2, D, 6 * D])
    ada_b = k.din("ada_b", [2, 6 * D])
    gmixF = k.din("gmixF", [2, 128, 8])
    gffn = k.din("gffn", [2, D])
    conv_in_w = k.din("conv_in_w", [D, 3 * D])
    convwF = k.din("convwF", [128, 24])
    conv_out_w = k.din("conv_out_w", [D, D])
    ident_in = k.din("ident", [128, 128])
    tri_in = k.din("tri", [128, 128])
    Wr_in = k.din("Wr", [2, D, 36])
    br_in = k.din("br", [2, 36])
    exp_gate_w = k.din("exp_gate_w", [2 * 32 * 128, 8 * 512])
    exp_up_w = k.din("exp_up_w", [2 * 32 * 128, 8 * 512])
    exp_down_w = k.din("exp_down_w", [2 * 32 * 128, 4 * D])
    pcol_in = k.din("pcol", [128, 1])
    attn_in_w = k.din("attn_in_w", [D, 4608])
    attn_out_w = k.din("attn_out_w", [512, D])
    ropeC_in = k.din("ropeC", [128, S])
    ropeS_in = k.din("ropeS", [128, S])
    aconst_in = k.din("aconst", [128, 640])
    fing_in = k.din("fing", [1, D])
    out_ap = k.dout("out", [T, D])
    NBLK = (2 * T) // 512 + 32
    hs = k.dscr("hs", [T, D], BF16)
    xs = k.dscr("xs", [NBLK * 512, D], BF16)
    ys = k.dscr("ys", [NBLK * 512, D], F32)
    xr = k.dout("xr", [T, D])
    modrow = k.dout("modrow", [2, NB, 6 * D])

    ident_f = k.sb("ident_f", [128, 128], F32)
    ident_b = k.sb("ident_b", [128, 128], BF16)
    cT = k.sb("cT", [128, 8 * NB], F32)
    B_ident_f, B_ident_b, B_cT = Buf(), Buf(), Buf()
    B_modrow = [Buf() for _ in range(2)]

    ld0 = k.dsem("ld0")
    k.dma("sp", ident_f[:], ident_in[:, :], ld0, writes=[B_ident_f], grp="init")
    k.dma("sp", cT[:], cT_in[:, :], ld0, writes=[B_cT], grp="init")
    k.op("dve", lambda e: e.tensor_copy(out=ident_b[:], in_=ident_f[:]), [B_ident_f], [B_ident_b])
    k.op("act", lambda e: e.activation(out=cT[:], in_=cT[:], func=AF.Silu), [B_cT], [B_cT])

    s.scope = "A_mod"
    NSA = 3
    aw = [k.sbp(f"aw{i}", [128, 8, 512], F32) for i in range(NSA)]
    adab = [k.sbp(f"adab{i}", [NB, 512], F32) for i in range(NSA)]
    modc = [k.sbp(f"modc{i}", [NB, 512], F32) for i in range(NSA)]
    B_aw, B_adab, B_modc = [Buf() for _ in range(NSA)], [Buf() for _ in range(NSA)], [Buf() for _ in range(NSA)]
    aw_sem = [k.dsem(f"aw{i}") for i in range(NSA)]
    adab_sem = [k.dsem(f"adab{i}") for i in range(NSA)]
    mod_st = [k.dsem(f"mod_st{i}", sw=True) for i in range(NSA)]
    ps_mod = [k.psp(f"ps_mod{i}", [128, 512], F32) for i in range(2)]
    B_psmod = [Buf(), Buf()]
    chunks = [(l, cc) for l in range(2) for cc in range(12)]

    def a_load(ci):
        l, cc = chunks[ci]
        sl = ci % NSA
        src_b = ada_b[l:l + 1, cc * 512:(cc + 1) * 512]
        k.dma("sp", adab[sl][:], src_b.partition_broadcast(NB) if NB > 1 else src_b, adab_sem[sl],
              writes=[B_adab[sl]])
        k.dma("sp", aw[sl][:], ada_w[l, :, cc * 512:(cc + 1) * 512].rearrange("(kc p) f -> p kc f", p=128),
              aw_sem[sl], writes=[B_aw[sl]])
    for ci in range(NSA - 1):
        a_load(ci)
    for ci, (l, cc) in enumerate(chunks):
        sl = ci % NSA
        pq = ci % 2
        if ci + NSA - 1 < len(chunks):
            a_load(ci + NSA - 1)
        for kc in range(8):
            k.mm(ps_mod[pq][0:NB, :], cT[:, kc * NB:(kc + 1) * NB], aw[sl][:, kc, :], kc == 0, kc == 7,
                 reads=[B_cT, B_aw[sl]], writes=[B_psmod[pq]])
        k.op("dve", lambda e, sl=sl, pq=pq: e.tensor_tensor(out=modc[sl][:], in0=ps_mod[pq][0:NB, :], in1=adab[sl][:],
                                                           op=ALU.add),
             [B_psmod[pq], B_adab[sl]], [B_modc[sl]])
        k.dma("pool", modrow[l, :, cc * 512:(cc + 1) * 512], modc[sl][:], mod_st[sl], reads=[B_modc[sl]],
              writes=[B_modrow[l]])
    k.phase()
    if stage == "A":
        return finish(k, [B_modrow[0], B_modrow[1]])

    s.scope = "L0_conv"
    L = 0
    w_in = k.sbp("w_in", [128, 8, 3 * D], BF16)
    w_stage = [k.sbp(f"w_stage{i}", [128, D], F32) for i in range(2)]
    w_out_b = [k.sbp(f"w_out_b{b}", [128, 8, D], BF16) for b in range(NB)]
    g1bc = [k.sbp(f"g1bc{b}", [128, D], F32) for b in range(NB)]
    A1 = [k.sbp(f"A1_{b}", [128, 8], F32) for b in range(NB)]
    sh1 = [k.sbp(f"sh1_{b}", [128, 8], F32) for b in range(NB)]
    sc1t = k.sbp("sc1t", [128, 8], F32)
    gmix = k.sbp("gmix", [128, 8], F32)
    convw = k.sbp("convw", [128, 24], F32)
    B_w_in, B_sc1t, B_gmix, B_convw = Buf(), Buf(), Buf(), Buf()
    B_w_stage = [Buf(), Buf()]
    B_g1bc = [Buf() for _ in range(NB)]
    B_w_out_b = [Buf() for _ in range(NB)]
    B_A1 = [Buf() for _ in range(NB)]
    B_sh1 = [Buf() for _ in range(NB)]
    wl = k.dsem("wl", sw=True)
    wst = [k.dsem("wst0"), k.dsem("wst1")]
    vl = k.dsem("vl")
    for kc in range(8):
        k.dma("pool", w_in[:, kc, :], conv_in_w[kc * 128:(kc + 1) * 128, :], wl, writes=[B_w_in], grp="w_in",
              max_dma_last_dim=4096)
    k.dma("sp", gmix[:], gmixF[L], vl, writes=[B_gmix], grp="v0")
    k.dma("sp", convw[:], convwF[:, :], vl, writes=[B_convw], grp="v0")
    for b in range(NB):
        k.dma("sp", sh1[b][:], modrow[L, b, 0:D].rearrange("(c p) -> p c", p=128), vl,
              reads=[B_modrow[L]], writes=[B_sh1[b]], allow_slow_non_contiguous=True)
        k.dma("sp", sc1t[:], modrow[L, b, D:2 * D].rearrange("(c p) -> p c", p=128), vl,
              reads=[B_modrow[L]], writes=[B_sc1t], allow_slow_non_contiguous=True)
        k.op("dve", lambda e, b=b: e.scalar_tensor_tensor(out=A1[b][:], in0=sc1t[:], scalar=1.0, in1=gmix[:],
                                                         op0=ALU.add, op1=ALU.mult),
             [B_sc1t, B_gmix], [B_A1[b]])
        k.dma("sp", g1bc[b][:], modrow[L, b:b + 1, 2 * D:3 * D].partition_broadcast(128), vl,
              reads=[B_modrow[L]], writes=[B_g1bc[b]])
    for kc in range(8):
        sl = kc % 2
        k.dma("sp", w_stage[sl][:], conv_out_w[kc * 128:(kc + 1) * 128, :], wst[sl], writes=[B_w_stage[sl]])
        for b in range(NB):
            k.op("dve", lambda e, b=b, kc=kc, sl=sl: e.tensor_tensor(out=w_out_b[b][:, kc, :], in0=w_stage[sl][:],
                                                                    in1=g1bc[b][:], op=ALU.mult),
                 [B_w_stage[sl], B_g1bc[b]], [B_w_out_b[b]])

    if stage == "L0setup":
        return finish(k, [B_modrow[0], B_modrow[1]] + B_w_out_b + B_A1 + B_sh1 + [B_w_in, B_convw])
    NXS = 3
    xt = [k.sbp(f"xt{i}", [128, 4, D], F32) for i in range(NXS)]
    B_xt = [Buf() for _ in range(NXS)]
    xt_ld = [k.dsem(f"xt_ld{i}") for i in range(NXS)]
    xt_st = [k.dsem(f"xt_st{i}") for i in range(NXS)]
    junk = k.sbp("junk", [128, D], BF16)
    B_junk = Buf()
    ss = k.sbp("ss", [128, 4], F32)
    rstd = k.sbp("rstd", [128, 4], F32)
    B_ss, B_rstd = Buf(), Buf()
    xn = [k.sbp(f"xn{i}", [128, 4, D], BF16) for i in range(2)]
    B_xn = [[Buf() for _ in range(4)] for _ in range(2)]
    pT = k.psp("pT", [128, 512], BF16)
    B_pT = Buf()
    hT = [k.sbp(f"hT{i}", [128, 8, 512], BF16) for i in range(2)]
    B_hT = [[Buf() for _ in range(8)] for _ in range(2)]
    psBCU = [[k.psp(f"ps{n}{i}", [128, 512]) for n in "BCU"] for i in range(2)]
    B_psBCU = [[Buf() for _ in range(3)] for _ in range(2)]
    Csb = [k.sbp(f"Csb{i}", [128, 512], F32) for i in range(2)]
    B_Csb = [Buf(), Buf()]
    zb = [k.sbp(f"zb{i}", [128, 514], F32) for i in range(2)]
    B_zb = [Buf(), Buf()]
    zh = k.sbp("zh", [128, 8, 2], F32)
    B_zh = [Buf() for _ in range(8)]
    zc = [k.sbp(f"zc{i}", [128, 512], F32) for i in range(2)]
    B_zc = [Buf(), Buf()]
    gT = [k.sbp(f"gT{i}", [128, 8, 512], BF16) for i in range(2)]
    B_gT = [[Buf() for _ in range(8)] for _ in range(2)]
    psY = k.psp("psY", [128, 512])
    B_psY = Buf()
    B_xr = [Buf() for _ in range(NG)]

    def load_x(G):
        sl = G % NXS
        k.dma("sp", xt[sl][:], x_in[G * 512:(G + 1) * 512, :].rearrange("(j p) d -> p j d", p=128),
              xt_ld[sl], writes=[B_xt[sl]])

    def norm_pre(G):
        sl = G % 2
        xt_t, B_x = xt[G % NXS], B_xt[G % NXS]
        for j in range(4):
            k.op("act", lambda e, j=j: e.activation(out=junk[:], in_=xt_t[:, j, :], func=AF.Square,
                                                    accum_out=ss[:, j:j + 1]),
                 [B_x], [B_junk, B_ss])
        k.op("dve", lambda e: e.tensor_scalar(out=rstd[:], in0=ss[:], scalar1=1.0 / D, scalar2=1e-6,
                                              op0=ALU.mult, op1=ALU.add), [B_ss], [B_rstd])
        k.op("act", lambda e: e.activation(out=rstd[:], in_=rstd[:], func=AF.Sqrt), [B_rstd], [B_rstd])
        k.op("dve", lambda e: e.reciprocal(out=rstd[:], in_=rstd[:]), [B_rstd], [B_rstd])
        for j in range(4):
            k.op("pool", lambda e, j=j: e.tensor_scalar(out=xn[sl][:, j, :], in0=xt_t[:, j, :],
                                                        scalar1=rstd[:, j:j + 1], scalar2=1.0,
                                                        op0=ALU.mult, op1=ALU.mult),
                 [B_x, B_rstd], [B_xn[sl][j]])

    def norm_tr(G, c):
        b = G // GPB
        sl = G % 2
        A, B_A, sh, B_sh = A1[b], B_A1[b], sh1[b], B_sh1[b]
        for j in range(4):
            k.tr(pT[:, j * 128:(j + 1) * 128], xn[sl][:, j, c * 128:(c + 1) * 128], ident_b[:],
                 reads=[B_xn[sl][j], B_ident_b], writes=[B_pT])
        k.op("act", lambda e, c=c: e.activation(out=hT[sl][:, c, :], in_=pT[:], func=AF.Identity,
                                                scale=A[:, c:c + 1], bias=sh[:, c:c + 1]),
             [B_pT, B_A, B_sh], [B_hT[sl][c]])

    def inproj(G, fcs):
        sl = G % 2
        first = (G % GPB == 0)
        for fc in fcs:
            q = fc % 2
            (psB, psC, psU), (B_psB, B_psC, B_psU) = psBCU[q], B_psBCU[q]
            for (pst, B_p, off) in ((psB, B_psB, 0), (psC, B_psC, D), (psU, B_psU, 2 * D)):
                for kc in range(8):
                    k.mm(pst[:], w_in[:, kc, off + fc * 128: off + (fc + 1) * 128], hT[sl][:, kc, :], kc == 0, kc == 7,
                         reads=[B_w_in, B_hT[sl][kc]], writes=[B_p])
            zt, Bz, zcb, Bzc, Cs, BCs = zb[q], B_zb[q], zc[q], B_zc[q], Csb[q], B_Csb[q]
            if first:
                k.op("pool", lambda e, zt=zt: e.memset(zt[:, 0:2], 0.0), [], [Bz])
            else:
                k.op("pool", lambda e, zt=zt, fc=fc: e.tensor_copy(out=zt[:, 0:2], in_=zh[:, fc, :]),
                     [B_zh[fc]], [Bz])
            k.op("act", lambda e, Cs=Cs, psC=psC: e.copy(out=Cs[:], in_=psC[:]), [B_psC], [BCs])
            k.op("dve", lambda e, zt=zt, Cs=Cs, psU=psU: e.tensor_tensor(out=zt[:, 2:514], in0=Cs[:], in1=psU[:],
                                                                        op=ALU.mult),
                 [BCs, B_psU, Bz], [Bz])
            k.op("pool", lambda e, zt=zt, fc=fc: e.tensor_copy(out=zh[:, fc, :], in_=zt[:, 512:514]),
                 [Bz], [B_zh[fc]])
            k.op("pool", lambda e, fc=fc, zt=zt, zcb=zcb: e.tensor_scalar(
                out=zcb[:], in0=zt[:, 2:514], scalar1=convw[:, fc * 3 + 2: fc * 3 + 3], scalar2=1.0,
                op0=ALU.mult, op1=ALU.mult), [Bz, B_convw], [Bzc])
            k.op("dve", lambda e, fc=fc, zt=zt, zcb=zcb: e.scalar_tensor_tensor(
                out=zcb[:], in0=zt[:, 1:513], scalar=convw[:, fc * 3 + 1: fc * 3 + 2], in1=zcb[:],
                op0=ALU.mult, op1=ALU.add), [Bz, B_convw, Bzc], [Bzc])
            k.op("dve", lambda e, fc=fc, zt=zt, zcb=zcb: e.scalar_tensor_tensor(
                out=zcb[:], in0=zt[:, 0:512], scalar=convw[:, fc * 3: fc * 3 + 1], in1=zcb[:],
                op0=ALU.mult, op1=ALU.add), [Bz, B_convw, Bzc], [Bzc])
            k.op("dve", lambda e, fc=fc, zcb=zcb, psB=psB, sl=sl: e.tensor_tensor(out=gT[sl][:, fc, :], in0=zcb[:],
                                                                                 in1=psB[:], op=ALU.mult),
                 [Bzc, B_psB], [B_gT[sl][fc]])

    def outproj_unit(G, u):
        b = G // GPB
        sl = G % 2
        xs_ = G % NXS
        j, dh = u // 2, u % 2
        for kc in range(8):
            k.mm(psY[:], gT[sl][:, kc, j * 128:(j + 1) * 128], w_out_b[b][:, kc, dh * 512:(dh + 1) * 512],
                 kc == 0, kc == 7, reads=[B_gT[sl][kc], B_w_out_b[b]], writes=[B_psY])
        k.op("dve", lambda e: e.tensor_tensor(
            out=xt[xs_][:, j, dh * 512:(dh + 1) * 512], in0=psY[:], in1=xt[xs_][:, j, dh * 512:(dh + 1) * 512],
            op=ALU.add), [B_psY, B_xt[xs_]], [B_xt[xs_]])
        if u == 7:
            k.dma("sp", xr[G * 512:(G + 1) * 512, :].rearrange("(j p) d -> p j d", p=128), xt[xs_][:], xt_st[xs_],
                  reads=[B_xt[xs_]], writes=[B_xr[G]])

    load_x(0)
    if NG > 1:
        load_x(1)
    norm_pre(0)
    for c in range(8):
        norm_tr(0, c)
    for G in range(NG):
        if G + 1 < NG:
            norm_pre(G + 1)
        for fc in range(8):
            inproj(G, [fc])
            if G + 1 < NG:
                norm_tr(G + 1, fc)
            if G > 0:
                outproj_unit(G - 1, fc)
        if G + 2 < NG:
            load_x(G + 2)
    for u in range(8):
        outproj_unit(NG - 1, u)

    if stage == "mix0":
        return finish(k, [B_xr[G] for G in range(NG)])
    k.phase()
    dbg = k.dout("dbg", [128, 4096]) if stage.startswith("M") else None
    r = moe_layer(k, 0, dict(stage=stage, dbg=dbg, xr=xr, B_xr=B_xr, modrow=modrow, B_modrow=B_modrow, gffn=gffn, Wr_in=Wr_in, br_in=br_in,
                         tri_in=tri_in, ident_f=ident_f, B_ident_f=B_ident_f, ident_b=ident_b, B_ident_b=B_ident_b,
                         hs=hs, xs=xs, ys=ys, gw=exp_gate_w, uw=exp_up_w, dw=exp_down_w, NBLK=NBLK, pcol_in=pcol_in))
    if r is not None:
        return finish(k, [B_xr[G] for G in range(NG)] + r)
    if stage == "ffn0":
        return finish(k, [B_xr[G] for G in range(NG)])
    k.phase()
    attn_layer(k, dict(xr=xr, B_xr=B_xr, modrow=modrow, B_modrow=B_modrow, gmixF=gmixF, ident_b=ident_b,
                       B_ident_b=B_ident_b, w_in=attn_in_w, w_out=attn_out_w, ropeC=ropeC_in, ropeS=ropeS_in,
                       aconst=aconst_in, pcol_in=pcol_in))
    if stage == "mix1":
        return finish(k, [B_xr[G] for G in range(NG)])
    k.phase()
    B_out = [Buf() for _ in range(NG)]
    moe_layer(k, 1, dict(stage=stage, dbg=None, final=dict(fing=fing_in, out=out_ap, B_out=B_out), xr=xr, B_xr=B_xr, modrow=modrow, B_modrow=B_modrow, gffn=gffn,
                         Wr_in=Wr_in, br_in=br_in, tri_in=tri_in, ident_f=ident_f, B_ident_f=B_ident_f,
                         ident_b=ident_b, B_ident_b=B_ident_b, hs=hs, xs=xs, ys=ys, gw=exp_gate_w, uw=exp_up_w,
                         dw=exp_down_w, NBLK=NBLK, pcol_in=pcol_in))
    return finish(k, [B_out[G] for G in range(NG)])


DIL = (1, 4, 16)
import os
SKIP = os.environ.get("ATT_SKIP", "").split(",")


def attn_layer(k, a):
    L = 1
    NB, S, T = k.NB, k.S, k.T
    GPB = S // 512
    xr, B_xr, modrow, B_modrow = a["xr"], a["B_xr"], a["modrow"], a["B_modrow"]
    ident_b, B_ident_b = a["ident_b"], a["B_ident_b"]
    w_in, w_out = a["w_in"], a["w_out"]
    hT = k.sbp("a_hT", [128, 8, S], BF16)
    oT = k.sbp("a_oT", [128, 4, S], BF16)
    B_hT = [[Buf() for _ in range(GPB)] for _ in range(8)]
    B_oT = [Buf() for _ in range(4)]
    vl = k.dsem("a_vl")
    for b in range(NB):
        k.s.scope = f"att{b}_1_hT"
        with contextlib.ExitStack() as c1:
            def sb1(name, shape, dt=F32):
                return c1.enter_context(k.nc.sbuf_tensor(f"s_a1_{b}_{name}", list(shape), dt))
            A1, sh1, sc1t, gmix = sb1("A1", [128, 8]), sb1("sh1", [128, 8]), sb1("sc1t", [128, 8]), sb1("gmix", [128, 8])
            B_A1, B_sh1, B_sc1t, B_gmix = Buf(), Buf(), Buf(), Buf()
            k.dma("sp", gmix[:], a["gmixF"][L], vl, writes=[B_gmix])
            k.dma("sp", sh1[:], modrow[L, b, 0:D].rearrange("(c p) -> p c", p=128), vl,
                  reads=[B_modrow[L]], writes=[B_sh1], allow_slow_non_contiguous=True)
            k.dma("sp", sc1t[:], modrow[L, b, D:2 * D].rearrange("(c p) -> p c", p=128), vl,
                  reads=[B_modrow[L]], writes=[B_sc1t], allow_slow_non_contiguous=True)
            k.op("dve", lambda e: e.scalar_tensor_tensor(out=A1[:], in0=sc1t[:], scalar=1.0, in1=gmix[:],
                                                         op0=ALU.add, op1=ALU.mult), [B_sc1t, B_gmix], [B_A1])
            xt = [sb1(f"xt{i}", [128, 4, D]) for i in range(2)]
            B_xt = [Buf(), Buf()]
            xt_ld = [k.dsem(f"a1_{b}_xt_ld0"), k.dsem(f"a1_{b}_xt_ld1")]
            junk = sb1("junk", [128, D], BF16)
            ss, rstd = sb1("ss", [128, 4]), sb1("rstd", [128, 4])
            xn = sb1("xn", [128, 4, D], BF16)
            B_junk, B_ss, B_rstd = Buf(), Buf(), Buf()
            B_xn = [Buf() for _ in range(4)]
            pT = [c1.enter_context(k.nc.psum_tensor(f"p_a1_{b}_pT{i}", [128, 512], BF16)) for i in range(2)]
            B_pT = [Buf(), Buf()]
            def a1_load(gi):
                G = b * GPB + gi
                sl = gi % 2
                k.dma("sp", xt[sl][:], xr[G * 512:(G + 1) * 512, :].rearrange("(j p) d -> p j d", p=128), xt_ld[sl],
                      reads=[B_xr[G]], writes=[B_xt[sl]])
            a1_load(0)
            for gi in range(GPB):
                G = b * GPB + gi
                sl = gi % 2
                if gi + 1 < GPB:
                    a1_load(gi + 1)
                xt_t, B_x = xt[sl], B_xt[sl]
                for j in range(4):
                    k.op("act", lambda e, j=j, xt_t=xt_t: e.activation(out=junk[:], in_=xt_t[:, j, :], func=AF.Square,
                                                                      accum_out=ss[:, j:j + 1]), [B_x], [B_junk, B_ss])
                k.op("dve", lambda e: e.tensor_scalar(out=rstd[:], in0=ss[:], scalar1=1.0 / D, scalar2=1e-6,
                                                      op0=ALU.mult, op1=ALU.add), [B_ss], [B_rstd])
                k.op("act", lambda e: e.activation(out=rstd[:], in_=rstd[:], func=AF.Sqrt), [B_rstd], [B_rstd])
                k.op("dve", lambda e: e.reciprocal(out=rstd[:], in_=rstd[:]), [B_rstd], [B_rstd])
                for j in range(4):
                    k.op("pool", lambda e, j=j, xt_t=xt_t: e.tensor_scalar(out=xn[:, j, :], in0=xt_t[:, j, :],
                                                                          scalar1=rstd[:, j:j + 1], scalar2=1.0,
                                                                          op0=ALU.mult, op1=ALU.mult),
                         [B_x, B_rstd], [B_xn[j]])
                for c in range(8):
                    p = c % 2
                    for j in range(4):
                        k.tr(pT[p][:, j * 128:(j + 1) * 128], xn[:, j, c * 128:(c + 1) * 128], ident_b[:],
                             reads=[B_xn[j], B_ident_b], writes=[B_pT[p]])
                    k.op("act", lambda e, c=c, p=p, gi=gi: e.activation(
                        out=hT[:, c, gi * 512:(gi + 1) * 512], in_=pT[p][:], func=AF.Identity,
                        scale=A1[:, c:c + 1], bias=sh1[:, c:c + 1]), [B_pT[p], B_A1, B_sh1], [B_hT[c][gi]])
            k.s.barrier()
        k.s.scope = f"att{b}_2_core"
        with contextlib.ExitStack() as c2:
            def sb2(name, shape, dt=F32):
                return c2.enter_context(k.nc.sbuf_tensor(f"s_a2_{b}_{name}", list(shape), dt))

            def ps2(name, shape, dt=F32):
                return c2.enter_context(k.nc.psum_tensor(f"p_a2_{b}_{name}", list(shape), dt))
            Ct, St = sb2("Ct", [128, S], BF16), sb2("St", [128, S], BF16)
            acb = sb2("acb", [128, 640], BF16)
            B_Ct, B_St, B_acb = Buf(), Buf(), Buf()
            vlp = k.dsem(f"a2_{b}_vlp", sw=True)
            k.dma("pool", Ct[:], a["ropeC"][:, :], vlp, writes=[B_Ct], max_dma_last_dim=4096)
            k.dma("pool", St[:], a["ropeS"][:, :], vlp, writes=[B_St], max_dma_last_dim=4096)
            k.dma("pool", acb[:], a["aconst"][:, :], vlp, writes=[B_acb])
            Rm, Mprev, Mcur = acb[:, 0:128], acb[:, 128:256], acb[:, 256:384]
            onesp = [acb[:, 384:512], acb[:, 512:640]]
            qz = [sb2(f"qz{h}", [128, S], BF16) for h in range(2)]
            kT = sb2("kT", [128, S], BF16)
            B_qz = [[Buf() for _ in range(GPB)] for _ in range(2)]
            B_kT = [Buf() for _ in range(GPB)]
            pc = sb2("pc", [128, 1])
            hm = sb2("hm", [128, 2])
            hmb = sb2("hmb", [128, 2], BF16)
            B_pc, B_hm = Buf(), Buf()
            k.dma("sp", pc[:], a["pcol_in"][:, :], vl, writes=[B_pc])
            k.op("dve", lambda e: e.tensor_scalar(out=hm[:, 0:1], in0=pc[:], scalar1=64.0, scalar2=None, op0=ALU.is_lt),
                 [B_pc], [B_hm])
            k.op("dve", lambda e: e.tensor_scalar(out=hm[:, 1:2], in0=pc[:], scalar1=64.0, scalar2=None, op0=ALU.is_ge),
                 [B_pc], [B_hm])
            k.op("dve", lambda e: e.tensor_copy(out=hmb[:], in_=hm[:]), [B_hm], [B_hm])
            NBK = S // 128
            Vp = sb2("Vp", [128, NBK, 2, 128], BF16)
            B_Vp = [Buf() for _ in range(NBK // 4)]
            k.op("pool", lambda e: e.memset(Vp[:].rearrange("p a h f -> p (a h f)"), 0.0), [], B_Vp)
            Nacc, Dacc = sb2("Nacc", [128, S]), sb2("Dacc", [128, S])
            B_Nacc, B_Dacc = Buf(), Buf()
            wq = [sb2(f"wq{i}", [128, 8, 384], BF16) for i in range(2)]
            B_wq = [Buf(), Buf()]
            wq_sem = [k.dsem(f"a2_{b}_wq0", sw=True), k.dsem(f"a2_{b}_wq1", sw=True)]
            qsb_ = [sb2(f"qsb{i}", [128, 512], BF16) for i in range(2)]
            t1s, t2s = sb2("t1s", [128, 512]), sb2("t2s", [128, 512])
            t1_, t2_ = [t1s, t1s], [t2s, t2s]
            Bt1, Bt2 = Buf(), Buf()
            B_qsb_, B_t1_, B_t2_ = [Buf(), Buf()], [Bt1, Bt1], [Bt2, Bt2]
            PT = [sb2(f"PT{i}", [128, 2, 2, 128], BF16) for i in range(2)]
            B_PT = [Buf(), Buf()]
            psQ_ = [ps2(f"psQ{i}", [128, 512]) for i in range(2)]
            psR0 = ps2("psR0", [128, 512])
            psR_ = [psR0, psR0]
            psV = ps2("psV", [128, 4, 128])
            psS = [ps2(f"psS{i}", [128, 2, 2, 128]) for i in range(2)]
            psND = [ps2(f"psND{i}", [128, 2, 128]) for i in range(2)]
            BpsR = Buf()
            B_psQ_, B_psR_, B_psV = [Buf(), Buf()], [BpsR, BpsR], Buf()
            B_psS, B_psND = [Buf(), Buf()], [Buf(), Buf()]
            pit = 0
            allhT = [B_hT[c][gi] for c in range(8) for gi in range(GPB)]
            it = 0

            def wq_load(hp, g):
                wsl = (hp * 3 + g) % 2
                for qi in range(3):
                    col = g * 1536 + qi * 512 + hp * 128
                    for kc in range(8):
                        k.dma("pool", wq[wsl][:, kc, qi * 128:(qi + 1) * 128],
                              w_in[kc * 128:(kc + 1) * 128, col:col + 128], wq_sem[wsl], writes=[B_wq[wsl]],
                              grp=("wq", hp, g))
            wq_load(0, 0)
            for hp in range(4):
                for g in range(3):
                    dil = DIL[g]
                    nb = S // dil // 128
                    wsl = (hp * 3 + g) % 2
                    nxt = hp * 3 + g + 1
                    if nxt < 12:
                        wq_load(nxt // 3, nxt % 3)
                    for qi in range(2 if "qk" not in SKIP else 0):
                        for tg in range(GPB):
                            pi = pit % 2
                            pit += 1
                            psQ, psR, qsb, t1, t2 = psQ_[pi], psR_[pi], qsb_[pi], t1_[pi], t2_[pi]
                            B_psQ, B_psR, B_qsb, B_t1, B_t2 = B_psQ_[pi], B_psR_[pi], B_qsb_[pi], B_t1_[pi], B_t2_[pi]
                            for kc in range(8):
                                k.mm(psQ[:], wq[wsl][:, kc, qi * 128:(qi + 1) * 128], hT[:, kc, tg * 512:(tg + 1) * 512],
                                     kc == 0, kc == 7, reads=[B_wq[wsl], B_hT[kc][tg]], writes=[B_psQ])
                            k.op("act", lambda e, qsb=qsb, psQ=psQ: e.copy(out=qsb[:], in_=psQ[:]), [B_psQ], [B_qsb])
                            k.mm(psR[:], Rm, qsb[:], True, True, reads=[B_acb, B_qsb], writes=[B_psR])
                            k.op("dve", lambda e, tg=tg, t1=t1, psQ=psQ: e.tensor_tensor(
                                out=t1[:], in0=psQ[:], in1=Ct[:, tg * 512:(tg + 1) * 512], op=ALU.mult),
                                 [B_psQ, B_Ct, B_qsb], [B_t1])
                            k.op("dve", lambda e, tg=tg, t2=t2, psR=psR: e.tensor_tensor(
                                out=t2[:], in0=psR[:], in1=St[:, tg * 512:(tg + 1) * 512], op=ALU.mult),
                                 [B_psR, B_St], [B_t2])
                            if qi == 0:
                                k.op("pool", lambda e, t1=t1, t2=t2, qsb=qsb: e.tensor_tensor(
                                    out=qsb[:], in0=t1[:], in1=t2[:], op=ALU.add), [B_t1, B_t2, B_qsb], [B_qsb])
                                for h in range(2):
                                    k.op("dve", lambda e, h=h, tg=tg, qsb=qsb: e.tensor_scalar(
                                        out=qz[h][:, tg * 512:(tg + 1) * 512], in0=qsb[:], scalar1=hmb[:, h:h + 1],
                                        scalar2=None, op0=ALU.mult), [B_qsb, B_hm], [B_qz[h][tg]])
                            else:
                                k.op("pool", lambda e, tg=tg, t1=t1, t2=t2: e.tensor_tensor(
                                    out=kT[:, tg * 512:(tg + 1) * 512], in0=t1[:], in1=t2[:], op=ALU.add),
                                     [B_t1, B_t2], [B_kT[tg]])
                    def tok(r, n):
                        st = r + n * 128 * dil
                        return slice(st, st + 127 * dil + 1, dil)
                    blks = [(r, n) for r in range(dil) for n in range(nb)]
                    for bi, (r, n) in enumerate(blks if "vproj" not in SKIP else []):
                        for kc in range(8):
                            k.mm(psV[:, bi % 4, :], hT[:, kc, tok(r, n)], wq[wsl][:, kc, 256:384], kc == 0, kc == 7,
                                 reads=[B_wq[wsl]] + [B_hT[kc][gi] for gi in range(GPB)], writes=[B_psV])
                        if bi % 4 == 3:
                            b4 = bi // 4
                            k.op("dve", lambda e, b4=b4: e.tensor_copy(out=Vp[:, b4 * 4:(b4 + 1) * 4, 0, 0:64],
                                                                      in_=psV[:, :, 0:64]), [B_psV], [B_Vp[b4]])
                            k.op("dve", lambda e, b4=b4: e.tensor_copy(out=Vp[:, b4 * 4:(b4 + 1) * 4, 1, 64:128],
                                                                      in_=psV[:, :, 64:128]), [B_psV], [B_Vp[b4]])
                    ablks = blks if "blocks" not in SKIP else []

                    def chunks_of(bi):
                        r, n = ablks[bi]
                        ch = [(1, tok(r, n), Mcur, bi)]
                        if n > 0:
                            ch.append((0, tok(r, n - 1), Mprev, bi - 1))
                        return ch

                    def emit_scores(bi, si):
                        r, n = ablks[bi]
                        cur = tok(r, n)
                        for h in range(2):
                            for (ci, ktok, Mk, vb) in chunks_of(bi):
                                k.mm(psS[si][:, h, ci, :], kT[:, ktok], qz[h][:, cur], True, False,
                                     reads=B_kT + B_qz[h], writes=[B_psS[si]])
                                k.mm(psS[si][:, h, ci, :], ident_b[:], Mk, False, True,
                                     reads=[B_ident_b, B_acb], writes=[B_psS[si]])

                    if ablks:
                        emit_scores(0, it % 2)
                    for bi, (r, n) in enumerate(ablks):
                        si = it % 2
                        it += 1
                        cur = tok(r, n)
                        chunks = chunks_of(bi)
                        if bi + 1 < len(ablks):
                            emit_scores(bi + 1, it % 2)
                        k.op("act", lambda e, si=si: e.activation(
                            out=PT[si][:].rearrange("p h c q -> p (h c q)"),
                            in_=psS[si][:].rearrange("p h c q -> p (h c q)"), func=AF.Exp, scale=0.125),
                            [B_psS[si]], [B_PT[si]])
                        nmm = 2 * len(chunks)
                        for which in range(2):
                            ii = 0
                            for h in range(2):
                                for (ci, ktok, Mk, vb) in chunks:
                                    lhs = Vp[:, vb, h, :] if which == 0 else onesp[h]
                                    k.mm(psND[si][:, which, :], lhs, PT[si][:, h, ci, :], ii == 0, ii == nmm - 1,
                                         reads=[B_Vp[vb // 4], B_PT[si], B_acb], writes=[B_psND[si]])
                                    ii += 1
                        if g == 0:
                            k.op("dve", lambda e, si=si, cur=cur: e.tensor_copy(out=Nacc[:, cur], in_=psND[si][:, 0, :]),
                                 [B_psND[si]], [B_Nacc])
                            k.op("dve", lambda e, si=si, cur=cur: e.tensor_copy(out=Dacc[:, cur], in_=psND[si][:, 1, :]),
                                 [B_psND[si]], [B_Dacc])
                        else:
                            k.op("dve", lambda e, si=si, cur=cur: e.tensor_tensor(out=Nacc[:, cur], in0=psND[si][:, 0, :],
                                                                                 in1=Nacc[:, cur], op=ALU.add),
                                 [B_psND[si], B_Nacc], [B_Nacc])
                            k.op("dve", lambda e, si=si, cur=cur: e.tensor_tensor(out=Dacc[:, cur], in0=psND[si][:, 1, :],
                                                                                 in1=Dacc[:, cur], op=ALU.add),
                                 [B_psND[si], B_Dacc], [B_Dacc])
                if "norm" in SKIP:
                    continue
                k.op("act", lambda e: e.activation(out=Dacc[:], in_=Dacc[:], func=AF.Ln), [B_Dacc], [B_Dacc])
                k.op("act", lambda e: e.activation(out=Dacc[:], in_=Dacc[:], func=AF.Exp, scale=-1.0),
                     [B_Dacc], [B_Dacc])
                k.op("dve", lambda e, hp=hp: e.tensor_tensor(out=oT[:, hp, :], in0=Nacc[:], in1=Dacc[:], op=ALU.mult),
                     [B_Nacc, B_Dacc], [B_oT[hp]])
            k.s.barrier()
        k.s.scope = f"att{b}_3_out"
        with contextlib.ExitStack() as c3:
            def sb3(name, shape, dt=F32):
                return c3.enter_context(k.nc.sbuf_tensor(f"s_a3_{b}_{name}", list(shape), dt))
            wof = sb3("wof", [128, 4, D])
            wob = sb3("wob", [128, 4, D], BF16)
            g1bc = sb3("g1bc", [128, D])
            B_wof, B_wob, B_g1bc = Buf(), Buf(), Buf()
            k.dma("sp", wof[:], w_out.rearrange("(c p) d -> p c d", p=128), vl, writes=[B_wof])
            k.dma("sp", g1bc[:], modrow[L, b:b + 1, 2 * D:3 * D].partition_broadcast(128), vl,
                  reads=[B_modrow[L]], writes=[B_g1bc])
            for c in range(4):
                k.op("dve", lambda e, c=c: e.tensor_tensor(out=wob[:, c, :], in0=wof[:, c, :], in1=g1bc[:], op=ALU.mult),
                     [B_wof, B_g1bc], [B_wob])
            xt = [sb3(f"xt{i}", [128, 4, D]) for i in range(3)]
            B_xt = [Buf(), Buf(), Buf()]
            xt_ld = [k.dsem(f"a3_{b}_xt_ld{i}") for i in range(3)]
            xt_st = [k.dsem(f"a3_{b}_xt_st{i}") for i in range(3)]
            psY = [c3.enter_context(k.nc.psum_tensor(f"p_a3_{b}_psY{i}", [128, 512], F32)) for i in range(2)]
            B_psY = [Buf(), Buf()]
            def a3_load(gi):
                G = b * GPB + gi
                sl = gi % 3
                k.dma("sp", xt[sl][:], xr[G * 512:(G + 1) * 512, :].rearrange("(j p) d -> p j d", p=128), xt_ld[sl],
                      reads=[B_xr[G]], writes=[B_xt[sl]])
            a3_load(0)
            for gi in range(GPB if "p3" not in SKIP else 0):
                G = b * GPB + gi
                sl = gi % 3
                if gi + 1 < GPB:
                    a3_load(gi + 1)
                for j in range(4):
                    for dh in range(2):
                        yi = (j * 2 + dh) % 2
                        t0 = gi * 512 + j * 128
                        for c in range(4):
                            k.mm(psY[yi][:], oT[:, c, t0:t0 + 128], wob[:, c, dh * 512:(dh + 1) * 512], c == 0, c == 3,
                                 reads=[B_oT[c], B_wob], writes=[B_psY[yi]])
                        k.op("dve", lambda e, j=j, dh=dh, yi=yi, sl=sl: e.tensor_tensor(
                            out=xt[sl][:, j, dh * 512:(dh + 1) * 512], in0=psY[yi][:],
                            in1=xt[sl][:, j, dh * 512:(dh + 1) * 512], op=ALU.add), [B_psY[yi], B_xt[sl]], [B_xt[sl]])
                k.dma("sp", xr[G * 512:(G + 1) * 512, :].rearrange("(j p) d -> p j d", p=128), xt[sl][:], xt_st[sl],
                      reads=[B_xt[sl]], writes=[B_xr[G]])
            k.s.barrier()


def moe_layer(k, L, a):
    NB, S, T = k.NB, k.S, k.T
    NT, NG, GPB = T // 128, T // 512, S // 512
    NBLK = a["NBLK"]
    xr, B_xr, modrow, B_modrow = a["xr"], a["B_xr"], a["modrow"], a["B_modrow"]
    ident_f, B_ident_f, ident_b, B_ident_b = a["ident_f"], a["B_ident_f"], a["ident_b"], a["B_ident_b"]
    hs, xs, ys = a["hs"], a["xs"], a["ys"]
    B_hs = [Buf() for _ in range(NG)]
    pfx = f"m{L}_"

    E1all = k.sbp(pfx + "E1all", [128, NT, 32], F32)
    E2all = k.sbp(pfx + "E2all", [128, NT, 32], F32)
    Oall = k.sbp(pfx + "Oall", [128, NT, 32], BF16)
    Wall = k.sbp(pfx + "Wall", [128, NT, 2], F32)
    B_E1 = [Buf() for _ in range(NT)]
    B_E2 = [Buf() for _ in range(NT)]
    B_O = [Buf() for _ in range(NT)]
    B_W = [Buf() for _ in range(NT)]
    g2bc = [k.sbp(pfx + f"g2bc{b}", [128, D], F32) for b in range(NB)]
    B_g2bc = [Buf() for _ in range(NB)]
    d1i = k.sbp(pfx + "d1i", [128, NT], I32)
    d2i = k.sbp(pfx + "d2i", [128, NT], I32)
    bei = k.sbp(pfx + "bei", [128, 2, NBLK], I32)
    B_d1i, B_d2i, B_bei = Buf(), Buf(), Buf()
    nusedi = k.sbp(pfx + "nusedi", [128, 1], I32)
    B_nusedi = Buf()
    vl = k.dsem(pfx + "vl")
    for b in range(NB):
        k.dma("sp", g2bc[b][:], modrow[L, b:b + 1, 5 * D:6 * D].partition_broadcast(128), vl,
              reads=[B_modrow[L]], writes=[B_g2bc[b]], grp="g2")

    k.s.scope = pfx + "M1_router"
    with contextlib.ExitStack() as c1:
        def sb1(name, shape, dt=F32):
            return c1.enter_context(k.nc.sbuf_tensor("s_" + pfx + name, list(shape), dt))

        def ps1(name, shape, dt=F32):
            return c1.enter_context(k.nc.psum_tensor("p_" + pfx + name, list(shape), dt))
        A2bc = [sb1(f"A2bc{b}", [128, D]) for b in range(NB)]
        sh2bc = [sb1(f"sh2bc{b}", [128, D]) for b in range(NB)]
        gfbc = sb1("gfbc", [128, D])
        B_A2bc = [Buf() for _ in range(NB)]
        B_sh2bc = [Buf() for _ in range(NB)]
        B_gfbc = Buf()
        Wr = sb1("Wr", [128, 8, 36])
        brbc = sb1("brbc", [128, 36])
        B_Wr, B_brbc = Buf(), Buf()
        k.dma("sp", gfbc[:], a["gffn"][L:L + 1, :].partition_broadcast(128), vl, writes=[B_gfbc], grp="g2")
        k.dma("sp", Wr[:], a["Wr_in"][L].rearrange("(kc p) f -> p kc f", p=128), vl, writes=[B_Wr], grp="g2")
        k.dma("sp", brbc[:], a["br_in"][L:L + 1, :].partition_broadcast(128), vl, writes=[B_brbc], grp="g2")
        for b in range(NB):
            k.dma("sp", sh2bc[b][:], modrow[L, b:b + 1, 3 * D:4 * D].partition_broadcast(128), vl,
                  reads=[B_modrow[L]], writes=[B_sh2bc[b]], grp="g2")
            k.dma("sp", A2bc[b][:], modrow[L, b:b + 1, 4 * D:5 * D].partition_broadcast(128), vl,
                  reads=[B_modrow[L]], writes=[B_A2bc[b]], grp="g2")
            k.op("dve", lambda e, b=b: e.scalar_tensor_tensor(out=A2bc[b][:], in0=A2bc[b][:], scalar=1.0, in1=gfbc[:],
                                                             op0=ALU.add, op1=ALU.mult),
                 [B_A2bc[b], B_gfbc], [B_A2bc[b]])
        xt = [sb1(f"xt{i}", [128, 4, D]) for i in range(2)]
        B_xt = [Buf(), Buf()]
        xt_ld = [k.dsem(pfx + "xt_ld0"), k.dsem(pfx + "xt_ld1")]
        junk = sb1("junk", [128, D], BF16)
        B_junk = Buf()
        ss, rstd = sb1("ss", [128, 4]), sb1("rstd", [128, 4])
        B_ss, B_rstd = Buf(), Buf()
        h2_ = [sb1(f"h2_{i}", [128, 4, D]) for i in range(2)]
        B_h2_ = [[Buf() for _ in range(4)] for _ in range(2)]
        h2b = [sb1(f"h2b{i}", [128, 4, D], BF16) for i in range(2)]
        B_h2b = [Buf(), Buf()]
        h2b_st = [k.dsem(pfx + "h2b_st0"), k.dsem(pfx + "h2b_st1")]
        pTf = [ps1(f"pTf{i}", [128, 512]) for i in range(2)]
        B_pTf = [Buf(), Buf()]
        hT2 = sb1("hT2", [128, 8, 512])
        B_hT2 = [Buf() for _ in range(8)]
        pslg = ps1("pslg", [128, 4, 36])
        B_pslg = Buf()
        lg4 = sb1("lg4", [128, 4, 36])
        gmax, gsum = sb1("gmax", [128, 4]), sb1("gsum", [128, 4])
        gsh = sb1("gsh", [128, 4, 4])
        ohg4 = sb1("ohg4", [128, 4, 4])
        tmp4 = sb1("tmp4", [128, 4, 4, 8])
        sel4, sel4b = sb1("sel4", [128, 4, 8]), sb1("sel4b", [128, 4, 8])
        oh1_4, oh2_4 = sb1("oh1_4", [128, 4, 8]), sb1("oh2_4", [128, 4, 8])
        m1, m2, dlt = sb1("m1", [128, 4]), sb1("m2", [128, 4]), sb1("dlt", [128, 4])
        B_lg, B_sm, B_sm2, B_ohg, B_ge, B_sel, B_selb, B_oh1, B_oh2, B_tmp4, B_m1, B_m2, B_dlt = (Buf() for _ in range(13))

        def load_x(G):
            sl = G % 2
            k.dma("sp", xt[sl][:], xr[G * 512:(G + 1) * 512, :].rearrange("(j p) d -> p j d", p=128),
                  xt_ld[sl], reads=[B_xr[G]], writes=[B_xt[sl]])
        def stage1(G):
            b = G // GPB
            sl = G % 2
            xt_t, B_x = xt[sl], B_xt[sl]
            h2, B_h2 = h2_[sl], B_h2_[sl]
            for j in range(4):
                k.op("act", lambda e, j=j, xt_t=xt_t: e.activation(out=junk[:], in_=xt_t[:, j, :], func=AF.Square,
                                                                  accum_out=ss[:, j:j + 1]), [B_x], [B_junk, B_ss])
            k.op("dve", lambda e: e.tensor_scalar(out=rstd[:], in0=ss[:], scalar1=1.0 / D, scalar2=1e-6,
                                                  op0=ALU.mult, op1=ALU.add), [B_ss], [B_rstd])
            k.op("act", lambda e: e.activation(out=rstd[:], in_=rstd[:], func=AF.Sqrt), [B_rstd], [B_rstd])
            k.op("dve", lambda e: e.reciprocal(out=rstd[:], in_=rstd[:]), [B_rstd], [B_rstd])
            for j in range(4):
                k.op("dve", lambda e, j=j, xt_t=xt_t, b=b, h2=h2: e.scalar_tensor_tensor(
                    out=h2[:, j, :], in0=xt_t[:, j, :], scalar=rstd[:, j:j + 1], in1=A2bc[b][:],
                    op0=ALU.mult, op1=ALU.mult), [B_x, B_rstd, B_A2bc[b]], [B_h2[j]])
                k.op("pool" if j % 2 else "dve", lambda e, j=j, b=b, h2=h2: e.tensor_tensor(
                    out=h2[:, j, :], in0=h2[:, j, :], in1=sh2bc[b][:], op=ALU.add),
                     [B_h2[j], B_sh2bc[b]], [B_h2[j]])

        def stage1b(G):
            sl = G % 2
            h2, B_h2 = h2_[sl], B_h2_[sl]
            for j in range(4):
                k.op("act", lambda e, j=j, sl=sl, h2=h2: e.copy(out=h2b[sl][:, j, :], in_=h2[:, j, :]),
                     [B_h2[j]], [B_h2b[sl]])
            k.dma("sp", hs[G * 512:(G + 1) * 512, :].rearrange("(j p) d -> p j d", p=128), h2b[sl][:], h2b_st[sl],
                  reads=[B_h2b[sl]], writes=[B_hs[G]])

        load_x(0)
        if NG > 1:
            load_x(1)
        stage1(0)
        stage1b(0)
        for G in range(NG):
            b = G // GPB
            sl = G % 2
            h2, B_h2 = h2_[sl], B_h2_[sl]
            for c in range(8):
                p = c % 2
                for j in range(4):
                    k.tr(pTf[p][:, j * 128:(j + 1) * 128], h2[:, j, c * 128:(c + 1) * 128], ident_f[:],
                         reads=[B_h2[j], B_ident_f], writes=[B_pTf[p]])
                if c % 2 == 0:
                    k.op("act", lambda e, c=c, p=p: e.copy(out=hT2[:, c, :], in_=pTf[p][:]), [B_pTf[p]], [B_hT2[c]])
                else:
                    k.op("dve", lambda e, c=c, p=p: e.tensor_copy(out=hT2[:, c, :], in_=pTf[p][:]),
                         [B_pTf[p]], [B_hT2[c]])
            for j in range(4):
                for kc in range(8):
                    k.mm(pslg[:, j, :], hT2[:, kc, j * 128:(j + 1) * 128], Wr[:, kc, :], kc == 0, kc == 7,
                         reads=[B_hT2[kc], B_Wr], writes=[B_pslg])
            if G + 1 < NG:
                stage1(G + 1)
            if G + 2 < NG:
                load_x(G + 2)
            V = "dve"
            i0_ = G * 4
            J = 4

            def bc(ap, shape):
                return ap.to_broadcast(list(shape))
            k.op(V, lambda e: e.tensor_tensor(out=lg4[:], in0=pslg[:], in1=bc(brbc[:].unsqueeze(1), [128, J, 36]),
                                              op=ALU.add), [B_pslg, B_brbc], [B_lg])
            k.op(V, lambda e: e.tensor_reduce(out=gmax[:], in_=lg4[:, :, 0:4], axis=AX.X, op=ALU.max), [B_lg], [B_sm])
            k.op(V, lambda e: e.tensor_tensor(out=gsh[:], in0=lg4[:, :, 0:4], in1=bc(gmax[:].unsqueeze(2), [128, J, 4]),
                                              op=ALU.subtract), [B_lg, B_sm], [B_ge])
            k.op(V, lambda e: e.tensor_tensor(out=ohg4[:], in0=lg4[:, :, 0:4], in1=bc(gmax[:].unsqueeze(2), [128, J, 4]),
                                              op=ALU.is_equal), [B_lg, B_sm], [B_ohg])
            k.op("act", lambda e: e.activation(out=gsh[:], in_=gsh[:], func=AF.Exp), [B_ge], [B_ge])
            k.op(V, lambda e: e.tensor_reduce(out=gsum[:], in_=gsh[:], axis=AX.X, op=ALU.add), [B_ge], [B_sm2])
            k.op(V, lambda e: e.reciprocal(out=gsum[:], in_=gsum[:]), [B_sm2], [B_sm2])
            k.op(V, lambda e: e.tensor_tensor(out=tmp4[:], in0=lg4[:, :, 4:36].rearrange("p j (g e) -> p j g e", g=4),
                                              in1=bc(ohg4[:].unsqueeze(3), [128, J, 4, 8]), op=ALU.mult),
                 [B_lg, B_ohg], [B_tmp4])
            k.op(V, lambda e: e.tensor_reduce(out=sel4[:], in_=tmp4[:].rearrange("p j g e -> p j e g"), axis=AX.X,
                                              op=ALU.add), [B_tmp4], [B_sel])
            k.op(V, lambda e: e.tensor_reduce(out=m1[:], in_=sel4[:], axis=AX.X, op=ALU.max), [B_sel], [B_m1])
            k.op(V, lambda e: e.tensor_tensor(out=oh1_4[:], in0=sel4[:], in1=bc(m1[:].unsqueeze(2), [128, J, 8]),
                                              op=ALU.is_equal), [B_sel, B_m1], [B_oh1])
            k.op(V, lambda e: e.scalar_tensor_tensor(out=sel4b[:], in0=oh1_4[:], scalar=-1.0e30, in1=sel4[:],
                                                     op0=ALU.mult, op1=ALU.add), [B_oh1, B_sel], [B_selb])
            k.op(V, lambda e: e.tensor_reduce(out=m2[:], in_=sel4b[:], axis=AX.X, op=ALU.max), [B_selb], [B_m2])
            k.op(V, lambda e: e.tensor_tensor(out=oh2_4[:], in0=sel4b[:], in1=bc(m2[:].unsqueeze(2), [128, J, 8]),
                                              op=ALU.is_equal), [B_selb, B_m2], [B_oh2])
            k.op(V, lambda e: e.tensor_tensor(out=dlt[:], in0=m2[:], in1=m1[:], op=ALU.subtract), [B_m1, B_m2], [B_dlt])
            k.op("act", lambda e: e.activation(out=dlt[:], in_=dlt[:], func=AF.Exp), [B_dlt], [B_dlt])
            k.op(V, lambda e: e.tensor_scalar(out=dlt[:], in0=dlt[:], scalar1=1.0, scalar2=None, op0=ALU.add),
                 [B_dlt], [B_dlt])
            k.op(V, lambda e: e.reciprocal(out=dlt[:], in_=dlt[:]), [B_dlt], [B_dlt])
            Bws = B_W[i0_:i0_ + J]
            k.op(V, lambda e, i0_=i0_: e.tensor_tensor(out=Wall[:, i0_:i0_ + J, 0], in0=gsum[:], in1=dlt[:], op=ALU.mult),
                 [B_sm2, B_dlt], Bws)
            k.op(V, lambda e, i0_=i0_: e.tensor_tensor(out=Wall[:, i0_:i0_ + J, 1], in0=gsum[:],
                                                      in1=Wall[:, i0_:i0_ + J, 0], op=ALU.subtract),
                 [B_sm2] + Bws, Bws)
            for (Eall, oh, B_oh, B_E) in ((E1all, oh1_4, B_oh1, B_E1), (E2all, oh2_4, B_oh2, B_E2)):
                k.op(V, lambda e, Eall=Eall, oh=oh, i0_=i0_: e.tensor_tensor(
                    out=Eall[:, i0_:i0_ + J, :].rearrange("p j (g e) -> p j g e", g=4),
                    in0=bc(ohg4[:].unsqueeze(3), [128, J, 4, 8]), in1=bc(oh[:].unsqueeze(2), [128, J, 4, 8]),
                    op=ALU.mult), [B_ohg, B_oh], B_E[i0_:i0_ + J])
            k.op("pool", lambda e, i0_=i0_: e.tensor_tensor(out=Oall[:, i0_:i0_ + J, :], in0=E1all[:, i0_:i0_ + J, :],
                                                           in1=E2all[:, i0_:i0_ + J, :], op=ALU.add),
                 B_E1[i0_:i0_ + J] + B_E2[i0_:i0_ + J], B_O[i0_:i0_ + J])
            if G + 1 < NG:
                stage1b(G + 1)
        k.s.barrier()
    dbgsem = k.dsem(pfx + "dbg")
    B_dbg = Buf()
    if a.get("stage") == "M1":
        dbg = a["dbg"]
        k.dma("sp", dbg[:, 0:NT * 2], Wall[:].rearrange("p i w -> p (i w)"), dbgsem, reads=B_W, writes=[B_dbg])
        k.dma("sp", dbg[:, 1024:1024 + NT * 32], E1all[:].rearrange("p i e -> p (i e)"), dbgsem, reads=B_E1, writes=[B_dbg])
        k.dma("sp", dbg[:, 2048:2048 + NT * 32], E2all[:].rearrange("p i e -> p (i e)"), dbgsem, reads=B_E2, writes=[B_dbg])
        return [B_dbg]

    k.s.scope = pfx + "M2_index"
    with contextlib.ExitStack() as c2:
        def sb2(name, shape, dt=F32):
            return c2.enter_context(k.nc.sbuf_tensor("s_" + pfx + name, list(shape), dt))

        def ps2(name, shape, dt=F32):
            return c2.enter_context(k.nc.psum_tensor("p_" + pfx + name, list(shape), dt))
        trif, trib, onesb = sb2("trif", [128, 128]), sb2("trib", [128, 128], BF16), sb2("onesb", [128, 128], BF16)
        B_trif, B_trib, B_onesb = Buf(), Buf(), Buf()
        k.dma("sp", trif[:], a["tri_in"][:, :], vl, writes=[B_trif])
        k.op("dve", lambda e: e.tensor_copy(out=trib[:], in_=trif[:]), [B_trif], [B_trib])
        k.op("dve", lambda e: e.memset(onesb[:], 1.0), [], [B_onesb])
        base = sb2("base", [128, NT, 32])
        tot = sb2("tot", [128, NT, 32])
        B_base, B_tot = Buf(), Buf()
        psw = [ps2(f"psw{i}", [128, 512]) for i in range(2)]
        B_psw = [Buf(), Buf()]
        TPC = 16
        nch = (NT + TPC - 1) // TPC
        for ci in range(nch):
            t0, t1 = ci * TPC, min(NT, (ci + 1) * TPC)
            w = (t1 - t0) * 32
            for which, (lhs, B_l, dst, B_d) in enumerate(((trib, B_trib, base, B_base), (onesb, B_onesb, tot, B_tot))):
                k.mm(psw[which][:, 0:w], lhs[:], Oall[:, t0:t1, :], True, True,
                     reads=[B_l] + B_O[t0:t1], writes=[B_psw[which]])
                k.op("dve", lambda e, which=which, dst=dst, t0=t0, t1=t1, w=w: e.tensor_copy(
                    out=dst[:, t0:t1, :], in_=psw[which][:, 0:w]), [B_psw[which]], [B_d])
        cnt = sb2("cnt", [128, 32])
        nblk = sb2("nblk", [128, 32])
        cum = sb2("cum", [128, 32])
        carry = sb2("carry", [128, 32])
        tmp32 = sb2("tmp32", [128, 32])
        B_cnt, B_nblk, B_cum, B_carry, B_tmp32 = Buf(), Buf(), Buf(), Buf(), Buf()
        V = "dve"
        k.op(V, lambda e: e.tensor_reduce(out=cnt[:], in_=tot[:].rearrange("p i e -> p e i"), axis=AX.X, op=ALU.add),
             [B_tot], [B_cnt])
        k.op(V, lambda e: e.tensor_scalar(out=nblk[:], in0=cnt[:], scalar1=0.0, scalar2=None, op0=ALU.is_gt),
             [B_cnt], [B_nblk])
        for j in range(1, (2 * T) // 512 + 1):
            k.op(V, lambda e, j=j: e.scalar_tensor_tensor(out=nblk[:], in0=cnt[:], scalar=512.0 * j, in1=nblk[:],
                                                         op0=ALU.is_gt, op1=ALU.add), [B_cnt, B_nblk], [B_nblk])
        k.op(V, lambda e: e.tensor_copy(out=cum[:], in_=nblk[:]), [B_nblk], [B_cum])
        for ee in range(1, 32):
            k.op(V, lambda e, ee=ee: e.tensor_tensor(out=cum[:, ee:ee + 1], in0=cum[:, ee - 1:ee], in1=nblk[:, ee:ee + 1],
                                                    op=ALU.add), [B_cum, B_nblk], [B_cum])
        k.op(V, lambda e: e.tensor_tensor(out=carry[:], in0=cum[:], in1=nblk[:], op=ALU.subtract),
             [B_cum, B_nblk], [B_carry])
        k.op(V, lambda e: e.tensor_scalar(out=carry[:], in0=carry[:], scalar1=512.0, scalar2=None, op0=ALU.mult),
             [B_carry], [B_carry])
        for i in range(NT):
            k.op(V, lambda e, i=i: e.tensor_tensor(out=base[:, i, :], in0=base[:, i, :], in1=carry[:], op=ALU.add),
                 [B_base, B_carry], [B_base])
            if i + 1 < NT:
                k.op(V, lambda e, i=i: e.tensor_tensor(out=carry[:], in0=carry[:], in1=tot[:, i, :], op=ALU.add),
                     [B_carry, B_tot], [B_carry])
        dtmp = sb2("dtmp", [128, NT, 32])
        dfl = sb2("dfl", [128, NT])
        B_dtmp, B_dfl = Buf(), Buf()
        for (Eall, B_E, di, B_di) in ((E1all, B_E1, d1i, B_d1i), (E2all, B_E2, d2i, B_d2i)):
            k.op(V, lambda e, Eall=Eall: e.tensor_tensor(out=dtmp[:], in0=Eall[:], in1=base[:], op=ALU.mult),
                 list(B_E) + [B_base], [B_dtmp])
            k.op(V, lambda e: e.tensor_reduce(out=dfl[:], in_=dtmp[:], axis=AX.X, op=ALU.add), [B_dtmp], [B_dfl])
            k.op(V, lambda e, di=di: e.tensor_copy(out=di[:], in_=dfl[:]), [B_dfl], [B_di])
        bef = sb2("bef", [128, NBLK])
        B_bef = Buf()
        for j in range(NBLK):
            k.op(V, lambda e, j=j: e.tensor_scalar(out=tmp32[:], in0=cum[:], scalar1=float(j), scalar2=None,
                                                  op0=ALU.is_le, op1=ALU.add, accum_out=bef[:, j:j + 1]),
                 [B_cum], [B_tmp32, B_bef])
        k.op(V, lambda e: e.tensor_scalar(out=bef[:], in0=bef[:], scalar1=31.0, scalar2=None, op0=ALU.min),
             [B_bef], [B_bef])
        k.op(V, lambda e: e.tensor_copy(out=nusedi[:], in_=cum[:, 31:32]), [B_cum], [B_nusedi])
        pcol = sb2("pcol", [128, 1])
        B_pcol = Buf()
        k.dma("sp", pcol[:], a["pcol_in"][:, :], vl, writes=[B_pcol])
        k.op(V, lambda e: e.tensor_scalar(out=bef[:], in0=bef[:], scalar1=float(L * 32), scalar2=128.0,
                                          op0=ALU.add, op1=ALU.mult), [B_bef], [B_bef])
        k.op(V, lambda e: e.tensor_scalar(out=bef[:], in0=bef[:], scalar1=pcol[:, 0:1], scalar2=None, op0=ALU.add),
             [B_bef, B_pcol], [B_bef])
        k.op(V, lambda e: e.tensor_scalar(out=bef[:], in0=bef[:], scalar1=2.0, scalar2=None, op0=ALU.mult),
             [B_bef], [B_bef])
        k.op(V, lambda e: e.tensor_copy(out=bei[:, 0, :], in_=bef[:]), [B_bef], [B_bei])
        k.op(V, lambda e: e.tensor_scalar(out=bef[:], in0=bef[:], scalar1=1.0, scalar2=None, op0=ALU.add),
             [B_bef], [B_bef])
        k.op(V, lambda e: e.tensor_copy(out=bei[:, 1, :], in_=bef[:]), [B_bef], [B_bei])
        k.s.barrier()
        if a.get("stage") == "M2":
            dbg = a["dbg"].bitcast(I32)
            k.dma("sp", dbg[:, 0:NT], d1i[:], dbgsem, reads=[B_d1i], writes=[B_dbg])
            k.dma("sp", dbg[:, 1024:1024 + NT], d2i[:], dbgsem, reads=[B_d2i], writes=[B_dbg])
            k.dma("sp", dbg[:, 2048:2048 + NBLK], bei[:, 0, :], dbgsem, reads=[B_bei], writes=[B_dbg])
            k.s.barrier()
    if a.get("stage") == "M2":
        return [B_dbg]

    k.s.scope = pfx + "M3_scatter"
    with contextlib.ExitStack() as c3:
        NS3 = 6
        hsb = [c3.enter_context(k.nc.sbuf_tensor(f"s_{pfx}hsb{i}", [128, D], BF16)) for i in range(NS3)]
        B_hsb = [Buf() for _ in range(NS3)]
        hsb_ld = [k.dsem(pfx + f"hsb_ld{i}") for i in range(NS3)]
        sc_sem = [k.dsem(pfx + f"sc{i}", sw=True) for i in range(NS3)]
        B_xs = Buf()

        def m3_load(i):
            sl = i % NS3
            k.dma("sp", hsb[sl][:], hs[i * 128:(i + 1) * 128, :], hsb_ld[sl], reads=[B_hs[i // 4]], writes=[B_hsb[sl]])
        for i in range(min(NS3 - 1, NT)):
            m3_load(i)
        for i in range(NT):
            sl = i % NS3
            if i + NS3 - 1 < NT:
                m3_load(i + NS3 - 1)
            for (di, B_di) in ((d1i, B_d1i), (d2i, B_d2i)):
                k.s.add("pool", lambda e, sl=sl, di=di, i=i: e.indirect_dma_start(
                    out=xs[:, :], out_offset=bass.IndirectOffsetOnAxis(ap=di[:, i:i + 1], axis=0),
                    in_=hsb[sl][:], in_offset=None), [B_hsb[sl], B_di], [B_xs], dsem=sc_sem[sl], grp=("sc", i))
        k.s.barrier()
    if a.get("stage") == "M3":
        return []

    k.s.scope = pfx + "M4_experts"
    with contextlib.ExitStack() as c4:
        def sb4(name, shape, dt=F32):
            return c4.enter_context(k.nc.sbuf_tensor("s_" + pfx + name, list(shape), dt))

        def ps4(name, shape, dt=F32):
            return c4.enter_context(k.nc.psum_tensor("p_" + pfx + name, list(shape), dt))
        wg = [sb4(f"wg{i}", [128, 8, 512], BF16) for i in range(2)]
        wu = [sb4(f"wu{i}", [128, 8, 512], BF16) for i in range(2)]
        wd = [sb4(f"wd{i}", [128, 4, D], BF16) for i in range(2)]
        B_wg, B_wu, B_wd = [Buf(), Buf()], [Buf(), Buf()], [Buf(), Buf()]
        wsem = [[k.dsem(pfx + f"w{n}{i}", sw=True) for i in range(2)] for n in "gud"]
        xb = [sb4(f"xb{i}", [128, 4, D], BF16) for i in range(2)]
        B_xb = [Buf(), Buf()]
        xb_ld = [k.dsem(pfx + "xb_ld0"), k.dsem(pfx + "xb_ld1")]
        xsT_ = [sb4(f"xsT{i}", [128, 8, 512], BF16) for i in range(2)]
        B_xsT_ = [[Buf() for _ in range(8)] for _ in range(2)]
        pTx = [ps4(f"pTx{i}", [128, 512], BF16) for i in range(2)]
        B_pTx = [Buf(), Buf()]
        psG = [ps4(f"psG{i}", [128, 512]) for i in range(2)]
        psU = [ps4(f"psU{i}", [128, 512]) for i in range(2)]
        B_psG, B_psU = [Buf(), Buf()], [Buf(), Buf()]
        sg = [sb4(f"sg{i}", [128, 512]) for i in range(2)]
        B_sg = [Buf(), Buf()]
        hTe = sb4("hTe", [128, 4, 512], BF16)
        B_hTe = [Buf() for _ in range(4)]
        psY = [ps4(f"psY{i}", [128, 512]) for i in range(2)]
        B_psY = [Buf(), Buf()]
        yb = [sb4(f"yb{i}", [128, 4, D]) for i in range(2)]
        B_yb = [Buf(), Buf()]
        yb_st = [k.dsem(pfx + "yb_st0"), k.dsem(pfx + "yb_st1")]
        B_ys = Buf()
        gw, uw, dw = a["gw"], a["uw"], a["dw"]
        gw2 = gw.rearrange("r (h f) -> (r h) f", h=2)
        uw2 = uw.rearrange("r (h f) -> (r h) f", h=2)
        dw2 = dw.rearrange("r (h f) -> (r h) f", h=2)

        def wload(j, sl):
            for (dst, B_d, tab, ws, nh) in ((wg[sl], B_wg[sl], gw2, wsem[0][sl], 4), (wu[sl], B_wu[sl], uw2, wsem[1][sl], 4),
                                            (wd[sl], B_wd[sl], dw2, wsem[2][sl], 2)):
                for h in range(2):
                    k.s.add("pool", lambda e, dst=dst, tab=tab, j=j, h=h, nh=nh: e.indirect_dma_start(
                        out=dst[:, h * nh:(h + 1) * nh, :].rearrange("p a f -> p (a f)"), out_offset=None, in_=tab,
                        in_offset=bass.IndirectOffsetOnAxis(ap=bei[:, h, j:j + 1], axis=0)), [B_bei], [B_d], dsem=ws,
                        grp=("w", j))

        def xload(j, sl):
            k.dma("sp", xb[sl][:], xs[j * 512:(j + 1) * 512, :].rearrange("(t p) d -> p t d", p=128), xb_ld[sl],
                  reads=[B_xs], writes=[B_xb[sl]])
        def xpose(j, cs):
            sl = j % 2
            for c in cs:
                p = c % 2
                for t in range(4):
                    k.tr(pTx[p][:, t * 128:(t + 1) * 128], xb[sl][:, t, c * 128:(c + 1) * 128], ident_b[:],
                         reads=[B_xb[sl], B_ident_b], writes=[B_pTx[p]])
                if c % 2 == 0:
                    k.op("act", lambda e, c=c, p=p, sl=sl: e.copy(out=xsT_[sl][:, c, :], in_=pTx[p][:]),
                         [B_pTx[p]], [B_xsT_[sl][c]])
                else:
                    k.op("dve", lambda e, c=c, p=p, sl=sl: e.tensor_copy(out=xsT_[sl][:, c, :], in_=pTx[p][:]),
                         [B_pTx[p]], [B_xsT_[sl][c]])
        regs = k.nused_regs()
        for eng_ in ENGS:
            k.s.add(eng_, lambda e, eng_=eng_: e.reg_load(regs[eng_], nusedi[0:1, 0:1]), [B_nusedi], [])
        JS = (2 * T) // 512
        wload(0, 0)
        xload(0, 0)
        if NBLK > 1:
            wload(1, 1)
            xload(1, 1)
        xpose(0, range(8))
        for j in range(NBLK):
            sl = j % 2
            k.s.guard = (regs, (j if not os.environ.get("DYN_NEVER") else -1)) if (j >= max(JS, int(os.environ.get("DYN_FROM", "0"))) and DYN_SKIP) else None
            xsT, B_xsT = xsT_[sl], B_xsT_[sl]
            for fc in range(4):
                q = fc % 2
                for kc in range(8):
                    k.mm(psG[q][:], wg[sl][:, kc, fc * 128:(fc + 1) * 128], xsT[:, kc, :], kc == 0, kc == 7,
                         reads=[B_wg[sl], B_xsT[kc]], writes=[B_psG[q]])
                for kc in range(8):
                    k.mm(psU[q][:], wu[sl][:, kc, fc * 128:(fc + 1) * 128], xsT[:, kc, :], kc == 0, kc == 7,
                         reads=[B_wu[sl], B_xsT[kc]], writes=[B_psU[q]])
                k.op("act", lambda e, q=q: e.activation(out=sg[q][:], in_=psG[q][:], func=AF.Silu),
                     [B_psG[q]], [B_sg[q]])
                k.op("dve", lambda e, q=q, fc=fc: e.tensor_tensor(out=hTe[:, fc, :], in0=sg[q][:], in1=psU[q][:],
                                                                 op=ALU.mult), [B_sg[q], B_psU[q]], [B_hTe[fc]])
                if j + 1 < NBLK:
                    xpose(j + 1, [2 * fc, 2 * fc + 1])
            for t in range(4):
                for dh in range(2):
                    yi = (t * 2 + dh) % 2
                    for fc in range(4):
                        k.mm(psY[yi][:], hTe[:, fc, t * 128:(t + 1) * 128], wd[sl][:, fc, dh * 512:(dh + 1) * 512],
                             fc == 0, fc == 3, reads=[B_hTe[fc], B_wd[sl]], writes=[B_psY[yi]])
                    if yi == 0:
                        k.op("act", lambda e, t=t, dh=dh, sl=sl: e.copy(out=yb[sl][:, t, dh * 512:(dh + 1) * 512],
                                                                       in_=psY[0][:]), [B_psY[0]], [B_yb[sl]])
                    else:
                        k.op("dve", lambda e, t=t, dh=dh, sl=sl: e.tensor_copy(
                            out=yb[sl][:, t, dh * 512:(dh + 1) * 512], in_=psY[1][:]), [B_psY[1]], [B_yb[sl]])
            if j + 2 < NBLK:
                wload(j + 2, sl)
                xload(j + 2, sl)
            k.dma("sp", ys[j * 512:(j + 1) * 512, :].rearrange("(t p) d -> p t d", p=128), yb[sl][:], yb_st[sl],
                  reads=[B_yb[sl]], writes=[B_ys])
        k.s.guard = None
        k.s.barrier()
    if a.get("stage") == "M4":
        return []

    k.s.scope = pfx + "M5_combine"
    fin = a.get("final")
    with contextlib.ExitStack() as c5:
        def sb5(name, shape, dt=F32):
            return c5.enter_context(k.nc.sbuf_tensor("s_" + pfx + name, list(shape), dt))
        NS = 6
        y1 = [sb5(f"y1_{i}", [128, D]) for i in range(NS)]
        y2 = [sb5(f"y2_{i}", [128, D]) for i in range(NS)]
        xc = [sb5(f"xc{i}", [128, D]) for i in range(NS)]
        B_y1, B_y2, B_xc = [Buf() for _ in range(NS)], [Buf() for _ in range(NS)], [Buf() for _ in range(NS)]
        g_sem = [[k.dsem(pfx + f"g{n}{i}", sw=True) for i in range(NS)] for n in "12"]
        xc_ld = [k.dsem(pfx + f"xc_ld{i}") for i in range(NS)]
        xc_st = [k.dsem(pfx + f"xc_st{i}") for i in range(NS)]
        if fin is not None:
            fg = sb5("fg", [128, D])
            fjunk = sb5("fjunk", [128, D], BF16)
            fss = [sb5(f"fss{i}", [128, 1]) for i in range(NS)]
            B_fg, B_fjunk = Buf(), Buf()
            B_fss = [Buf() for _ in range(NS)]
            k.dma("sp", fg[:], fin["fing"][0:1, :].partition_broadcast(128), vl, writes=[B_fg])
        def m5_load(i):
            sl = i % NS
            G = i // 4
            k.s.add("pool", lambda e, sl=sl, i=i: e.indirect_dma_start(
                out=y1[sl][:], out_offset=None, in_=ys[:, :],
                in_offset=bass.IndirectOffsetOnAxis(ap=d1i[:, i:i + 1], axis=0)), [B_ys, B_d1i], [B_y1[sl]],
                dsem=g_sem[0][sl])
            k.s.add("pool", lambda e, sl=sl, i=i: e.indirect_dma_start(
                out=y2[sl][:], out_offset=None, in_=ys[:, :],
                in_offset=bass.IndirectOffsetOnAxis(ap=d2i[:, i:i + 1], axis=0)), [B_ys, B_d2i], [B_y2[sl]],
                dsem=g_sem[1][sl])
            k.dma("sp", xc[sl][:], xr[i * 128:(i + 1) * 128, :], xc_ld[sl], reads=[B_xr[G]], writes=[B_xc[sl]])
        for i in range(min(NS - 1, NT)):
            m5_load(i)
        for i in range(NT):
            sl = i % NS
            b = (i * 128) // S
            G = i // 4
            if i + NS - 1 < NT:
                m5_load(i + NS - 1)
            k.op("act", lambda e, sl=sl, i=i: e.activation(out=y1[sl][:], in_=y1[sl][:], func=AF.Copy,
                                                          scale=Wall[:, i, 0:1]), [B_y1[sl], B_W[i]], [B_y1[sl]])
            k.op("dve", lambda e, sl=sl, i=i: e.scalar_tensor_tensor(out=y1[sl][:], in0=y2[sl][:],
                                                                    scalar=Wall[:, i, 1:2], in1=y1[sl][:],
                                                                    op0=ALU.mult, op1=ALU.add),
                 [B_y1[sl], B_y2[sl], B_W[i]], [B_y1[sl]])
            k.op("dve", lambda e, sl=sl, b=b: e.tensor_tensor(out=y1[sl][:], in0=y1[sl][:], in1=g2bc[b][:],
                                                             op=ALU.mult), [B_y1[sl], B_g2bc[b]], [B_y1[sl]])
            k.op("dve", lambda e, sl=sl: e.tensor_tensor(out=xc[sl][:], in0=xc[sl][:], in1=y1[sl][:], op=ALU.add),
                 [B_xc[sl], B_y1[sl]], [B_xc[sl]])
            if fin is None:
                k.dma("sp", xr[i * 128:(i + 1) * 128, :], xc[sl][:], xc_st[sl], reads=[B_xc[sl]], writes=[B_xr[G]])
            else:
                k.op("act", lambda e, sl=sl: e.activation(out=fjunk[:], in_=xc[sl][:], func=AF.Square,
                                                          accum_out=fss[sl][:, 0:1]), [B_xc[sl]], [B_fjunk, B_fss[sl]])
                k.op("dve", lambda e, sl=sl: e.tensor_scalar(out=fss[sl][:], in0=fss[sl][:], scalar1=1.0 / D,
                                                             scalar2=1e-6, op0=ALU.mult, op1=ALU.add),
                     [B_fss[sl]], [B_fss[sl]])
                k.op("act", lambda e, sl=sl: e.activation(out=fss[sl][:], in_=fss[sl][:], func=AF.Sqrt),
                     [B_fss[sl]], [B_fss[sl]])
                k.op("dve", lambda e, sl=sl: e.reciprocal(out=fss[sl][:], in_=fss[sl][:]), [B_fss[sl]], [B_fss[sl]])
                k.op("dve", lambda e, sl=sl: e.scalar_tensor_tensor(out=xc[sl][:], in0=xc[sl][:], scalar=fss[sl][:, 0:1],
                                                                    in1=fg[:], op0=ALU.mult, op1=ALU.mult),
                     [B_xc[sl], B_fss[sl], B_fg], [B_xc[sl]])
                k.dma("sp", fin["out"][i * 128:(i + 1) * 128, :], xc[sl][:], xc_st[sl], reads=[B_xc[sl]],
                      writes=[fin["B_out"][G]])
        k.s.barrier()


def finish(k, out_bufs):
    k.s.add("sp", None, reads=out_bufs)
    esem = {e: k.sem("e_" + e) for e in ENGS}
    stuck = k.s.simulate()
    assert not stuck, f"schedule deadlock: {stuck}"
    k.s.emit(k.nc, esem)
    k.pctx.close()
    k.ctx.close()
    return k.nc


_RL = {}


def _relayout(w, key, nch):
    if key not in _RL or _RL[key][0] is not w:
        Lr, E, R, N = w.shape
        _RL[key] = (w, np.ascontiguousarray(w.reshape(Lr, E, nch, 128, N).transpose(0, 1, 3, 2, 4)).reshape(
            Lr * E * 128, nch * N))
    return _RL[key][1]


def host_inputs(inp, core, NB, S):
    b0 = core * NB
    f = np.float32
    m = {}
    m["x"] = np.ascontiguousarray(inp["x"][b0:b0 + NB, :S].reshape(NB * S, D))
    c = inp["c"][b0:b0 + NB]
    m["cT"] = np.ascontiguousarray(c.reshape(NB, 8, 128).transpose(2, 1, 0).reshape(128, 8 * NB))
    m["ada_w"] = inp["ada_w"]
    m["ada_b"] = inp["ada_b"]
    m["gmixF"] = np.ascontiguousarray(inp["norm_mix_g"].reshape(2, 8, 128).transpose(0, 2, 1))
    m["gffn"] = inp["norm_ffn_g"]
    m["conv_in_w"] = inp["conv_in_w"][0]
    m["convwF"] = np.ascontiguousarray(inp["conv_w"][0].reshape(3, 8, 128).transpose(2, 1, 0).reshape(128, 24))
    m["conv_out_w"] = inp["conv_out_w"][0]
    m["ident"] = np.eye(128, dtype=f)
    m["tri"] = np.triu(np.ones((128, 128), dtype=f), 1)
    m["Wr"] = np.ascontiguousarray(np.concatenate(
        [inp["router_grp_w"], inp["router_exp_w"].transpose(0, 2, 1, 3).reshape(2, D, 32)], axis=2))
    m["br"] = np.ascontiguousarray(np.concatenate([inp["router_grp_b"], inp["router_exp_b"].reshape(2, 32)], axis=1))
    m["exp_gate_w"] = _relayout(inp["exp_gate_w"], "g", 8)
    m["exp_up_w"] = _relayout(inp["exp_up_w"], "u", 8)
    m["exp_down_w"] = _relayout(inp["exp_down_w"], "d", 4)
    m["pcol"] = np.arange(128, dtype=f).reshape(128, 1)
    m["attn_in_w"] = inp["attn_in_w"][0]
    m["attn_out_w"] = inp["attn_out_w"][0]
    pos = np.arange(S, dtype=f)
    inv = np.power(f(500000.0), -np.arange(0, 16, 2, dtype=f) / f(16)).astype(f)
    ang = pos[None, :] * inv[:, None]
    C = np.ones((128, S), f)
    Sg = np.zeros((128, S), f)
    Rm = np.zeros((128, 128), f)
    for h in range(2):
        for e in range(16):
            C[h * 64 + e] = np.cos(ang[e % 8])
            Sg[h * 64 + e] = (-np.sin(ang[e % 8])) if e < 8 else np.sin(ang[e % 8])
            Rm[h * 64 + (e + 8 if e < 8 else e - 8), h * 64 + e] = 1.0
    m["ropeC"], m["ropeS"] = C, Sg
    kk = np.arange(128)[:, None]
    qq = np.arange(128)[None, :]
    NEG = f(-30000.0)
    Mprev = np.where(kk >= qq, f(0), NEG).astype(f)
    Mcur = np.where(kk <= qq, f(0), NEG).astype(f)
    o0 = np.zeros((128, 128), f)
    o0[:, :64] = 1
    o1 = np.zeros((128, 128), f)
    o1[:, 64:] = 1
    m["aconst"] = np.ascontiguousarray(np.concatenate([Rm, Mprev, Mcur, o0, o1], axis=1))
    m["fing"] = inp["final_norm_g"].reshape(1, D)
    return m


def kernel(**inputs):
    NB, S = 2, 4096
    nc = build(NB, S)
    in_maps = [host_inputs(inputs, c, NB, S) for c in range(NCORES)]
    res = run_bass_kernel_spmd(nc, in_maps, core_ids=list(range(NCORES)))
    out = np.stack([r["out"].reshape(NB, S, D) for r in res.results]).reshape(NCORES * NB, S, D)
    return out.astype(np.float32)
```

```python
import contextlib
import os
import numpy as np
import concourse.bass as bass
import concourse.mybir as mybir
from concourse.bass_utils import run_bass_kernel_spmd

F32 = mybir.dt.float32
BF16 = mybir.dt.bfloat16
I32 = mybir.dt.int32
AF = mybir.ActivationFunctionType
ALU = mybir.AluOpType
AX = mybir.AxisListType

D = 1024
NCORES = 8
ENGS = ("pe", "act", "dve", "pool", "sp")
SAME_ENG_SYNC = True
DYN_SKIP = False


class Buf:
    __slots__ = ("name", "lw", "rd")

    def __init__(self, name=""):
        self.name = name
        self.lw = None
        self.rd = {}


class DSem:
    def __init__(self, h):
        self.h = h
        self.groups = []


class Ins:
    __slots__ = ("eng", "fn", "dsem", "signal", "sig", "target", "deps", "gk", "scope", "guard")


class Sched:
    def __init__(self):
        self.L = {e: [] for e in ENGS}
        self.n = 0
        self.dsems = []
        self.scope = None
        self.trace_scopes = False
        self.guard = None


    def dsem(self, h):
        d = DSem(h)
        self.dsems.append(d)
        return d

    def add(self, eng, fn, reads=(), writes=(), dsem=None, grp=None):
        ins = Ins()
        ins.eng, ins.fn, ins.dsem = eng, fn, dsem
        ins.signal, ins.sig, ins.target = False, 0, 0
        ins.gk = (id(dsem), grp) if (dsem is not None and grp is not None) else None
        ins.scope = self.scope
        ins.guard = self.guard
        deps = {}

        def need(d, kind):
            if d.dsem is not None:
                return not (ins.gk is not None and d.gk == ins.gk)
            if d.eng == eng:
                if dsem is not None:
                    return True
                if eng == "pe":
                    return False
                return kind == "raw" and SAME_ENG_SYNC
            return True

        for b in reads:
            if b.lw is not None and need(b.lw, "raw"):
                deps[id(b.lw)] = b.lw
        for b in writes:
            if b.lw is not None and need(b.lw, "waw"):
                deps[id(b.lw)] = b.lw
            for r in b.rd.values():
                if need(r, "war"):
                    deps[id(r)] = r
        if dsem is not None:
            assert getattr(dsem, "sw", False) == (eng == "pool"), f"DMA semaphore class mismatch on {eng}"
            g = dsem.groups
            if g and grp is not None and g[-1][0] == grp:
                g[-1][1].append(ins)
            else:
                if g:
                    prev = g[-1][1][-1]
                    deps[id(prev)] = prev
                g.append((grp if grp is not None else object(), [ins]))
        ins.deps = list(deps.values())
        for d in ins.deps:
            d.signal = True
        for b in reads:
            key = eng if dsem is None else ("dma", self.n)
            b.rd[key] = ins
        for b in writes:
            b.lw = ins
            b.rd = {}
        self.L[eng].append(ins)
        self.n += 1
        return ins

    def barrier(self):
        lasts = []
        for e in ENGS:
            for ins in reversed(self.L[e]):
                if ins.fn is not None and ins.dsem is None:
                    lasts.append(ins)
                    break
        for ds in self.dsems:
            if ds.groups:
                lasts.append(ds.groups[-1][1][-1])
        for e in ENGS:
            ins = Ins()
            ins.eng, ins.fn, ins.dsem = e, None, None
            ins.signal, ins.sig, ins.target = False, 0, 0
            ins.gk = None
            ins.scope = self.scope
            ins.guard = None
            ins.deps = [d for d in lasts if not (d.dsem is None and d.eng == e)]
            for d in ins.deps:
                d.signal = True
            self.L[e].append(ins)

    def simulate(self):
        for e in ENGS:
            c = 0
            for ins in self.L[e]:
                if ins.dsem is None and ins.signal:
                    c += 1
                    ins.sig = c
        for ds in self.dsems:
            tot = 0
            for _, lst in ds.groups:
                tot += 16 * len(lst)
                for i in lst:
                    i.target = tot
        pc = {e: 0 for e in ENGS}
        ev = {e: 0 for e in ENGS}
        dv = {id(ds): 0 for ds in self.dsems}
        prog = True
        while prog:
            prog = False
            for e in ENGS:
                while pc[e] < len(self.L[e]):
                    ins = self.L[e][pc[e]]
                    ok = True
                    for d in ins.deps:
                        if d.dsem is not None:
                            if dv[id(d.dsem)] < d.target:
                                ok = False
                        elif ev[d.eng] < d.sig:
                            ok = False
                    if not ok:
                        break
                    if ins.fn is not None:
                        if ins.dsem is not None:
                            dv[id(ins.dsem)] += 16
                        elif ins.signal:
                            ev[e] += 1
                    pc[e] += 1
                    prog = True
        stuck = {e: (pc[e], len(self.L[e])) for e in ENGS if pc[e] < len(self.L[e])}
        return stuck

    def emit(self, nc, esem):
        for e in ENGS:
            c = 0
            for ins in self.L[e]:
                if ins.dsem is None and ins.signal:
                    c += 1
                    ins.sig = c
        for ds in self.dsems:
            tot = 0
            for _, lst in ds.groups:
                tot += 16 * len(lst)
                for i in lst:
                    i.target = tot

        def emit_one(e, eo, ins, seen):
            for d in ins.deps:
                if d.dsem is not None:
                    key, val, h = ("d", id(d.dsem)), d.target, d.dsem.h
                else:
                    key, val, h = ("e", d.eng), d.sig, esem[d.eng]
                if seen.get(key, 0) < val:
                    eo.wait_ge(h, val)
                    seen[key] = val
            if ins.fn is None:
                return
            r = ins.fn(eo)
            if ins.dsem is not None:
                r.then_inc(ins.dsem.h, 16)
            elif ins.signal:
                r.then_inc(esem[e], 1)

        def run(e, eo):
            seen = {}
            cur = [None, None]
            L_ = self.L[e]
            i = 0
            while i < len(L_):
                ins = L_[i]
                if self.trace_scopes and ins.scope != cur[0]:
                    if cur[1] is not None:
                        cur[1].__exit__(None, None, None)
                    cur[0] = ins.scope
                    cur[1] = nc.named_scope(f"{ins.scope}") if ins.scope else None
                    if cur[1] is not None:
                        cur[1].__enter__()
                if ins.guard is None:
                    emit_one(e, eo, ins, seen)
                    i += 1
                    continue
                g = ins.guard
                j = i
                while j < len(L_) and L_[j].guard is g and L_[j].scope == ins.scope:
                    j += 1
                region = L_[i:j]
                nsig = sum(1 for x in region if x.dsem is None and x.signal and x.fn is not None)
                dcount = {}
                for x in region:
                    if x.dsem is not None:
                        dcount[id(x.dsem)] = (x.dsem, dcount.get(id(x.dsem), (x.dsem, 0))[1] + 16)
                regs, thresh = g
                snap = dict(seen)
                with eo.If_lt(regs[e], thresh + 1):
                    for _ in range(nsig):
                        eo.nop(nofuse=True).then_inc(esem[e], 1)
                    for ds, n in dcount.values():
                        for _ in range(n // 16):
                            eo.nop(nofuse=True).then_inc(ds.h, 16)
                with eo.Else():
                    for x in region:
                        emit_one(e, eo, x, seen)
                seen = snap
                i = j
            if cur[1] is not None:
                cur[1].__exit__(None, None, None)

        with nc.Block() as block:
            @block.sync
            def _(eo):
                run("sp", eo)

            @block.tensor
            def _(eo):
                run("pe", eo)

            @block.scalar
            def _(eo):
                run("act", eo)

            @block.vector
            def _(eo):
                run("dve", eo)

            @block.gpsimd
            def _(eo):
                run("pool", eo)


class K:
    def __init__(self, NB, S, dbg=None):
        self.NB, self.S, self.T = NB, S, NB * S
        self.dbg = dbg
        self.nc = bass.Bass("TRN2", target_bir_lowering=False)
        self.ctx = contextlib.ExitStack()
        self.s = Sched()
        self.pctx = contextlib.ExitStack()
        self.nsem = 0
        self.dpool = []
        self.dcur = 0
        self.dpool_sw = []
        self.dcur_sw = 0
        self._regs = None

    def din(self, name, shape, dt=F32):
        return self.nc.dram_tensor(name, list(shape), dt, kind="ExternalInput").ap()

    def dout(self, name, shape, dt=F32):
        return self.nc.dram_tensor(name, list(shape), dt, kind="ExternalOutput").ap()

    def dscr(self, name, shape, dt=F32):
        return self.nc.dram_tensor(name, list(shape), dt, kind="Internal").ap()

    def phase(self):
        self.s.barrier()
        self.pctx.close()
        self.pctx = contextlib.ExitStack()
        self.dcur = 0
        self.dcur_sw = 0

    def sbp(self, name, shape, dt=F32):
        return self.pctx.enter_context(self.nc.sbuf_tensor("s_" + name, list(shape), dt))

    def psp(self, name, shape, dt=F32):
        return self.pctx.enter_context(self.nc.psum_tensor("p_" + name, list(shape), dt))

    def sb(self, name, shape, dt=F32):
        return self.ctx.enter_context(self.nc.sbuf_tensor("s_" + name, list(shape), dt))

    def ps(self, name, shape, dt=F32):
        return self.ctx.enter_context(self.nc.psum_tensor("p_" + name, list(shape), dt))

    def sem(self, name):
        self.nsem += 1
        return self.ctx.enter_context(self.nc.semaphore(name))

    def nused_regs(self):
        if self._regs is None:
            nc = self.nc
            eng = {"pe": nc.tensor, "act": nc.scalar, "dve": nc.vector, "pool": nc.gpsimd, "sp": nc.sync}
            self._regs = {e: self.ctx.enter_context(eng[e].register("nused_" + e)) for e in ENGS}
        return self._regs

    def dsem(self, name, sw=False):
        pool, cur = (self.dpool_sw, self.dcur_sw) if sw else (self.dpool, self.dcur)
        if cur < len(pool):
            d = pool[cur]
        else:
            d = self.s.dsem(self.sem(f"dma{'s' if sw else 'h'}{len(pool)}"))
            d.sw = sw
            pool.append(d)
        if sw:
            self.dcur_sw += 1
        else:
            self.dcur += 1
        return d

    def dma(self, eng, out, in_, dsem, reads=(), writes=(), grp=None, **kw):
        return self.s.add(eng, lambda e: e.dma_start(out=out, in_=in_, **kw), reads, writes, dsem=dsem, grp=grp)

    def mm(self, out, lhsT, rhs, start, stop, reads=(), writes=()):
        return self.s.add("pe", lambda e: e.matmul(out, lhsT=lhsT, rhs=rhs, start=start, stop=stop), reads, writes)

    def tr(self, out, in_, ident, reads=(), writes=()):
        return self.s.add("pe", lambda e: e.transpose(out=out, in_=in_, identity=ident), reads, writes)

    def op(self, eng, fn, reads=(), writes=()):
        return self.s.add(eng, fn, reads, writes)


def build(NB, S, stage="all", trace_scopes=False):
    k = K(NB, S)
    k.s.trace_scopes = trace_scopes
    nc, s = k.nc, k.s
    T = NB * S
    NT = T // 128
    NG = T // 512
    GPB = S // 512

    x_in = k.din("x", [T, D])
    cT_in = k.din("cT", [128, 8 * NB])
    ada_w = k.din("ada_w", [2, D, 6 * D])
    ada_b = k.din("ada_b", [2, 6 * D])
    gmixF = k.din("gmixF", [2, 128, 8])
    gffn = k.din("gffn", [2, D])
    conv_in_w = k.din("conv_in_w", [D, 3 * D])
    convwF = k.din("convwF", [128, 24])
    conv_out_w = k.din("conv_out_w", [D, D])
    ident_in = k.din("ident", [128, 128])
    tri_in = k.din("tri", [128, 128])
    Wr_in = k.din("Wr", [2, D, 36])
    br_in = k.din("br", [2, 36])
    exp_gate_w = k.din("exp_gate_w", [2 * 32 * 128, 8 * 512])
    exp_up_w = k.din("exp_up_w", [2 * 32 * 128, 8 * 512])
    exp_down_w = k.din("exp_down_w", [2 * 32 * 128, 4 * D])
    pcol_in = k.din("pcol", [128, 1])
    attn_in_w = k.din("attn_in_w", [D, 4608])
    attn_out_w = k.din("attn_out_w", [512, D])
    ropeC_in = k.din("ropeC", [128, S])
    ropeS_in = k.din("ropeS", [128, S])
    aconst_in = k.din("aconst", [128, 640])
    fing_in = k.din("fing", [1, D])
    out_ap = k.dout("out", [T, D])
    NBLK = (2 * T) // 512 + 32
    hs = k.dscr("hs", [T, D], BF16)
    xs = k.dscr("xs", [NBLK * 512, D], BF16)
    ys = k.dscr("ys", [NBLK * 512, D], F32)
    xr = k.dout("xr", [T, D])
    modrow = k.dout("modrow", [2, NB, 6 * D])

    ident_f = k.sb("ident_f", [128, 128], F32)
    ident_b = k.sb("ident_b", [128, 128], BF16)
    cT = k.sb("cT", [128, 8 * NB], F32)
    B_ident_f, B_ident_b, B_cT = Buf(), Buf(), Buf()
    B_modrow = [Buf() for _ in range(2)]

    ld0 = k.dsem("ld0")
    k.dma("sp", ident_f[:], ident_in[:, :], ld0, writes=[B_ident_f], grp="init")
    k.dma("sp", cT[:], cT_in[:, :], ld0, writes=[B_cT], grp="init")
    k.op("dve", lambda e: e.tensor_copy(out=ident_b[:], in_=ident_f[:]), [B_ident_f], [B_ident_b])
    k.op("act", lambda e: e.activation(out=cT[:], in_=cT[:], func=AF.Silu), [B_cT], [B_cT])

    s.scope = "A_mod"
    NSA = 3
    aw = [k.sbp(f"aw{i}", [128, 8, 512], F32) for i in range(NSA)]
    adab = [k.sbp(f"adab{i}", [NB, 512], F32) for i in range(NSA)]
    modc = [k.sbp(f"modc{i}", [NB, 512], F32) for i in range(NSA)]
    B_aw, B_adab, B_modc = [Buf() for _ in range(NSA)], [Buf() for _ in range(NSA)], [Buf() for _ in range(NSA)]
    aw_sem = [k.dsem(f"aw{i}") for i in range(NSA)]
    adab_sem = [k.dsem(f"adab{i}") for i in range(NSA)]
    mod_st = [k.dsem(f"mod_st{i}", sw=True) for i in range(NSA)]
    ps_mod = [k.psp(f"ps_mod{i}", [128, 512], F32) for i in range(2)]
    B_psmod = [Buf(), Buf()]
    chunks = [(l, cc) for l in range(2) for cc in range(12)]

    def a_load(ci):
        l, cc = chunks[ci]
        sl = ci % NSA
        src_b = ada_b[l:l + 1, cc * 512:(cc + 1) * 512]
        k.dma("sp", adab[sl][:], src_b.partition_broadcast(NB) if NB > 1 else src_b, adab_sem[sl],
              writes=[B_adab[sl]])
        k.dma("sp", aw[sl][:], ada_w[l, :, cc * 512:(cc + 1) * 512].rearrange("(kc p) f -> p kc f", p=128),
              aw_sem[sl], writes=[B_aw[sl]])
    for ci in range(NSA - 1):
        a_load(ci)
    for ci, (l, cc) in enumerate(chunks):
        sl = ci % NSA
        pq = ci % 2
        if ci + NSA - 1 < len(chunks):
            a_load(ci + NSA - 1)
        for kc in range(8):
            k.mm(ps_mod[pq][0:NB, :], cT[:, kc * NB:(kc + 1) * NB], aw[sl][:, kc, :], kc == 0, kc == 7,
                 reads=[B_cT, B_aw[sl]], writes=[B_psmod[pq]])
        k.op("dve", lambda e, sl=sl, pq=pq: e.tensor_tensor(out=modc[sl][:], in0=ps_mod[pq][0:NB, :], in1=adab[sl][:],
                                                           op=ALU.add),
             [B_psmod[pq], B_adab[sl]], [B_modc[sl]])
        k.dma("pool", modrow[l, :, cc * 512:(cc + 1) * 512], modc[sl][:], mod_st[sl], reads=[B_modc[sl]],
              writes=[B_modrow[l]])
    k.phase()
    if stage == "A":
        return finish(k, [B_modrow[0], B_modrow[1]])

    s.scope = "L0_conv"
    L = 0
    w_in = k.sbp("w_in", [128, 8, 3 * D], BF16)
    w_stage = [k.sbp(f"w_stage{i}", [128, D], F32) for i in range(2)]
    w_out_b1 = k.sbp("w_out_b", [128, 8, D], BF16)
    w_out_b = [w_out_b1 for b in range(NB)]
    g1bc = [k.sbp(f"g1bc{b}", [128, D], F32) for b in range(NB)]
    A1 = [k.sbp(f"A1_{b}", [128, 8], F32) for b in range(NB)]
    sh1 = [k.sbp(f"sh1_{b}", [128, 8], F32) for b in range(NB)]
    sc1t = k.sbp("sc1t", [128, 8], F32)
    gmix = k.sbp("gmix", [128, 8], F32)
    convw = k.sbp("convw", [128, 24], F32)
    B_w_in, B_sc1t, B_gmix, B_convw = Buf(), Buf(), Buf(), Buf()
    B_w_stage = [Buf(), Buf()]
    B_g1bc = [Buf() for _ in range(NB)]
    B_w_out_b1 = Buf()
    B_w_out_b = [B_w_out_b1 for _ in range(NB)]
    B_A1 = [Buf() for _ in range(NB)]
    B_sh1 = [Buf() for _ in range(NB)]
    wl = k.dsem("wl", sw=True)
    wst = [k.dsem("wst0"), k.dsem("wst1")]
    vl = k.dsem("vl")
    for kc in range(8):
        k.dma("pool", w_in[:, kc, :], conv_in_w[kc * 128:(kc + 1) * 128, :], wl, writes=[B_w_in], grp="w_in",
              max_dma_last_dim=4096)
    k.dma("sp", gmix[:], gmixF[L], vl, writes=[B_gmix], grp="v0")
    k.dma("sp", convw[:], convwF[:, :], vl, writes=[B_convw], grp="v0")
    for b in range(NB):
        k.dma("sp", sh1[b][:], modrow[L, b, 0:D].rearrange("(c p) -> p c", p=128), vl,
              reads=[B_modrow[L]], writes=[B_sh1[b]], allow_slow_non_contiguous=True)
        k.dma("sp", sc1t[:], modrow[L, b, D:2 * D].rearrange("(c p) -> p c", p=128), vl,
              reads=[B_modrow[L]], writes=[B_sc1t], allow_slow_non_contiguous=True)
        k.op("dve", lambda e, b=b: e.scalar_tensor_tensor(out=A1[b][:], in0=sc1t[:], scalar=1.0, in1=gmix[:],
                                                         op0=ALU.add, op1=ALU.mult),
             [B_sc1t, B_gmix], [B_A1[b]])
        k.dma("sp", g1bc[b][:], modrow[L, b:b + 1, 2 * D:3 * D].partition_broadcast(128), vl,
              reads=[B_modrow[L]], writes=[B_g1bc[b]])
    def build_w_out(b):
        for kc in range(8):
            sl = kc % 2
            k.dma("sp", w_stage[sl][:], conv_out_w[kc * 128:(kc + 1) * 128, :], wst[sl], writes=[B_w_stage[sl]])
            k.op("dve", lambda e, b=b, kc=kc, sl=sl: e.tensor_tensor(out=w_out_b[b][:, kc, :], in0=w_stage[sl][:],
                                                                    in1=g1bc[b][:], op=ALU.mult),
                 [B_w_stage[sl], B_g1bc[b]], [B_w_out_b[b]])
    build_w_out(0)

    if stage == "L0setup":
        return finish(k, [B_modrow[0], B_modrow[1]] + B_w_out_b + B_A1 + B_sh1 + [B_w_in, B_convw])
    NXS = 4
    xt = [k.sbp(f"xt{i}", [128, 4, D], F32) for i in range(NXS)]
    B_xt = [Buf() for _ in range(NXS)]
    xt_ld = [k.dsem(f"xt_ld{i}") for i in range(NXS)]
    xt_st = [k.dsem(f"xt_st{i}") for i in range(NXS)]
    junk = k.sbp("junk", [128, D], BF16)
    B_junk = Buf()
    ss = k.sbp("ss", [128, 4], F32)
    rstd = k.sbp("rstd", [128, 4], F32)
    B_ss, B_rstd = Buf(), Buf()
    xn = [k.sbp(f"xn{i}", [128, 4, D], BF16) for i in range(2)]
    B_xn = [[Buf() for _ in range(4)] for _ in range(2)]
    pT = k.psp("pT", [128, 512], BF16)
    B_pT = Buf()
    hT = [k.sbp(f"hT{i}", [128, 8, 512], BF16) for i in range(2)]
    B_hT = [[Buf() for _ in range(8)] for _ in range(2)]
    psBCU = [[k.psp(f"ps{n}{i}", [128, 512]) for n in "BCU"] for i in range(2)]
    B_psBCU = [[Buf() for _ in range(3)] for _ in range(2)]
    Csb = [k.sbp(f"Csb{i}", [128, 512], F32) for i in range(2)]
    B_Csb = [Buf(), Buf()]
    zb = [k.sbp(f"zb{i}", [128, 514], F32) for i in range(2)]
    B_zb = [Buf(), Buf()]
    zh = k.sbp("zh", [128, 8, 2], F32)
    B_zh = [Buf() for _ in range(8)]
    zc = [k.sbp(f"zc{i}", [128, 512], F32) for i in range(2)]
    B_zc = [Buf(), Buf()]
    gT = [k.sbp(f"gT{i}", [128, 8, 512], BF16) for i in range(2)]
    B_gT = [[Buf() for _ in range(8)] for _ in range(2)]
    psY = k.psp("psY", [128, 512])
    B_psY = Buf()
    B_xr = [Buf() for _ in range(NG)]

    def load_x(G):
        sl = G % NXS
        k.dma("sp", xt[sl][:], x_in[G * 512:(G + 1) * 512, :].rearrange("(j p) d -> p j d", p=128),
              xt_ld[sl], writes=[B_xt[sl]])

    def norm_pre(G):
        sl = G % 2
        xt_t, B_x = xt[G % NXS], B_xt[G % NXS]
        for j in range(4):
            k.op("act", lambda e, j=j: e.activation(out=junk[:], in_=xt_t[:, j, :], func=AF.Square,
                                                    accum_out=ss[:, j:j + 1]),
                 [B_x], [B_junk, B_ss])
        k.op("dve", lambda e: e.tensor_scalar(out=rstd[:], in0=ss[:], scalar1=1.0 / D, scalar2=1e-6,
                                              op0=ALU.mult, op1=ALU.add), [B_ss], [B_rstd])
        k.op("act", lambda e: e.activation(out=rstd[:], in_=rstd[:], func=AF.Sqrt), [B_rstd], [B_rstd])
        k.op("dve", lambda e: e.reciprocal(out=rstd[:], in_=rstd[:]), [B_rstd], [B_rstd])
        for j in range(4):
            k.op("act", lambda e, j=j: e.activation(out=xn[sl][:, j, :], in_=xt_t[:, j, :], func=AF.Copy,
                                                    scale=rstd[:, j:j + 1]),
                 [B_x, B_rstd], [B_xn[sl][j]])

    def norm_tr(G, c):
        b = G // GPB
        sl = G % 2
        A, B_A, sh, B_sh = A1[b], B_A1[b], sh1[b], B_sh1[b]
        for j in range(4):
            k.tr(pT[:, j * 128:(j + 1) * 128], xn[sl][:, j, c * 128:(c + 1) * 128], ident_b[:],
                 reads=[B_xn[sl][j], B_ident_b], writes=[B_pT])
        k.op("act", lambda e, c=c: e.activation(out=hT[sl][:, c, :], in_=pT[:], func=AF.Identity,
                                                scale=A[:, c:c + 1], bias=sh[:, c:c + 1]),
             [B_pT, B_A, B_sh], [B_hT[sl][c]])

    def inproj(G, fcs):
        sl = G % 2
        first = (G % GPB == 0)
        for fc in fcs:
            q = fc % 2
            (psB, psC, psU), (B_psB, B_psC, B_psU) = psBCU[q], B_psBCU[q]
            for (pst, B_p, off) in ((psB, B_psB, 0), (psC, B_psC, D), (psU, B_psU, 2 * D)):
                for kc in range(8):
                    k.mm(pst[:], w_in[:, kc, off + fc * 128: off + (fc + 1) * 128], hT[sl][:, kc, :], kc == 0, kc == 7,
                         reads=[B_w_in, B_hT[sl][kc]], writes=[B_p])
            zt, Bz, zcb, Bzc, Cs, BCs = zb[q], B_zb[q], zc[q], B_zc[q], Csb[q], B_Csb[q]
            if first:
                k.op("pool", lambda e, zt=zt: e.memset(zt[:, 0:2], 0.0), [], [Bz])
            else:
                k.op("pool", lambda e, zt=zt, fc=fc: e.tensor_copy(out=zt[:, 0:2], in_=zh[:, fc, :]),
                     [B_zh[fc]], [Bz])
            k.op("act", lambda e, Cs=Cs, psC=psC: e.copy(out=Cs[:], in_=psC[:]), [B_psC], [BCs])
            k.op("dve", lambda e, zt=zt, Cs=Cs, psU=psU: e.tensor_tensor(out=zt[:, 2:514], in0=Cs[:], in1=psU[:],
                                                                        op=ALU.mult),
                 [BCs, B_psU, Bz], [Bz])
            k.op("pool", lambda e, zt=zt, fc=fc: e.tensor_copy(out=zh[:, fc, :], in_=zt[:, 512:514]),
                 [Bz], [B_zh[fc]])
            k.op("pool", lambda e, fc=fc, zt=zt, zcb=zcb: e.tensor_scalar(
                out=zcb[:], in0=zt[:, 2:514], scalar1=convw[:, fc * 3 + 2: fc * 3 + 3], scalar2=1.0,
                op0=ALU.mult, op1=ALU.mult), [Bz, B_convw], [Bzc])
            k.op("dve", lambda e, fc=fc, zt=zt, zcb=zcb: e.scalar_tensor_tensor(
                out=zcb[:], in0=zt[:, 1:513], scalar=convw[:, fc * 3 + 1: fc * 3 + 2], in1=zcb[:],
                op0=ALU.mult, op1=ALU.add), [Bz, B_convw, Bzc], [Bzc])
            k.op("dve", lambda e, fc=fc, zt=zt, zcb=zcb: e.scalar_tensor_tensor(
                out=zcb[:], in0=zt[:, 0:512], scalar=convw[:, fc * 3: fc * 3 + 1], in1=zcb[:],
                op0=ALU.mult, op1=ALU.add), [Bz, B_convw, Bzc], [Bzc])
            k.op("dve", lambda e, fc=fc, zcb=zcb, psB=psB, sl=sl: e.tensor_tensor(out=gT[sl][:, fc, :], in0=zcb[:],
                                                                                 in1=psB[:], op=ALU.mult),
                 [Bzc, B_psB], [B_gT[sl][fc]])

    def outproj_unit(G, u):
        b = G // GPB
        sl = G % 2
        xs_ = G % NXS
        j, dh = u // 2, u % 2
        for kc in range(8):
            k.mm(psY[:], gT[sl][:, kc, j * 128:(j + 1) * 128], w_out_b[b][:, kc, dh * 512:(dh + 1) * 512],
                 kc == 0, kc == 7, reads=[B_gT[sl][kc], B_w_out_b[b]], writes=[B_psY])
        k.op("dve", lambda e: e.tensor_tensor(
            out=xt[xs_][:, j, dh * 512:(dh + 1) * 512], in0=psY[:], in1=xt[xs_][:, j, dh * 512:(dh + 1) * 512],
            op=ALU.add), [B_psY, B_xt[xs_]], [B_xt[xs_]])
        if u == 7:
            k.dma("sp", xr[G * 512:(G + 1) * 512, :].rearrange("(j p) d -> p j d", p=128), xt[xs_][:], xt_st[xs_],
                  reads=[B_xt[xs_]], writes=[B_xr[G]])

    load_x(0)
    if NG > 1:
        load_x(1)
    norm_pre(0)
    for c in range(8):
        norm_tr(0, c)
    for G in range(NG):
        if G + 1 < NG:
            norm_pre(G + 1)
        if G + 2 < NG:
            load_x(G + 2)
        for fc in range(8):
            inproj(G, [fc])
            if G + 1 < NG and fc > 0:
                norm_tr(G + 1, fc - 1)
            if G > 0:
                outproj_unit(G - 1, fc)
        if G + 1 < NG:
            norm_tr(G + 1, 7)
        if G > 0 and (G % GPB) == 0 and G // GPB < NB:
            build_w_out(G // GPB)
    for u in range(8):
        outproj_unit(NG - 1, u)

    if stage == "mix0":
        return finish(k, [B_xr[G] for G in range(NG)])
    k.phase()
    dbg = k.dout("dbg", [128, 4096]) if stage.startswith("M") else None
    r = moe_layer(k, 0, dict(stage=stage, dbg=dbg, xr=xr, B_xr=B_xr, modrow=modrow, B_modrow=B_modrow, gffn=gffn, Wr_in=Wr_in, br_in=br_in,
                         tri_in=tri_in, ident_f=ident_f, B_ident_f=B_ident_f, ident_b=ident_b, B_ident_b=B_ident_b,
                         hs=hs, xs=xs, ys=ys, gw=exp_gate_w, uw=exp_up_w, dw=exp_down_w, NBLK=NBLK, pcol_in=pcol_in))
    if r is not None:
        return finish(k, [B_xr[G] for G in range(NG)] + r)
    if stage == "ffn0":
        return finish(k, [B_xr[G] for G in range(NG)])
    k.phase()
    attn_layer(k, dict(xr=xr, B_xr=B_xr, modrow=modrow, B_modrow=B_modrow, gmixF=gmixF, ident_b=ident_b,
                       B_ident_b=B_ident_b, w_in=attn_in_w, w_out=attn_out_w, ropeC=ropeC_in, ropeS=ropeS_in,
                       aconst=aconst_in, pcol_in=pcol_in))
    if stage == "mix1":
        return finish(k, [B_xr[G] for G in range(NG)])
    k.phase()
    B_out = [Buf() for _ in range(NG)]
    moe_layer(k, 1, dict(stage=stage, dbg=None, final=dict(fing=fing_in, out=out_ap, B_out=B_out), xr=xr, B_xr=B_xr, modrow=modrow, B_modrow=B_modrow, gffn=gffn,
                         Wr_in=Wr_in, br_in=br_in, tri_in=tri_in, ident_f=ident_f, B_ident_f=B_ident_f,
                         ident_b=ident_b, B_ident_b=B_ident_b, hs=hs, xs=xs, ys=ys, gw=exp_gate_w, uw=exp_up_w,
                         dw=exp_down_w, NBLK=NBLK, pcol_in=pcol_in))
    return finish(k, [B_out[G] for G in range(NG)])


DIL = (1, 4, 16)
import os
SKIP = os.environ.get("ATT_SKIP", "").split(",")


def attn_layer(k, a):
    L = 1
    NB, S, T = k.NB, k.S, k.T
    GPB = S // 512
    xr, B_xr, modrow, B_modrow = a["xr"], a["B_xr"], a["modrow"], a["B_modrow"]
    ident_b, B_ident_b = a["ident_b"], a["B_ident_b"]
    w_in, w_out = a["w_in"], a["w_out"]
    hT = k.sbp("a_hT", [128, 8, S], BF16)
    oT = k.sbp("a_oT", [128, 4, S], BF16)
    B_hT = [[Buf() for _ in range(GPB)] for _ in range(8)]
    B_oT = [Buf() for _ in range(4)]
    vl = k.dsem("a_vl")
    for b in range(NB):
        k.s.scope = f"att{b}_1_hT"
        with contextlib.ExitStack() as c1:
            def sb1(name, shape, dt=F32):
                return c1.enter_context(k.nc.sbuf_tensor(f"s_a1_{b}_{name}", list(shape), dt))
            A1, sh1, sc1t, gmix = sb1("A1", [128, 8]), sb1("sh1", [128, 8]), sb1("sc1t", [128, 8]), sb1("gmix", [128, 8])
            B_A1, B_sh1, B_sc1t, B_gmix = Buf(), Buf(), Buf(), Buf()
            k.dma("sp", gmix[:], a["gmixF"][L], vl, writes=[B_gmix])
            k.dma("sp", sh1[:], modrow[L, b, 0:D].rearrange("(c p) -> p c", p=128), vl,
                  reads=[B_modrow[L]], writes=[B_sh1], allow_slow_non_contiguous=True)
            k.dma("sp", sc1t[:], modrow[L, b, D:2 * D].rearrange("(c p) -> p c", p=128), vl,
                  reads=[B_modrow[L]], writes=[B_sc1t], allow_slow_non_contiguous=True)
            k.op("dve", lambda e: e.scalar_tensor_tensor(out=A1[:], in0=sc1t[:], scalar=1.0, in1=gmix[:],
                                                         op0=ALU.add, op1=ALU.mult), [B_sc1t, B_gmix], [B_A1])
            xt = [sb1(f"xt{i}", [128, 4, D]) for i in range(2)]
            B_xt = [Buf(), Buf()]
            xt_ld = [k.dsem(f"a1_{b}_xt_ld0"), k.dsem(f"a1_{b}_xt_ld1")]
            junk = sb1("junk", [128, D], BF16)
            ss, rstd = sb1("ss", [128, 4]), sb1("rstd", [128, 4])
            xn = sb1("xn", [128, 4, D], BF16)
            B_junk, B_ss, B_rstd = Buf(), Buf(), Buf()
            B_xn = [Buf() for _ in range(4)]
            pT = [c1.enter_context(k.nc.psum_tensor(f"p_a1_{b}_pT{i}", [128, 512], BF16)) for i in range(2)]
            B_pT = [Buf(), Buf()]
            def a1_load(gi):
                G = b * GPB + gi
                sl = gi % 2
                k.dma("sp", xt[sl][:], xr[G * 512:(G + 1) * 512, :].rearrange("(j p) d -> p j d", p=128), xt_ld[sl],
                      reads=[B_xr[G]], writes=[B_xt[sl]])
            a1_load(0)
            for gi in range(GPB):
                G = b * GPB + gi
                sl = gi % 2
                if gi + 1 < GPB:
                    a1_load(gi + 1)
                xt_t, B_x = xt[sl], B_xt[sl]
                for j in range(4):
                    k.op("act", lambda e, j=j, xt_t=xt_t: e.activation(out=junk[:], in_=xt_t[:, j, :], func=AF.Square,
                                                                      accum_out=ss[:, j:j + 1]), [B_x], [B_junk, B_ss])
                k.op("dve", lambda e: e.tensor_scalar(out=rstd[:], in0=ss[:], scalar1=1.0 / D, scalar2=1e-6,
                                                      op0=ALU.mult, op1=ALU.add), [B_ss], [B_rstd])
                k.op("act", lambda e: e.activation(out=rstd[:], in_=rstd[:], func=AF.Sqrt), [B_rstd], [B_rstd])
                k.op("dve", lambda e: e.reciprocal(out=rstd[:], in_=rstd[:]), [B_rstd], [B_rstd])
                for j in range(4):
                    k.op("act" if j % 2 == 0 else "pool", (lambda e, j=j, xt_t=xt_t: e.activation(
                        out=xn[:, j, :], in_=xt_t[:, j, :], func=AF.Copy, scale=rstd[:, j:j + 1])) if j % 2 == 0 else (
                        lambda e, j=j, xt_t=xt_t: e.tensor_scalar(out=xn[:, j, :], in0=xt_t[:, j, :],
                                                                  scalar1=rstd[:, j:j + 1], scalar2=1.0,
                                                                  op0=ALU.mult, op1=ALU.mult)),
                         [B_x, B_rstd], [B_xn[j]])
                for c in range(8):
                    p = c % 2
                    for j in range(4):
                        k.tr(pT[p][:, j * 128:(j + 1) * 128], xn[:, j, c * 128:(c + 1) * 128], ident_b[:],
                             reads=[B_xn[j], B_ident_b], writes=[B_pT[p]])
                    k.op("act", lambda e, c=c, p=p, gi=gi: e.activation(
                        out=hT[:, c, gi * 512:(gi + 1) * 512], in_=pT[p][:], func=AF.Identity,
                        scale=A1[:, c:c + 1], bias=sh1[:, c:c + 1]), [B_pT[p], B_A1, B_sh1], [B_hT[c][gi]])
            k.s.barrier()
        k.s.scope = f"att{b}_2_core"
        with contextlib.ExitStack() as c2:
            def sb2(name, shape, dt=F32):
                return c2.enter_context(k.nc.sbuf_tensor(f"s_a2_{b}_{name}", list(shape), dt))

            def ps2(name, shape, dt=F32):
                return c2.enter_context(k.nc.psum_tensor(f"p_a2_{b}_{name}", list(shape), dt))
            Ct, St = sb2("Ct", [128, S], BF16), sb2("St", [128, S], BF16)
            acb = sb2("acb", [128, 640], BF16)
            B_Ct, B_St, B_acb = Buf(), Buf(), Buf()
            vlp = k.dsem(f"a2_{b}_vlp", sw=True)
            k.dma("pool", Ct[:], a["ropeC"][:, :], vlp, writes=[B_Ct], max_dma_last_dim=4096)
            k.dma("pool", St[:], a["ropeS"][:, :], vlp, writes=[B_St], max_dma_last_dim=4096)
            k.dma("pool", acb[:], a["aconst"][:, :], vlp, writes=[B_acb])
            Rm, Mprev, Mcur = acb[:, 0:128], acb[:, 128:256], acb[:, 256:384]
            onesp = [acb[:, 384:512], acb[:, 512:640]]
            qz = [sb2(f"qz{h}", [128, S], BF16) for h in range(2)]
            kT = sb2("kT", [128, S], BF16)
            B_qz = [[Buf() for _ in range(GPB)] for _ in range(2)]
            B_kT = [Buf() for _ in range(GPB)]
            pc = sb2("pc", [128, 1])
            hm = sb2("hm", [128, 2])
            hmb = sb2("hmb", [128, 2], BF16)
            B_pc, B_hm = Buf(), Buf()
            k.dma("sp", pc[:], a["pcol_in"][:, :], vl, writes=[B_pc])
            k.op("dve", lambda e: e.tensor_scalar(out=hm[:, 0:1], in0=pc[:], scalar1=64.0, scalar2=None, op0=ALU.is_lt),
                 [B_pc], [B_hm])
            k.op("dve", lambda e: e.tensor_scalar(out=hm[:, 1:2], in0=pc[:], scalar1=64.0, scalar2=None, op0=ALU.is_ge),
                 [B_pc], [B_hm])
            k.op("dve", lambda e: e.tensor_copy(out=hmb[:], in_=hm[:]), [B_hm], [B_hm])
            NBK = S // 128
            Vp = sb2("Vp", [128, NBK, 2, 128], BF16)
            B_Vp = [Buf() for _ in range(NBK // 4)]
            k.op("pool", lambda e: e.memset(Vp[:].rearrange("p a h f -> p (a h f)"), 0.0), [], B_Vp)
            Nacc, Dacc = sb2("Nacc", [128, S]), sb2("Dacc", [128, S])
            B_Nacc, B_Dacc = Buf(), Buf()
            wq = [sb2(f"wq{i}", [128, 8, 384], BF16) for i in range(2)]
            B_wq = [Buf(), Buf()]
            wq_sem = [k.dsem(f"a2_{b}_wq0", sw=True), k.dsem(f"a2_{b}_wq1", sw=True)]
            qsb_ = [sb2(f"qsb{i}", [128, 512], BF16) for i in range(2)]
            t1s, t2s = sb2("t1s", [128, 512]), sb2("t2s", [128, 512])
            t1_, t2_ = [t1s, t1s], [t2s, t2s]
            Bt1, Bt2 = Buf(), Buf()
            B_qsb_, B_t1_, B_t2_ = [Buf(), Buf()], [Bt1, Bt1], [Bt2, Bt2]
            PT = [sb2(f"PT{i}", [128, 2, 2, 128], BF16) for i in range(2)]
            B_PT = [Buf(), Buf()]
            psQ_ = [ps2(f"psQ{i}", [128, 512]) for i in range(2)]
            psR0 = ps2("psR0", [128, 512])
            psR_ = [psR0, psR0]
            psV = ps2("psV", [128, 4, 128])
            psS = [ps2(f"psS{i}", [128, 2, 2, 128]) for i in range(2)]
            psND = [ps2(f"psND{i}", [128, 2, 128]) for i in range(2)]
            BpsR = Buf()
            B_psQ_, B_psR_, B_psV = [Buf(), Buf()], [BpsR, BpsR], Buf()
            B_psS, B_psND = [Buf(), Buf()], [Buf(), Buf()]
            pit = 0
            allhT = [B_hT[c][gi] for c in range(8) for gi in range(GPB)]
            it = 0

            def wq_load(hp, g):
                wsl = (hp * 3 + g) % 2
                for qi in range(3):
                    col = g * 1536 + qi * 512 + hp * 128
                    for kc in range(8):
                        k.dma("pool", wq[wsl][:, kc, qi * 128:(qi + 1) * 128],
                              w_in[kc * 128:(kc + 1) * 128, col:col + 128], wq_sem[wsl], writes=[B_wq[wsl]],
                              grp=("wq", hp, g))
            wq_load(0, 0)
            for hp in range(4):
                for g in range(3):
                    dil = DIL[g]
                    nb = S // dil // 128
                    wsl = (hp * 3 + g) % 2
                    nxt = hp * 3 + g + 1
                    if nxt < 12:
                        wq_load(nxt // 3, nxt % 3)
                    for qi in range(2 if "qk" not in SKIP else 0):
                        for tg in range(GPB):
                            pi = pit % 2
                            pit += 1
                            psQ, psR, qsb, t1, t2 = psQ_[pi], psR_[pi], qsb_[pi], t1_[pi], t2_[pi]
                            B_psQ, B_psR, B_qsb, B_t1, B_t2 = B_psQ_[pi], B_psR_[pi], B_qsb_[pi], B_t1_[pi], B_t2_[pi]
                            for kc in range(8):
                                k.mm(psQ[:], wq[wsl][:, kc, qi * 128:(qi + 1) * 128], hT[:, kc, tg * 512:(tg + 1) * 512],
                                     kc == 0, kc == 7, reads=[B_wq[wsl], B_hT[kc][tg]], writes=[B_psQ])
                            k.op("act", lambda e, qsb=qsb, psQ=psQ: e.copy(out=qsb[:], in_=psQ[:]), [B_psQ], [B_qsb])
                            k.mm(psR[:], Rm, qsb[:], True, True, reads=[B_acb, B_qsb], writes=[B_psR])
                            k.op("dve", lambda e, tg=tg, t1=t1, psQ=psQ: e.tensor_tensor(
                                out=t1[:], in0=psQ[:], in1=Ct[:, tg * 512:(tg + 1) * 512], op=ALU.mult),
                                 [B_psQ, B_Ct, B_qsb], [B_t1])
                            k.op("dve", lambda e, tg=tg, t2=t2, psR=psR: e.tensor_tensor(
                                out=t2[:], in0=psR[:], in1=St[:, tg * 512:(tg + 1) * 512], op=ALU.mult),
                                 [B_psR, B_St], [B_t2])
                            if qi == 0:
                                k.op("pool", lambda e, t1=t1, t2=t2, qsb=qsb: e.tensor_tensor(
                                    out=qsb[:], in0=t1[:], in1=t2[:], op=ALU.add), [B_t1, B_t2, B_qsb], [B_qsb])
                                for h in range(2):
                                    k.op("dve", lambda e, h=h, tg=tg, qsb=qsb: e.tensor_scalar(
                                        out=qz[h][:, tg * 512:(tg + 1) * 512], in0=qsb[:], scalar1=hmb[:, h:h + 1],
                                        scalar2=None, op0=ALU.mult), [B_qsb, B_hm], [B_qz[h][tg]])
                            else:
                                k.op("pool", lambda e, tg=tg, t1=t1, t2=t2: e.tensor_tensor(
                                    out=kT[:, tg * 512:(tg + 1) * 512], in0=t1[:], in1=t2[:], op=ALU.add),
                                     [B_t1, B_t2], [B_kT[tg]])
                    def tok(r, n):
                        st = r + n * 128 * dil
                        return slice(st, st + 127 * dil + 1, dil)
                    blks = [(r, n) for r in range(dil) for n in range(nb)]
                    for bi, (r, n) in enumerate(blks if "vproj" not in SKIP else []):
                        for kc in range(8):
                            k.mm(psV[:, bi % 4, :], hT[:, kc, tok(r, n)], wq[wsl][:, kc, 256:384], kc == 0, kc == 7,
                                 reads=[B_wq[wsl]] + [B_hT[kc][gi] for gi in range(GPB)], writes=[B_psV])
                        if bi % 4 == 3:
                            b4 = bi // 4
                            k.op("dve", lambda e, b4=b4: e.tensor_copy(out=Vp[:, b4 * 4:(b4 + 1) * 4, 0, 0:64],
                                                                      in_=psV[:, :, 0:64]), [B_psV], [B_Vp[b4]])
                            k.op("dve", lambda e, b4=b4: e.tensor_copy(out=Vp[:, b4 * 4:(b4 + 1) * 4, 1, 64:128],
                                                                      in_=psV[:, :, 64:128]), [B_psV], [B_Vp[b4]])
                    ablks = blks if "blocks" not in SKIP else []

                    def chunks_of(bi):
                        r, n = ablks[bi]
                        ch = [(1, tok(r, n), Mcur, bi)]
                        if n > 0:
                            ch.append((0, tok(r, n - 1), Mprev, bi - 1))
                        return ch

                    def emit_scores(bi, si):
                        r, n = ablks[bi]
                        cur = tok(r, n)
                        for h in range(2):
                            for (ci, ktok, Mk, vb) in chunks_of(bi):
                                k.mm(psS[si][:, h, ci, :], kT[:, ktok], qz[h][:, cur], True, False,
                                     reads=B_kT + B_qz[h], writes=[B_psS[si]])
                                k.mm(psS[si][:, h, ci, :], ident_b[:], Mk, False, True,
                                     reads=[B_ident_b, B_acb], writes=[B_psS[si]])

                    if ablks:
                        emit_scores(0, it % 2)
                    for bi, (r, n) in enumerate(ablks):
                        si = it % 2
                        it += 1
                        cur = tok(r, n)
                        chunks = chunks_of(bi)
                        if bi + 1 < len(ablks):
                            emit_scores(bi + 1, it % 2)
                        k.op("act", lambda e, si=si: e.activation(
                            out=PT[si][:].rearrange("p h c q -> p (h c q)"),
                            in_=psS[si][:].rearrange("p h c q -> p (h c q)"), func=AF.Exp, scale=0.125),
                            [B_psS[si]], [B_PT[si]])
                        nmm = 2 * len(chunks)
                        for which in range(2):
                            ii = 0
                            for h in range(2):
                                for (ci, ktok, Mk, vb) in chunks:
                                    lhs = Vp[:, vb, h, :] if which == 0 else onesp[h]
                                    k.mm(psND[si][:, which, :], lhs, PT[si][:, h, ci, :], ii == 0, ii == nmm - 1,
                                         reads=[B_Vp[vb // 4], B_PT[si], B_acb], writes=[B_psND[si]])
                                    ii += 1
                        if g == 0:
                            k.op("dve", lambda e, si=si, cur=cur: e.tensor_copy(out=Nacc[:, cur], in_=psND[si][:, 0, :]),
                                 [B_psND[si]], [B_Nacc])
                            k.op("dve", lambda e, si=si, cur=cur: e.tensor_copy(out=Dacc[:, cur], in_=psND[si][:, 1, :]),
                                 [B_psND[si]], [B_Dacc])
                        else:
                            k.op("dve", lambda e, si=si, cur=cur: e.tensor_tensor(out=Nacc[:, cur], in0=psND[si][:, 0, :],
                                                                                 in1=Nacc[:, cur], op=ALU.add),
                                 [B_psND[si], B_Nacc], [B_Nacc])
                            k.op("dve", lambda e, si=si, cur=cur: e.tensor_tensor(out=Dacc[:, cur], in0=psND[si][:, 1, :],
                                                                                 in1=Dacc[:, cur], op=ALU.add),
                                 [B_psND[si], B_Dacc], [B_Dacc])
                if "norm" in SKIP:
                    continue
                k.op("act", lambda e: e.activation(out=Dacc[:], in_=Dacc[:], func=AF.Ln), [B_Dacc], [B_Dacc])
                k.op("act", lambda e: e.activation(out=Dacc[:], in_=Dacc[:], func=AF.Exp, scale=-1.0),
                     [B_Dacc], [B_Dacc])
                k.op("dve", lambda e, hp=hp: e.tensor_tensor(out=oT[:, hp, :], in0=Nacc[:], in1=Dacc[:], op=ALU.mult),
                     [B_Nacc, B_Dacc], [B_oT[hp]])
            k.s.barrier()
        k.s.scope = f"att{b}_3_out"
        with contextlib.ExitStack() as c3:
            def sb3(name, shape, dt=F32):
                return c3.enter_context(k.nc.sbuf_tensor(f"s_a3_{b}_{name}", list(shape), dt))
            wof = sb3("wof", [128, 4, D])
            wob = sb3("wob", [128, 4, D], BF16)
            g1bc = sb3("g1bc", [128, D])
            B_wof, B_wob, B_g1bc = Buf(), Buf(), Buf()
            k.dma("sp", wof[:], w_out.rearrange("(c p) d -> p c d", p=128), vl, writes=[B_wof])
            k.dma("sp", g1bc[:], modrow[L, b:b + 1, 2 * D:3 * D].partition_broadcast(128), vl,
                  reads=[B_modrow[L]], writes=[B_g1bc])
            for c in range(4):
                k.op("dve", lambda e, c=c: e.tensor_tensor(out=wob[:, c, :], in0=wof[:, c, :], in1=g1bc[:], op=ALU.mult),
                     [B_wof, B_g1bc], [B_wob])
            xt = [sb3(f"xt{i}", [128, 4, D]) for i in range(3)]
            B_xt = [Buf(), Buf(), Buf()]
            xt_ld = [k.dsem(f"a3_{b}_xt_ld{i}") for i in range(3)]
            xt_st = [k.dsem(f"a3_{b}_xt_st{i}") for i in range(3)]
            psY = [c3.enter_context(k.nc.psum_tensor(f"p_a3_{b}_psY{i}", [128, 512], F32)) for i in range(2)]
            B_psY = [Buf(), Buf()]
            def a3_load(gi):
                G = b * GPB + gi
                sl = gi % 3
                k.dma("sp", xt[sl][:], xr[G * 512:(G + 1) * 512, :].rearrange("(j p) d -> p j d", p=128), xt_ld[sl],
                      reads=[B_xr[G]], writes=[B_xt[sl]])
            a3_load(0)
            for gi in range(GPB if "p3" not in SKIP else 0):
                G = b * GPB + gi
                sl = gi % 3
                if gi + 1 < GPB:
                    a3_load(gi + 1)
                for j in range(4):
                    for dh in range(2):
                        yi = (j * 2 + dh) % 2
                        t0 = gi * 512 + j * 128
                        for c in range(4):
                            k.mm(psY[yi][:], oT[:, c, t0:t0 + 128], wob[:, c, dh * 512:(dh + 1) * 512], c == 0, c == 3,
                                 reads=[B_oT[c], B_wob], writes=[B_psY[yi]])
                        k.op("dve", lambda e, j=j, dh=dh, yi=yi, sl=sl: e.tensor_tensor(
                            out=xt[sl][:, j, dh * 512:(dh + 1) * 512], in0=psY[yi][:],
                            in1=xt[sl][:, j, dh * 512:(dh + 1) * 512], op=ALU.add), [B_psY[yi], B_xt[sl]], [B_xt[sl]])
                k.dma("sp", xr[G * 512:(G + 1) * 512, :].rearrange("(j p) d -> p j d", p=128), xt[sl][:], xt_st[sl],
                      reads=[B_xt[sl]], writes=[B_xr[G]])
            k.s.barrier()


def moe_layer(k, L, a):
    NB, S, T = k.NB, k.S, k.T
    NT, NG, GPB = T // 128, T // 512, S // 512
    NBLK = a["NBLK"]
    xr, B_xr, modrow, B_modrow = a["xr"], a["B_xr"], a["modrow"], a["B_modrow"]
    ident_f, B_ident_f, ident_b, B_ident_b = a["ident_f"], a["B_ident_f"], a["ident_b"], a["B_ident_b"]
    hs, xs, ys = a["hs"], a["xs"], a["ys"]
    B_hs = [Buf() for _ in range(NG)]
    pfx = f"m{L}_"

    E1all = k.sbp(pfx + "E1all", [128, NT, 32], F32)
    E2all = k.sbp(pfx + "E2all", [128, NT, 32], F32)
    Oall = k.sbp(pfx + "Oall", [128, NT, 32], BF16)
    Wall = k.sbp(pfx + "Wall", [128, NT, 2], F32)
    B_E1 = [Buf() for _ in range(NT)]
    B_E2 = [Buf() for _ in range(NT)]
    B_O = [Buf() for _ in range(NT)]
    B_W = [Buf() for _ in range(NT)]
    g2bc = [k.sbp(pfx + f"g2bc{b}", [128, D], F32) for b in range(NB)]
    B_g2bc = [Buf() for _ in range(NB)]
    d1i = k.sbp(pfx + "d1i", [128, NT], I32)
    d2i = k.sbp(pfx + "d2i", [128, NT], I32)
    bei = k.sbp(pfx + "bei", [128, 2, NBLK], I32)
    B_d1i, B_d2i, B_bei = Buf(), Buf(), Buf()
    nusedi = k.sbp(pfx + "nusedi", [128, 1], I32)
    B_nusedi = Buf()
    vl = k.dsem(pfx + "vl")
    for b in range(NB):
        k.dma("sp", g2bc[b][:], modrow[L, b:b + 1, 5 * D:6 * D].partition_broadcast(128), vl,
              reads=[B_modrow[L]], writes=[B_g2bc[b]], grp="g2")

    k.s.scope = pfx + "M1_router"
    with contextlib.ExitStack() as c1:
        def sb1(name, shape, dt=F32):
            return c1.enter_context(k.nc.sbuf_tensor("s_" + pfx + name, list(shape), dt))

        def ps1(name, shape, dt=F32):
            return c1.enter_context(k.nc.psum_tensor("p_" + pfx + name, list(shape), dt))
        A2bc = [sb1(f"A2bc{b}", [128, D]) for b in range(NB)]
        sh2bc = [sb1(f"sh2bc{b}", [128, D]) for b in range(NB)]
        gfbc = sb1("gfbc", [128, D])
        B_A2bc = [Buf() for _ in range(NB)]
        B_sh2bc = [Buf() for _ in range(NB)]
        B_gfbc = Buf()
        Wr = sb1("Wr", [128, 8, 36])
        brbc = sb1("brbc", [128, 36])
        B_Wr, B_brbc = Buf(), Buf()
        k.dma("sp", gfbc[:], a["gffn"][L:L + 1, :].partition_broadcast(128), vl, writes=[B_gfbc], grp="g2")
        k.dma("sp", Wr[:], a["Wr_in"][L].rearrange("(kc p) f -> p kc f", p=128), vl, writes=[B_Wr], grp="g2")
        k.dma("sp", brbc[:], a["br_in"][L:L + 1, :].partition_broadcast(128), vl, writes=[B_brbc], grp="g2")
        for b in range(NB):
            k.dma("sp", sh2bc[b][:], modrow[L, b:b + 1, 3 * D:4 * D].partition_broadcast(128), vl,
                  reads=[B_modrow[L]], writes=[B_sh2bc[b]], grp="g2")
            k.dma("sp", A2bc[b][:], modrow[L, b:b + 1, 4 * D:5 * D].partition_broadcast(128), vl,
                  reads=[B_modrow[L]], writes=[B_A2bc[b]], grp="g2")
            k.op("dve", lambda e, b=b: e.scalar_tensor_tensor(out=A2bc[b][:], in0=A2bc[b][:], scalar=1.0, in1=gfbc[:],
                                                             op0=ALU.add, op1=ALU.mult),
                 [B_A2bc[b], B_gfbc], [B_A2bc[b]])
        xt = [sb1(f"xt{i}", [128, 4, D]) for i in range(2)]
        B_xt = [Buf(), Buf()]
        xt_ld = [k.dsem(pfx + "xt_ld0"), k.dsem(pfx + "xt_ld1")]
        junk = sb1("junk", [128, D], BF16)
        B_junk = Buf()
        ss, rstd = sb1("ss", [128, 4]), sb1("rstd", [128, 4])
        B_ss, B_rstd = Buf(), Buf()
        h2_ = [sb1(f"h2_{i}", [128, 4, D]) for i in range(2)]
        B_h2_ = [[Buf() for _ in range(4)] for _ in range(2)]
        h2b = [sb1(f"h2b{i}", [128, 4, D], BF16) for i in range(2)]
        B_h2b = [Buf(), Buf()]
        h2b_st = [k.dsem(pfx + "h2b_st0"), k.dsem(pfx + "h2b_st1")]
        pTf = [ps1(f"pTf{i}", [128, 512]) for i in range(2)]
        B_pTf = [Buf(), Buf()]
        hT2 = sb1("hT2", [128, 8, 512])
        B_hT2 = [Buf() for _ in range(8)]
        pslg = ps1("pslg", [128, 4, 36])
        B_pslg = Buf()
        lg4 = sb1("lg4", [128, 4, 36])
        gmax, gsum = sb1("gmax", [128, 4]), sb1("gsum", [128, 4])
        gsh = sb1("gsh", [128, 4, 4])
        ohg4 = sb1("ohg4", [128, 4, 4])
        tmp4 = sb1("tmp4", [128, 4, 4, 8])
        sel4, sel4b = sb1("sel4", [128, 4, 8]), sb1("sel4b", [128, 4, 8])
        oh1_4, oh2_4 = sb1("oh1_4", [128, 4, 8]), sb1("oh2_4", [128, 4, 8])
        m1, m2, dlt = sb1("m1", [128, 4]), sb1("m2", [128, 4]), sb1("dlt", [128, 4])
        B_lg, B_sm, B_sm2, B_ohg, B_ge, B_sel, B_selb, B_oh1, B_oh2, B_tmp4, B_m1, B_m2, B_dlt = (Buf() for _ in range(13))

        def load_x(G):
            sl = G % 2
            k.dma("sp", xt[sl][:], xr[G * 512:(G + 1) * 512, :].rearrange("(j p) d -> p j d", p=128),
                  xt_ld[sl], reads=[B_xr[G]], writes=[B_xt[sl]])
        def stage1(G):
            b = G // GPB
            sl = G % 2
            xt_t, B_x = xt[sl], B_xt[sl]
            h2, B_h2 = h2_[sl], B_h2_[sl]
            for j in range(4):
                k.op("act", lambda e, j=j, xt_t=xt_t: e.activation(out=junk[:], in_=xt_t[:, j, :], func=AF.Square,
                                                                  accum_out=ss[:, j:j + 1]), [B_x], [B_junk, B_ss])
            k.op("dve", lambda e: e.tensor_scalar(out=rstd[:], in0=ss[:], scalar1=1.0 / D, scalar2=1e-6,
                                                  op0=ALU.mult, op1=ALU.add), [B_ss], [B_rstd])
            k.op("act", lambda e: e.activation(out=rstd[:], in_=rstd[:], func=AF.Sqrt), [B_rstd], [B_rstd])
            k.op("dve", lambda e: e.reciprocal(out=rstd[:], in_=rstd[:]), [B_rstd], [B_rstd])
            for j in range(4):
                k.op("dve", lambda e, j=j, xt_t=xt_t, b=b, h2=h2: e.scalar_tensor_tensor(
                    out=h2[:, j, :], in0=xt_t[:, j, :], scalar=rstd[:, j:j + 1], in1=A2bc[b][:],
                    op0=ALU.mult, op1=ALU.mult), [B_x, B_rstd, B_A2bc[b]], [B_h2[j]])
                k.op("pool" if j % 2 else "dve", lambda e, j=j, b=b, h2=h2: e.tensor_tensor(
                    out=h2[:, j, :], in0=h2[:, j, :], in1=sh2bc[b][:], op=ALU.add),
                     [B_h2[j], B_sh2bc[b]], [B_h2[j]])

        def stage1b(G):
            sl = G % 2
            h2, B_h2 = h2_[sl], B_h2_[sl]
            for j in range(4):
                k.op("act", lambda e, j=j, sl=sl, h2=h2: e.copy(out=h2b[sl][:, j, :], in_=h2[:, j, :]),
                     [B_h2[j]], [B_h2b[sl]])
            k.dma("sp", hs[G * 512:(G + 1) * 512, :].rearrange("(j p) d -> p j d", p=128), h2b[sl][:], h2b_st[sl],
                  reads=[B_h2b[sl]], writes=[B_hs[G]])

        load_x(0)
        if NG > 1:
            load_x(1)
        stage1(0)
        stage1b(0)
        for G in range(NG):
            b = G // GPB
            sl = G % 2
            h2, B_h2 = h2_[sl], B_h2_[sl]
            for c in range(8):
                p = c % 2
                for j in range(4):
                    k.tr(pTf[p][:, j * 128:(j + 1) * 128], h2[:, j, c * 128:(c + 1) * 128], ident_f[:],
                         reads=[B_h2[j], B_ident_f], writes=[B_pTf[p]])
                if c % 2 == 0:
                    k.op("act", lambda e, c=c, p=p: e.copy(out=hT2[:, c, :], in_=pTf[p][:]), [B_pTf[p]], [B_hT2[c]])
                else:
                    k.op("dve", lambda e, c=c, p=p: e.tensor_copy(out=hT2[:, c, :], in_=pTf[p][:]),
                         [B_pTf[p]], [B_hT2[c]])
            for j in range(4):
                for kc in range(8):
                    k.mm(pslg[:, j, :], hT2[:, kc, j * 128:(j + 1) * 128], Wr[:, kc, :], kc == 0, kc == 7,
                         reads=[B_hT2[kc], B_Wr], writes=[B_pslg])
            if G + 1 < NG:
                stage1(G + 1)
            if G + 2 < NG:
                load_x(G + 2)
            V = "dve"
            i0_ = G * 4
            J = 4

            def bc(ap, shape):
                return ap.to_broadcast(list(shape))
            k.op(V, lambda e: e.tensor_tensor(out=lg4[:], in0=pslg[:], in1=bc(brbc[:].unsqueeze(1), [128, J, 36]),
                                              op=ALU.add), [B_pslg, B_brbc], [B_lg])
            k.op(V, lambda e: e.tensor_reduce(out=gmax[:], in_=lg4[:, :, 0:4], axis=AX.X, op=ALU.max), [B_lg], [B_sm])
            k.op(V, lambda e: e.tensor_tensor(out=gsh[:], in0=lg4[:, :, 0:4], in1=bc(gmax[:].unsqueeze(2), [128, J, 4]),
                                              op=ALU.subtract), [B_lg, B_sm], [B_ge])
            k.op(V, lambda e: e.tensor_tensor(out=ohg4[:], in0=lg4[:, :, 0:4], in1=bc(gmax[:].unsqueeze(2), [128, J, 4]),
                                              op=ALU.is_equal), [B_lg, B_sm], [B_ohg])
            k.op("act", lambda e: e.activation(out=gsh[:], in_=gsh[:], func=AF.Exp), [B_ge], [B_ge])
            k.op(V, lambda e: e.tensor_reduce(out=gsum[:], in_=gsh[:], axis=AX.X, op=ALU.add), [B_ge], [B_sm2])
            k.op(V, lambda e: e.reciprocal(out=gsum[:], in_=gsum[:]), [B_sm2], [B_sm2])
            k.op(V, lambda e: e.tensor_tensor(out=tmp4[:], in0=lg4[:, :, 4:36].rearrange("p j (g e) -> p j g e", g=4),
                                              in1=bc(ohg4[:].unsqueeze(3), [128, J, 4, 8]), op=ALU.mult),
                 [B_lg, B_ohg], [B_tmp4])
            k.op(V, lambda e: e.tensor_reduce(out=sel4[:], in_=tmp4[:].rearrange("p j g e -> p j e g"), axis=AX.X,
                                              op=ALU.add), [B_tmp4], [B_sel])
            k.op(V, lambda e: e.tensor_reduce(out=m1[:], in_=sel4[:], axis=AX.X, op=ALU.max), [B_sel], [B_m1])
            k.op(V, lambda e: e.tensor_tensor(out=oh1_4[:], in0=sel4[:], in1=bc(m1[:].unsqueeze(2), [128, J, 8]),
                                              op=ALU.is_equal), [B_sel, B_m1], [B_oh1])
            k.op(V, lambda e: e.scalar_tensor_tensor(out=sel4b[:], in0=oh1_4[:], scalar=-1.0e30, in1=sel4[:],
                                                     op0=ALU.mult, op1=ALU.add), [B_oh1, B_sel], [B_selb])
            k.op(V, lambda e: e.tensor_reduce(out=m2[:], in_=sel4b[:], axis=AX.X, op=ALU.max), [B_selb], [B_m2])
            k.op(V, lambda e: e.tensor_tensor(out=oh2_4[:], in0=sel4b[:], in1=bc(m2[:].unsqueeze(2), [128, J, 8]),
                                              op=ALU.is_equal), [B_selb, B_m2], [B_oh2])
            k.op(V, lambda e: e.tensor_tensor(out=dlt[:], in0=m2[:], in1=m1[:], op=ALU.subtract), [B_m1, B_m2], [B_dlt])
            k.op("act", lambda e: e.activation(out=dlt[:], in_=dlt[:], func=AF.Exp), [B_dlt], [B_dlt])
            k.op(V, lambda e: e.tensor_scalar(out=dlt[:], in0=dlt[:], scalar1=1.0, scalar2=None, op0=ALU.add),
                 [B_dlt], [B_dlt])
            k.op(V, lambda e: e.reciprocal(out=dlt[:], in_=dlt[:]), [B_dlt], [B_dlt])
            Bws = B_W[i0_:i0_ + J]
            k.op(V, lambda e, i0_=i0_: e.tensor_tensor(out=Wall[:, i0_:i0_ + J, 0], in0=gsum[:], in1=dlt[:], op=ALU.mult),
                 [B_sm2, B_dlt], Bws)
            k.op(V, lambda e, i0_=i0_: e.tensor_tensor(out=Wall[:, i0_:i0_ + J, 1], in0=gsum[:],
                                                      in1=Wall[:, i0_:i0_ + J, 0], op=ALU.subtract),
                 [B_sm2] + Bws, Bws)
            for (Eall, oh, B_oh, B_E) in ((E1all, oh1_4, B_oh1, B_E1), (E2all, oh2_4, B_oh2, B_E2)):
                k.op(V, lambda e, Eall=Eall, oh=oh, i0_=i0_: e.tensor_tensor(
                    out=Eall[:, i0_:i0_ + J, :].rearrange("p j (g e) -> p j g e", g=4),
                    in0=bc(ohg4[:].unsqueeze(3), [128, J, 4, 8]), in1=bc(oh[:].unsqueeze(2), [128, J, 4, 8]),
                    op=ALU.mult), [B_ohg, B_oh], B_E[i0_:i0_ + J])
            k.op("pool", lambda e, i0_=i0_: e.tensor_tensor(out=Oall[:, i0_:i0_ + J, :], in0=E1all[:, i0_:i0_ + J, :],
                                                           in1=E2all[:, i0_:i0_ + J, :], op=ALU.add),
                 B_E1[i0_:i0_ + J] + B_E2[i0_:i0_ + J], B_O[i0_:i0_ + J])
            if G + 1 < NG:
                stage1b(G + 1)
        k.s.barrier()
    dbgsem = k.dsem(pfx + "dbg")
    B_dbg = Buf()
    if a.get("stage") == "M1":
        dbg = a["dbg"]
        k.dma("sp", dbg[:, 0:NT * 2], Wall[:].rearrange("p i w -> p (i w)"), dbgsem, reads=B_W, writes=[B_dbg])
        k.dma("sp", dbg[:, 1024:1024 + NT * 32], E1all[:].rearrange("p i e -> p (i e)"), dbgsem, reads=B_E1, writes=[B_dbg])
        k.dma("sp", dbg[:, 2048:2048 + NT * 32], E2all[:].rearrange("p i e -> p (i e)"), dbgsem, reads=B_E2, writes=[B_dbg])
        return [B_dbg]

    k.s.scope = pfx + "M2_index"
    with contextlib.ExitStack() as c2:
        def sb2(name, shape, dt=F32):
            return c2.enter_context(k.nc.sbuf_tensor("s_" + pfx + name, list(shape), dt))

        def ps2(name, shape, dt=F32):
            return c2.enter_context(k.nc.psum_tensor("p_" + pfx + name, list(shape), dt))
        trif, trib, onesb = sb2("trif", [128, 128]), sb2("trib", [128, 128], BF16), sb2("onesb", [128, 128], BF16)
        B_trif, B_trib, B_onesb = Buf(), Buf(), Buf()
        k.dma("sp", trif[:], a["tri_in"][:, :], vl, writes=[B_trif])
        k.op("dve", lambda e: e.tensor_copy(out=trib[:], in_=trif[:]), [B_trif], [B_trib])
        k.op("dve", lambda e: e.memset(onesb[:], 1.0), [], [B_onesb])
        base = sb2("base", [128, NT, 32])
        tot = sb2("tot", [128, NT, 32])
        B_base, B_tot = Buf(), Buf()
        psw = [ps2(f"psw{i}", [128, 512]) for i in range(2)]
        B_psw = [Buf(), Buf()]
        TPC = 16
        nch = (NT + TPC - 1) // TPC
        for ci in range(nch):
            t0, t1 = ci * TPC, min(NT, (ci + 1) * TPC)
            w = (t1 - t0) * 32
            for which, (lhs, B_l, dst, B_d) in enumerate(((trib, B_trib, base, B_base), (onesb, B_onesb, tot, B_tot))):
                k.mm(psw[which][:, 0:w], lhs[:], Oall[:, t0:t1, :], True, True,
                     reads=[B_l] + B_O[t0:t1], writes=[B_psw[which]])
                k.op("dve", lambda e, which=which, dst=dst, t0=t0, t1=t1, w=w: e.tensor_copy(
                    out=dst[:, t0:t1, :], in_=psw[which][:, 0:w]), [B_psw[which]], [B_d])
        cnt = sb2("cnt", [128, 32])
        nblk = sb2("nblk", [128, 32])
        cum = sb2("cum", [128, 32])
        carry = sb2("carry", [128, 32])
        tmp32 = sb2("tmp32", [128, 32])
        B_cnt, B_nblk, B_cum, B_carry, B_tmp32 = Buf(), Buf(), Buf(), Buf(), Buf()
        V = "dve"
        k.op(V, lambda e: e.tensor_reduce(out=cnt[:], in_=tot[:].rearrange("p i e -> p e i"), axis=AX.X, op=ALU.add),
             [B_tot], [B_cnt])
        k.op(V, lambda e: e.tensor_scalar(out=nblk[:], in0=cnt[:], scalar1=0.0, scalar2=None, op0=ALU.is_gt),
             [B_cnt], [B_nblk])
        for j in range(1, (2 * T) // 512 + 1):
            k.op(V, lambda e, j=j: e.scalar_tensor_tensor(out=nblk[:], in0=cnt[:], scalar=512.0 * j, in1=nblk[:],
                                                         op0=ALU.is_gt, op1=ALU.add), [B_cnt, B_nblk], [B_nblk])
        k.op(V, lambda e: e.tensor_copy(out=cum[:], in_=nblk[:]), [B_nblk], [B_cum])
        for ee in range(1, 32):
            k.op(V, lambda e, ee=ee: e.tensor_tensor(out=cum[:, ee:ee + 1], in0=cum[:, ee - 1:ee], in1=nblk[:, ee:ee + 1],
                                                    op=ALU.add), [B_cum, B_nblk], [B_cum])
        k.op(V, lambda e: e.tensor_tensor(out=carry[:], in0=cum[:], in1=nblk[:], op=ALU.subtract),
             [B_cum, B_nblk], [B_carry])
        k.op(V, lambda e: e.tensor_scalar(out=carry[:], in0=carry[:], scalar1=512.0, scalar2=None, op0=ALU.mult),
             [B_carry], [B_carry])
        for i in range(NT):
            k.op(V, lambda e, i=i: e.tensor_tensor(out=base[:, i, :], in0=base[:, i, :], in1=carry[:], op=ALU.add),
                 [B_base, B_carry], [B_base])
            if i + 1 < NT:
                k.op(V, lambda e, i=i: e.tensor_tensor(out=carry[:], in0=carry[:], in1=tot[:, i, :], op=ALU.add),
                     [B_carry, B_tot], [B_carry])
        dtmp = sb2("dtmp", [128, NT, 32])
        dfl = sb2("dfl", [128, NT])
        B_dtmp, B_dfl = Buf(), Buf()
        for (Eall, B_E, di, B_di) in ((E1all, B_E1, d1i, B_d1i), (E2all, B_E2, d2i, B_d2i)):
            k.op(V, lambda e, Eall=Eall: e.tensor_tensor(out=dtmp[:], in0=Eall[:], in1=base[:], op=ALU.mult),
                 list(B_E) + [B_base], [B_dtmp])
            k.op(V, lambda e: e.tensor_reduce(out=dfl[:], in_=dtmp[:], axis=AX.X, op=ALU.add), [B_dtmp], [B_dfl])
            k.op(V, lambda e, di=di: e.tensor_copy(out=di[:], in_=dfl[:]), [B_dfl], [B_di])
        bef = sb2("bef", [128, NBLK])
        B_bef = Buf()
        for j in range(NBLK):
            k.op(V, lambda e, j=j: e.tensor_scalar(out=tmp32[:], in0=cum[:], scalar1=float(j), scalar2=None,
                                                  op0=ALU.is_le, op1=ALU.add, accum_out=bef[:, j:j + 1]),
                 [B_cum], [B_tmp32, B_bef])
        k.op(V, lambda e: e.tensor_scalar(out=bef[:], in0=bef[:], scalar1=31.0, scalar2=None, op0=ALU.min),
             [B_bef], [B_bef])
        k.op(V, lambda e: e.tensor_copy(out=nusedi[:], in_=cum[:, 31:32]), [B_cum], [B_nusedi])
        pcol = sb2("pcol", [128, 1])
        B_pcol = Buf()
        k.dma("sp", pcol[:], a["pcol_in"][:, :], vl, writes=[B_pcol])
        k.op(V, lambda e: e.tensor_scalar(out=bef[:], in0=bef[:], scalar1=float(L * 32), scalar2=128.0,
                                          op0=ALU.add, op1=ALU.mult), [B_bef], [B_bef])
        k.op(V, lambda e: e.tensor_scalar(out=bef[:], in0=bef[:], scalar1=pcol[:, 0:1], scalar2=None, op0=ALU.add),
             [B_bef, B_pcol], [B_bef])
        k.op(V, lambda e: e.tensor_scalar(out=bef[:], in0=bef[:], scalar1=2.0, scalar2=None, op0=ALU.mult),
             [B_bef], [B_bef])
        k.op(V, lambda e: e.tensor_copy(out=bei[:, 0, :], in_=bef[:]), [B_bef], [B_bei])
        k.op(V, lambda e: e.tensor_scalar(out=bef[:], in0=bef[:], scalar1=1.0, scalar2=None, op0=ALU.add),
             [B_bef], [B_bef])
        k.op(V, lambda e: e.tensor_copy(out=bei[:, 1, :], in_=bef[:]), [B_bef], [B_bei])
        k.s.barrier()
        if a.get("stage") == "M2":
            dbg = a["dbg"].bitcast(I32)
            k.dma("sp", dbg[:, 0:NT], d1i[:], dbgsem, reads=[B_d1i], writes=[B_dbg])
            k.dma("sp", dbg[:, 1024:1024 + NT], d2i[:], dbgsem, reads=[B_d2i], writes=[B_dbg])
            k.dma("sp", dbg[:, 2048:2048 + NBLK], bei[:, 0, :], dbgsem, reads=[B_bei], writes=[B_dbg])
            k.s.barrier()
    if a.get("stage") == "M2":
        return [B_dbg]

    k.s.scope = pfx + "M3_scatter"
    with contextlib.ExitStack() as c3:
        NS3 = 6
        hsb = [c3.enter_context(k.nc.sbuf_tensor(f"s_{pfx}hsb{i}", [128, D], BF16)) for i in range(NS3)]
        B_hsb = [Buf() for _ in range(NS3)]
        hsb_ld = [k.dsem(pfx + f"hsb_ld{i}") for i in range(NS3)]
        sc_sem = [k.dsem(pfx + f"sc{i}", sw=True) for i in range(NS3)]
        B_xs = Buf()

        def m3_load(i):
            sl = i % NS3
            k.dma("sp", hsb[sl][:], hs[i * 128:(i + 1) * 128, :], hsb_ld[sl], reads=[B_hs[i // 4]], writes=[B_hsb[sl]])
        for i in range(min(NS3 - 1, NT)):
            m3_load(i)
        for i in range(NT):
            sl = i % NS3
            if i + NS3 - 1 < NT:
                m3_load(i + NS3 - 1)
            for (di, B_di) in ((d1i, B_d1i), (d2i, B_d2i)):
                k.s.add("pool", lambda e, sl=sl, di=di, i=i: e.indirect_dma_start(
                    out=xs[:, :], out_offset=bass.IndirectOffsetOnAxis(ap=di[:, i:i + 1], axis=0),
                    in_=hsb[sl][:], in_offset=None), [B_hsb[sl], B_di], [B_xs], dsem=sc_sem[sl], grp=("sc", i))
        k.s.barrier()
    if a.get("stage") == "M3":
        return []

    k.s.scope = pfx + "M4_experts"
    with contextlib.ExitStack() as c4:
        def sb4(name, shape, dt=F32):
            return c4.enter_context(k.nc.sbuf_tensor("s_" + pfx + name, list(shape), dt))

        def ps4(name, shape, dt=F32):
            return c4.enter_context(k.nc.psum_tensor("p_" + pfx + name, list(shape), dt))
        wg = [sb4(f"wg{i}", [128, 8, 512], BF16) for i in range(2)]
        wu = [sb4(f"wu{i}", [128, 8, 512], BF16) for i in range(2)]
        wd = [sb4(f"wd{i}", [128, 4, D], BF16) for i in range(2)]
        B_wg, B_wu, B_wd = [Buf(), Buf()], [Buf(), Buf()], [Buf(), Buf()]
        wsem = [[k.dsem(pfx + f"w{n}{i}", sw=True) for i in range(2)] for n in "gud"]
        xb = [sb4(f"xb{i}", [128, 4, D], BF16) for i in range(2)]
        B_xb = [Buf(), Buf()]
        xb_ld = [k.dsem(pfx + "xb_ld0"), k.dsem(pfx + "xb_ld1")]
        xsT_ = [sb4(f"xsT{i}", [128, 8, 512], BF16) for i in range(2)]
        B_xsT_ = [[Buf() for _ in range(8)] for _ in range(2)]
        pTx = [ps4(f"pTx{i}", [128, 512], BF16) for i in range(2)]
        B_pTx = [Buf(), Buf()]
        psG = [ps4(f"psG{i}", [128, 512]) for i in range(2)]
        psU = [ps4(f"psU{i}", [128, 512]) for i in range(2)]
        B_psG, B_psU = [Buf(), Buf()], [Buf(), Buf()]
        sg = [sb4(f"sg{i}", [128, 512]) for i in range(2)]
        B_sg = [Buf(), Buf()]
        hTe = sb4("hTe", [128, 4, 512], BF16)
        B_hTe = [Buf() for _ in range(4)]
        psY = [ps4(f"psY{i}", [128, 512]) for i in range(2)]
        B_psY = [Buf(), Buf()]
        yb = [sb4(f"yb{i}", [128, 4, D]) for i in range(2)]
        B_yb = [Buf(), Buf()]
        yb_st = [k.dsem(pfx + "yb_st0"), k.dsem(pfx + "yb_st1")]
        B_ys = Buf()
        gw, uw, dw = a["gw"], a["uw"], a["dw"]
        gw2 = gw.rearrange("r (h f) -> (r h) f", h=2)
        uw2 = uw.rearrange("r (h f) -> (r h) f", h=2)
        dw2 = dw.rearrange("r (h f) -> (r h) f", h=2)

        def wload(j, sl):
            for (dst, B_d, tab, ws, nh) in ((wg[sl], B_wg[sl], gw2, wsem[0][sl], 4), (wu[sl], B_wu[sl], uw2, wsem[1][sl], 4),
                                            (wd[sl], B_wd[sl], dw2, wsem[2][sl], 2)):
                for h in range(2):
                    k.s.add("pool", lambda e, dst=dst, tab=tab, j=j, h=h, nh=nh: e.indirect_dma_start(
                        out=dst[:, h * nh:(h + 1) * nh, :].rearrange("p a f -> p (a f)"), out_offset=None, in_=tab,
                        in_offset=bass.IndirectOffsetOnAxis(ap=bei[:, h, j:j + 1], axis=0)), [B_bei], [B_d], dsem=ws,
                        grp=("w", j))

        def xload(j, sl):
            k.dma("sp", xb[sl][:], xs[j * 512:(j + 1) * 512, :].rearrange("(t p) d -> p t d", p=128), xb_ld[sl],
                  reads=[B_xs], writes=[B_xb[sl]])
        def xpose(j, cs):
            sl = j % 2
            for c in cs:
                p = c % 2
                for t in range(4):
                    k.tr(pTx[p][:, t * 128:(t + 1) * 128], xb[sl][:, t, c * 128:(c + 1) * 128], ident_b[:],
                         reads=[B_xb[sl], B_ident_b], writes=[B_pTx[p]])
                if c % 2 == 0:
                    k.op("act", lambda e, c=c, p=p, sl=sl: e.copy(out=xsT_[sl][:, c, :], in_=pTx[p][:]),
                         [B_pTx[p]], [B_xsT_[sl][c]])
                else:
                    k.op("dve", lambda e, c=c, p=p, sl=sl: e.tensor_copy(out=xsT_[sl][:, c, :], in_=pTx[p][:]),
                         [B_pTx[p]], [B_xsT_[sl][c]])
        regs = k.nused_regs()
        for eng_ in ENGS:
            k.s.add(eng_, lambda e, eng_=eng_: e.reg_load(regs[eng_], nusedi[0:1, 0:1]), [B_nusedi], [])
        JS = (2 * T) // 512
        wload(0, 0)
        xload(0, 0)
        if NBLK > 1:
            wload(1, 1)
            xload(1, 1)
        xpose(0, range(8))
        for j in range(NBLK):
            sl = j % 2
            k.s.guard = (regs, (j if not os.environ.get("DYN_NEVER") else -1)) if (j >= max(JS, int(os.environ.get("DYN_FROM", "0"))) and DYN_SKIP) else None
            xsT, B_xsT = xsT_[sl], B_xsT_[sl]
            for fc in range(4):
                q = fc % 2
                for kc in range(8):
                    k.mm(psG[q][:], wg[sl][:, kc, fc * 128:(fc + 1) * 128], xsT[:, kc, :], kc == 0, kc == 7,
                         reads=[B_wg[sl], B_xsT[kc]], writes=[B_psG[q]])
                for kc in range(8):
                    k.mm(psU[q][:], wu[sl][:, kc, fc * 128:(fc + 1) * 128], xsT[:, kc, :], kc == 0, kc == 7,
                         reads=[B_wu[sl], B_xsT[kc]], writes=[B_psU[q]])
                k.op("act", lambda e, q=q: e.activation(out=sg[q][:], in_=psG[q][:], func=AF.Silu),
                     [B_psG[q]], [B_sg[q]])
                k.op("dve", lambda e, q=q, fc=fc: e.tensor_tensor(out=hTe[:, fc, :], in0=sg[q][:], in1=psU[q][:],
                                                                 op=ALU.mult), [B_sg[q], B_psU[q]], [B_hTe[fc]])
                if j + 1 < NBLK:
                    xpose(j + 1, [2 * fc, 2 * fc + 1])
            for t in range(4):
                for dh in range(2):
                    yi = (t * 2 + dh) % 2
                    for fc in range(4):
                        k.mm(psY[yi][:], hTe[:, fc, t * 128:(t + 1) * 128], wd[sl][:, fc, dh * 512:(dh + 1) * 512],
                             fc == 0, fc == 3, reads=[B_hTe[fc], B_wd[sl]], writes=[B_psY[yi]])
                    if yi == 0:
                        k.op("act", lambda e, t=t, dh=dh, sl=sl: e.copy(out=yb[sl][:, t, dh * 512:(dh + 1) * 512],
                                                                       in_=psY[0][:]), [B_psY[0]], [B_yb[sl]])
                    else:
                        k.op("dve", lambda e, t=t, dh=dh, sl=sl: e.tensor_copy(
                            out=yb[sl][:, t, dh * 512:(dh + 1) * 512], in_=psY[1][:]), [B_psY[1]], [B_yb[sl]])
            if j + 2 < NBLK:
                wload(j + 2, sl)
                xload(j + 2, sl)
            k.dma("sp", ys[j * 512:(j + 1) * 512, :].rearrange("(t p) d -> p t d", p=128), yb[sl][:], yb_st[sl],
                  reads=[B_yb[sl]], writes=[B_ys])
        k.s.guard = None
        k.s.barrier()
    if a.get("stage") == "M4":
        return []

    k.s.scope = pfx + "M5_combine"
    fin = a.get("final")
    with contextlib.ExitStack() as c5:
        def sb5(name, shape, dt=F32):
            return c5.enter_context(k.nc.sbuf_tensor("s_" + pfx + name, list(shape), dt))
        NS = 6
        y1 = [sb5(f"y1_{i}", [128, D]) for i in range(NS)]
        y2 = [sb5(f"y2_{i}", [128, D]) for i in range(NS)]
        xc = [sb5(f"xc{i}", [128, D]) for i in range(NS)]
        B_y1, B_y2, B_xc = [Buf() for _ in range(NS)], [Buf() for _ in range(NS)], [Buf() for _ in range(NS)]
        g_sem = [[k.dsem(pfx + f"g{n}{i}", sw=True) for i in range(NS)] for n in "12"]
        xc_ld = [k.dsem(pfx + f"xc_ld{i}") for i in range(NS)]
        xc_st = [k.dsem(pfx + f"xc_st{i}") for i in range(NS)]
        if fin is not None:
            fg = sb5("fg", [128, D])
            fjunk = sb5("fjunk", [128, D], BF16)
            fss = [sb5(f"fss{i}", [128, 1]) for i in range(NS)]
            B_fg, B_fjunk = Buf(), Buf()
            B_fss = [Buf() for _ in range(NS)]
            k.dma("sp", fg[:], fin["fing"][0:1, :].partition_broadcast(128), vl, writes=[B_fg])
        def m5_load(i):
            sl = i % NS
            G = i // 4
            k.s.add("pool", lambda e, sl=sl, i=i: e.indirect_dma_start(
                out=y1[sl][:], out_offset=None, in_=ys[:, :],
                in_offset=bass.IndirectOffsetOnAxis(ap=d1i[:, i:i + 1], axis=0)), [B_ys, B_d1i], [B_y1[sl]],
                dsem=g_sem[0][sl])
            k.s.add("pool", lambda e, sl=sl, i=i: e.indirect_dma_start(
                out=y2[sl][:], out_offset=None, in_=ys[:, :],
                in_offset=bass.IndirectOffsetOnAxis(ap=d2i[:, i:i + 1], axis=0)), [B_ys, B_d2i], [B_y2[sl]],
                dsem=g_sem[1][sl])
            k.dma("sp", xc[sl][:], xr[i * 128:(i + 1) * 128, :], xc_ld[sl], reads=[B_xr[G]], writes=[B_xc[sl]])
        for i in range(min(NS - 1, NT)):
            m5_load(i)
        for i in range(NT):
            sl = i % NS
            b = (i * 128) // S
            G = i // 4
            if i + NS - 1 < NT:
                m5_load(i + NS - 1)
            k.op("act", lambda e, sl=sl, i=i: e.activation(out=y1[sl][:], in_=y1[sl][:], func=AF.Copy,
                                                          scale=Wall[:, i, 0:1]), [B_y1[sl], B_W[i]], [B_y1[sl]])
            k.op("dve", lambda e, sl=sl, i=i: e.scalar_tensor_tensor(out=y1[sl][:], in0=y2[sl][:],
                                                                    scalar=Wall[:, i, 1:2], in1=y1[sl][:],
                                                                    op0=ALU.mult, op1=ALU.add),
                 [B_y1[sl], B_y2[sl], B_W[i]], [B_y1[sl]])
            k.op("dve", lambda e, sl=sl, b=b: e.tensor_tensor(out=y1[sl][:], in0=y1[sl][:], in1=g2bc[b][:],
                                                             op=ALU.mult), [B_y1[sl], B_g2bc[b]], [B_y1[sl]])
            k.op("dve", lambda e, sl=sl: e.tensor_tensor(out=xc[sl][:], in0=xc[sl][:], in1=y1[sl][:], op=ALU.add),
                 [B_xc[sl], B_y1[sl]], [B_xc[sl]])
            if fin is None:
                k.dma("sp", xr[i * 128:(i + 1) * 128, :], xc[sl][:], xc_st[sl], reads=[B_xc[sl]], writes=[B_xr[G]])
            else:
                k.op("act", lambda e, sl=sl: e.activation(out=fjunk[:], in_=xc[sl][:], func=AF.Square,
                                                          accum_out=fss[sl][:, 0:1]), [B_xc[sl]], [B_fjunk, B_fss[sl]])
                k.op("dve", lambda e, sl=sl: e.tensor_scalar(out=fss[sl][:], in0=fss[sl][:], scalar1=1.0 / D,
                                                             scalar2=1e-6, op0=ALU.mult, op1=ALU.add),
                     [B_fss[sl]], [B_fss[sl]])
                k.op("act", lambda e, sl=sl: e.activation(out=fss[sl][:], in_=fss[sl][:], func=AF.Sqrt),
                     [B_fss[sl]], [B_fss[sl]])
                k.op("dve", lambda e, sl=sl: e.reciprocal(out=fss[sl][:], in_=fss[sl][:]), [B_fss[sl]], [B_fss[sl]])
                k.op("dve", lambda e, sl=sl: e.scalar_tensor_tensor(out=xc[sl][:], in0=xc[sl][:], scalar=fss[sl][:, 0:1],
                                                                    in1=fg[:], op0=ALU.mult, op1=ALU.mult),
                     [B_xc[sl], B_fss[sl], B_fg], [B_xc[sl]])
                k.dma("sp", fin["out"][i * 128:(i + 1) * 128, :], xc[sl][:], xc_st[sl], reads=[B_xc[sl]],
                      writes=[fin["B_out"][G]])
        k.s.barrier()


def finish(k, out_bufs):
    k.s.add("sp", None, reads=out_bufs)
    esem = {e: k.sem("e_" + e) for e in ENGS}
    stuck = k.s.simulate()
    assert not stuck, f"schedule deadlock: {stuck}"
    k.s.emit(k.nc, esem)
    k.pctx.close()
    k.ctx.close()
    return k.nc


_RL = {}


def _relayout(w, key, nch):
    if key not in _RL or _RL[key][0] is not w:
        Lr, E, R, N = w.shape
        _RL[key] = (w, np.ascontiguousarray(w.reshape(Lr, E, nch, 128, N).transpose(0, 1, 3, 2, 4)).reshape(
            Lr * E * 128, nch * N))
    return _RL[key][1]


def host_inputs(inp, core, NB, S):
    b0 = core * NB
    f = np.float32
    m = {}
    m["x"] = np.ascontiguousarray(inp["x"][b0:b0 + NB, :S].reshape(NB * S, D))
    c = inp["c"][b0:b0 + NB]
    m["cT"] = np.ascontiguousarray(c.reshape(NB, 8, 128).transpose(2, 1, 0).reshape(128, 8 * NB))
    m["ada_w"] = inp["ada_w"]
    m["ada_b"] = inp["ada_b"]
    m["gmixF"] = np.ascontiguousarray(inp["norm_mix_g"].reshape(2, 8, 128).transpose(0, 2, 1))
    m["gffn"] = inp["norm_ffn_g"]
    m["conv_in_w"] = inp["conv_in_w"][0]
    m["convwF"] = np.ascontiguousarray(inp["conv_w"][0].reshape(3, 8, 128).transpose(2, 1, 0).reshape(128, 24))
    m["conv_out_w"] = inp["conv_out_w"][0]
    m["ident"] = np.eye(128, dtype=f)
    m["tri"] = np.triu(np.ones((128, 128), dtype=f), 1)
    m["Wr"] = np.ascontiguousarray(np.concatenate(
        [inp["router_grp_w"], inp["router_exp_w"].transpose(0, 2, 1, 3).reshape(2, D, 32)], axis=2))
    m["br"] = np.ascontiguousarray(np.concatenate([inp["router_grp_b"], inp["router_exp_b"].reshape(2, 32)], axis=1))
    m["exp_gate_w"] = _relayout(inp["exp_gate_w"], "g", 8)
    m["exp_up_w"] = _relayout(inp["exp_up_w"], "u", 8)
    m["exp_down_w"] = _relayout(inp["exp_down_w"], "d", 4)
    m["pcol"] = np.arange(128, dtype=f).reshape(128, 1)
    m["attn_in_w"] = inp["attn_in_w"][0]
    m["attn_out_w"] = inp["attn_out_w"][0]
    pos = np.arange(S, dtype=f)
    inv = np.power(f(500000.0), -np.arange(0, 16, 2, dtype=f) / f(16)).astype(f)
    ang = pos[None, :] * inv[:, None]
    C = np.ones((128, S), f)
    Sg = np.zeros((128, S), f)
    Rm = np.zeros((128, 128), f)
    for h in range(2):
        for e in range(16):
            C[h * 64 + e] = np.cos(ang[e % 8])
            Sg[h * 64 + e] = (-np.sin(ang[e % 8])) if e < 8 else np.sin(ang[e % 8])
            Rm[h * 64 + (e + 8 if e < 8 else e - 8), h * 64 + e] = 1.0
    m["ropeC"], m["ropeS"] = C, Sg
    kk = np.arange(128)[:, None]
    qq = np.arange(128)[None, :]
    NEG = f(-30000.0)
    Mprev = np.where(kk >= qq, f(0), NEG).astype(f)
    Mcur = np.where(kk <= qq, f(0), NEG).astype(f)
    o0 = np.zeros((128, 128), f)
    o0[:, :64] = 1
    o1 = np.zeros((128, 128), f)
    o1[:, 64:] = 1
    m["aconst"] = np.ascontiguousarray(np.concatenate([Rm, Mprev, Mcur, o0, o1], axis=1))
    m["fing"] = inp["final_norm_g"].reshape(1, D)
    return m


def kernel(**inputs):
    NB, S = 2, 4096
    nc = build(NB, S)
    in_maps = [host_inputs(inputs, c, NB, S) for c in range(NCORES)]
    res = run_bass_kernel_spmd(nc, in_maps, core_ids=list(range(NCORES)))
    out = np.stack([r["out"].reshape(NB, S, D) for r in res.results]).reshape(NCORES * NB, S, D)
    return out.astype(np.float32)
```

```python
import contextlib
import os
import numpy as np
import concourse.bass as bass
import concourse.mybir as mybir
from concourse.bass_utils import run_bass_kernel_spmd

F32 = mybir.dt.float32
BF16 = mybir.dt.bfloat16
I32 = mybir.dt.int32
AF = mybir.ActivationFunctionType
ALU = mybir.AluOpType
AX = mybir.AxisListType

D = 1024
NCORES = 8
ENGS = ("pe", "act", "dve", "pool", "sp")
SAME_ENG_SYNC = True
OOB_BIG = 1000000
DYN_SKIP = False


class Buf:
    __slots__ = ("name", "lw", "rd")

    def __init__(self, name=""):
        self.name = name
        self.lw = None
        self.rd = {}


class DSem:
    def __init__(self, h):
        self.h = h
        self.groups = []


class Ins:
    __slots__ = ("eng", "fn", "dsem", "signal", "sig", "target", "deps", "gk", "scope", "guard")


class Sched:
    def __init__(self):
        self.L = {e: [] for e in ENGS}
        self.n = 0
        self.dsems = []
        self.scope = None
        self.trace_scopes = False
        self.guard = None


    def dsem(self, h):
        d = DSem(h)
        self.dsems.append(d)
        return d

    def add(self, eng, fn, reads=(), writes=(), dsem=None, grp=None):
        ins = Ins()
        ins.eng, ins.fn, ins.dsem = eng, fn, dsem
        ins.signal, ins.sig, ins.target = False, 0, 0
        ins.gk = (id(dsem), grp) if (dsem is not None and grp is not None) else None
        ins.scope = self.scope
        ins.guard = self.guard
        deps = {}

        def need(d, kind):
            if d.dsem is not None:
                return not (ins.gk is not None and d.gk == ins.gk)
            if d.eng == eng:
                if dsem is not None:
                    return True
                if eng == "pe":
                    return False
                return kind == "raw" and SAME_ENG_SYNC
            return True

        for b in reads:
            if b.lw is not None and need(b.lw, "raw"):
                deps[id(b.lw)] = b.lw
        for b in writes:
            if b.lw is not None and need(b.lw, "waw"):
                deps[id(b.lw)] = b.lw
            for r in b.rd.values():
                if need(r, "war"):
                    deps[id(r)] = r
        if dsem is not None:
            assert getattr(dsem, "sw", False) == (eng == "pool"), f"DMA semaphore class mismatch on {eng}"
            g = dsem.groups
            if g and grp is not None and g[-1][0] == grp:
                g[-1][1].append(ins)
            else:
                if g:
                    prev = g[-1][1][-1]
                    deps[id(prev)] = prev
                g.append((grp if grp is not None else object(), [ins]))
        ins.deps = list(deps.values())
        for d in ins.deps:
            d.signal = True
        for b in reads:
            key = eng if dsem is None else ("dma", self.n)
            b.rd[key] = ins
        for b in writes:
            b.lw = ins
            b.rd = {}
        self.L[eng].append(ins)
        self.n += 1
        return ins

    def barrier(self):
        lasts = []
        for e in ENGS:
            for ins in reversed(self.L[e]):
                if ins.fn is not None and ins.dsem is None:
                    lasts.append(ins)
                    break
        for ds in self.dsems:
            if ds.groups:
                lasts.append(ds.groups[-1][1][-1])
        for e in ENGS:
            ins = Ins()
            ins.eng, ins.fn, ins.dsem = e, None, None
            ins.signal, ins.sig, ins.target = False, 0, 0
            ins.gk = None
            ins.scope = self.scope
            ins.guard = None
            ins.deps = [d for d in lasts if not (d.dsem is None and d.eng == e)]
            for d in ins.deps:
                d.signal = True
            self.L[e].append(ins)

    def simulate(self):
        for e in ENGS:
            c = 0
            for ins in self.L[e]:
                if ins.dsem is None and ins.signal:
                    c += 1
                    ins.sig = c
        for ds in self.dsems:
            tot = 0
            for _, lst in ds.groups:
                tot += 16 * len(lst)
                for i in lst:
                    i.target = tot
        pc = {e: 0 for e in ENGS}
        ev = {e: 0 for e in ENGS}
        dv = {id(ds): 0 for ds in self.dsems}
        prog = True
        while prog:
            prog = False
            for e in ENGS:
                while pc[e] < len(self.L[e]):
                    ins = self.L[e][pc[e]]
                    ok = True
                    for d in ins.deps:
                        if d.dsem is not None:
                            if dv[id(d.dsem)] < d.target:
                                ok = False
                        elif ev[d.eng] < d.sig:
                            ok = False
                    if not ok:
                        break
                    if ins.fn is not None:
                        if ins.dsem is not None:
                            dv[id(ins.dsem)] += 16
                        elif ins.signal:
                            ev[e] += 1
                    pc[e] += 1
                    prog = True
        stuck = {e: (pc[e], len(self.L[e])) for e in ENGS if pc[e] < len(self.L[e])}
        return stuck

    def emit(self, nc, esem):
        for e in ENGS:
            c = 0
            for ins in self.L[e]:
                if ins.dsem is None and ins.signal:
                    c += 1
                    ins.sig = c
        for ds in self.dsems:
            tot = 0
            for _, lst in ds.groups:
                tot += 16 * len(lst)
                for i in lst:
                    i.target = tot

        def emit_one(e, eo, ins, seen):
            for d in ins.deps:
                if d.dsem is not None:
                    key, val, h = ("d", id(d.dsem)), d.target, d.dsem.h
                else:
                    key, val, h = ("e", d.eng), d.sig, esem[d.eng]
                if seen.get(key, 0) < val:
                    eo.wait_ge(h, val)
                    seen[key] = val
            if ins.fn is None:
                return
            r = ins.fn(eo)
            if ins.dsem is not None:
                r.then_inc(ins.dsem.h, 16)
            elif ins.signal:
                r.then_inc(esem[e], 1)

        def run(e, eo):
            seen = {}
            cur = [None, None]
            L_ = self.L[e]
            i = 0
            while i < len(L_):
                ins = L_[i]
                if self.trace_scopes and ins.scope != cur[0]:
                    if cur[1] is not None:
                        cur[1].__exit__(None, None, None)
                    cur[0] = ins.scope
                    cur[1] = nc.named_scope(f"{ins.scope}") if ins.scope else None
                    if cur[1] is not None:
                        cur[1].__enter__()
                if ins.guard is None:
                    emit_one(e, eo, ins, seen)
                    i += 1
                    continue
                g = ins.guard
                j = i
                while j < len(L_) and L_[j].guard is g and L_[j].scope == ins.scope:
                    j += 1
                region = L_[i:j]
                nsig = sum(1 for x in region if x.dsem is None and x.signal and x.fn is not None)
                dcount = {}
                for x in region:
                    if x.dsem is not None:
                        dcount[id(x.dsem)] = (x.dsem, dcount.get(id(x.dsem), (x.dsem, 0))[1] + 16)
                regs, thresh = g
                snap = dict(seen)
                with eo.If_lt(regs[e], thresh + 1):
                    for _ in range(nsig):
                        eo.nop(nofuse=True).then_inc(esem[e], 1)
                    for ds, n in dcount.values():
                        for _ in range(n // 16):
                            eo.nop(nofuse=True).then_inc(ds.h, 16)
                with eo.Else():
                    for x in region:
                        emit_one(e, eo, x, seen)
                seen = snap
                i = j
            if cur[1] is not None:
                cur[1].__exit__(None, None, None)

        with nc.Block() as block:
            @block.sync
            def _(eo):
                run("sp", eo)

            @block.tensor
            def _(eo):
                run("pe", eo)

            @block.scalar
            def _(eo):
                run("act", eo)

            @block.vector
            def _(eo):
                run("dve", eo)

            @block.gpsimd
            def _(eo):
                run("pool", eo)


class K:
    def __init__(self, NB, S, dbg=None):
        self.NB, self.S, self.T = NB, S, NB * S
        self.dbg = dbg
        self.nc = bass.Bass("TRN2", target_bir_lowering=False)
        self.ctx = contextlib.ExitStack()
        self.s = Sched()
        self.pctx = contextlib.ExitStack()
        self.nsem = 0
        self.dpool = []
        self.dcur = 0
        self.dpool_sw = []
        self.dcur_sw = 0
        self._regs = None
        self._breg = None

    def din(self, name, shape, dt=F32):
        return self.nc.dram_tensor(name, list(shape), dt, kind="ExternalInput").ap()

    def dout(self, name, shape, dt=F32):
        return self.nc.dram_tensor(name, list(shape), dt, kind="ExternalOutput").ap()

    def dscr(self, name, shape, dt=F32):
        return self.nc.dram_tensor(name, list(shape), dt, kind="Internal").ap()

    def phase(self):
        self.s.barrier()
        self.pctx.close()
        self.pctx = contextlib.ExitStack()
        self.dcur = 0
        self.dcur_sw = 0

    def sbp(self, name, shape, dt=F32):
        return self.pctx.enter_context(self.nc.sbuf_tensor("s_" + name, list(shape), dt))

    def psp(self, name, shape, dt=F32):
        return self.pctx.enter_context(self.nc.psum_tensor("p_" + name, list(shape), dt))

    def sb(self, name, shape, dt=F32):
        return self.ctx.enter_context(self.nc.sbuf_tensor("s_" + name, list(shape), dt))

    def ps(self, name, shape, dt=F32):
        return self.ctx.enter_context(self.nc.psum_tensor("p_" + name, list(shape), dt))

    def sem(self, name):
        self.nsem += 1
        return self.ctx.enter_context(self.nc.semaphore(name))

    def bound_reg(self):
        if self._breg is None:
            self._breg = self.ctx.enter_context(self.nc.gpsimd.register("oob_bound"))
        return self._breg

    def nused_regs(self):
        if self._regs is None:
            nc = self.nc
            eng = {"pe": nc.tensor, "act": nc.scalar, "dve": nc.vector, "pool": nc.gpsimd, "sp": nc.sync}
            self._regs = {e: self.ctx.enter_context(eng[e].register("nused_" + e)) for e in ENGS}
        return self._regs

    def dsem(self, name, sw=False):
        pool, cur = (self.dpool_sw, self.dcur_sw) if sw else (self.dpool, self.dcur)
        if cur < len(pool):
            d = pool[cur]
        else:
            d = self.s.dsem(self.sem(f"dma{'s' if sw else 'h'}{len(pool)}"))
            d.sw = sw
            pool.append(d)
        if sw:
            self.dcur_sw += 1
        else:
            self.dcur += 1
        return d

    def dma(self, eng, out, in_, dsem, reads=(), writes=(), grp=None, **kw):
        return self.s.add(eng, lambda e: e.dma_start(out=out, in_=in_, **kw), reads, writes, dsem=dsem, grp=grp)

    def mm(self, out, lhsT, rhs, start, stop, reads=(), writes=()):
        return self.s.add("pe", lambda e: e.matmul(out, lhsT=lhsT, rhs=rhs, start=start, stop=stop), reads, writes)

    def tr(self, out, in_, ident, reads=(), writes=()):
        return self.s.add("pe", lambda e: e.transpose(out=out, in_=in_, identity=ident), reads, writes)

    def op(self, eng, fn, reads=(), writes=()):
        return self.s.add(eng, fn, reads, writes)


def build(NB, S, stage="all", trace_scopes=False):
    k = K(NB, S)
    k.s.trace_scopes = trace_scopes
    nc, s = k.nc, k.s
    T = NB * S
    NT = T // 128
    NG = T // 512
    GPB = S // 512

    x_in = k.din("x", [T, D])
    cT_in = k.din("cT", [128, 8 * NB])
    ada_w = k.din("ada_w", [2, D, 6 * D])
    ada_b = k.din("ada_b", [2, 6 * D])
    gmixF = k.din("gmixF", [2, 128, 8])
    gffn = k.din("gffn", [2, D])
    conv_in_w = k.din("conv_in_w", [D, 3 * D])
    convwF = k.din("convwF", [128, 24])
    conv_out_w = k.din("conv_out_w", [D, D])
    ident_in = k.din("ident", [128, 128])
    tri_in = k.din("tri", [128, 128])
    Wr_in = k.din("Wr", [2, D, 36])
    br_in = k.din("br", [2, 36])
    exp_gate_w = k.din("exp_gate_w", [2 * 32 * 128, 8 * 512])
    exp_up_w = k.din("exp_up_w", [2 * 32 * 128, 8 * 512])
    exp_down_w = k.din("exp_down_w", [2 * 32 * 128, 4 * D])
    pcol_in = k.din("pcol", [128, 1])
    attn_in_w = k.din("attn_in_w", [D, 4608])
    attn_out_w = k.din("attn_out_w", [512, D])
    ropeC_in = k.din("ropeC", [128, S])
    ropeS_in = k.din("ropeS", [128, S])
    aconst_in = k.din("aconst", [128, 640])
    fing_in = k.din("fing", [1, D])
    out_ap = k.dout("out", [T, D])
    NBLK = (2 * T) // 512 + 32
    hs = k.dscr("hs", [T, D], BF16)
    xs = k.dscr("xs", [NBLK * 512, D], BF16)
    ys = k.dscr("ys", [NBLK * 512, D], F32)
    xr = k.dout("xr", [T, D])
    modrow = k.dout("modrow", [2, NB, 6 * D])

    ident_f = k.sb("ident_f", [128, 128], F32)
    ident_b = k.sb("ident_b", [128, 128], BF16)
    cT = k.sb("cT", [128, 8 * NB], F32)
    B_ident_f, B_ident_b, B_cT = Buf(), Buf(), Buf()
    B_modrow = [Buf() for _ in range(2)]

    ld0 = k.dsem("ld0")
    k.dma("sp", ident_f[:], ident_in[:, :], ld0, writes=[B_ident_f], grp="init")
    k.dma("sp", cT[:], cT_in[:, :], ld0, writes=[B_cT], grp="init")
    k.op("dve", lambda e: e.tensor_copy(out=ident_b[:], in_=ident_f[:]), [B_ident_f], [B_ident_b])
    k.op("act", lambda e: e.activation(out=cT[:], in_=cT[:], func=AF.Silu), [B_cT], [B_cT])

    s.scope = "A_mod"
    NSA = 3
    aw = [k.sbp(f"aw{i}", [128, 8, 512], F32) for i in range(NSA)]
    adab = [k.sbp(f"adab{i}", [NB, 512], F32) for i in range(NSA)]
    modc = [k.sbp(f"modc{i}", [NB, 512], F32) for i in range(NSA)]
    B_aw, B_adab, B_modc = [Buf() for _ in range(NSA)], [Buf() for _ in range(NSA)], [Buf() for _ in range(NSA)]
    aw_sem = [k.dsem(f"aw{i}") for i in range(NSA)]
    adab_sem = [k.dsem(f"adab{i}") for i in range(NSA)]
    mod_st = [k.dsem(f"mod_st{i}", sw=True) for i in range(NSA)]
    ps_mod = [k.psp(f"ps_mod{i}", [128, 512], F32) for i in range(2)]
    B_psmod = [Buf(), Buf()]
    chunks = [(l, cc) for l in range(2) for cc in range(12)]

    def a_load(ci):
        l, cc = chunks[ci]
        sl = ci % NSA
        src_b = ada_b[l:l + 1, cc * 512:(cc + 1) * 512]
        k.dma("sp", adab[sl][:], src_b.partition_broadcast(NB) if NB > 1 else src_b, adab_sem[sl],
              writes=[B_adab[sl]])
        k.dma("sp", aw[sl][:], ada_w[l, :, cc * 512:(cc + 1) * 512].rearrange("(kc p) f -> p kc f", p=128),
              aw_sem[sl], writes=[B_aw[sl]])
    for ci in range(NSA - 1):
        a_load(ci)
    for ci, (l, cc) in enumerate(chunks):
        sl = ci % NSA
        pq = ci % 2
        if ci + NSA - 1 < len(chunks):
            a_load(ci + NSA - 1)
        for kc in range(8):
            k.mm(ps_mod[pq][0:NB, :], cT[:, kc * NB:(kc + 1) * NB], aw[sl][:, kc, :], kc == 0, kc == 7,
                 reads=[B_cT, B_aw[sl]], writes=[B_psmod[pq]])
        k.op("dve", lambda e, sl=sl, pq=pq: e.tensor_tensor(out=modc[sl][:], in0=ps_mod[pq][0:NB, :], in1=adab[sl][:],
                                                           op=ALU.add),
             [B_psmod[pq], B_adab[sl]], [B_modc[sl]])
        k.dma("pool", modrow[l, :, cc * 512:(cc + 1) * 512], modc[sl][:], mod_st[sl], reads=[B_modc[sl]],
              writes=[B_modrow[l]])
    k.phase()
    if stage == "A":
        return finish(k, [B_modrow[0], B_modrow[1]])

    s.scope = "L0_conv"
    L = 0
    w_in = k.sbp("w_in", [128, 8, 3 * D], BF16)
    w_stage = [k.sbp(f"w_stage{i}", [128, D], F32) for i in range(2)]
    w_out_b1 = k.sbp("w_out_b", [128, 8, D], BF16)
    w_out_b = [w_out_b1 for b in range(NB)]
    g1bc = [k.sbp(f"g1bc{b}", [128, D], F32) for b in range(NB)]
    A1 = [k.sbp(f"A1_{b}", [128, 8], F32) for b in range(NB)]
    sh1 = [k.sbp(f"sh1_{b}", [128, 8], F32) for b in range(NB)]
    sc1t = k.sbp("sc1t", [128, 8], F32)
    gmix = k.sbp("gmix", [128, 8], F32)
    convw = k.sbp("convw", [128, 24], F32)
    B_w_in, B_sc1t, B_gmix, B_convw = Buf(), Buf(), Buf(), Buf()
    B_w_stage = [Buf(), Buf()]
    B_g1bc = [Buf() for _ in range(NB)]
    B_w_out_b1 = Buf()
    B_w_out_b = [B_w_out_b1 for _ in range(NB)]
    B_A1 = [Buf() for _ in range(NB)]
    B_sh1 = [Buf() for _ in range(NB)]
    wl = k.dsem("wl", sw=True)
    wst = [k.dsem("wst0"), k.dsem("wst1")]
    vl = k.dsem("vl")
    for kc in range(8):
        k.dma("pool", w_in[:, kc, :], conv_in_w[kc * 128:(kc + 1) * 128, :], wl, writes=[B_w_in], grp="w_in",
              max_dma_last_dim=4096)
    k.dma("sp", gmix[:], gmixF[L], vl, writes=[B_gmix], grp="v0")
    k.dma("sp", convw[:], convwF[:, :], vl, writes=[B_convw], grp="v0")
    for b in range(NB):
        k.dma("sp", sh1[b][:], modrow[L, b, 0:D].rearrange("(c p) -> p c", p=128), vl,
              reads=[B_modrow[L]], writes=[B_sh1[b]], allow_slow_non_contiguous=True)
        k.dma("sp", sc1t[:], modrow[L, b, D:2 * D].rearrange("(c p) -> p c", p=128), vl,
              reads=[B_modrow[L]], writes=[B_sc1t], allow_slow_non_contiguous=True)
        k.op("dve", lambda e, b=b: e.scalar_tensor_tensor(out=A1[b][:], in0=sc1t[:], scalar=1.0, in1=gmix[:],
                                                         op0=ALU.add, op1=ALU.mult),
             [B_sc1t, B_gmix], [B_A1[b]])
        k.dma("sp", g1bc[b][:], modrow[L, b:b + 1, 2 * D:3 * D].partition_broadcast(128), vl,
              reads=[B_modrow[L]], writes=[B_g1bc[b]])
    def build_w_out(b):
        for kc in range(8):
            sl = kc % 2
            k.dma("sp", w_stage[sl][:], conv_out_w[kc * 128:(kc + 1) * 128, :], wst[sl], writes=[B_w_stage[sl]])
            k.op("dve", lambda e, b=b, kc=kc, sl=sl: e.tensor_tensor(out=w_out_b[b][:, kc, :], in0=w_stage[sl][:],
                                                                    in1=g1bc[b][:], op=ALU.mult),
                 [B_w_stage[sl], B_g1bc[b]], [B_w_out_b[b]])
    build_w_out(0)

    if stage == "L0setup":
        return finish(k, [B_modrow[0], B_modrow[1]] + B_w_out_b + B_A1 + B_sh1 + [B_w_in, B_convw])
    NXS = 4
    xt = [k.sbp(f"xt{i}", [128, 4, D], F32) for i in range(NXS)]
    B_xt = [Buf() for _ in range(NXS)]
    xt_ld = [k.dsem(f"xt_ld{i}") for i in range(NXS)]
    xt_st = [k.dsem(f"xt_st{i}") for i in range(NXS)]
    junk = k.sbp("junk", [128, D], BF16)
    B_junk = Buf()
    ss = k.sbp("ss", [128, 4], F32)
    rstd = k.sbp("rstd", [128, 4], F32)
    B_ss, B_rstd = Buf(), Buf()
    xn = [k.sbp(f"xn{i}", [128, 4, D], BF16) for i in range(2)]
    B_xn = [[Buf() for _ in range(4)] for _ in range(2)]
    pT = k.psp("pT", [128, 512], BF16)
    B_pT = Buf()
    hT = [k.sbp(f"hT{i}", [128, 8, 512], BF16) for i in range(2)]
    B_hT = [[Buf() for _ in range(8)] for _ in range(2)]
    psBCU = [[k.psp(f"ps{n}{i}", [128, 512]) for n in "BCU"] for i in range(2)]
    B_psBCU = [[Buf() for _ in range(3)] for _ in range(2)]
    Csb = [k.sbp(f"Csb{i}", [128, 512], F32) for i in range(2)]
    B_Csb = [Buf(), Buf()]
    zb = [k.sbp(f"zb{i}", [128, 514], F32) for i in range(2)]
    B_zb = [Buf(), Buf()]
    zh = k.sbp("zh", [128, 8, 2], F32)
    B_zh = [Buf() for _ in range(8)]
    zc = [k.sbp(f"zc{i}", [128, 512], F32) for i in range(2)]
    B_zc = [Buf(), Buf()]
    gT = [k.sbp(f"gT{i}", [128, 8, 512], BF16) for i in range(2)]
    B_gT = [[Buf() for _ in range(8)] for _ in range(2)]
    psY = k.psp("psY", [128, 512])
    B_psY = Buf()
    B_xr = [Buf() for _ in range(NG)]

    def load_x(G):
        sl = G % NXS
        k.dma("sp", xt[sl][:], x_in[G * 512:(G + 1) * 512, :].rearrange("(j p) d -> p j d", p=128),
              xt_ld[sl], writes=[B_xt[sl]])

    def norm_pre(G):
        sl = G % 2
        xt_t, B_x = xt[G % NXS], B_xt[G % NXS]
        for j in range(4):
            k.op("act", lambda e, j=j: e.activation(out=junk[:], in_=xt_t[:, j, :], func=AF.Square,
                                                    accum_out=ss[:, j:j + 1]),
                 [B_x], [B_junk, B_ss])
        k.op("dve", lambda e: e.tensor_scalar(out=rstd[:], in0=ss[:], scalar1=1.0 / D, scalar2=1e-6,
                                              op0=ALU.mult, op1=ALU.add), [B_ss], [B_rstd])
        k.op("act", lambda e: e.activation(out=rstd[:], in_=rstd[:], func=AF.Sqrt), [B_rstd], [B_rstd])
        k.op("dve", lambda e: e.reciprocal(out=rstd[:], in_=rstd[:]), [B_rstd], [B_rstd])
        for j in range(4):
            k.op("act", lambda e, j=j: e.activation(out=xn[sl][:, j, :], in_=xt_t[:, j, :], func=AF.Copy,
                                                    scale=rstd[:, j:j + 1]),
                 [B_x, B_rstd], [B_xn[sl][j]])

    def norm_tr(G, c):
        b = G // GPB
        sl = G % 2
        A, B_A, sh, B_sh = A1[b], B_A1[b], sh1[b], B_sh1[b]
        for j in range(4):
            k.tr(pT[:, j * 128:(j + 1) * 128], xn[sl][:, j, c * 128:(c + 1) * 128], ident_b[:],
                 reads=[B_xn[sl][j], B_ident_b], writes=[B_pT])
        k.op("act", lambda e, c=c: e.activation(out=hT[sl][:, c, :], in_=pT[:], func=AF.Identity,
                                                scale=A[:, c:c + 1], bias=sh[:, c:c + 1]),
             [B_pT, B_A, B_sh], [B_hT[sl][c]])

    def inproj(G, fcs):
        sl = G % 2
        first = (G % GPB == 0)
        for fc in fcs:
            q = fc % 2
            (psB, psC, psU), (B_psB, B_psC, B_psU) = psBCU[q], B_psBCU[q]
            for (pst, B_p, off) in ((psB, B_psB, 0), (psC, B_psC, D), (psU, B_psU, 2 * D)):
                for kc in range(8):
                    k.mm(pst[:], w_in[:, kc, off + fc * 128: off + (fc + 1) * 128], hT[sl][:, kc, :], kc == 0, kc == 7,
                         reads=[B_w_in, B_hT[sl][kc]], writes=[B_p])
            zt, Bz, zcb, Bzc, Cs, BCs = zb[q], B_zb[q], zc[q], B_zc[q], Csb[q], B_Csb[q]
            if first:
                k.op("pool", lambda e, zt=zt: e.memset(zt[:, 0:2], 0.0), [], [Bz])
            else:
                k.op("pool", lambda e, zt=zt, fc=fc: e.tensor_copy(out=zt[:, 0:2], in_=zh[:, fc, :]),
                     [B_zh[fc]], [Bz])
            k.op("act", lambda e, Cs=Cs, psC=psC: e.copy(out=Cs[:], in_=psC[:]), [B_psC], [BCs])
            k.op("dve", lambda e, zt=zt, Cs=Cs, psU=psU: e.tensor_tensor(out=zt[:, 2:514], in0=Cs[:], in1=psU[:],
                                                                        op=ALU.mult),
                 [BCs, B_psU, Bz], [Bz])
            k.op("pool", lambda e, zt=zt, fc=fc: e.tensor_copy(out=zh[:, fc, :], in_=zt[:, 512:514]),
                 [Bz], [B_zh[fc]])
            k.op("pool", lambda e, fc=fc, zt=zt, zcb=zcb: e.tensor_scalar(
                out=zcb[:], in0=zt[:, 2:514], scalar1=convw[:, fc * 3 + 2: fc * 3 + 3], scalar2=1.0,
                op0=ALU.mult, op1=ALU.mult), [Bz, B_convw], [Bzc])
            k.op("dve", lambda e, fc=fc, zt=zt, zcb=zcb: e.scalar_tensor_tensor(
                out=zcb[:], in0=zt[:, 1:513], scalar=convw[:, fc * 3 + 1: fc * 3 + 2], in1=zcb[:],
                op0=ALU.mult, op1=ALU.add), [Bz, B_convw, Bzc], [Bzc])
            k.op("dve", lambda e, fc=fc, zt=zt, zcb=zcb: e.scalar_tensor_tensor(
                out=zcb[:], in0=zt[:, 0:512], scalar=convw[:, fc * 3: fc * 3 + 1], in1=zcb[:],
                op0=ALU.mult, op1=ALU.add), [Bz, B_convw, Bzc], [Bzc])
            k.op("dve", lambda e, fc=fc, zcb=zcb, psB=psB, sl=sl: e.tensor_tensor(out=gT[sl][:, fc, :], in0=zcb[:],
                                                                                 in1=psB[:], op=ALU.mult),
                 [Bzc, B_psB], [B_gT[sl][fc]])

    def outproj_unit(G, u):
        b = G // GPB
        sl = G % 2
        xs_ = G % NXS
        j, dh = u // 2, u % 2
        for kc in range(8):
            k.mm(psY[:], gT[sl][:, kc, j * 128:(j + 1) * 128], w_out_b[b][:, kc, dh * 512:(dh + 1) * 512],
                 kc == 0, kc == 7, reads=[B_gT[sl][kc], B_w_out_b[b]], writes=[B_psY])
        k.op("dve", lambda e: e.tensor_tensor(
            out=xt[xs_][:, j, dh * 512:(dh + 1) * 512], in0=psY[:], in1=xt[xs_][:, j, dh * 512:(dh + 1) * 512],
            op=ALU.add), [B_psY, B_xt[xs_]], [B_xt[xs_]])
        if u == 7:
            k.dma("sp", xr[G * 512:(G + 1) * 512, :].rearrange("(j p) d -> p j d", p=128), xt[xs_][:], xt_st[xs_],
                  reads=[B_xt[xs_]], writes=[B_xr[G]])

    load_x(0)
    if NG > 1:
        load_x(1)
    norm_pre(0)
    for c in range(8):
        norm_tr(0, c)
    for G in range(NG):
        if G + 1 < NG:
            norm_pre(G + 1)
        if G + 2 < NG:
            load_x(G + 2)
        for fc in range(8):
            inproj(G, [fc])
            if G + 1 < NG and fc > 0:
                norm_tr(G + 1, fc - 1)
            if G > 0:
                outproj_unit(G - 1, fc)
        if G + 1 < NG:
            norm_tr(G + 1, 7)
        if G > 0 and (G % GPB) == 0 and G // GPB < NB:
            build_w_out(G // GPB)
    for u in range(8):
        outproj_unit(NG - 1, u)

    if stage == "mix0":
        return finish(k, [B_xr[G] for G in range(NG)])
    k.phase()
    dbg = k.dout("dbg", [128, 4096]) if stage.startswith("M") else None
    r = moe_layer(k, 0, dict(stage=stage, dbg=dbg, xr=xr, B_xr=B_xr, modrow=modrow, B_modrow=B_modrow, gffn=gffn, Wr_in=Wr_in, br_in=br_in,
                         tri_in=tri_in, ident_f=ident_f, B_ident_f=B_ident_f, ident_b=ident_b, B_ident_b=B_ident_b,
                         hs=hs, xs=xs, ys=ys, gw=exp_gate_w, uw=exp_up_w, dw=exp_down_w, NBLK=NBLK, pcol_in=pcol_in))
    if r is not None:
        return finish(k, [B_xr[G] for G in range(NG)] + r)
    if stage == "ffn0":
        return finish(k, [B_xr[G] for G in range(NG)])
    k.phase()
    attn_layer(k, dict(xr=xr, B_xr=B_xr, modrow=modrow, B_modrow=B_modrow, gmixF=gmixF, ident_b=ident_b,
                       B_ident_b=B_ident_b, w_in=attn_in_w, w_out=attn_out_w, ropeC=ropeC_in, ropeS=ropeS_in,
                       aconst=aconst_in, pcol_in=pcol_in))
    if stage == "mix1":
        return finish(k, [B_xr[G] for G in range(NG)])
    k.phase()
    B_out = [Buf() for _ in range(NG)]
    moe_layer(k, 1, dict(stage=stage, dbg=None, final=dict(fing=fing_in, out=out_ap, B_out=B_out), xr=xr, B_xr=B_xr, modrow=modrow, B_modrow=B_modrow, gffn=gffn,
                         Wr_in=Wr_in, br_in=br_in, tri_in=tri_in, ident_f=ident_f, B_ident_f=B_ident_f,
                         ident_b=ident_b, B_ident_b=B_ident_b, hs=hs, xs=xs, ys=ys, gw=exp_gate_w, uw=exp_up_w,
                         dw=exp_down_w, NBLK=NBLK, pcol_in=pcol_in))
    return finish(k, [B_out[G] for G in range(NG)])


DIL = (1, 4, 16)
import os
SKIP = os.environ.get("ATT_SKIP", "").split(",")


def attn_layer(k, a):
    L = 1
    NB, S, T = k.NB, k.S, k.T
    GPB = S // 512
    xr, B_xr, modrow, B_modrow = a["xr"], a["B_xr"], a["modrow"], a["B_modrow"]
    ident_b, B_ident_b = a["ident_b"], a["B_ident_b"]
    w_in, w_out = a["w_in"], a["w_out"]
    hT = k.sbp("a_hT", [128, 8, S], BF16)
    oT = k.sbp("a_oT", [128, 4, S], BF16)
    B_hT = [[Buf() for _ in range(GPB)] for _ in range(8)]
    B_oT = [Buf() for _ in range(4)]
    vl = k.dsem("a_vl")
    for b in range(NB):
        k.s.scope = f"att{b}_1_hT"
        with contextlib.ExitStack() as c1:
            def sb1(name, shape, dt=F32):
                return c1.enter_context(k.nc.sbuf_tensor(f"s_a1_{b}_{name}", list(shape), dt))
            A1, sh1, sc1t, gmix = sb1("A1", [128, 8]), sb1("sh1", [128, 8]), sb1("sc1t", [128, 8]), sb1("gmix", [128, 8])
            B_A1, B_sh1, B_sc1t, B_gmix = Buf(), Buf(), Buf(), Buf()
            k.dma("sp", gmix[:], a["gmixF"][L], vl, writes=[B_gmix])
            k.dma("sp", sh1[:], modrow[L, b, 0:D].rearrange("(c p) -> p c", p=128), vl,
                  reads=[B_modrow[L]], writes=[B_sh1], allow_slow_non_contiguous=True)
            k.dma("sp", sc1t[:], modrow[L, b, D:2 * D].rearrange("(c p) -> p c", p=128), vl,
                  reads=[B_modrow[L]], writes=[B_sc1t], allow_slow_non_contiguous=True)
            k.op("dve", lambda e: e.scalar_tensor_tensor(out=A1[:], in0=sc1t[:], scalar=1.0, in1=gmix[:],
                                                         op0=ALU.add, op1=ALU.mult), [B_sc1t, B_gmix], [B_A1])
            xt = [sb1(f"xt{i}", [128, 4, D]) for i in range(2)]
            B_xt = [Buf(), Buf()]
            xt_ld = [k.dsem(f"a1_{b}_xt_ld0"), k.dsem(f"a1_{b}_xt_ld1")]
            junk = sb1("junk", [128, D], BF16)
            ss, rstd = sb1("ss", [128, 4]), sb1("rstd", [128, 4])
            xn = sb1("xn", [128, 4, D], BF16)
            B_junk, B_ss, B_rstd = Buf(), Buf(), Buf()
            B_xn = [Buf() for _ in range(4)]
            pT = [c1.enter_context(k.nc.psum_tensor(f"p_a1_{b}_pT{i}", [128, 512], BF16)) for i in range(2)]
            B_pT = [Buf(), Buf()]
            def a1_load(gi):
                G = b * GPB + gi
                sl = gi % 2
                k.dma("sp", xt[sl][:], xr[G * 512:(G + 1) * 512, :].rearrange("(j p) d -> p j d", p=128), xt_ld[sl],
                      reads=[B_xr[G]], writes=[B_xt[sl]])
            a1_load(0)
            for gi in range(GPB):
                G = b * GPB + gi
                sl = gi % 2
                if gi + 1 < GPB:
                    a1_load(gi + 1)
                xt_t, B_x = xt[sl], B_xt[sl]
                for j in range(4):
                    k.op("act", lambda e, j=j, xt_t=xt_t: e.activation(out=junk[:], in_=xt_t[:, j, :], func=AF.Square,
                                                                      accum_out=ss[:, j:j + 1]), [B_x], [B_junk, B_ss])
                k.op("dve", lambda e: e.tensor_scalar(out=rstd[:], in0=ss[:], scalar1=1.0 / D, scalar2=1e-6,
                                                      op0=ALU.mult, op1=ALU.add), [B_ss], [B_rstd])
                k.op("act", lambda e: e.activation(out=rstd[:], in_=rstd[:], func=AF.Sqrt), [B_rstd], [B_rstd])
                k.op("dve", lambda e: e.reciprocal(out=rstd[:], in_=rstd[:]), [B_rstd], [B_rstd])
                for j in range(4):
                    k.op("act" if j % 2 == 0 else "pool", (lambda e, j=j, xt_t=xt_t: e.activation(
                        out=xn[:, j, :], in_=xt_t[:, j, :], func=AF.Copy, scale=rstd[:, j:j + 1])) if j % 2 == 0 else (
                        lambda e, j=j, xt_t=xt_t: e.tensor_scalar(out=xn[:, j, :], in0=xt_t[:, j, :],
                                                                  scalar1=rstd[:, j:j + 1], scalar2=1.0,
                                                                  op0=ALU.mult, op1=ALU.mult)),
                         [B_x, B_rstd], [B_xn[j]])
                for c in range(8):
                    p = c % 2
                    for j in range(4):
                        k.tr(pT[p][:, j * 128:(j + 1) * 128], xn[:, j, c * 128:(c + 1) * 128], ident_b[:],
                             reads=[B_xn[j], B_ident_b], writes=[B_pT[p]])
                    k.op("act", lambda e, c=c, p=p, gi=gi: e.activation(
                        out=hT[:, c, gi * 512:(gi + 1) * 512], in_=pT[p][:], func=AF.Identity,
                        scale=A1[:, c:c + 1], bias=sh1[:, c:c + 1]), [B_pT[p], B_A1, B_sh1], [B_hT[c][gi]])
            k.s.barrier()
        k.s.scope = f"att{b}_2_core"
        with contextlib.ExitStack() as c2:
            def sb2(name, shape, dt=F32):
                return c2.enter_context(k.nc.sbuf_tensor(f"s_a2_{b}_{name}", list(shape), dt))

            def ps2(name, shape, dt=F32):
                return c2.enter_context(k.nc.psum_tensor(f"p_a2_{b}_{name}", list(shape), dt))
            Ct, St = sb2("Ct", [128, S], BF16), sb2("St", [128, S], BF16)
            acb = sb2("acb", [128, 640], BF16)
            B_Ct, B_St, B_acb = Buf(), Buf(), Buf()
            vlp = k.dsem(f"a2_{b}_vlp", sw=True)
            k.dma("pool", Ct[:], a["ropeC"][:, :], vlp, writes=[B_Ct], max_dma_last_dim=4096)
            k.dma("pool", St[:], a["ropeS"][:, :], vlp, writes=[B_St], max_dma_last_dim=4096)
            k.dma("pool", acb[:], a["aconst"][:, :], vlp, writes=[B_acb])
            Rm, Mprev, Mcur = acb[:, 0:128], acb[:, 128:256], acb[:, 256:384]
            onesp = [acb[:, 384:512], acb[:, 512:640]]
            qz = [sb2(f"qz{h}", [128, S], BF16) for h in range(2)]
            kT = sb2("kT", [128, S], BF16)
            B_qz = [[Buf() for _ in range(GPB)] for _ in range(2)]
            B_kT = [Buf() for _ in range(GPB)]
            pc = sb2("pc", [128, 1])
            hm = sb2("hm", [128, 2])
            hmb = sb2("hmb", [128, 2], BF16)
            B_pc, B_hm = Buf(), Buf()
            k.dma("sp", pc[:], a["pcol_in"][:, :], vl, writes=[B_pc])
            k.op("dve", lambda e: e.tensor_scalar(out=hm[:, 0:1], in0=pc[:], scalar1=64.0, scalar2=None, op0=ALU.is_lt),
                 [B_pc], [B_hm])
            k.op("dve", lambda e: e.tensor_scalar(out=hm[:, 1:2], in0=pc[:], scalar1=64.0, scalar2=None, op0=ALU.is_ge),
                 [B_pc], [B_hm])
            k.op("dve", lambda e: e.tensor_copy(out=hmb[:], in_=hm[:]), [B_hm], [B_hm])
            NBK = S // 128
            Vp = sb2("Vp", [128, NBK, 2, 128], BF16)
            B_Vp = [Buf() for _ in range(NBK // 4)]
            k.op("pool", lambda e: e.memset(Vp[:].rearrange("p a h f -> p (a h f)"), 0.0), [], B_Vp)
            Nacc, Dacc = sb2("Nacc", [128, S]), sb2("Dacc", [128, S])
            B_Nacc, B_Dacc = Buf(), Buf()
            wq = [sb2(f"wq{i}", [128, 8, 384], BF16) for i in range(2)]
            B_wq = [Buf(), Buf()]
            wq_sem = [k.dsem(f"a2_{b}_wq0", sw=True), k.dsem(f"a2_{b}_wq1", sw=True)]
            qsb_ = [sb2(f"qsb{i}", [128, 512], BF16) for i in range(2)]
            t1s, t2s = sb2("t1s", [128, 512]), sb2("t2s", [128, 512])
            t1_, t2_ = [t1s, t1s], [t2s, t2s]
            Bt1, Bt2 = Buf(), Buf()
            B_qsb_, B_t1_, B_t2_ = [Buf(), Buf()], [Bt1, Bt1], [Bt2, Bt2]
            PT = [sb2(f"PT{i}", [128, 2, 2, 128], BF16) for i in range(2)]
            B_PT = [Buf(), Buf()]
            psQ_ = [ps2(f"psQ{i}", [128, 512]) for i in range(2)]
            psR0 = ps2("psR0", [128, 512])
            psR_ = [psR0, psR0]
            psV = ps2("psV", [128, 4, 128])
            psS = [ps2(f"psS{i}", [128, 2, 2, 128]) for i in range(2)]
            psND = [ps2(f"psND{i}", [128, 2, 128]) for i in range(2)]
            BpsR = Buf()
            B_psQ_, B_psR_, B_psV = [Buf(), Buf()], [BpsR, BpsR], Buf()
            B_psS, B_psND = [Buf(), Buf()], [Buf(), Buf()]
            pit = 0
            allhT = [B_hT[c][gi] for c in range(8) for gi in range(GPB)]
            it = 0

            def wq_load(hp, g):
                wsl = (hp * 3 + g) % 2
                for qi in range(3):
                    col = g * 1536 + qi * 512 + hp * 128
                    for kc in range(8):
                        k.dma("pool", wq[wsl][:, kc, qi * 128:(qi + 1) * 128],
                              w_in[kc * 128:(kc + 1) * 128, col:col + 128], wq_sem[wsl], writes=[B_wq[wsl]],
                              grp=("wq", hp, g))
            wq_load(0, 0)
            for hp in range(4):
                for g in range(3):
                    dil = DIL[g]
                    nb = S // dil // 128
                    wsl = (hp * 3 + g) % 2
                    nxt = hp * 3 + g + 1
                    if nxt < 12:
                        wq_load(nxt // 3, nxt % 3)
                    for qi in range(2 if "qk" not in SKIP else 0):
                        for tg in range(GPB):
                            pi = pit % 2
                            pit += 1
                            psQ, psR, qsb, t1, t2 = psQ_[pi], psR_[pi], qsb_[pi], t1_[pi], t2_[pi]
                            B_psQ, B_psR, B_qsb, B_t1, B_t2 = B_psQ_[pi], B_psR_[pi], B_qsb_[pi], B_t1_[pi], B_t2_[pi]
                            for kc in range(8):
                                k.mm(psQ[:], wq[wsl][:, kc, qi * 128:(qi + 1) * 128], hT[:, kc, tg * 512:(tg + 1) * 512],
                                     kc == 0, kc == 7, reads=[B_wq[wsl], B_hT[kc][tg]], writes=[B_psQ])
                            k.op("act", lambda e, qsb=qsb, psQ=psQ: e.copy(out=qsb[:], in_=psQ[:]), [B_psQ], [B_qsb])
                            k.mm(psR[:], Rm, qsb[:], True, True, reads=[B_acb, B_qsb], writes=[B_psR])
                            k.op("dve", lambda e, tg=tg, t1=t1, psQ=psQ: e.tensor_tensor(
                                out=t1[:], in0=psQ[:], in1=Ct[:, tg * 512:(tg + 1) * 512], op=ALU.mult),
                                 [B_psQ, B_Ct, B_qsb], [B_t1])
                            k.op("dve", lambda e, tg=tg, t2=t2, psR=psR: e.tensor_tensor(
                                out=t2[:], in0=psR[:], in1=St[:, tg * 512:(tg + 1) * 512], op=ALU.mult),
                                 [B_psR, B_St], [B_t2])
                            if qi == 0:
                                k.op("pool", lambda e, t1=t1, t2=t2, qsb=qsb: e.tensor_tensor(
                                    out=qsb[:], in0=t1[:], in1=t2[:], op=ALU.add), [B_t1, B_t2, B_qsb], [B_qsb])
                                for h in range(2):
                                    k.op("dve", lambda e, h=h, tg=tg, qsb=qsb: e.tensor_scalar(
                                        out=qz[h][:, tg * 512:(tg + 1) * 512], in0=qsb[:], scalar1=hmb[:, h:h + 1],
                                        scalar2=None, op0=ALU.mult), [B_qsb, B_hm], [B_qz[h][tg]])
                            else:
                                k.op("pool", lambda e, tg=tg, t1=t1, t2=t2: e.tensor_tensor(
                                    out=kT[:, tg * 512:(tg + 1) * 512], in0=t1[:], in1=t2[:], op=ALU.add),
                                     [B_t1, B_t2], [B_kT[tg]])
                    def tok(r, n):
                        st = r + n * 128 * dil
                        return slice(st, st + 127 * dil + 1, dil)
                    blks = [(r, n) for r in range(dil) for n in range(nb)]
                    for bi, (r, n) in enumerate(blks if "vproj" not in SKIP else []):
                        for kc in range(8):
                            k.mm(psV[:, bi % 4, :], hT[:, kc, tok(r, n)], wq[wsl][:, kc, 256:384], kc == 0, kc == 7,
                                 reads=[B_wq[wsl]] + [B_hT[kc][gi] for gi in range(GPB)], writes=[B_psV])
                        if bi % 4 == 3:
                            b4 = bi // 4
                            k.op("dve", lambda e, b4=b4: e.tensor_copy(out=Vp[:, b4 * 4:(b4 + 1) * 4, 0, 0:64],
                                                                      in_=psV[:, :, 0:64]), [B_psV], [B_Vp[b4]])
                            k.op("dve", lambda e, b4=b4: e.tensor_copy(out=Vp[:, b4 * 4:(b4 + 1) * 4, 1, 64:128],
                                                                      in_=psV[:, :, 64:128]), [B_psV], [B_Vp[b4]])
                    ablks = blks if "blocks" not in SKIP else []

                    def chunks_of(bi):
                        r, n = ablks[bi]
                        ch = [(1, tok(r, n), Mcur, bi)]
                        if n > 0:
                            ch.append((0, tok(r, n - 1), Mprev, bi - 1))
                        return ch

                    def emit_scores(bi, si):
                        r, n = ablks[bi]
                        cur = tok(r, n)
                        for h in range(2):
                            for (ci, ktok, Mk, vb) in chunks_of(bi):
                                k.mm(psS[si][:, h, ci, :], kT[:, ktok], qz[h][:, cur], True, False,
                                     reads=B_kT + B_qz[h], writes=[B_psS[si]])
                                k.mm(psS[si][:, h, ci, :], ident_b[:], Mk, False, True,
                                     reads=[B_ident_b, B_acb], writes=[B_psS[si]])

                    if ablks:
                        emit_scores(0, it % 2)
                    for bi, (r, n) in enumerate(ablks):
                        si = it % 2
                        it += 1
                        cur = tok(r, n)
                        chunks = chunks_of(bi)
                        if bi + 1 < len(ablks):
                            emit_scores(bi + 1, it % 2)
                        k.op("act", lambda e, si=si: e.activation(
                            out=PT[si][:].rearrange("p h c q -> p (h c q)"),
                            in_=psS[si][:].rearrange("p h c q -> p (h c q)"), func=AF.Exp, scale=0.125),
                            [B_psS[si]], [B_PT[si]])
                        nmm = 2 * len(chunks)
                        for which in range(2):
                            ii = 0
                            for h in range(2):
                                for (ci, ktok, Mk, vb) in chunks:
                                    lhs = Vp[:, vb, h, :] if which == 0 else onesp[h]
                                    k.mm(psND[si][:, which, :], lhs, PT[si][:, h, ci, :], ii == 0, ii == nmm - 1,
                                         reads=[B_Vp[vb // 4], B_PT[si], B_acb], writes=[B_psND[si]])
                                    ii += 1
                        if g == 0:
                            k.op("dve", lambda e, si=si, cur=cur: e.tensor_copy(out=Nacc[:, cur], in_=psND[si][:, 0, :]),
                                 [B_psND[si]], [B_Nacc])
                            k.op("dve", lambda e, si=si, cur=cur: e.tensor_copy(out=Dacc[:, cur], in_=psND[si][:, 1, :]),
                                 [B_psND[si]], [B_Dacc])
                        else:
                            k.op("dve", lambda e, si=si, cur=cur: e.tensor_tensor(out=Nacc[:, cur], in0=psND[si][:, 0, :],
                                                                                 in1=Nacc[:, cur], op=ALU.add),
                                 [B_psND[si], B_Nacc], [B_Nacc])
                            k.op("dve", lambda e, si=si, cur=cur: e.tensor_tensor(out=Dacc[:, cur], in0=psND[si][:, 1, :],
                                                                                 in1=Dacc[:, cur], op=ALU.add),
                                 [B_psND[si], B_Dacc], [B_Dacc])
                if "norm" in SKIP:
                    continue
                k.op("act", lambda e: e.activation(out=Dacc[:], in_=Dacc[:], func=AF.Ln), [B_Dacc], [B_Dacc])
                k.op("act", lambda e: e.activation(out=Dacc[:], in_=Dacc[:], func=AF.Exp, scale=-1.0),
                     [B_Dacc], [B_Dacc])
                k.op("dve", lambda e, hp=hp: e.tensor_tensor(out=oT[:, hp, :], in0=Nacc[:], in1=Dacc[:], op=ALU.mult),
                     [B_Nacc, B_Dacc], [B_oT[hp]])
            k.s.barrier()
        k.s.scope = f"att{b}_3_out"
        with contextlib.ExitStack() as c3:
            def sb3(name, shape, dt=F32):
                return c3.enter_context(k.nc.sbuf_tensor(f"s_a3_{b}_{name}", list(shape), dt))
            wof = sb3("wof", [128, 4, D])
            wob = sb3("wob", [128, 4, D], BF16)
            g1bc = sb3("g1bc", [128, D])
            B_wof, B_wob, B_g1bc = Buf(), Buf(), Buf()
            k.dma("sp", wof[:], w_out.rearrange("(c p) d -> p c d", p=128), vl, writes=[B_wof])
            k.dma("sp", g1bc[:], modrow[L, b:b + 1, 2 * D:3 * D].partition_broadcast(128), vl,
                  reads=[B_modrow[L]], writes=[B_g1bc])
            for c in range(4):
                k.op("dve", lambda e, c=c: e.tensor_tensor(out=wob[:, c, :], in0=wof[:, c, :], in1=g1bc[:], op=ALU.mult),
                     [B_wof, B_g1bc], [B_wob])
            xt = [sb3(f"xt{i}", [128, 4, D]) for i in range(3)]
            B_xt = [Buf(), Buf(), Buf()]
            xt_ld = [k.dsem(f"a3_{b}_xt_ld{i}") for i in range(3)]
            xt_st = [k.dsem(f"a3_{b}_xt_st{i}") for i in range(3)]
            psY = [c3.enter_context(k.nc.psum_tensor(f"p_a3_{b}_psY{i}", [128, 512], F32)) for i in range(2)]
            B_psY = [Buf(), Buf()]
            def a3_load(gi):
                G = b * GPB + gi
                sl = gi % 3
                k.dma("sp", xt[sl][:], xr[G * 512:(G + 1) * 512, :].rearrange("(j p) d -> p j d", p=128), xt_ld[sl],
                      reads=[B_xr[G]], writes=[B_xt[sl]])
            a3_load(0)
            for gi in range(GPB if "p3" not in SKIP else 0):
                G = b * GPB + gi
                sl = gi % 3
                if gi + 1 < GPB:
                    a3_load(gi + 1)
                for j in range(4):
                    for dh in range(2):
                        yi = (j * 2 + dh) % 2
                        t0 = gi * 512 + j * 128
                        for c in range(4):
                            k.mm(psY[yi][:], oT[:, c, t0:t0 + 128], wob[:, c, dh * 512:(dh + 1) * 512], c == 0, c == 3,
                                 reads=[B_oT[c], B_wob], writes=[B_psY[yi]])
                        k.op("dve", lambda e, j=j, dh=dh, yi=yi, sl=sl: e.tensor_tensor(
                            out=xt[sl][:, j, dh * 512:(dh + 1) * 512], in0=psY[yi][:],
                            in1=xt[sl][:, j, dh * 512:(dh + 1) * 512], op=ALU.add), [B_psY[yi], B_xt[sl]], [B_xt[sl]])
                k.dma("sp", xr[G * 512:(G + 1) * 512, :].rearrange("(j p) d -> p j d", p=128), xt[sl][:], xt_st[sl],
                      reads=[B_xt[sl]], writes=[B_xr[G]])
            k.s.barrier()


def moe_layer(k, L, a):
    NB, S, T = k.NB, k.S, k.T
    NT, NG, GPB = T // 128, T // 512, S // 512
    NBLK = a["NBLK"]
    xr, B_xr, modrow, B_modrow = a["xr"], a["B_xr"], a["modrow"], a["B_modrow"]
    ident_f, B_ident_f, ident_b, B_ident_b = a["ident_f"], a["B_ident_f"], a["ident_b"], a["B_ident_b"]
    hs, xs, ys = a["hs"], a["xs"], a["ys"]
    B_hs = [Buf() for _ in range(NG)]
    pfx = f"m{L}_"

    E1all = k.sbp(pfx + "E1all", [128, NT, 32], F32)
    E2all = k.sbp(pfx + "E2all", [128, NT, 32], F32)
    Oall = k.sbp(pfx + "Oall", [128, NT, 32], BF16)
    Wall = k.sbp(pfx + "Wall", [128, NT, 2], F32)
    B_E1 = [Buf() for _ in range(NT)]
    B_E2 = [Buf() for _ in range(NT)]
    B_O = [Buf() for _ in range(NT)]
    B_W = [Buf() for _ in range(NT)]
    g2bc = [k.sbp(pfx + f"g2bc{b}", [128, D], F32) for b in range(NB)]
    B_g2bc = [Buf() for _ in range(NB)]
    d1i = k.sbp(pfx + "d1i", [128, NT], I32)
    d2i = k.sbp(pfx + "d2i", [128, NT], I32)
    bei = k.sbp(pfx + "bei", [128, 2, NBLK], I32)
    B_d1i, B_d2i, B_bei = Buf(), Buf(), Buf()
    nusedi = k.sbp(pfx + "nusedi", [128, 1], I32)
    B_nusedi = Buf()
    vl = k.dsem(pfx + "vl")
    for b in range(NB):
        k.dma("sp", g2bc[b][:], modrow[L, b:b + 1, 5 * D:6 * D].partition_broadcast(128), vl,
              reads=[B_modrow[L]], writes=[B_g2bc[b]], grp="g2")

    k.s.scope = pfx + "M1_router"
    with contextlib.ExitStack() as c1:
        def sb1(name, shape, dt=F32):
            return c1.enter_context(k.nc.sbuf_tensor("s_" + pfx + name, list(shape), dt))

        def ps1(name, shape, dt=F32):
            return c1.enter_context(k.nc.psum_tensor("p_" + pfx + name, list(shape), dt))
        A2bc = [sb1(f"A2bc{b}", [128, D]) for b in range(NB)]
        sh2bc = [sb1(f"sh2bc{b}", [128, D]) for b in range(NB)]
        gfbc = sb1("gfbc", [128, D])
        B_A2bc = [Buf() for _ in range(NB)]
        B_sh2bc = [Buf() for _ in range(NB)]
        B_gfbc = Buf()
        Wr = sb1("Wr", [128, 8, 36])
        brbc = sb1("brbc", [128, 36])
        B_Wr, B_brbc = Buf(), Buf()
        k.dma("sp", gfbc[:], a["gffn"][L:L + 1, :].partition_broadcast(128), vl, writes=[B_gfbc], grp="g2")
        k.dma("sp", Wr[:], a["Wr_in"][L].rearrange("(kc p) f -> p kc f", p=128), vl, writes=[B_Wr], grp="g2")
        k.dma("sp", brbc[:], a["br_in"][L:L + 1, :].partition_broadcast(128), vl, writes=[B_brbc], grp="g2")
        for b in range(NB):
            k.dma("sp", sh2bc[b][:], modrow[L, b:b + 1, 3 * D:4 * D].partition_broadcast(128), vl,
                  reads=[B_modrow[L]], writes=[B_sh2bc[b]], grp="g2")
            k.dma("sp", A2bc[b][:], modrow[L, b:b + 1, 4 * D:5 * D].partition_broadcast(128), vl,
                  reads=[B_modrow[L]], writes=[B_A2bc[b]], grp="g2")
            k.op("dve", lambda e, b=b: e.scalar_tensor_tensor(out=A2bc[b][:], in0=A2bc[b][:], scalar=1.0, in1=gfbc[:],
                                                             op0=ALU.add, op1=ALU.mult),
                 [B_A2bc[b], B_gfbc], [B_A2bc[b]])
        xt = [sb1(f"xt{i}", [128, 4, D]) for i in range(2)]
        B_xt = [Buf(), Buf()]
        xt_ld = [k.dsem(pfx + "xt_ld0"), k.dsem(pfx + "xt_ld1")]
        junk = sb1("junk", [128, D], BF16)
        B_junk = Buf()
        ss, rstd = sb1("ss", [128, 4]), sb1("rstd", [128, 4])
        B_ss, B_rstd = Buf(), Buf()
        h2_ = [sb1(f"h2_{i}", [128, 4, D]) for i in range(2)]
        B_h2_ = [[Buf() for _ in range(4)] for _ in range(2)]
        h2b = [sb1(f"h2b{i}", [128, 4, D], BF16) for i in range(2)]
        B_h2b = [Buf(), Buf()]
        h2b_st = [k.dsem(pfx + "h2b_st0"), k.dsem(pfx + "h2b_st1")]
        pTf = [ps1(f"pTf{i}", [128, 512]) for i in range(2)]
        B_pTf = [Buf(), Buf()]
        hT2 = sb1("hT2", [128, 8, 512])
        B_hT2 = [Buf() for _ in range(8)]
        pslg = ps1("pslg", [128, 4, 36])
        B_pslg = Buf()
        lg4 = sb1("lg4", [128, 4, 36])
        gmax, gsum = sb1("gmax", [128, 4]), sb1("gsum", [128, 4])
        gsh = sb1("gsh", [128, 4, 4])
        ohg4 = sb1("ohg4", [128, 4, 4])
        tmp4 = sb1("tmp4", [128, 4, 4, 8])
        sel4, sel4b = sb1("sel4", [128, 4, 8]), sb1("sel4b", [128, 4, 8])
        oh1_4, oh2_4 = sb1("oh1_4", [128, 4, 8]), sb1("oh2_4", [128, 4, 8])
        m1, m2, dlt = sb1("m1", [128, 4]), sb1("m2", [128, 4]), sb1("dlt", [128, 4])
        B_lg, B_sm, B_sm2, B_ohg, B_ge, B_sel, B_selb, B_oh1, B_oh2, B_tmp4, B_m1, B_m2, B_dlt = (Buf() for _ in range(13))

        def load_x(G):
            sl = G % 2
            k.dma("sp", xt[sl][:], xr[G * 512:(G + 1) * 512, :].rearrange("(j p) d -> p j d", p=128),
                  xt_ld[sl], reads=[B_xr[G]], writes=[B_xt[sl]])
        def stage1(G):
            b = G // GPB
            sl = G % 2
            xt_t, B_x = xt[sl], B_xt[sl]
            h2, B_h2 = h2_[sl], B_h2_[sl]
            for j in range(4):
                k.op("act", lambda e, j=j, xt_t=xt_t: e.activation(out=junk[:], in_=xt_t[:, j, :], func=AF.Square,
                                                                  accum_out=ss[:, j:j + 1]), [B_x], [B_junk, B_ss])
            k.op("dve", lambda e: e.tensor_scalar(out=rstd[:], in0=ss[:], scalar1=1.0 / D, scalar2=1e-6,
                                                  op0=ALU.mult, op1=ALU.add), [B_ss], [B_rstd])
            k.op("act", lambda e: e.activation(out=rstd[:], in_=rstd[:], func=AF.Sqrt), [B_rstd], [B_rstd])
            k.op("dve", lambda e: e.reciprocal(out=rstd[:], in_=rstd[:]), [B_rstd], [B_rstd])
            for j in range(4):
                k.op("dve", lambda e, j=j, xt_t=xt_t, b=b, h2=h2: e.scalar_tensor_tensor(
                    out=h2[:, j, :], in0=xt_t[:, j, :], scalar=rstd[:, j:j + 1], in1=A2bc[b][:],
                    op0=ALU.mult, op1=ALU.mult), [B_x, B_rstd, B_A2bc[b]], [B_h2[j]])
                k.op("pool" if j % 2 else "dve", lambda e, j=j, b=b, h2=h2: e.tensor_tensor(
                    out=h2[:, j, :], in0=h2[:, j, :], in1=sh2bc[b][:], op=ALU.add),
                     [B_h2[j], B_sh2bc[b]], [B_h2[j]])

        def stage1b(G):
            sl = G % 2
            h2, B_h2 = h2_[sl], B_h2_[sl]
            for j in range(4):
                k.op("act", lambda e, j=j, sl=sl, h2=h2: e.copy(out=h2b[sl][:, j, :], in_=h2[:, j, :]),
                     [B_h2[j]], [B_h2b[sl]])
            k.dma("sp", hs[G * 512:(G + 1) * 512, :].rearrange("(j p) d -> p j d", p=128), h2b[sl][:], h2b_st[sl],
                  reads=[B_h2b[sl]], writes=[B_hs[G]])

        load_x(0)
        if NG > 1:
            load_x(1)
        stage1(0)
        stage1b(0)
        for G in range(NG):
            b = G // GPB
            sl = G % 2
            h2, B_h2 = h2_[sl], B_h2_[sl]
            for c in range(8):
                p = c % 2
                for j in range(4):
                    k.tr(pTf[p][:, j * 128:(j + 1) * 128], h2[:, j, c * 128:(c + 1) * 128], ident_f[:],
                         reads=[B_h2[j], B_ident_f], writes=[B_pTf[p]])
                if c % 2 == 0:
                    k.op("act", lambda e, c=c, p=p: e.copy(out=hT2[:, c, :], in_=pTf[p][:]), [B_pTf[p]], [B_hT2[c]])
                else:
                    k.op("dve", lambda e, c=c, p=p: e.tensor_copy(out=hT2[:, c, :], in_=pTf[p][:]),
                         [B_pTf[p]], [B_hT2[c]])
            for j in range(4):
                for kc in range(8):
                    k.mm(pslg[:, j, :], hT2[:, kc, j * 128:(j + 1) * 128], Wr[:, kc, :], kc == 0, kc == 7,
                         reads=[B_hT2[kc], B_Wr], writes=[B_pslg])
            if G + 1 < NG:
                stage1(G + 1)
            if G + 2 < NG:
                load_x(G + 2)
            V = "dve"
            i0_ = G * 4
            J = 4

            def bc(ap, shape):
                return ap.to_broadcast(list(shape))
            k.op(V, lambda e: e.tensor_tensor(out=lg4[:], in0=pslg[:], in1=bc(brbc[:].unsqueeze(1), [128, J, 36]),
                                              op=ALU.add), [B_pslg, B_brbc], [B_lg])
            k.op(V, lambda e: e.tensor_reduce(out=gmax[:], in_=lg4[:, :, 0:4], axis=AX.X, op=ALU.max), [B_lg], [B_sm])
            k.op(V, lambda e: e.tensor_tensor(out=gsh[:], in0=lg4[:, :, 0:4], in1=bc(gmax[:].unsqueeze(2), [128, J, 4]),
                                              op=ALU.subtract), [B_lg, B_sm], [B_ge])
            k.op(V, lambda e: e.tensor_tensor(out=ohg4[:], in0=lg4[:, :, 0:4], in1=bc(gmax[:].unsqueeze(2), [128, J, 4]),
                                              op=ALU.is_equal), [B_lg, B_sm], [B_ohg])
            k.op("act", lambda e: e.activation(out=gsh[:], in_=gsh[:], func=AF.Exp), [B_ge], [B_ge])
            k.op(V, lambda e: e.tensor_reduce(out=gsum[:], in_=gsh[:], axis=AX.X, op=ALU.add), [B_ge], [B_sm2])
            k.op(V, lambda e: e.reciprocal(out=gsum[:], in_=gsum[:]), [B_sm2], [B_sm2])
            k.op(V, lambda e: e.tensor_tensor(out=tmp4[:], in0=lg4[:, :, 4:36].rearrange("p j (g e) -> p j g e", g=4),
                                              in1=bc(ohg4[:].unsqueeze(3), [128, J, 4, 8]), op=ALU.mult),
                 [B_lg, B_ohg], [B_tmp4])
            k.op(V, lambda e: e.tensor_reduce(out=sel4[:], in_=tmp4[:].rearrange("p j g e -> p j e g"), axis=AX.X,
                                              op=ALU.add), [B_tmp4], [B_sel])
            k.op(V, lambda e: e.tensor_reduce(out=m1[:], in_=sel4[:], axis=AX.X, op=ALU.max), [B_sel], [B_m1])
            k.op(V, lambda e: e.tensor_tensor(out=oh1_4[:], in0=sel4[:], in1=bc(m1[:].unsqueeze(2), [128, J, 8]),
                                              op=ALU.is_equal), [B_sel, B_m1], [B_oh1])
            k.op(V, lambda e: e.scalar_tensor_tensor(out=sel4b[:], in0=oh1_4[:], scalar=-1.0e30, in1=sel4[:],
                                                     op0=ALU.mult, op1=ALU.add), [B_oh1, B_sel], [B_selb])
            k.op(V, lambda e: e.tensor_reduce(out=m2[:], in_=sel4b[:], axis=AX.X, op=ALU.max), [B_selb], [B_m2])
            k.op(V, lambda e: e.tensor_tensor(out=oh2_4[:], in0=sel4b[:], in1=bc(m2[:].unsqueeze(2), [128, J, 8]),
                                              op=ALU.is_equal), [B_selb, B_m2], [B_oh2])
            k.op(V, lambda e: e.tensor_tensor(out=dlt[:], in0=m2[:], in1=m1[:], op=ALU.subtract), [B_m1, B_m2], [B_dlt])
            k.op("act", lambda e: e.activation(out=dlt[:], in_=dlt[:], func=AF.Exp), [B_dlt], [B_dlt])
            k.op(V, lambda e: e.tensor_scalar(out=dlt[:], in0=dlt[:], scalar1=1.0, scalar2=None, op0=ALU.add),
                 [B_dlt], [B_dlt])
            k.op(V, lambda e: e.reciprocal(out=dlt[:], in_=dlt[:]), [B_dlt], [B_dlt])
            Bws = B_W[i0_:i0_ + J]
            k.op(V, lambda e, i0_=i0_: e.tensor_tensor(out=Wall[:, i0_:i0_ + J, 0], in0=gsum[:], in1=dlt[:], op=ALU.mult),
                 [B_sm2, B_dlt], Bws)
            k.op(V, lambda e, i0_=i0_: e.tensor_tensor(out=Wall[:, i0_:i0_ + J, 1], in0=gsum[:],
                                                      in1=Wall[:, i0_:i0_ + J, 0], op=ALU.subtract),
                 [B_sm2] + Bws, Bws)
            for (Eall, oh, B_oh, B_E) in ((E1all, oh1_4, B_oh1, B_E1), (E2all, oh2_4, B_oh2, B_E2)):
                k.op(V, lambda e, Eall=Eall, oh=oh, i0_=i0_: e.tensor_tensor(
                    out=Eall[:, i0_:i0_ + J, :].rearrange("p j (g e) -> p j g e", g=4),
                    in0=bc(ohg4[:].unsqueeze(3), [128, J, 4, 8]), in1=bc(oh[:].unsqueeze(2), [128, J, 4, 8]),
                    op=ALU.mult), [B_ohg, B_oh], B_E[i0_:i0_ + J])
            k.op("pool", lambda e, i0_=i0_: e.tensor_tensor(out=Oall[:, i0_:i0_ + J, :], in0=E1all[:, i0_:i0_ + J, :],
                                                           in1=E2all[:, i0_:i0_ + J, :], op=ALU.add),
                 B_E1[i0_:i0_ + J] + B_E2[i0_:i0_ + J], B_O[i0_:i0_ + J])
            if G + 1 < NG:
                stage1b(G + 1)
        k.s.barrier()
    dbgsem = k.dsem(pfx + "dbg")
    B_dbg = Buf()
    if a.get("stage") == "M1":
        dbg = a["dbg"]
        k.dma("sp", dbg[:, 0:NT * 2], Wall[:].rearrange("p i w -> p (i w)"), dbgsem, reads=B_W, writes=[B_dbg])
        k.dma("sp", dbg[:, 1024:1024 + NT * 32], E1all[:].rearrange("p i e -> p (i e)"), dbgsem, reads=B_E1, writes=[B_dbg])
        k.dma("sp", dbg[:, 2048:2048 + NT * 32], E2all[:].rearrange("p i e -> p (i e)"), dbgsem, reads=B_E2, writes=[B_dbg])
        return [B_dbg]

    k.s.scope = pfx + "M2_index"
    with contextlib.ExitStack() as c2:
        def sb2(name, shape, dt=F32):
            return c2.enter_context(k.nc.sbuf_tensor("s_" + pfx + name, list(shape), dt))

        def ps2(name, shape, dt=F32):
            return c2.enter_context(k.nc.psum_tensor("p_" + pfx + name, list(shape), dt))
        trif, trib, onesb = sb2("trif", [128, 128]), sb2("trib", [128, 128], BF16), sb2("onesb", [128, 128], BF16)
        B_trif, B_trib, B_onesb = Buf(), Buf(), Buf()
        k.dma("sp", trif[:], a["tri_in"][:, :], vl, writes=[B_trif])
        k.op("dve", lambda e: e.tensor_copy(out=trib[:], in_=trif[:]), [B_trif], [B_trib])
        k.op("dve", lambda e: e.memset(onesb[:], 1.0), [], [B_onesb])
        base = sb2("base", [128, NT, 32])
        tot = sb2("tot", [128, NT, 32])
        B_base, B_tot = Buf(), Buf()
        psw = [ps2(f"psw{i}", [128, 512]) for i in range(2)]
        B_psw = [Buf(), Buf()]
        TPC = 16
        nch = (NT + TPC - 1) // TPC
        for ci in range(nch):
            t0, t1 = ci * TPC, min(NT, (ci + 1) * TPC)
            w = (t1 - t0) * 32
            for which, (lhs, B_l, dst, B_d) in enumerate(((trib, B_trib, base, B_base), (onesb, B_onesb, tot, B_tot))):
                k.mm(psw[which][:, 0:w], lhs[:], Oall[:, t0:t1, :], True, True,
                     reads=[B_l] + B_O[t0:t1], writes=[B_psw[which]])
                k.op("dve", lambda e, which=which, dst=dst, t0=t0, t1=t1, w=w: e.tensor_copy(
                    out=dst[:, t0:t1, :], in_=psw[which][:, 0:w]), [B_psw[which]], [B_d])
        cnt = sb2("cnt", [128, 32])
        nblk = sb2("nblk", [128, 32])
        cum = sb2("cum", [128, 32])
        carry = sb2("carry", [128, 32])
        tmp32 = sb2("tmp32", [128, 32])
        B_cnt, B_nblk, B_cum, B_carry, B_tmp32 = Buf(), Buf(), Buf(), Buf(), Buf()
        V = "dve"
        k.op(V, lambda e: e.tensor_reduce(out=cnt[:], in_=tot[:].rearrange("p i e -> p e i"), axis=AX.X, op=ALU.add),
             [B_tot], [B_cnt])
        k.op(V, lambda e: e.tensor_scalar(out=nblk[:], in0=cnt[:], scalar1=0.0, scalar2=None, op0=ALU.is_gt),
             [B_cnt], [B_nblk])
        for j in range(1, (2 * T) // 512 + 1):
            k.op(V, lambda e, j=j: e.scalar_tensor_tensor(out=nblk[:], in0=cnt[:], scalar=512.0 * j, in1=nblk[:],
                                                         op0=ALU.is_gt, op1=ALU.add), [B_cnt, B_nblk], [B_nblk])
        k.op(V, lambda e: e.tensor_copy(out=cum[:], in_=nblk[:]), [B_nblk], [B_cum])
        for ee in range(1, 32):
            k.op(V, lambda e, ee=ee: e.tensor_tensor(out=cum[:, ee:ee + 1], in0=cum[:, ee - 1:ee], in1=nblk[:, ee:ee + 1],
                                                    op=ALU.add), [B_cum, B_nblk], [B_cum])
        k.op(V, lambda e: e.tensor_tensor(out=carry[:], in0=cum[:], in1=nblk[:], op=ALU.subtract),
             [B_cum, B_nblk], [B_carry])
        k.op(V, lambda e: e.tensor_scalar(out=carry[:], in0=carry[:], scalar1=512.0, scalar2=None, op0=ALU.mult),
             [B_carry], [B_carry])
        for i in range(NT):
            k.op(V, lambda e, i=i: e.tensor_tensor(out=base[:, i, :], in0=base[:, i, :], in1=carry[:], op=ALU.add),
                 [B_base, B_carry], [B_base])
            if i + 1 < NT:
                k.op(V, lambda e, i=i: e.tensor_tensor(out=carry[:], in0=carry[:], in1=tot[:, i, :], op=ALU.add),
                     [B_carry, B_tot], [B_carry])
        dtmp = sb2("dtmp", [128, NT, 32])
        dfl = sb2("dfl", [128, NT])
        B_dtmp, B_dfl = Buf(), Buf()
        for (Eall, B_E, di, B_di) in ((E1all, B_E1, d1i, B_d1i), (E2all, B_E2, d2i, B_d2i)):
            k.op(V, lambda e, Eall=Eall: e.tensor_tensor(out=dtmp[:], in0=Eall[:], in1=base[:], op=ALU.mult),
                 list(B_E) + [B_base], [B_dtmp])
            k.op(V, lambda e: e.tensor_reduce(out=dfl[:], in_=dtmp[:], axis=AX.X, op=ALU.add), [B_dtmp], [B_dfl])
            k.op(V, lambda e, di=di: e.tensor_copy(out=di[:], in_=dfl[:]), [B_dfl], [B_di])
        bef = sb2("bef", [128, NBLK])
        B_bef = Buf()
        for j in range(NBLK):
            k.op(V, lambda e, j=j: e.tensor_scalar(out=tmp32[:], in0=cum[:], scalar1=float(j), scalar2=None,
                                                  op0=ALU.is_le, op1=ALU.add, accum_out=bef[:, j:j + 1]),
                 [B_cum], [B_tmp32, B_bef])
        oob = sb2("oob", [128, NBLK])
        B_oob = Buf()
        k.op(V, lambda e: e.tensor_scalar(out=oob[:], in0=bef[:], scalar1=32.0, scalar2=float(OOB_BIG),
                                          op0=ALU.is_ge, op1=ALU.mult), [B_bef], [B_oob])
        k.op(V, lambda e: e.tensor_scalar(out=bef[:], in0=bef[:], scalar1=31.0, scalar2=None, op0=ALU.min),
             [B_bef], [B_bef])
        k.op(V, lambda e: e.tensor_copy(out=nusedi[:], in_=cum[:, 31:32]), [B_cum], [B_nusedi])
        pcol = sb2("pcol", [128, 1])
        B_pcol = Buf()
        k.dma("sp", pcol[:], a["pcol_in"][:, :], vl, writes=[B_pcol])
        k.op(V, lambda e: e.tensor_scalar(out=bef[:], in0=bef[:], scalar1=float(L * 32), scalar2=128.0,
                                          op0=ALU.add, op1=ALU.mult), [B_bef], [B_bef])
        k.op(V, lambda e: e.tensor_scalar(out=bef[:], in0=bef[:], scalar1=pcol[:, 0:1], scalar2=None, op0=ALU.add),
             [B_bef, B_pcol], [B_bef])
        k.op(V, lambda e: e.tensor_scalar(out=bef[:], in0=bef[:], scalar1=2.0, scalar2=None, op0=ALU.mult),
             [B_bef], [B_bef])
        k.op(V, lambda e: e.tensor_tensor(out=bef[:], in0=bef[:], in1=oob[:], op=ALU.add), [B_bef, B_oob], [B_bef])
        k.op(V, lambda e: e.tensor_copy(out=bei[:, 0, :], in_=bef[:]), [B_bef], [B_bei])
        k.op(V, lambda e: e.tensor_scalar(out=bef[:], in0=bef[:], scalar1=1.0, scalar2=None, op0=ALU.add),
             [B_bef], [B_bef])
        k.op(V, lambda e: e.tensor_copy(out=bei[:, 1, :], in_=bef[:]), [B_bef], [B_bei])
        k.s.barrier()
        if a.get("stage") == "M2":
            dbg = a["dbg"].bitcast(I32)
            k.dma("sp", dbg[:, 0:NT], d1i[:], dbgsem, reads=[B_d1i], writes=[B_dbg])
            k.dma("sp", dbg[:, 1024:1024 + NT], d2i[:], dbgsem, reads=[B_d2i], writes=[B_dbg])
            k.dma("sp", dbg[:, 2048:2048 + NBLK], bei[:, 0, :], dbgsem, reads=[B_bei], writes=[B_dbg])
            k.s.barrier()
    if a.get("stage") == "M2":
        return [B_dbg]

    k.s.scope = pfx + "M3_scatter"
    with contextlib.ExitStack() as c3:
        NS3 = 6
        hsb = [c3.enter_context(k.nc.sbuf_tensor(f"s_{pfx}hsb{i}", [128, D], BF16)) for i in range(NS3)]
        B_hsb = [Buf() for _ in range(NS3)]
        hsb_ld = [k.dsem(pfx + f"hsb_ld{i}") for i in range(NS3)]
        sc_sem = [k.dsem(pfx + f"sc{i}", sw=True) for i in range(NS3)]
        B_xs = Buf()

        def m3_load(i):
            sl = i % NS3
            k.dma("sp", hsb[sl][:], hs[i * 128:(i + 1) * 128, :], hsb_ld[sl], reads=[B_hs[i // 4]], writes=[B_hsb[sl]])
        for i in range(min(NS3 - 1, NT)):
            m3_load(i)
        for i in range(NT):
            sl = i % NS3
            if i + NS3 - 1 < NT:
                m3_load(i + NS3 - 1)
            for (di, B_di) in ((d1i, B_d1i), (d2i, B_d2i)):
                k.s.add("pool", lambda e, sl=sl, di=di, i=i: e.indirect_dma_start(
                    out=xs[:, :], out_offset=bass.IndirectOffsetOnAxis(ap=di[:, i:i + 1], axis=0),
                    in_=hsb[sl][:], in_offset=None), [B_hsb[sl], B_di], [B_xs], dsem=sc_sem[sl], grp=("sc", i))
        k.s.barrier()
    if a.get("stage") == "M3":
        return []

    k.s.scope = pfx + "M4_experts"
    with contextlib.ExitStack() as c4:
        def sb4(name, shape, dt=F32):
            return c4.enter_context(k.nc.sbuf_tensor("s_" + pfx + name, list(shape), dt))

        def ps4(name, shape, dt=F32):
            return c4.enter_context(k.nc.psum_tensor("p_" + pfx + name, list(shape), dt))
        wg = [sb4(f"wg{i}", [128, 8, 512], BF16) for i in range(2)]
        wu = [sb4(f"wu{i}", [128, 8, 512], BF16) for i in range(2)]
        wd = [sb4(f"wd{i}", [128, 4, D], BF16) for i in range(2)]
        B_wg, B_wu, B_wd = [Buf(), Buf()], [Buf(), Buf()], [Buf(), Buf()]
        wsem = [[k.dsem(pfx + f"w{n}{i}", sw=True) for i in range(2)] for n in "gud"]
        xb = [sb4(f"xb{i}", [128, 4, D], BF16) for i in range(2)]
        B_xb = [Buf(), Buf()]
        xb_ld = [k.dsem(pfx + "xb_ld0"), k.dsem(pfx + "xb_ld1")]
        xsT_ = [sb4(f"xsT{i}", [128, 8, 512], BF16) for i in range(2)]
        B_xsT_ = [[Buf() for _ in range(8)] for _ in range(2)]
        pTx = [ps4(f"pTx{i}", [128, 512], BF16) for i in range(2)]
        B_pTx = [Buf(), Buf()]
        psG = [ps4(f"psG{i}", [128, 512]) for i in range(2)]
        psU = [ps4(f"psU{i}", [128, 512]) for i in range(2)]
        B_psG, B_psU = [Buf(), Buf()], [Buf(), Buf()]
        sg = [sb4(f"sg{i}", [128, 512]) for i in range(2)]
        B_sg = [Buf(), Buf()]
        hTe = sb4("hTe", [128, 4, 512], BF16)
        B_hTe = [Buf() for _ in range(4)]
        psY = [ps4(f"psY{i}", [128, 512]) for i in range(2)]
        B_psY = [Buf(), Buf()]
        yb = [sb4(f"yb{i}", [128, 4, D]) for i in range(2)]
        B_yb = [Buf(), Buf()]
        yb_st = [k.dsem(pfx + "yb_st0"), k.dsem(pfx + "yb_st1")]
        B_ys = Buf()
        gw, uw, dw = a["gw"], a["uw"], a["dw"]
        gw2 = gw.rearrange("r (h f) -> (r h) f", h=2)
        uw2 = uw.rearrange("r (h f) -> (r h) f", h=2)
        dw2 = dw.rearrange("r (h f) -> (r h) f", h=2)

        breg = k.bound_reg()
        k.s.add("pool", lambda e: e.reg_mov(breg, 2 * 2 * 32 * 128 - 1), [], [])

        def wload(j, sl):
            for (dst, B_d, tab, ws, nh) in ((wg[sl], B_wg[sl], gw2, wsem[0][sl], 4), (wu[sl], B_wu[sl], uw2, wsem[1][sl], 4),
                                            (wd[sl], B_wd[sl], dw2, wsem[2][sl], 2)):
                for h in range(2):
                    k.s.add("pool", lambda e, dst=dst, tab=tab, j=j, h=h, nh=nh: e.indirect_dma_start(
                        out=dst[:, h * nh:(h + 1) * nh, :].rearrange("p a f -> p (a f)"), out_offset=None, in_=tab,
                        in_offset=bass.IndirectOffsetOnAxis(ap=bei[:, h, j:j + 1], axis=0),
                        bounds_check=breg, oob_is_err=False), [B_bei], [B_d], dsem=ws,
                        grp=("w", j))

        def xload(j, sl):
            k.dma("sp", xb[sl][:], xs[j * 512:(j + 1) * 512, :].rearrange("(t p) d -> p t d", p=128), xb_ld[sl],
                  reads=[B_xs], writes=[B_xb[sl]])
        def xpose(j, cs):
            sl = j % 2
            for c in cs:
                p = c % 2
                for t in range(4):
                    k.tr(pTx[p][:, t * 128:(t + 1) * 128], xb[sl][:, t, c * 128:(c + 1) * 128], ident_b[:],
                         reads=[B_xb[sl], B_ident_b], writes=[B_pTx[p]])
                if c % 2 == 0:
                    k.op("act", lambda e, c=c, p=p, sl=sl: e.copy(out=xsT_[sl][:, c, :], in_=pTx[p][:]),
                         [B_pTx[p]], [B_xsT_[sl][c]])
                else:
                    k.op("dve", lambda e, c=c, p=p, sl=sl: e.tensor_copy(out=xsT_[sl][:, c, :], in_=pTx[p][:]),
                         [B_pTx[p]], [B_xsT_[sl][c]])
        regs = k.nused_regs()
        for eng_ in ENGS:
            k.s.add(eng_, lambda e, eng_=eng_: e.reg_load(regs[eng_], nusedi[0:1, 0:1]), [B_nusedi], [])
        JS = (2 * T) // 512
        wload(0, 0)
        xload(0, 0)
        if NBLK > 1:
            wload(1, 1)
            xload(1, 1)
        xpose(0, range(8))
        for j in range(NBLK):
            sl = j % 2
            k.s.guard = (regs, (j if not os.environ.get("DYN_NEVER") else -1)) if (j >= max(JS, int(os.environ.get("DYN_FROM", "0"))) and DYN_SKIP) else None
            xsT, B_xsT = xsT_[sl], B_xsT_[sl]
            for fc in range(4):
                q = fc % 2
                for kc in range(8):
                    k.mm(psG[q][:], wg[sl][:, kc, fc * 128:(fc + 1) * 128], xsT[:, kc, :], kc == 0, kc == 7,
                         reads=[B_wg[sl], B_xsT[kc]], writes=[B_psG[q]])
                for kc in range(8):
                    k.mm(psU[q][:], wu[sl][:, kc, fc * 128:(fc + 1) * 128], xsT[:, kc, :], kc == 0, kc == 7,
                         reads=[B_wu[sl], B_xsT[kc]], writes=[B_psU[q]])
                k.op("act", lambda e, q=q: e.activation(out=sg[q][:], in_=psG[q][:], func=AF.Silu),
                     [B_psG[q]], [B_sg[q]])
                k.op("dve", lambda e, q=q, fc=fc: e.tensor_tensor(out=hTe[:, fc, :], in0=sg[q][:], in1=psU[q][:],
                                                                 op=ALU.mult), [B_sg[q], B_psU[q]], [B_hTe[fc]])
                if j + 1 < NBLK:
                    xpose(j + 1, [2 * fc, 2 * fc + 1])
            for t in range(4):
                for dh in range(2):
                    yi = (t * 2 + dh) % 2
                    for fc in range(4):
                        k.mm(psY[yi][:], hTe[:, fc, t * 128:(t + 1) * 128], wd[sl][:, fc, dh * 512:(dh + 1) * 512],
                             fc == 0, fc == 3, reads=[B_hTe[fc], B_wd[sl]], writes=[B_psY[yi]])
                    if yi == 0:
                        k.op("act", lambda e, t=t, dh=dh, sl=sl: e.copy(out=yb[sl][:, t, dh * 512:(dh + 1) * 512],
                                                                       in_=psY[0][:]), [B_psY[0]], [B_yb[sl]])
                    else:
                        k.op("dve", lambda e, t=t, dh=dh, sl=sl: e.tensor_copy(
                            out=yb[sl][:, t, dh * 512:(dh + 1) * 512], in_=psY[1][:]), [B_psY[1]], [B_yb[sl]])
            if j + 2 < NBLK:
                wload(j + 2, sl)
                xload(j + 2, sl)
            k.dma("sp", ys[j * 512:(j + 1) * 512, :].rearrange("(t p) d -> p t d", p=128), yb[sl][:], yb_st[sl],
                  reads=[B_yb[sl]], writes=[B_ys])
        k.s.guard = None
        k.s.barrier()
    if a.get("stage") == "M4":
        return []

    k.s.scope = pfx + "M5_combine"
    fin = a.get("final")
    with contextlib.ExitStack() as c5:
        def sb5(name, shape, dt=F32):
            return c5.enter_context(k.nc.sbuf_tensor("s_" + pfx + name, list(shape), dt))
        NS = 6
        y1 = [sb5(f"y1_{i}", [128, D]) for i in range(NS)]
        y2 = [sb5(f"y2_{i}", [128, D]) for i in range(NS)]
        xc = [sb5(f"xc{i}", [128, D]) for i in range(NS)]
        B_y1, B_y2, B_xc = [Buf() for _ in range(NS)], [Buf() for _ in range(NS)], [Buf() for _ in range(NS)]
        g_sem = [[k.dsem(pfx + f"g{n}{i}", sw=True) for i in range(NS)] for n in "12"]
        xc_ld = [k.dsem(pfx + f"xc_ld{i}") for i in range(NS)]
        xc_st = [k.dsem(pfx + f"xc_st{i}") for i in range(NS)]
        if fin is not None:
            fg = sb5("fg", [128, D])
            fjunk = sb5("fjunk", [128, D], BF16)
            fss = [sb5(f"fss{i}", [128, 1]) for i in range(NS)]
            B_fg, B_fjunk = Buf(), Buf()
            B_fss = [Buf() for _ in range(NS)]
            k.dma("sp", fg[:], fin["fing"][0:1, :].partition_broadcast(128), vl, writes=[B_fg])
        def m5_load(i):
            sl = i % NS
            G = i // 4
            k.s.add("pool", lambda e, sl=sl, i=i: e.indirect_dma_start(
                out=y1[sl][:], out_offset=None, in_=ys[:, :],
                in_offset=bass.IndirectOffsetOnAxis(ap=d1i[:, i:i + 1], axis=0)), [B_ys, B_d1i], [B_y1[sl]],
                dsem=g_sem[0][sl])
            k.s.add("pool", lambda e, sl=sl, i=i: e.indirect_dma_start(
                out=y2[sl][:], out_offset=None, in_=ys[:, :],
                in_offset=bass.IndirectOffsetOnAxis(ap=d2i[:, i:i + 1], axis=0)), [B_ys, B_d2i], [B_y2[sl]],
                dsem=g_sem[1][sl])
            k.dma("sp", xc[sl][:], xr[i * 128:(i + 1) * 128, :], xc_ld[sl], reads=[B_xr[G]], writes=[B_xc[sl]])
        for i in range(min(NS - 1, NT)):
            m5_load(i)
        for i in range(NT):
            sl = i % NS
            b = (i * 128) // S
            G = i // 4
            if i + NS - 1 < NT:
                m5_load(i + NS - 1)
            k.op("act", lambda e, sl=sl, i=i: e.activation(out=y1[sl][:], in_=y1[sl][:], func=AF.Copy,
                                                          scale=Wall[:, i, 0:1]), [B_y1[sl], B_W[i]], [B_y1[sl]])
            k.op("dve", lambda e, sl=sl, i=i: e.scalar_tensor_tensor(out=y1[sl][:], in0=y2[sl][:],
                                                                    scalar=Wall[:, i, 1:2], in1=y1[sl][:],
                                                                    op0=ALU.mult, op1=ALU.add),
                 [B_y1[sl], B_y2[sl], B_W[i]], [B_y1[sl]])
            k.op("dve", lambda e, sl=sl, b=b: e.tensor_tensor(out=y1[sl][:], in0=y1[sl][:], in1=g2bc[b][:],
                                                             op=ALU.mult), [B_y1[sl], B_g2bc[b]], [B_y1[sl]])
            k.op("dve", lambda e, sl=sl: e.tensor_tensor(out=xc[sl][:], in0=xc[sl][:], in1=y1[sl][:], op=ALU.add),
                 [B_xc[sl], B_y1[sl]], [B_xc[sl]])
            if fin is None:
                k.dma("sp", xr[i * 128:(i + 1) * 128, :], xc[sl][:], xc_st[sl], reads=[B_xc[sl]], writes=[B_xr[G]])
            else:
                k.op("act", lambda e, sl=sl: e.activation(out=fjunk[:], in_=xc[sl][:], func=AF.Square,
                                                          accum_out=fss[sl][:, 0:1]), [B_xc[sl]], [B_fjunk, B_fss[sl]])
                k.op("dve", lambda e, sl=sl: e.tensor_scalar(out=fss[sl][:], in0=fss[sl][:], scalar1=1.0 / D,
                                                             scalar2=1e-6, op0=ALU.mult, op1=ALU.add),
                     [B_fss[sl]], [B_fss[sl]])
                k.op("act", lambda e, sl=sl: e.activation(out=fss[sl][:], in_=fss[sl][:], func=AF.Sqrt),
                     [B_fss[sl]], [B_fss[sl]])
                k.op("dve", lambda e, sl=sl: e.reciprocal(out=fss[sl][:], in_=fss[sl][:]), [B_fss[sl]], [B_fss[sl]])
                k.op("dve", lambda e, sl=sl: e.scalar_tensor_tensor(out=xc[sl][:], in0=xc[sl][:], scalar=fss[sl][:, 0:1],
                                                                    in1=fg[:], op0=ALU.mult, op1=ALU.mult),
                     [B_xc[sl], B_fss[sl], B_fg], [B_xc[sl]])
                k.dma("sp", fin["out"][i * 128:(i + 1) * 128, :], xc[sl][:], xc_st[sl], reads=[B_xc[sl]],
                      writes=[fin["B_out"][G]])
        k.s.barrier()


def finish(k, out_bufs):
    k.s.add("sp", None, reads=out_bufs)
    esem = {e: k.sem("e_" + e) for e in ENGS}
    stuck = k.s.simulate()
    assert not stuck, f"schedule deadlock: {stuck}"
    k.s.emit(k.nc, esem)
    k.pctx.close()
    k.ctx.close()
    return k.nc


_RL = {}


def _relayout(w, key, nch):
    if key not in _RL or _RL[key][0] is not w:
        Lr, E, R, N = w.shape
        _RL[key] = (w, np.ascontiguousarray(w.reshape(Lr, E, nch, 128, N).transpose(0, 1, 3, 2, 4)).reshape(
            Lr * E * 128, nch * N))
    return _RL[key][1]


def host_inputs(inp, core, NB, S):
    b0 = core * NB
    f = np.float32
    m = {}
    m["x"] = np.ascontiguousarray(inp["x"][b0:b0 + NB, :S].reshape(NB * S, D))
    c = inp["c"][b0:b0 + NB]
    m["cT"] = np.ascontiguousarray(c.reshape(NB, 8, 128).transpose(2, 1, 0).reshape(128, 8 * NB))
    m["ada_w"] = inp["ada_w"]
    m["ada_b"] = inp["ada_b"]
    m["gmixF"] = np.ascontiguousarray(inp["norm_mix_g"].reshape(2, 8, 128).transpose(0, 2, 1))
    m["gffn"] = inp["norm_ffn_g"]
    m["conv_in_w"] = inp["conv_in_w"][0]
    m["convwF"] = np.ascontiguousarray(inp["conv_w"][0].reshape(3, 8, 128).transpose(2, 1, 0).reshape(128, 24))
    m["conv_out_w"] = inp["conv_out_w"][0]
    m["ident"] = np.eye(128, dtype=f)
    m["tri"] = np.triu(np.ones((128, 128), dtype=f), 1)
    m["Wr"] = np.ascontiguousarray(np.concatenate(
        [inp["router_grp_w"], inp["router_exp_w"].transpose(0, 2, 1, 3).reshape(2, D, 32)], axis=2))
    m["br"] = np.ascontiguousarray(np.concatenate([inp["router_grp_b"], inp["router_exp_b"].reshape(2, 32)], axis=1))
    m["exp_gate_w"] = _relayout(inp["exp_gate_w"], "g", 8)
    m["exp_up_w"] = _relayout(inp["exp_up_w"], "u", 8)
    m["exp_down_w"] = _relayout(inp["exp_down_w"], "d", 4)
    m["pcol"] = np.arange(128, dtype=f).reshape(128, 1)
    m["attn_in_w"] = inp["attn_in_w"][0]
    m["attn_out_w"] = inp["attn_out_w"][0]
    pos = np.arange(S, dtype=f)
    inv = np.power(f(500000.0), -np.arange(0, 16, 2, dtype=f) / f(16)).astype(f)
    ang = pos[None, :] * inv[:, None]
    C = np.ones((128, S), f)
    Sg = np.zeros((128, S), f)
    Rm = np.zeros((128, 128), f)
    for h in range(2):
        for e in range(16):
            C[h * 64 + e] = np.cos(ang[e % 8])
            Sg[h * 64 + e] = (-np.sin(ang[e % 8])) if e < 8 else np.sin(ang[e % 8])
            Rm[h * 64 + (e + 8 if e < 8 else e - 8), h * 64 + e] = 1.0
    m["ropeC"], m["ropeS"] = C, Sg
    kk = np.arange(128)[:, None]
    qq = np.arange(128)[None, :]
    NEG = f(-30000.0)
    Mprev = np.where(kk >= qq, f(0), NEG).astype(f)
    Mcur = np.where(kk <= qq, f(0), NEG).astype(f)
    o0 = np.zeros((128, 128), f)
    o0[:, :64] = 1
    o1 = np.zeros((128, 128), f)
    o1[:, 64:] = 1
    m["aconst"] = np.ascontiguousarray(np.concatenate([Rm, Mprev, Mcur, o0, o1], axis=1))
    m["fing"] = inp["final_norm_g"].reshape(1, D)
    return m


def kernel(**inputs):
    NB, S = 2, 4096
    nc = build(NB, S)
    in_maps = [host_inputs(inputs, c, NB, S) for c in range(NCORES)]
    res = run_bass_kernel_spmd(nc, in_maps, core_ids=list(range(NCORES)))
    out = np.stack([r["out"].reshape(NB, S, D) for r in res.results]).reshape(NCORES * NB, S, D)
    return out.astype(np.float32)
```

```python
import contextlib
import os
import numpy as np
import concourse.bass as bass
import concourse.mybir as mybir
from concourse.bass_utils import run_bass_kernel_spmd

F32 = mybir.dt.float32
BF16 = mybir.dt.bfloat16
I32 = mybir.dt.int32
AF = mybir.ActivationFunctionType
ALU = mybir.AluOpType
AX = mybir.AxisListType

D = 1024
NCORES = 8
ENGS = ("pe", "act", "dve", "pool", "sp")
SAME_ENG_SYNC = True
OOB_BIG = 1000000
DYN_SKIP = False


class Buf:
    __slots__ = ("name", "lw", "rd")

    def __init__(self, name=""):
        self.name = name
        self.lw = None
        self.rd = {}


class DSem:
    def __init__(self, h):
        self.h = h
        self.groups = []


class Ins:
    __slots__ = ("eng", "fn", "dsem", "signal", "sig", "target", "deps", "gk", "scope", "guard")


class Sched:
    def __init__(self):
        self.L = {e: [] for e in ENGS}
        self.n = 0
        self.dsems = []
        self.scope = None
        self.trace_scopes = False
        self.guard = None


    def dsem(self, h):
        d = DSem(h)
        self.dsems.append(d)
        return d

    def add(self, eng, fn, reads=(), writes=(), dsem=None, grp=None):
        ins = Ins()
        ins.eng, ins.fn, ins.dsem = eng, fn, dsem
        ins.signal, ins.sig, ins.target = False, 0, 0
        ins.gk = (id(dsem), grp) if (dsem is not None and grp is not None) else None
        ins.scope = self.scope
        ins.guard = self.guard
        deps = {}

        def need(d, kind):
            if d.dsem is not None:
                return not (ins.gk is not None and d.gk == ins.gk)
            if d.eng == eng:
                if dsem is not None:
                    return True
                if eng == "pe":
                    return False
                return kind == "raw" and SAME_ENG_SYNC
            return True

        for b in reads:
            if b.lw is not None and need(b.lw, "raw"):
                deps[id(b.lw)] = b.lw
        for b in writes:
            if b.lw is not None and need(b.lw, "waw"):
                deps[id(b.lw)] = b.lw
            for r in b.rd.values():
                if need(r, "war"):
                    deps[id(r)] = r
        if dsem is not None:
            assert getattr(dsem, "sw", False) == (eng == "pool"), f"DMA semaphore class mismatch on {eng}"
            g = dsem.groups
            if g and grp is not None and g[-1][0] == grp:
                g[-1][1].append(ins)
            else:
                if g:
                    prev = g[-1][1][-1]
                    deps[id(prev)] = prev
                g.append((grp if grp is not None else object(), [ins]))
        ins.deps = list(deps.values())
        for d in ins.deps:
            d.signal = True
        for b in reads:
            key = eng if dsem is None else ("dma", self.n)
            b.rd[key] = ins
        for b in writes:
            b.lw = ins
            b.rd = {}
        self.L[eng].append(ins)
        self.n += 1
        return ins

    def barrier(self):
        lasts = []
        for e in ENGS:
            for ins in reversed(self.L[e]):
                if ins.fn is not None and ins.dsem is None:
                    lasts.append(ins)
                    break
        for ds in self.dsems:
            if ds.groups:
                lasts.append(ds.groups[-1][1][-1])
        for e in ENGS:
            ins = Ins()
            ins.eng, ins.fn, ins.dsem = e, None, None
            ins.signal, ins.sig, ins.target = False, 0, 0
            ins.gk = None
            ins.scope = self.scope
            ins.guard = None
            ins.deps = [d for d in lasts if not (d.dsem is None and d.eng == e)]
            for d in ins.deps:
                d.signal = True
            self.L[e].append(ins)

    def simulate(self):
        for e in ENGS:
            c = 0
            for ins in self.L[e]:
                if ins.dsem is None and ins.signal:
                    c += 1
                    ins.sig = c
        for ds in self.dsems:
            tot = 0
            for _, lst in ds.groups:
                tot += 16 * len(lst)
                for i in lst:
                    i.target = tot
        pc = {e: 0 for e in ENGS}
        ev = {e: 0 for e in ENGS}
        dv = {id(ds): 0 for ds in self.dsems}
        prog = True
        while prog:
            prog = False
            for e in ENGS:
                while pc[e] < len(self.L[e]):
                    ins = self.L[e][pc[e]]
                    ok = True
                    for d in ins.deps:
                        if d.dsem is not None:
                            if dv[id(d.dsem)] < d.target:
                                ok = False
                        elif ev[d.eng] < d.sig:
                            ok = False
                    if not ok:
                        break
                    if ins.fn is not None:
                        if ins.dsem is not None:
                            dv[id(ins.dsem)] += 16
                        elif ins.signal:
                            ev[e] += 1
                    pc[e] += 1
                    prog = True
        stuck = {e: (pc[e], len(self.L[e])) for e in ENGS if pc[e] < len(self.L[e])}
        return stuck

    def emit(self, nc, esem):
        for e in ENGS:
            c = 0
            for ins in self.L[e]:
                if ins.dsem is None and ins.signal:
                    c += 1
                    ins.sig = c
        for ds in self.dsems:
            tot = 0
            for _, lst in ds.groups:
                tot += 16 * len(lst)
                for i in lst:
                    i.target = tot

        def emit_one(e, eo, ins, seen):
            for d in ins.deps:
                if d.dsem is not None:
                    key, val, h = ("d", id(d.dsem)), d.target, d.dsem.h
                else:
                    key, val, h = ("e", d.eng), d.sig, esem[d.eng]
                if seen.get(key, 0) < val:
                    eo.wait_ge(h, val)
                    seen[key] = val
            if ins.fn is None:
                return
            r = ins.fn(eo)
            if ins.dsem is not None:
                r.then_inc(ins.dsem.h, 16)
            elif ins.signal:
                r.then_inc(esem[e], 1)

        def run(e, eo):
            seen = {}
            cur = [None, None]
            L_ = self.L[e]
            i = 0
            while i < len(L_):
                ins = L_[i]
                if self.trace_scopes and ins.scope != cur[0]:
                    if cur[1] is not None:
                        cur[1].__exit__(None, None, None)
                    cur[0] = ins.scope
                    cur[1] = nc.named_scope(f"{ins.scope}") if ins.scope else None
                    if cur[1] is not None:
                        cur[1].__enter__()
                if ins.guard is None:
                    emit_one(e, eo, ins, seen)
                    i += 1
                    continue
                g = ins.guard
                j = i
                while j < len(L_) and L_[j].guard is g and L_[j].scope == ins.scope:
                    j += 1
                region = L_[i:j]
                nsig = sum(1 for x in region if x.dsem is None and x.signal and x.fn is not None)
                dcount = {}
                for x in region:
                    if x.dsem is not None:
                        dcount[id(x.dsem)] = (x.dsem, dcount.get(id(x.dsem), (x.dsem, 0))[1] + 16)
                regs, thresh = g
                snap = dict(seen)
                with eo.If_lt(regs[e], thresh + 1):
                    for _ in range(nsig):
                        eo.nop(nofuse=True).then_inc(esem[e], 1)
                    for ds, n in dcount.values():
                        for _ in range(n // 16):
                            eo.nop(nofuse=True).then_inc(ds.h, 16)
                with eo.Else():
                    for x in region:
                        emit_one(e, eo, x, seen)
                seen = snap
                i = j
            if cur[1] is not None:
                cur[1].__exit__(None, None, None)

        with nc.Block() as block:
            @block.sync
            def _(eo):
                run("sp", eo)

            @block.tensor
            def _(eo):
                run("pe", eo)

            @block.scalar
            def _(eo):
                run("act", eo)

            @block.vector
            def _(eo):
                run("dve", eo)

            @block.gpsimd
            def _(eo):
                run("pool", eo)


class K:
    def __init__(self, NB, S, dbg=None):
        self.NB, self.S, self.T = NB, S, NB * S
        self.dbg = dbg
        self.nc = bass.Bass("TRN2", target_bir_lowering=False)
        self.ctx = contextlib.ExitStack()
        self.s = Sched()
        self.pctx = contextlib.ExitStack()
        self.nsem = 0
        self.dpool = []
        self.dcur = 0
        self.dpool_sw = []
        self.dcur_sw = 0
        self._regs = None
        self._breg = None

    def din(self, name, shape, dt=F32):
        return self.nc.dram_tensor(name, list(shape), dt, kind="ExternalInput").ap()

    def dout(self, name, shape, dt=F32):
        return self.nc.dram_tensor(name, list(shape), dt, kind="ExternalOutput").ap()

    def dscr(self, name, shape, dt=F32):
        return self.nc.dram_tensor(name, list(shape), dt, kind="Internal").ap()

    def phase(self):
        self.s.barrier()
        self.pctx.close()
        self.pctx = contextlib.ExitStack()
        self.dcur = 0
        self.dcur_sw = 0

    def sbp(self, name, shape, dt=F32):
        return self.pctx.enter_context(self.nc.sbuf_tensor("s_" + name, list(shape), dt))

    def psp(self, name, shape, dt=F32):
        return self.pctx.enter_context(self.nc.psum_tensor("p_" + name, list(shape), dt))

    def sb(self, name, shape, dt=F32):
        return self.ctx.enter_context(self.nc.sbuf_tensor("s_" + name, list(shape), dt))

    def ps(self, name, shape, dt=F32):
        return self.ctx.enter_context(self.nc.psum_tensor("p_" + name, list(shape), dt))

    def sem(self, name):
        self.nsem += 1
        return self.ctx.enter_context(self.nc.semaphore(name))

    def bound_reg(self):
        if self._breg is None:
            self._breg = self.ctx.enter_context(self.nc.gpsimd.register("oob_bound"))
        return self._breg

    def nused_regs(self):
        if self._regs is None:
            nc = self.nc
            eng = {"pe": nc.tensor, "act": nc.scalar, "dve": nc.vector, "pool": nc.gpsimd, "sp": nc.sync}
            self._regs = {e: self.ctx.enter_context(eng[e].register("nused_" + e)) for e in ENGS}
        return self._regs

    def dsem(self, name, sw=False):
        pool, cur = (self.dpool_sw, self.dcur_sw) if sw else (self.dpool, self.dcur)
        if cur < len(pool):
            d = pool[cur]
        else:
            d = self.s.dsem(self.sem(f"dma{'s' if sw else 'h'}{len(pool)}"))
            d.sw = sw
            pool.append(d)
        if sw:
            self.dcur_sw += 1
        else:
            self.dcur += 1
        return d

    def dma(self, eng, out, in_, dsem, reads=(), writes=(), grp=None, **kw):
        return self.s.add(eng, lambda e: e.dma_start(out=out, in_=in_, **kw), reads, writes, dsem=dsem, grp=grp)

    def mm(self, out, lhsT, rhs, start, stop, reads=(), writes=()):
        return self.s.add("pe", lambda e: e.matmul(out, lhsT=lhsT, rhs=rhs, start=start, stop=stop), reads, writes)

    def tr(self, out, in_, ident, reads=(), writes=()):
        return self.s.add("pe", lambda e: e.transpose(out=out, in_=in_, identity=ident), reads, writes)

    def op(self, eng, fn, reads=(), writes=()):
        return self.s.add(eng, fn, reads, writes)


def build(NB, S, stage="all", trace_scopes=False):
    k = K(NB, S)
    k.s.trace_scopes = trace_scopes
    nc, s = k.nc, k.s
    T = NB * S
    NT = T // 128
    NG = T // 512
    GPB = S // 512

    x_in = k.din("x", [T, D])
    cT_in = k.din("cT", [128, 8 * NB])
    ada_w = k.din("ada_w", [2, D, 6 * D])
    ada_b = k.din("ada_b", [2, 6 * D])
    gmixF = k.din("gmixF", [2, 128, 8])
    gffn = k.din("gffn", [2, D])
    conv_in_w = k.din("conv_in_w", [D, 3 * D])
    convwF = k.din("convwF", [128, 24])
    conv_out_w = k.din("conv_out_w", [D, D])
    ident_in = k.din("ident", [128, 128])
    tri_in = k.din("tri", [128, 128])
    Wr_in = k.din("Wr", [2, D, 36])
    br_in = k.din("br", [2, 36])
    exp_gate_w = k.din("exp_gate_w", [2 * 32 * 128, 8 * 512])
    exp_up_w = k.din("exp_up_w", [2 * 32 * 128, 8 * 512])
    exp_down_w = k.din("exp_down_w", [2 * 32 * 128, 4 * D])
    pcol_in = k.din("pcol", [128, 1])
    attn_in_w = k.din("attn_in_w", [D, 4608])
    attn_out_w = k.din("attn_out_w", [512, D])
    ropeC_in = k.din("ropeC", [128, S])
    ropeS_in = k.din("ropeS", [128, S])
    aconst_in = k.din("aconst", [128, 640])
    fing_in = k.din("fing", [1, D])
    out_ap = k.dout("out", [T, D])
    NBLK = (2 * T) // 512 + 32
    hs = k.dscr("hs", [T, D], BF16)
    xs = k.dscr("xs", [NBLK * 512, D], BF16)
    ys = k.dscr("ys", [NBLK * 512, D], F32)
    xr = k.dout("xr", [T, D])
    modrow = k.dout("modrow", [2, NB, 6 * D])

    ident_f = k.sb("ident_f", [128, 128], F32)
    ident_b = k.sb("ident_b", [128, 128], BF16)
    cT = k.sb("cT", [128, 8 * NB], F32)
    B_ident_f, B_ident_b, B_cT = Buf(), Buf(), Buf()
    B_modrow = [Buf() for _ in range(2)]

    ld0 = k.dsem("ld0")
    k.dma("sp", ident_f[:], ident_in[:, :], ld0, writes=[B_ident_f], grp="init")
    k.dma("sp", cT[:], cT_in[:, :], ld0, writes=[B_cT], grp="init")
    k.op("dve", lambda e: e.tensor_copy(out=ident_b[:], in_=ident_f[:]), [B_ident_f], [B_ident_b])
    k.op("act", lambda e: e.activation(out=cT[:], in_=cT[:], func=AF.Silu), [B_cT], [B_cT])

    s.scope = "A_mod"
    NSA = 3
    aw = [k.sbp(f"aw{i}", [128, 8, 512], F32) for i in range(NSA)]
    adab = [k.sbp(f"adab{i}", [NB, 512], F32) for i in range(NSA)]
    modc = [k.sbp(f"modc{i}", [NB, 512], F32) for i in range(NSA)]
    B_aw, B_adab, B_modc = [Buf() for _ in range(NSA)], [Buf() for _ in range(NSA)], [Buf() for _ in range(NSA)]
    aw_sem = [k.dsem(f"aw{i}") for i in range(NSA)]
    adab_sem = [k.dsem(f"adab{i}") for i in range(NSA)]
    mod_st = [k.dsem(f"mod_st{i}", sw=True) for i in range(NSA)]
    ps_mod = [k.psp(f"ps_mod{i}", [128, 512], F32) for i in range(2)]
    B_psmod = [Buf(), Buf()]
    chunks = [(l, cc) for l in range(2) for cc in range(12)]

    def a_load(ci):
        l, cc = chunks[ci]
        sl = ci % NSA
        src_b = ada_b[l:l + 1, cc * 512:(cc + 1) * 512]
        k.dma("sp", adab[sl][:], src_b.partition_broadcast(NB) if NB > 1 else src_b, adab_sem[sl],
              writes=[B_adab[sl]])
        k.dma("sp", aw[sl][:], ada_w[l, :, cc * 512:(cc + 1) * 512].rearrange("(kc p) f -> p kc f", p=128),
              aw_sem[sl], writes=[B_aw[sl]])
    for ci in range(NSA - 1):
        a_load(ci)
    for ci, (l, cc) in enumerate(chunks):
        sl = ci % NSA
        pq = ci % 2
        if ci + NSA - 1 < len(chunks):
            a_load(ci + NSA - 1)
        for kc in range(8):
            k.mm(ps_mod[pq][0:NB, :], cT[:, kc * NB:(kc + 1) * NB], aw[sl][:, kc, :], kc == 0, kc == 7,
                 reads=[B_cT, B_aw[sl]], writes=[B_psmod[pq]])
        k.op("dve", lambda e, sl=sl, pq=pq: e.tensor_tensor(out=modc[sl][:], in0=ps_mod[pq][0:NB, :], in1=adab[sl][:],
                                                           op=ALU.add),
             [B_psmod[pq], B_adab[sl]], [B_modc[sl]])
        k.dma("pool", modrow[l, :, cc * 512:(cc + 1) * 512], modc[sl][:], mod_st[sl], reads=[B_modc[sl]],
              writes=[B_modrow[l]])
    k.phase()
    if stage == "A":
        return finish(k, [B_modrow[0], B_modrow[1]])

    s.scope = "L0_conv"
    L = 0
    w_in = k.sbp("w_in", [128, 8, 3 * D], BF16)
    w_stage = [k.sbp(f"w_stage{i}", [128, D], F32) for i in range(2)]
    w_out_b1 = k.sbp("w_out_b", [128, 8, D], BF16)
    w_out_b = [w_out_b1 for b in range(NB)]
    g1bc = [k.sbp(f"g1bc{b}", [128, D], F32) for b in range(NB)]
    A1 = [k.sbp(f"A1_{b}", [128, 8], F32) for b in range(NB)]
    sh1 = [k.sbp(f"sh1_{b}", [128, 8], F32) for b in range(NB)]
    sc1t = k.sbp("sc1t", [128, 8], F32)
    gmix = k.sbp("gmix", [128, 8], F32)
    convw = k.sbp("convw", [128, 24], F32)
    B_w_in, B_sc1t, B_gmix, B_convw = Buf(), Buf(), Buf(), Buf()
    B_w_stage = [Buf(), Buf()]
    B_g1bc = [Buf() for _ in range(NB)]
    B_w_out_b1 = Buf()
    B_w_out_b = [B_w_out_b1 for _ in range(NB)]
    B_A1 = [Buf() for _ in range(NB)]
    B_sh1 = [Buf() for _ in range(NB)]
    wl = k.dsem("wl", sw=True)
    wst = [k.dsem("wst0"), k.dsem("wst1")]
    vl = k.dsem("vl")
    for kc in range(8):
        k.dma("pool", w_in[:, kc, :], conv_in_w[kc * 128:(kc + 1) * 128, :], wl, writes=[B_w_in], grp="w_in",
              max_dma_last_dim=4096)
    k.dma("sp", gmix[:], gmixF[L], vl, writes=[B_gmix], grp="v0")
    k.dma("sp", convw[:], convwF[:, :], vl, writes=[B_convw], grp="v0")
    for b in range(NB):
        k.dma("sp", sh1[b][:], modrow[L, b, 0:D].rearrange("(c p) -> p c", p=128), vl,
              reads=[B_modrow[L]], writes=[B_sh1[b]], allow_slow_non_contiguous=True)
        k.dma("sp", sc1t[:], modrow[L, b, D:2 * D].rearrange("(c p) -> p c", p=128), vl,
              reads=[B_modrow[L]], writes=[B_sc1t], allow_slow_non_contiguous=True)
        k.op("dve", lambda e, b=b: e.scalar_tensor_tensor(out=A1[b][:], in0=sc1t[:], scalar=1.0, in1=gmix[:],
                                                         op0=ALU.add, op1=ALU.mult),
             [B_sc1t, B_gmix], [B_A1[b]])
        k.dma("sp", g1bc[b][:], modrow[L, b:b + 1, 2 * D:3 * D].partition_broadcast(128), vl,
              reads=[B_modrow[L]], writes=[B_g1bc[b]])
    def build_w_out(b):
        for kc in range(8):
            sl = kc % 2
            k.dma("sp", w_stage[sl][:], conv_out_w[kc * 128:(kc + 1) * 128, :], wst[sl], writes=[B_w_stage[sl]])
            k.op("dve", lambda e, b=b, kc=kc, sl=sl: e.tensor_tensor(out=w_out_b[b][:, kc, :], in0=w_stage[sl][:],
                                                                    in1=g1bc[b][:], op=ALU.mult),
                 [B_w_stage[sl], B_g1bc[b]], [B_w_out_b[b]])
    build_w_out(0)

    if stage == "L0setup":
        return finish(k, [B_modrow[0], B_modrow[1]] + B_w_out_b + B_A1 + B_sh1 + [B_w_in, B_convw])
    NXS = 4
    xt = [k.sbp(f"xt{i}", [128, 4, D], F32) for i in range(NXS)]
    B_xt = [Buf() for _ in range(NXS)]
    xt_ld = [k.dsem(f"xt_ld{i}") for i in range(NXS)]
    xt_st = [k.dsem(f"xt_st{i}") for i in range(NXS)]
    junk = k.sbp("junk", [128, D], BF16)
    B_junk = Buf()
    ss = k.sbp("ss", [128, 4], F32)
    rstd = k.sbp("rstd", [128, 4], F32)
    B_ss, B_rstd = Buf(), Buf()
    xn = [k.sbp(f"xn{i}", [128, 4, D], BF16) for i in range(2)]
    B_xn = [[Buf() for _ in range(4)] for _ in range(2)]
    pT = k.psp("pT", [128, 512], BF16)
    B_pT = Buf()
    hT = [k.sbp(f"hT{i}", [128, 8, 512], BF16) for i in range(2)]
    B_hT = [[Buf() for _ in range(8)] for _ in range(2)]
    psBCU = [[k.psp(f"ps{n}{i}", [128, 512]) for n in "BCU"] for i in range(2)]
    B_psBCU = [[Buf() for _ in range(3)] for _ in range(2)]
    Csb = [k.sbp(f"Csb{i}", [128, 512], F32) for i in range(2)]
    B_Csb = [Buf(), Buf()]
    zb = [k.sbp(f"zb{i}", [128, 514], F32) for i in range(2)]
    B_zb = [Buf(), Buf()]
    zh = k.sbp("zh", [128, 8, 2], F32)
    B_zh = [Buf() for _ in range(8)]
    zc = [k.sbp(f"zc{i}", [128, 512], F32) for i in range(2)]
    B_zc = [Buf(), Buf()]
    gT = [k.sbp(f"gT{i}", [128, 8, 512], BF16) for i in range(2)]
    B_gT = [[Buf() for _ in range(8)] for _ in range(2)]
    psY = k.psp("psY", [128, 512])
    B_psY = Buf()
    B_xr = [Buf() for _ in range(NG)]

    def load_x(G):
        sl = G % NXS
        k.dma("sp", xt[sl][:], x_in[G * 512:(G + 1) * 512, :].rearrange("(j p) d -> p j d", p=128),
              xt_ld[sl], writes=[B_xt[sl]])

    def norm_pre(G):
        sl = G % 2
        xt_t, B_x = xt[G % NXS], B_xt[G % NXS]
        for j in range(4):
            k.op("act", lambda e, j=j: e.activation(out=junk[:], in_=xt_t[:, j, :], func=AF.Square,
                                                    accum_out=ss[:, j:j + 1]),
                 [B_x], [B_junk, B_ss])
        k.op("dve", lambda e: e.tensor_scalar(out=rstd[:], in0=ss[:], scalar1=1.0 / D, scalar2=1e-6,
                                              op0=ALU.mult, op1=ALU.add), [B_ss], [B_rstd])
        k.op("act", lambda e: e.activation(out=rstd[:], in_=rstd[:], func=AF.Sqrt), [B_rstd], [B_rstd])
        k.op("dve", lambda e: e.reciprocal(out=rstd[:], in_=rstd[:]), [B_rstd], [B_rstd])
        for j in range(4):
            k.op("act", lambda e, j=j: e.activation(out=xn[sl][:, j, :], in_=xt_t[:, j, :], func=AF.Copy,
                                                    scale=rstd[:, j:j + 1]),
                 [B_x, B_rstd], [B_xn[sl][j]])

    def norm_tr(G, c):
        b = G // GPB
        sl = G % 2
        A, B_A, sh, B_sh = A1[b], B_A1[b], sh1[b], B_sh1[b]
        for j in range(4):
            k.tr(pT[:, j * 128:(j + 1) * 128], xn[sl][:, j, c * 128:(c + 1) * 128], ident_b[:],
                 reads=[B_xn[sl][j], B_ident_b], writes=[B_pT])
        k.op("act", lambda e, c=c: e.activation(out=hT[sl][:, c, :], in_=pT[:], func=AF.Identity,
                                                scale=A[:, c:c + 1], bias=sh[:, c:c + 1]),
             [B_pT, B_A, B_sh], [B_hT[sl][c]])

    def inproj(G, fcs):
        sl = G % 2
        first = (G % GPB == 0)
        for fc in fcs:
            q = fc % 2
            (psB, psC, psU), (B_psB, B_psC, B_psU) = psBCU[q], B_psBCU[q]
            for (pst, B_p, off) in ((psB, B_psB, 0), (psC, B_psC, D), (psU, B_psU, 2 * D)):
                for kc in range(8):
                    k.mm(pst[:], w_in[:, kc, off + fc * 128: off + (fc + 1) * 128], hT[sl][:, kc, :], kc == 0, kc == 7,
                         reads=[B_w_in, B_hT[sl][kc]], writes=[B_p])
            zt, Bz, zcb, Bzc, Cs, BCs = zb[q], B_zb[q], zc[q], B_zc[q], Csb[q], B_Csb[q]
            if first:
                k.op("pool", lambda e, zt=zt: e.memset(zt[:, 0:2], 0.0), [], [Bz])
            else:
                k.op("pool", lambda e, zt=zt, fc=fc: e.tensor_copy(out=zt[:, 0:2], in_=zh[:, fc, :]),
                     [B_zh[fc]], [Bz])
            k.op("act", lambda e, Cs=Cs, psC=psC: e.copy(out=Cs[:], in_=psC[:]), [B_psC], [BCs])
            k.op("dve", lambda e, zt=zt, Cs=Cs, psU=psU: e.tensor_tensor(out=zt[:, 2:514], in0=Cs[:], in1=psU[:],
                                                                        op=ALU.mult),
                 [BCs, B_psU, Bz], [Bz])
            k.op("pool", lambda e, zt=zt, fc=fc: e.tensor_copy(out=zh[:, fc, :], in_=zt[:, 512:514]),
                 [Bz], [B_zh[fc]])
            k.op("pool", lambda e, fc=fc, zt=zt, zcb=zcb: e.tensor_scalar(
                out=zcb[:], in0=zt[:, 2:514], scalar1=convw[:, fc * 3 + 2: fc * 3 + 3], scalar2=1.0,
                op0=ALU.mult, op1=ALU.mult), [Bz, B_convw], [Bzc])
            k.op("dve", lambda e, fc=fc, zt=zt, zcb=zcb: e.scalar_tensor_tensor(
                out=zcb[:], in0=zt[:, 1:513], scalar=convw[:, fc * 3 + 1: fc * 3 + 2], in1=zcb[:],
                op0=ALU.mult, op1=ALU.add), [Bz, B_convw, Bzc], [Bzc])
            k.op("dve", lambda e, fc=fc, zt=zt, zcb=zcb: e.scalar_tensor_tensor(
                out=zcb[:], in0=zt[:, 0:512], scalar=convw[:, fc * 3: fc * 3 + 1], in1=zcb[:],
                op0=ALU.mult, op1=ALU.add), [Bz, B_convw, Bzc], [Bzc])
            k.op("dve", lambda e, fc=fc, zcb=zcb, psB=psB, sl=sl: e.tensor_tensor(out=gT[sl][:, fc, :], in0=zcb[:],
                                                                                 in1=psB[:], op=ALU.mult),
                 [Bzc, B_psB], [B_gT[sl][fc]])

    def outproj_unit(G, u):
        b = G // GPB
        sl = G % 2
        xs_ = G % NXS
        j, dh = u // 2, u % 2
        for kc in range(8):
            k.mm(psY[:], gT[sl][:, kc, j * 128:(j + 1) * 128], w_out_b[b][:, kc, dh * 512:(dh + 1) * 512],
                 kc == 0, kc == 7, reads=[B_gT[sl][kc], B_w_out_b[b]], writes=[B_psY])
        k.op("dve", lambda e: e.tensor_tensor(
            out=xt[xs_][:, j, dh * 512:(dh + 1) * 512], in0=psY[:], in1=xt[xs_][:, j, dh * 512:(dh + 1) * 512],
            op=ALU.add), [B_psY, B_xt[xs_]], [B_xt[xs_]])
        if u == 7:
            k.dma("sp", xr[G * 512:(G + 1) * 512, :].rearrange("(j p) d -> p j d", p=128), xt[xs_][:], xt_st[xs_],
                  reads=[B_xt[xs_]], writes=[B_xr[G]])

    load_x(0)
    if NG > 1:
        load_x(1)
    norm_pre(0)
    for c in range(8):
        norm_tr(0, c)
    for G in range(NG):
        if G + 1 < NG:
            norm_pre(G + 1)
        if G + 2 < NG:
            load_x(G + 2)
        for fc in range(8):
            inproj(G, [fc])
            if G + 1 < NG and fc > 0:
                norm_tr(G + 1, fc - 1)
            if G > 0:
                outproj_unit(G - 1, fc)
        if G + 1 < NG:
            norm_tr(G + 1, 7)
        if G > 0 and (G % GPB) == 0 and G // GPB < NB:
            build_w_out(G // GPB)
    for u in range(8):
        outproj_unit(NG - 1, u)

    if stage == "mix0":
        return finish(k, [B_xr[G] for G in range(NG)])
    k.phase()
    dbg = k.dout("dbg", [128, 4096]) if stage.startswith("M") else None
    r = moe_layer(k, 0, dict(stage=stage, dbg=dbg, xr=xr, B_xr=B_xr, modrow=modrow, B_modrow=B_modrow, gffn=gffn, Wr_in=Wr_in, br_in=br_in,
                         tri_in=tri_in, ident_f=ident_f, B_ident_f=B_ident_f, ident_b=ident_b, B_ident_b=B_ident_b,
                         hs=hs, xs=xs, ys=ys, gw=exp_gate_w, uw=exp_up_w, dw=exp_down_w, NBLK=NBLK, pcol_in=pcol_in))
    if r is not None:
        return finish(k, [B_xr[G] for G in range(NG)] + r)
    if stage == "ffn0":
        return finish(k, [B_xr[G] for G in range(NG)])
    k.phase()
    attn_layer(k, dict(xr=xr, B_xr=B_xr, modrow=modrow, B_modrow=B_modrow, gmixF=gmixF, ident_b=ident_b,
                       B_ident_b=B_ident_b, w_in=attn_in_w, w_out=attn_out_w, ropeC=ropeC_in, ropeS=ropeS_in,
                       aconst=aconst_in, pcol_in=pcol_in))
    if stage == "mix1":
        return finish(k, [B_xr[G] for G in range(NG)])
    k.phase()
    B_out = [Buf() for _ in range(NG)]
    moe_layer(k, 1, dict(stage=stage, dbg=None, final=dict(fing=fing_in, out=out_ap, B_out=B_out), xr=xr, B_xr=B_xr, modrow=modrow, B_modrow=B_modrow, gffn=gffn,
                         Wr_in=Wr_in, br_in=br_in, tri_in=tri_in, ident_f=ident_f, B_ident_f=B_ident_f,
                         ident_b=ident_b, B_ident_b=B_ident_b, hs=hs, xs=xs, ys=ys, gw=exp_gate_w, uw=exp_up_w,
                         dw=exp_down_w, NBLK=NBLK, pcol_in=pcol_in))
    return finish(k, [B_out[G] for G in range(NG)])


DIL = (1, 4, 16)
import os
SKIP = os.environ.get("ATT_SKIP", "").split(",")


def attn_layer(k, a):
    L = 1
    NB, S, T = k.NB, k.S, k.T
    GPB = S // 512
    xr, B_xr, modrow, B_modrow = a["xr"], a["B_xr"], a["modrow"], a["B_modrow"]
    ident_b, B_ident_b = a["ident_b"], a["B_ident_b"]
    w_in, w_out = a["w_in"], a["w_out"]
    hT = k.sbp("a_hT", [128, 8, S], BF16)
    oT = k.sbp("a_oT", [128, 4, S], BF16)
    B_hT = [[Buf() for _ in range(GPB)] for _ in range(8)]
    B_oT = [Buf() for _ in range(4)]
    vl = k.dsem("a_vl")
    for b in range(NB):
        k.s.scope = f"att{b}_1_hT"
        with contextlib.ExitStack() as c1:
            def sb1(name, shape, dt=F32):
                return c1.enter_context(k.nc.sbuf_tensor(f"s_a1_{b}_{name}", list(shape), dt))
            A1, sh1, sc1t, gmix = sb1("A1", [128, 8]), sb1("sh1", [128, 8]), sb1("sc1t", [128, 8]), sb1("gmix", [128, 8])
            B_A1, B_sh1, B_sc1t, B_gmix = Buf(), Buf(), Buf(), Buf()
            k.dma("sp", gmix[:], a["gmixF"][L], vl, writes=[B_gmix])
            k.dma("sp", sh1[:], modrow[L, b, 0:D].rearrange("(c p) -> p c", p=128), vl,
                  reads=[B_modrow[L]], writes=[B_sh1], allow_slow_non_contiguous=True)
            k.dma("sp", sc1t[:], modrow[L, b, D:2 * D].rearrange("(c p) -> p c", p=128), vl,
                  reads=[B_modrow[L]], writes=[B_sc1t], allow_slow_non_contiguous=True)
            k.op("dve", lambda e: e.scalar_tensor_tensor(out=A1[:], in0=sc1t[:], scalar=1.0, in1=gmix[:],
                                                         op0=ALU.add, op1=ALU.mult), [B_sc1t, B_gmix], [B_A1])
            xt = [sb1(f"xt{i}", [128, 4, D]) for i in range(2)]
            B_xt = [Buf(), Buf()]
            xt_ld = [k.dsem(f"a1_{b}_xt_ld0"), k.dsem(f"a1_{b}_xt_ld1")]
            junk = sb1("junk", [128, D], BF16)
            ss, rstd = sb1("ss", [128, 4]), sb1("rstd", [128, 4])
            xn = sb1("xn", [128, 4, D], BF16)
            B_junk, B_ss, B_rstd = Buf(), Buf(), Buf()
            B_xn = [Buf() for _ in range(4)]
            pT = [c1.enter_context(k.nc.psum_tensor(f"p_a1_{b}_pT{i}", [128, 512], BF16)) for i in range(2)]
            B_pT = [Buf(), Buf()]
            def a1_load(gi):
                G = b * GPB + gi
                sl = gi % 2
                k.dma("sp", xt[sl][:], xr[G * 512:(G + 1) * 512, :].rearrange("(j p) d -> p j d", p=128), xt_ld[sl],
                      reads=[B_xr[G]], writes=[B_xt[sl]])
            a1_load(0)
            for gi in range(GPB):
                G = b * GPB + gi
                sl = gi % 2
                if gi + 1 < GPB:
                    a1_load(gi + 1)
                xt_t, B_x = xt[sl], B_xt[sl]
                for j in range(4):
                    k.op("act", lambda e, j=j, xt_t=xt_t: e.activation(out=junk[:], in_=xt_t[:, j, :], func=AF.Square,
                                                                      accum_out=ss[:, j:j + 1]), [B_x], [B_junk, B_ss])
                k.op("dve", lambda e: e.tensor_scalar(out=rstd[:], in0=ss[:], scalar1=1.0 / D, scalar2=1e-6,
                                                      op0=ALU.mult, op1=ALU.add), [B_ss], [B_rstd])
                k.op("act", lambda e: e.activation(out=rstd[:], in_=rstd[:], func=AF.Sqrt), [B_rstd], [B_rstd])
                k.op("dve", lambda e: e.reciprocal(out=rstd[:], in_=rstd[:]), [B_rstd], [B_rstd])
                for j in range(4):
                    k.op("act" if j % 2 == 0 else "pool", (lambda e, j=j, xt_t=xt_t: e.activation(
                        out=xn[:, j, :], in_=xt_t[:, j, :], func=AF.Copy, scale=rstd[:, j:j + 1])) if j % 2 == 0 else (
                        lambda e, j=j, xt_t=xt_t: e.tensor_scalar(out=xn[:, j, :], in0=xt_t[:, j, :],
                                                                  scalar1=rstd[:, j:j + 1], scalar2=1.0,
                                                                  op0=ALU.mult, op1=ALU.mult)),
                         [B_x, B_rstd], [B_xn[j]])
                for c in range(8):
                    p = c % 2
                    for j in range(4):
                        k.tr(pT[p][:, j * 128:(j + 1) * 128], xn[:, j, c * 128:(c + 1) * 128], ident_b[:],
                             reads=[B_xn[j], B_ident_b], writes=[B_pT[p]])
                    k.op("act", lambda e, c=c, p=p, gi=gi: e.activation(
                        out=hT[:, c, gi * 512:(gi + 1) * 512], in_=pT[p][:], func=AF.Identity,
                        scale=A1[:, c:c + 1], bias=sh1[:, c:c + 1]), [B_pT[p], B_A1, B_sh1], [B_hT[c][gi]])
            k.s.barrier()
        k.s.scope = f"att{b}_2_core"
        with contextlib.ExitStack() as c2:
            def sb2(name, shape, dt=F32):
                return c2.enter_context(k.nc.sbuf_tensor(f"s_a2_{b}_{name}", list(shape), dt))

            def ps2(name, shape, dt=F32):
                return c2.enter_context(k.nc.psum_tensor(f"p_a2_{b}_{name}", list(shape), dt))
            Ct, St = sb2("Ct", [128, S], BF16), sb2("St", [128, S], BF16)
            acb = sb2("acb", [128, 640], BF16)
            B_Ct, B_St, B_acb = Buf(), Buf(), Buf()
            vlp = k.dsem(f"a2_{b}_vlp", sw=True)
            k.dma("pool", Ct[:], a["ropeC"][:, :], vlp, writes=[B_Ct], max_dma_last_dim=4096)
            k.dma("pool", St[:], a["ropeS"][:, :], vlp, writes=[B_St], max_dma_last_dim=4096)
            k.dma("pool", acb[:], a["aconst"][:, :], vlp, writes=[B_acb])
            Rm, Mprev, Mcur = acb[:, 0:128], acb[:, 128:256], acb[:, 256:384]
            onesp = [acb[:, 384:512], acb[:, 512:640]]
            qz = [sb2(f"qz{h}", [128, S], BF16) for h in range(2)]
            kT = sb2("kT", [128, S], BF16)
            B_qz = [[Buf() for _ in range(GPB)] for _ in range(2)]
            B_kT = [Buf() for _ in range(GPB)]
            pc = sb2("pc", [128, 1])
            hm = sb2("hm", [128, 2])
            hmb = sb2("hmb", [128, 2], BF16)
            B_pc, B_hm = Buf(), Buf()
            k.dma("sp", pc[:], a["pcol_in"][:, :], vl, writes=[B_pc])
            k.op("dve", lambda e: e.tensor_scalar(out=hm[:, 0:1], in0=pc[:], scalar1=64.0, scalar2=None, op0=ALU.is_lt),
                 [B_pc], [B_hm])
            k.op("dve", lambda e: e.tensor_scalar(out=hm[:, 1:2], in0=pc[:], scalar1=64.0, scalar2=None, op0=ALU.is_ge),
                 [B_pc], [B_hm])
            k.op("dve", lambda e: e.tensor_copy(out=hmb[:], in_=hm[:]), [B_hm], [B_hm])
            NBK = S // 128
            Vp = sb2("Vp", [128, NBK, 2, 128], BF16)
            B_Vp = [Buf() for _ in range(NBK // 4)]
            k.op("pool", lambda e: e.memset(Vp[:].rearrange("p a h f -> p (a h f)"), 0.0), [], B_Vp)
            Nacc, Dacc = sb2("Nacc", [128, S]), sb2("Dacc", [128, S])
            B_Nacc, B_Dacc = Buf(), Buf()
            wq = [sb2(f"wq{i}", [128, 8, 384], BF16) for i in range(2)]
            B_wq = [Buf(), Buf()]
            wq_sem = [k.dsem(f"a2_{b}_wq0", sw=True), k.dsem(f"a2_{b}_wq1", sw=True)]
            qsb_ = [sb2(f"qsb{i}", [128, 512], BF16) for i in range(2)]
            t1s, t2s = sb2("t1s", [128, 512]), sb2("t2s", [128, 512])
            t1_, t2_ = [t1s, t1s], [t2s, t2s]
            Bt1, Bt2 = Buf(), Buf()
            B_qsb_, B_t1_, B_t2_ = [Buf(), Buf()], [Bt1, Bt1], [Bt2, Bt2]
            PT = [sb2(f"PT{i}", [128, 2, 2, 128], BF16) for i in range(2)]
            B_PT = [Buf(), Buf()]
            psQ_ = [ps2(f"psQ{i}", [128, 512]) for i in range(2)]
            psR0 = ps2("psR0", [128, 512])
            psR_ = [psR0, psR0]
            psV = ps2("psV", [128, 4, 128])
            psS = [ps2(f"psS{i}", [128, 2, 2, 128]) for i in range(2)]
            psND = [ps2(f"psND{i}", [128, 2, 128]) for i in range(2)]
            BpsR = Buf()
            B_psQ_, B_psR_, B_psV = [Buf(), Buf()], [BpsR, BpsR], Buf()
            B_psS, B_psND = [Buf(), Buf()], [Buf(), Buf()]
            pit = 0
            allhT = [B_hT[c][gi] for c in range(8) for gi in range(GPB)]
            it = 0

            def wq_load(hp, g):
                wsl = (hp * 3 + g) % 2
                for qi in range(3):
                    col = g * 1536 + qi * 512 + hp * 128
                    for kc in range(8):
                        k.dma("pool", wq[wsl][:, kc, qi * 128:(qi + 1) * 128],
                              w_in[kc * 128:(kc + 1) * 128, col:col + 128], wq_sem[wsl], writes=[B_wq[wsl]],
                              grp=("wq", hp, g))
            wq_load(0, 0)
            for hp in range(4):
                for g in range(3):
                    dil = DIL[g]
                    nb = S // dil // 128
                    wsl = (hp * 3 + g) % 2
                    nxt = hp * 3 + g + 1
                    if nxt < 12:
                        wq_load(nxt // 3, nxt % 3)
                    def tok(r, n, dil=dil):
                        st = r + n * 128 * dil
                        return slice(st, st + 127 * dil + 1, dil)
                    blks = [(r, n) for r in range(dil) for n in range(nb)]
                    vnext = [0]

                    def emit_v(wsl=wsl, blks=blks, tok=tok, vnext=vnext):
                        bi = vnext[0]
                        vnext[0] += 1
                        r, n = blks[bi]
                        for kc in range(8):
                            k.mm(psV[:, bi % 4, :], hT[:, kc, tok(r, n)], wq[wsl][:, kc, 256:384], kc == 0, kc == 7,
                                 reads=[B_wq[wsl]] + [B_hT[kc][gi] for gi in range(GPB)], writes=[B_psV])
                        if bi % 4 == 3:
                            b4 = bi // 4
                            k.op("dve", lambda e, b4=b4: e.tensor_copy(out=Vp[:, b4 * 4:(b4 + 1) * 4, 0, 0:64],
                                                                      in_=psV[:, :, 0:64]), [B_psV], [B_Vp[b4]])
                            k.op("dve", lambda e, b4=b4: e.tensor_copy(out=Vp[:, b4 * 4:(b4 + 1) * 4, 1, 64:128],
                                                                      in_=psV[:, :, 64:128]), [B_psV], [B_Vp[b4]])
                    vper = -(-len(blks) // (2 * GPB))
                    for qi in range(2 if "qk" not in SKIP else 0):
                        for tg in range(GPB):
                            for _ in range(vper):
                                if vnext[0] < len(blks):
                                    emit_v()
                            pi = pit % 2
                            pit += 1
                            psQ, psR, qsb, t1, t2 = psQ_[pi], psR_[pi], qsb_[pi], t1_[pi], t2_[pi]
                            B_psQ, B_psR, B_qsb, B_t1, B_t2 = B_psQ_[pi], B_psR_[pi], B_qsb_[pi], B_t1_[pi], B_t2_[pi]
                            for kc in range(8):
                                k.mm(psQ[:], wq[wsl][:, kc, qi * 128:(qi + 1) * 128], hT[:, kc, tg * 512:(tg + 1) * 512],
                                     kc == 0, kc == 7, reads=[B_wq[wsl], B_hT[kc][tg]], writes=[B_psQ])
                            k.op("act", lambda e, qsb=qsb, psQ=psQ: e.copy(out=qsb[:], in_=psQ[:]), [B_psQ], [B_qsb])
                            k.mm(psR[:], Rm, qsb[:], True, True, reads=[B_acb, B_qsb], writes=[B_psR])
                            k.op("dve", lambda e, tg=tg, t1=t1, psQ=psQ: e.tensor_tensor(
                                out=t1[:], in0=psQ[:], in1=Ct[:, tg * 512:(tg + 1) * 512], op=ALU.mult),
                                 [B_psQ, B_Ct, B_qsb], [B_t1])
                            k.op("dve", lambda e, tg=tg, t2=t2, psR=psR: e.tensor_tensor(
                                out=t2[:], in0=psR[:], in1=St[:, tg * 512:(tg + 1) * 512], op=ALU.mult),
                                 [B_psR, B_St], [B_t2])
                            if qi == 0:
                                k.op("pool", lambda e, t1=t1, t2=t2, qsb=qsb: e.tensor_tensor(
                                    out=qsb[:], in0=t1[:], in1=t2[:], op=ALU.add), [B_t1, B_t2, B_qsb], [B_qsb])
                                for h in range(2):
                                    k.op("dve", lambda e, h=h, tg=tg, qsb=qsb: e.tensor_scalar(
                                        out=qz[h][:, tg * 512:(tg + 1) * 512], in0=qsb[:], scalar1=hmb[:, h:h + 1],
                                        scalar2=None, op0=ALU.mult), [B_qsb, B_hm], [B_qz[h][tg]])
                            else:
                                k.op("pool", lambda e, tg=tg, t1=t1, t2=t2: e.tensor_tensor(
                                    out=kT[:, tg * 512:(tg + 1) * 512], in0=t1[:], in1=t2[:], op=ALU.add),
                                     [B_t1, B_t2], [B_kT[tg]])
                    while vnext[0] < len(blks):
                        emit_v()
                    ablks = blks if "blocks" not in SKIP else []

                    def chunks_of(bi):
                        r, n = ablks[bi]
                        ch = [(1, tok(r, n), Mcur, bi)]
                        if n > 0:
                            ch.append((0, tok(r, n - 1), Mprev, bi - 1))
                        return ch

                    def emit_scores(bi, si):
                        r, n = ablks[bi]
                        cur = tok(r, n)
                        for h in range(2):
                            for (ci, ktok, Mk, vb) in chunks_of(bi):
                                k.mm(psS[si][:, h, ci, :], kT[:, ktok], qz[h][:, cur], True, False,
                                     reads=B_kT + B_qz[h], writes=[B_psS[si]])
                                k.mm(psS[si][:, h, ci, :], ident_b[:], Mk, False, True,
                                     reads=[B_ident_b, B_acb], writes=[B_psS[si]])

                    if ablks:
                        emit_scores(0, it % 2)
                    for bi, (r, n) in enumerate(ablks):
                        si = it % 2
                        it += 1
                        cur = tok(r, n)
                        chunks = chunks_of(bi)
                        if bi + 1 < len(ablks):
                            emit_scores(bi + 1, it % 2)
                        k.op("act", lambda e, si=si: e.activation(
                            out=PT[si][:].rearrange("p h c q -> p (h c q)"),
                            in_=psS[si][:].rearrange("p h c q -> p (h c q)"), func=AF.Exp, scale=0.125),
                            [B_psS[si]], [B_PT[si]])
                        nmm = 2 * len(chunks)
                        for which in range(2):
                            ii = 0
                            for h in range(2):
                                for (ci, ktok, Mk, vb) in chunks:
                                    lhs = Vp[:, vb, h, :] if which == 0 else onesp[h]
                                    k.mm(psND[si][:, which, :], lhs, PT[si][:, h, ci, :], ii == 0, ii == nmm - 1,
                                         reads=[B_Vp[vb // 4], B_PT[si], B_acb], writes=[B_psND[si]])
                                    ii += 1
                        if g == 0:
                            k.op("dve", lambda e, si=si, cur=cur: e.tensor_copy(out=Nacc[:, cur], in_=psND[si][:, 0, :]),
                                 [B_psND[si]], [B_Nacc])
                            k.op("dve", lambda e, si=si, cur=cur: e.tensor_copy(out=Dacc[:, cur], in_=psND[si][:, 1, :]),
                                 [B_psND[si]], [B_Dacc])
                        else:
                            k.op("dve", lambda e, si=si, cur=cur: e.tensor_tensor(out=Nacc[:, cur], in0=psND[si][:, 0, :],
                                                                                 in1=Nacc[:, cur], op=ALU.add),
                                 [B_psND[si], B_Nacc], [B_Nacc])
                            k.op("dve", lambda e, si=si, cur=cur: e.tensor_tensor(out=Dacc[:, cur], in0=psND[si][:, 1, :],
                                                                                 in1=Dacc[:, cur], op=ALU.add),
                                 [B_psND[si], B_Dacc], [B_Dacc])
                if "norm" in SKIP:
                    continue
                k.op("act", lambda e: e.activation(out=Dacc[:], in_=Dacc[:], func=AF.Ln), [B_Dacc], [B_Dacc])
                k.op("act", lambda e: e.activation(out=Dacc[:], in_=Dacc[:], func=AF.Exp, scale=-1.0),
                     [B_Dacc], [B_Dacc])
                k.op("dve", lambda e, hp=hp: e.tensor_tensor(out=oT[:, hp, :], in0=Nacc[:], in1=Dacc[:], op=ALU.mult),
                     [B_Nacc, B_Dacc], [B_oT[hp]])
            k.s.barrier()
        k.s.scope = f"att{b}_3_out"
        with contextlib.ExitStack() as c3:
            def sb3(name, shape, dt=F32):
                return c3.enter_context(k.nc.sbuf_tensor(f"s_a3_{b}_{name}", list(shape), dt))
            wof = sb3("wof", [128, 4, D])
            wob = sb3("wob", [128, 4, D], BF16)
            g1bc = sb3("g1bc", [128, D])
            B_wof, B_wob, B_g1bc = Buf(), Buf(), Buf()
            k.dma("sp", wof[:], w_out.rearrange("(c p) d -> p c d", p=128), vl, writes=[B_wof])
            k.dma("sp", g1bc[:], modrow[L, b:b + 1, 2 * D:3 * D].partition_broadcast(128), vl,
                  reads=[B_modrow[L]], writes=[B_g1bc])
            for c in range(4):
                k.op("dve", lambda e, c=c: e.tensor_tensor(out=wob[:, c, :], in0=wof[:, c, :], in1=g1bc[:], op=ALU.mult),
                     [B_wof, B_g1bc], [B_wob])
            xt = [sb3(f"xt{i}", [128, 4, D]) for i in range(3)]
            B_xt = [Buf(), Buf(), Buf()]
            xt_ld = [k.dsem(f"a3_{b}_xt_ld{i}") for i in range(3)]
            xt_st = [k.dsem(f"a3_{b}_xt_st{i}") for i in range(3)]
            psY = [c3.enter_context(k.nc.psum_tensor(f"p_a3_{b}_psY{i}", [128, 512], F32)) for i in range(2)]
            B_psY = [Buf(), Buf()]
            def a3_load(gi):
                G = b * GPB + gi
                sl = gi % 3
                k.dma("sp", xt[sl][:], xr[G * 512:(G + 1) * 512, :].rearrange("(j p) d -> p j d", p=128), xt_ld[sl],
                      reads=[B_xr[G]], writes=[B_xt[sl]])
            a3_load(0)
            for gi in range(GPB if "p3" not in SKIP else 0):
                G = b * GPB + gi
                sl = gi % 3
                if gi + 1 < GPB:
                    a3_load(gi + 1)
                for j in range(4):
                    for dh in range(2):
                        yi = (j * 2 + dh) % 2
                        t0 = gi * 512 + j * 128
                        for c in range(4):
                            k.mm(psY[yi][:], oT[:, c, t0:t0 + 128], wob[:, c, dh * 512:(dh + 1) * 512], c == 0, c == 3,
                                 reads=[B_oT[c], B_wob], writes=[B_psY[yi]])
                        k.op("dve", lambda e, j=j, dh=dh, yi=yi, sl=sl: e.tensor_tensor(
                            out=xt[sl][:, j, dh * 512:(dh + 1) * 512], in0=psY[yi][:],
                            in1=xt[sl][:, j, dh * 512:(dh + 1) * 512], op=ALU.add), [B_psY[yi], B_xt[sl]], [B_xt[sl]])
                k.dma("sp", xr[G * 512:(G + 1) * 512, :].rearrange("(j p) d -> p j d", p=128), xt[sl][:], xt_st[sl],
                      reads=[B_xt[sl]], writes=[B_xr[G]])
            k.s.barrier()


def moe_layer(k, L, a):
    NB, S, T = k.NB, k.S, k.T
    NT, NG, GPB = T // 128, T // 512, S // 512
    NBLK = a["NBLK"]
    xr, B_xr, modrow, B_modrow = a["xr"], a["B_xr"], a["modrow"], a["B_modrow"]
    ident_f, B_ident_f, ident_b, B_ident_b = a["ident_f"], a["B_ident_f"], a["ident_b"], a["B_ident_b"]
    hs, xs, ys = a["hs"], a["xs"], a["ys"]
    B_hs = [Buf() for _ in range(NG)]
    pfx = f"m{L}_"

    E1all = k.sbp(pfx + "E1all", [128, NT, 32], F32)
    E2all = k.sbp(pfx + "E2all", [128, NT, 32], F32)
    Oall = k.sbp(pfx + "Oall", [128, NT, 32], BF16)
    Wall = k.sbp(pfx + "Wall", [128, NT, 2], F32)
    B_E1 = [Buf() for _ in range(NT)]
    B_E2 = [Buf() for _ in range(NT)]
    B_O = [Buf() for _ in range(NT)]
    B_W = [Buf() for _ in range(NT)]
    g2bc = [k.sbp(pfx + f"g2bc{b}", [128, D], F32) for b in range(NB)]
    B_g2bc = [Buf() for _ in range(NB)]
    d1i = k.sbp(pfx + "d1i", [128, NT], I32)
    d2i = k.sbp(pfx + "d2i", [128, NT], I32)
    bei = k.sbp(pfx + "bei", [128, 2, NBLK], I32)
    B_d1i, B_d2i, B_bei = Buf(), Buf(), Buf()
    nusedi = k.sbp(pfx + "nusedi", [128, 1], I32)
    B_nusedi = Buf()
    vl = k.dsem(pfx + "vl")
    for b in range(NB):
        k.dma("sp", g2bc[b][:], modrow[L, b:b + 1, 5 * D:6 * D].partition_broadcast(128), vl,
              reads=[B_modrow[L]], writes=[B_g2bc[b]], grp="g2")

    k.s.scope = pfx + "M1_router"
    with contextlib.ExitStack() as c1:
        def sb1(name, shape, dt=F32):
            return c1.enter_context(k.nc.sbuf_tensor("s_" + pfx + name, list(shape), dt))

        def ps1(name, shape, dt=F32):
            return c1.enter_context(k.nc.psum_tensor("p_" + pfx + name, list(shape), dt))
        A2bc = [sb1(f"A2bc{b}", [128, D]) for b in range(NB)]
        sh2bc = [sb1(f"sh2bc{b}", [128, D]) for b in range(NB)]
        gfbc = sb1("gfbc", [128, D])
        B_A2bc = [Buf() for _ in range(NB)]
        B_sh2bc = [Buf() for _ in range(NB)]
        B_gfbc = Buf()
        Wr = sb1("Wr", [128, 8, 36])
        brbc = sb1("brbc", [128, 36])
        B_Wr, B_brbc = Buf(), Buf()
        k.dma("sp", gfbc[:], a["gffn"][L:L + 1, :].partition_broadcast(128), vl, writes=[B_gfbc], grp="g2")
        k.dma("sp", Wr[:], a["Wr_in"][L].rearrange("(kc p) f -> p kc f", p=128), vl, writes=[B_Wr], grp="g2")
        k.dma("sp", brbc[:], a["br_in"][L:L + 1, :].partition_broadcast(128), vl, writes=[B_brbc], grp="g2")
        for b in range(NB):
            k.dma("sp", sh2bc[b][:], modrow[L, b:b + 1, 3 * D:4 * D].partition_broadcast(128), vl,
                  reads=[B_modrow[L]], writes=[B_sh2bc[b]], grp="g2")
            k.dma("sp", A2bc[b][:], modrow[L, b:b + 1, 4 * D:5 * D].partition_broadcast(128), vl,
                  reads=[B_modrow[L]], writes=[B_A2bc[b]], grp="g2")
            k.op("dve", lambda e, b=b: e.scalar_tensor_tensor(out=A2bc[b][:], in0=A2bc[b][:], scalar=1.0, in1=gfbc[:],
                                                             op0=ALU.add, op1=ALU.mult),
                 [B_A2bc[b], B_gfbc], [B_A2bc[b]])
        xt = [sb1(f"xt{i}", [128, 4, D]) for i in range(2)]
        B_xt = [Buf(), Buf()]
        xt_ld = [k.dsem(pfx + "xt_ld0"), k.dsem(pfx + "xt_ld1")]
        junk = sb1("junk", [128, D], BF16)
        B_junk = Buf()
        ss, rstd = sb1("ss", [128, 4]), sb1("rstd", [128, 4])
        B_ss, B_rstd = Buf(), Buf()
        h2_ = [sb1(f"h2_{i}", [128, 4, D]) for i in range(2)]
        B_h2_ = [[Buf() for _ in range(4)] for _ in range(2)]
        h2b = [sb1(f"h2b{i}", [128, 4, D], BF16) for i in range(2)]
        B_h2b = [Buf(), Buf()]
        h2b_st = [k.dsem(pfx + "h2b_st0"), k.dsem(pfx + "h2b_st1")]
        pTf = [ps1(f"pTf{i}", [128, 512]) for i in range(2)]
        B_pTf = [Buf(), Buf()]
        hT2 = sb1("hT2", [128, 8, 512])
        B_hT2 = [Buf() for _ in range(8)]
        pslg = ps1("pslg", [128, 4, 36])
        B_pslg = Buf()
        lg4 = sb1("lg4", [128, 4, 36])
        gmax, gsum = sb1("gmax", [128, 4]), sb1("gsum", [128, 4])
        gsh = sb1("gsh", [128, 4, 4])
        ohg4 = sb1("ohg4", [128, 4, 4])
        tmp4 = sb1("tmp4", [128, 4, 4, 8])
        sel4, sel4b = sb1("sel4", [128, 4, 8]), sb1("sel4b", [128, 4, 8])
        oh1_4, oh2_4 = sb1("oh1_4", [128, 4, 8]), sb1("oh2_4", [128, 4, 8])
        m1, m2, dlt = sb1("m1", [128, 4]), sb1("m2", [128, 4]), sb1("dlt", [128, 4])
        B_lg, B_sm, B_sm2, B_ohg, B_ge, B_sel, B_selb, B_oh1, B_oh2, B_tmp4, B_m1, B_m2, B_dlt = (Buf() for _ in range(13))

        def load_x(G):
            sl = G % 2
            k.dma("sp", xt[sl][:], xr[G * 512:(G + 1) * 512, :].rearrange("(j p) d -> p j d", p=128),
                  xt_ld[sl], reads=[B_xr[G]], writes=[B_xt[sl]])
        def stage1(G):
            b = G // GPB
            sl = G % 2
            xt_t, B_x = xt[sl], B_xt[sl]
            h2, B_h2 = h2_[sl], B_h2_[sl]
            for j in range(4):
                k.op("act", lambda e, j=j, xt_t=xt_t: e.activation(out=junk[:], in_=xt_t[:, j, :], func=AF.Square,
                                                                  accum_out=ss[:, j:j + 1]), [B_x], [B_junk, B_ss])
            k.op("dve", lambda e: e.tensor_scalar(out=rstd[:], in0=ss[:], scalar1=1.0 / D, scalar2=1e-6,
                                                  op0=ALU.mult, op1=ALU.add), [B_ss], [B_rstd])
            k.op("act", lambda e: e.activation(out=rstd[:], in_=rstd[:], func=AF.Sqrt), [B_rstd], [B_rstd])
            k.op("dve", lambda e: e.reciprocal(out=rstd[:], in_=rstd[:]), [B_rstd], [B_rstd])
            for j in range(4):
                k.op("dve", lambda e, j=j, xt_t=xt_t, b=b, h2=h2: e.scalar_tensor_tensor(
                    out=h2[:, j, :], in0=xt_t[:, j, :], scalar=rstd[:, j:j + 1], in1=A2bc[b][:],
                    op0=ALU.mult, op1=ALU.mult), [B_x, B_rstd, B_A2bc[b]], [B_h2[j]])
                k.op("pool" if j > 0 else "dve", lambda e, j=j, b=b, h2=h2: e.tensor_tensor(
                    out=h2[:, j, :], in0=h2[:, j, :], in1=sh2bc[b][:], op=ALU.add),
                     [B_h2[j], B_sh2bc[b]], [B_h2[j]])

        def stage1b(G):
            sl = G % 2
            h2, B_h2 = h2_[sl], B_h2_[sl]
            for j in range(4):
                k.op("act", lambda e, j=j, sl=sl, h2=h2: e.copy(out=h2b[sl][:, j, :], in_=h2[:, j, :]),
                     [B_h2[j]], [B_h2b[sl]])
            k.dma("sp", hs[G * 512:(G + 1) * 512, :].rearrange("(j p) d -> p j d", p=128), h2b[sl][:], h2b_st[sl],
                  reads=[B_h2b[sl]], writes=[B_hs[G]])

        load_x(0)
        if NG > 1:
            load_x(1)
        stage1(0)
        stage1b(0)
        for G in range(NG):
            b = G // GPB
            sl = G % 2
            h2, B_h2 = h2_[sl], B_h2_[sl]
            for c in range(8):
                p = c % 2
                for j in range(4):
                    k.tr(pTf[p][:, j * 128:(j + 1) * 128], h2[:, j, c * 128:(c + 1) * 128], ident_f[:],
                         reads=[B_h2[j], B_ident_f], writes=[B_pTf[p]])
                if c % 4 != 3:
                    k.op("act", lambda e, c=c, p=p: e.copy(out=hT2[:, c, :], in_=pTf[p][:]), [B_pTf[p]], [B_hT2[c]])
                else:
                    k.op("dve", lambda e, c=c, p=p: e.tensor_copy(out=hT2[:, c, :], in_=pTf[p][:]),
                         [B_pTf[p]], [B_hT2[c]])
            for j in range(4):
                for kc in range(8):
                    k.mm(pslg[:, j, :], hT2[:, kc, j * 128:(j + 1) * 128], Wr[:, kc, :], kc == 0, kc == 7,
                         reads=[B_hT2[kc], B_Wr], writes=[B_pslg])
            if G + 1 < NG:
                stage1(G + 1)
            if G + 2 < NG:
                load_x(G + 2)
            V = "dve"
            i0_ = G * 4
            J = 4

            def bc(ap, shape):
                return ap.to_broadcast(list(shape))
            k.op(V, lambda e: e.tensor_tensor(out=lg4[:], in0=pslg[:], in1=bc(brbc[:].unsqueeze(1), [128, J, 36]),
                                              op=ALU.add), [B_pslg, B_brbc], [B_lg])
            k.op(V, lambda e: e.tensor_reduce(out=gmax[:], in_=lg4[:, :, 0:4], axis=AX.X, op=ALU.max), [B_lg], [B_sm])
            k.op(V, lambda e: e.tensor_tensor(out=gsh[:], in0=lg4[:, :, 0:4], in1=bc(gmax[:].unsqueeze(2), [128, J, 4]),
                                              op=ALU.subtract), [B_lg, B_sm], [B_ge])
            k.op(V, lambda e: e.tensor_tensor(out=ohg4[:], in0=lg4[:, :, 0:4], in1=bc(gmax[:].unsqueeze(2), [128, J, 4]),
                                              op=ALU.is_equal), [B_lg, B_sm], [B_ohg])
            k.op("act", lambda e: e.activation(out=gsh[:], in_=gsh[:], func=AF.Exp), [B_ge], [B_ge])
            k.op(V, lambda e: e.tensor_reduce(out=gsum[:], in_=gsh[:], axis=AX.X, op=ALU.add), [B_ge], [B_sm2])
            k.op(V, lambda e: e.reciprocal(out=gsum[:], in_=gsum[:]), [B_sm2], [B_sm2])
            k.op(V, lambda e: e.tensor_tensor(out=tmp4[:], in0=lg4[:, :, 4:36].rearrange("p j (g e) -> p j g e", g=4),
                                              in1=bc(ohg4[:].unsqueeze(3), [128, J, 4, 8]), op=ALU.mult),
                 [B_lg, B_ohg], [B_tmp4])
            k.op(V, lambda e: e.tensor_reduce(out=sel4[:], in_=tmp4[:].rearrange("p j g e -> p j e g"), axis=AX.X,
                                              op=ALU.add), [B_tmp4], [B_sel])
            k.op(V, lambda e: e.tensor_reduce(out=m1[:], in_=sel4[:], axis=AX.X, op=ALU.max), [B_sel], [B_m1])
            k.op(V, lambda e: e.tensor_tensor(out=oh1_4[:], in0=sel4[:], in1=bc(m1[:].unsqueeze(2), [128, J, 8]),
                                              op=ALU.is_equal), [B_sel, B_m1], [B_oh1])
            k.op(V, lambda e: e.scalar_tensor_tensor(out=sel4b[:], in0=oh1_4[:], scalar=-1.0e30, in1=sel4[:],
                                                     op0=ALU.mult, op1=ALU.add), [B_oh1, B_sel], [B_selb])
            k.op(V, lambda e: e.tensor_reduce(out=m2[:], in_=sel4b[:], axis=AX.X, op=ALU.max), [B_selb], [B_m2])
            k.op(V, lambda e: e.tensor_tensor(out=oh2_4[:], in0=sel4b[:], in1=bc(m2[:].unsqueeze(2), [128, J, 8]),
                                              op=ALU.is_equal), [B_selb, B_m2], [B_oh2])
            k.op(V, lambda e: e.tensor_tensor(out=dlt[:], in0=m2[:], in1=m1[:], op=ALU.subtract), [B_m1, B_m2], [B_dlt])
            k.op("act", lambda e: e.activation(out=dlt[:], in_=dlt[:], func=AF.Exp), [B_dlt], [B_dlt])
            k.op(V, lambda e: e.tensor_scalar(out=dlt[:], in0=dlt[:], scalar1=1.0, scalar2=None, op0=ALU.add),
                 [B_dlt], [B_dlt])
            k.op(V, lambda e: e.reciprocal(out=dlt[:], in_=dlt[:]), [B_dlt], [B_dlt])
            Bws = B_W[i0_:i0_ + J]
            k.op(V, lambda e, i0_=i0_: e.tensor_tensor(out=Wall[:, i0_:i0_ + J, 0], in0=gsum[:], in1=dlt[:], op=ALU.mult),
                 [B_sm2, B_dlt], Bws)
            k.op(V, lambda e, i0_=i0_: e.tensor_tensor(out=Wall[:, i0_:i0_ + J, 1], in0=gsum[:],
                                                      in1=Wall[:, i0_:i0_ + J, 0], op=ALU.subtract),
                 [B_sm2] + Bws, Bws)
            for (Eall, oh, B_oh, B_E) in ((E1all, oh1_4, B_oh1, B_E1), (E2all, oh2_4, B_oh2, B_E2)):
                k.op(V, lambda e, Eall=Eall, oh=oh, i0_=i0_: e.tensor_tensor(
                    out=Eall[:, i0_:i0_ + J, :].rearrange("p j (g e) -> p j g e", g=4),
                    in0=bc(ohg4[:].unsqueeze(3), [128, J, 4, 8]), in1=bc(oh[:].unsqueeze(2), [128, J, 4, 8]),
                    op=ALU.mult), [B_ohg, B_oh], B_E[i0_:i0_ + J])
            k.op("pool", lambda e, i0_=i0_: e.tensor_tensor(out=Oall[:, i0_:i0_ + J, :], in0=E1all[:, i0_:i0_ + J, :],
                                                           in1=E2all[:, i0_:i0_ + J, :], op=ALU.add),
                 B_E1[i0_:i0_ + J] + B_E2[i0_:i0_ + J], B_O[i0_:i0_ + J])
            if G + 1 < NG:
                stage1b(G + 1)
        k.s.barrier()
    dbgsem = k.dsem(pfx + "dbg")
    B_dbg = Buf()
    if a.get("stage") == "M1":
        dbg = a["dbg"]
        k.dma("sp", dbg[:, 0:NT * 2], Wall[:].rearrange("p i w -> p (i w)"), dbgsem, reads=B_W, writes=[B_dbg])
        k.dma("sp", dbg[:, 1024:1024 + NT * 32], E1all[:].rearrange("p i e -> p (i e)"), dbgsem, reads=B_E1, writes=[B_dbg])
        k.dma("sp", dbg[:, 2048:2048 + NT * 32], E2all[:].rearrange("p i e -> p (i e)"), dbgsem, reads=B_E2, writes=[B_dbg])
        return [B_dbg]

    k.s.scope = pfx + "M2_index"
    with contextlib.ExitStack() as c2:
        def sb2(name, shape, dt=F32):
            return c2.enter_context(k.nc.sbuf_tensor("s_" + pfx + name, list(shape), dt))

        def ps2(name, shape, dt=F32):
            return c2.enter_context(k.nc.psum_tensor("p_" + pfx + name, list(shape), dt))
        trif, trib, onesb = sb2("trif", [128, 128]), sb2("trib", [128, 128], BF16), sb2("onesb", [128, 128], BF16)
        B_trif, B_trib, B_onesb = Buf(), Buf(), Buf()
        k.dma("sp", trif[:], a["tri_in"][:, :], vl, writes=[B_trif])
        k.op("dve", lambda e: e.tensor_copy(out=trib[:], in_=trif[:]), [B_trif], [B_trib])
        k.op("dve", lambda e: e.memset(onesb[:], 1.0), [], [B_onesb])
        base = sb2("base", [128, NT, 32])
        tot = sb2("tot", [128, NT, 32])
        B_base, B_tot = Buf(), Buf()
        psw = [ps2(f"psw{i}", [128, 512]) for i in range(2)]
        B_psw = [Buf(), Buf()]
        TPC = 16
        nch = (NT + TPC - 1) // TPC
        for ci in range(nch):
            t0, t1 = ci * TPC, min(NT, (ci + 1) * TPC)
            w = (t1 - t0) * 32
            for which, (lhs, B_l, dst, B_d) in enumerate(((trib, B_trib, base, B_base), (onesb, B_onesb, tot, B_tot))):
                k.mm(psw[which][:, 0:w], lhs[:], Oall[:, t0:t1, :], True, True,
                     reads=[B_l] + B_O[t0:t1], writes=[B_psw[which]])
                k.op("dve", lambda e, which=which, dst=dst, t0=t0, t1=t1, w=w: e.tensor_copy(
                    out=dst[:, t0:t1, :], in_=psw[which][:, 0:w]), [B_psw[which]], [B_d])
        cnt = sb2("cnt", [128, 32])
        nblk = sb2("nblk", [128, 32])
        cum = sb2("cum", [128, 32])
        carry = sb2("carry", [128, 32])
        tmp32 = sb2("tmp32", [128, 32])
        B_cnt, B_nblk, B_cum, B_carry, B_tmp32 = Buf(), Buf(), Buf(), Buf(), Buf()
        V = "dve"
        k.op(V, lambda e: e.tensor_reduce(out=cnt[:], in_=tot[:].rearrange("p i e -> p e i"), axis=AX.X, op=ALU.add),
             [B_tot], [B_cnt])
        k.op(V, lambda e: e.tensor_scalar(out=nblk[:], in0=cnt[:], scalar1=0.0, scalar2=None, op0=ALU.is_gt),
             [B_cnt], [B_nblk])
        for j in range(1, (2 * T) // 512 + 1):
            k.op(V, lambda e, j=j: e.scalar_tensor_tensor(out=nblk[:], in0=cnt[:], scalar=512.0 * j, in1=nblk[:],
                                                         op0=ALU.is_gt, op1=ALU.add), [B_cnt, B_nblk], [B_nblk])
        k.op(V, lambda e: e.tensor_copy(out=cum[:], in_=nblk[:]), [B_nblk], [B_cum])
        for ee in range(1, 32):
            k.op(V, lambda e, ee=ee: e.tensor_tensor(out=cum[:, ee:ee + 1], in0=cum[:, ee - 1:ee], in1=nblk[:, ee:ee + 1],
                                                    op=ALU.add), [B_cum, B_nblk], [B_cum])
        k.op(V, lambda e: e.tensor_tensor(out=carry[:], in0=cum[:], in1=nblk[:], op=ALU.subtract),
             [B_cum, B_nblk], [B_carry])
        k.op(V, lambda e: e.tensor_scalar(out=carry[:], in0=carry[:], scalar1=512.0, scalar2=None, op0=ALU.mult),
             [B_carry], [B_carry])
        for i in range(NT):
            k.op(V, lambda e, i=i: e.tensor_tensor(out=base[:, i, :], in0=base[:, i, :], in1=carry[:], op=ALU.add),
                 [B_base, B_carry], [B_base])
            if i + 1 < NT:
                k.op(V, lambda e, i=i: e.tensor_tensor(out=carry[:], in0=carry[:], in1=tot[:, i, :], op=ALU.add),
                     [B_carry, B_tot], [B_carry])
        dtmp = sb2("dtmp", [128, NT, 32])
        dfl = sb2("dfl", [128, NT])
        B_dtmp, B_dfl = Buf(), Buf()
        for (Eall, B_E, di, B_di) in ((E1all, B_E1, d1i, B_d1i), (E2all, B_E2, d2i, B_d2i)):
            k.op(V, lambda e, Eall=Eall: e.tensor_tensor(out=dtmp[:], in0=Eall[:], in1=base[:], op=ALU.mult),
                 list(B_E) + [B_base], [B_dtmp])
            k.op(V, lambda e: e.tensor_reduce(out=dfl[:], in_=dtmp[:], axis=AX.X, op=ALU.add), [B_dtmp], [B_dfl])
            k.op(V, lambda e, di=di: e.tensor_copy(out=di[:], in_=dfl[:]), [B_dfl], [B_di])
        bef = sb2("bef", [128, NBLK])
        B_bef = Buf()
        for j in range(NBLK):
            k.op(V, lambda e, j=j: e.tensor_scalar(out=tmp32[:], in0=cum[:], scalar1=float(j), scalar2=None,
                                                  op0=ALU.is_le, op1=ALU.add, accum_out=bef[:, j:j + 1]),
                 [B_cum], [B_tmp32, B_bef])
        oob = sb2("oob", [128, NBLK])
        B_oob = Buf()
        k.op(V, lambda e: e.tensor_scalar(out=oob[:], in0=bef[:], scalar1=32.0, scalar2=float(OOB_BIG),
                                          op0=ALU.is_ge, op1=ALU.mult), [B_bef], [B_oob])
        k.op(V, lambda e: e.tensor_scalar(out=bef[:], in0=bef[:], scalar1=31.0, scalar2=None, op0=ALU.min),
             [B_bef], [B_bef])
        k.op(V, lambda e: e.tensor_copy(out=nusedi[:], in_=cum[:, 31:32]), [B_cum], [B_nusedi])
        pcol = sb2("pcol", [128, 1])
        B_pcol = Buf()
        k.dma("sp", pcol[:], a["pcol_in"][:, :], vl, writes=[B_pcol])
        k.op(V, lambda e: e.tensor_scalar(out=bef[:], in0=bef[:], scalar1=float(L * 32), scalar2=128.0,
                                          op0=ALU.add, op1=ALU.mult), [B_bef], [B_bef])
        k.op(V, lambda e: e.tensor_scalar(out=bef[:], in0=bef[:], scalar1=pcol[:, 0:1], scalar2=None, op0=ALU.add),
             [B_bef, B_pcol], [B_bef])
        k.op(V, lambda e: e.tensor_scalar(out=bef[:], in0=bef[:], scalar1=2.0, scalar2=None, op0=ALU.mult),
             [B_bef], [B_bef])
        k.op(V, lambda e: e.tensor_tensor(out=bef[:], in0=bef[:], in1=oob[:], op=ALU.add), [B_bef, B_oob], [B_bef])
        k.op(V, lambda e: e.tensor_copy(out=bei[:, 0, :], in_=bef[:]), [B_bef], [B_bei])
        k.op(V, lambda e: e.tensor_scalar(out=bef[:], in0=bef[:], scalar1=1.0, scalar2=None, op0=ALU.add),
             [B_bef], [B_bef])
        k.op(V, lambda e: e.tensor_copy(out=bei[:, 1, :], in_=bef[:]), [B_bef], [B_bei])
        k.s.barrier()
        if a.get("stage") == "M2":
            dbg = a["dbg"].bitcast(I32)
            k.dma("sp", dbg[:, 0:NT], d1i[:], dbgsem, reads=[B_d1i], writes=[B_dbg])
            k.dma("sp", dbg[:, 1024:1024 + NT], d2i[:], dbgsem, reads=[B_d2i], writes=[B_dbg])
            k.dma("sp", dbg[:, 2048:2048 + NBLK], bei[:, 0, :], dbgsem, reads=[B_bei], writes=[B_dbg])
            k.s.barrier()
    if a.get("stage") == "M2":
        return [B_dbg]

    k.s.scope = pfx + "M3_scatter"
    with contextlib.ExitStack() as c3:
        NS3 = 6
        hsb = [c3.enter_context(k.nc.sbuf_tensor(f"s_{pfx}hsb{i}", [128, D], BF16)) for i in range(NS3)]
        B_hsb = [Buf() for _ in range(NS3)]
        hsb_ld = [k.dsem(pfx + f"hsb_ld{i}") for i in range(NS3)]
        sc_sem = [k.dsem(pfx + f"sc{i}", sw=True) for i in range(NS3)]
        B_xs = Buf()

        def m3_load(i):
            sl = i % NS3
            k.dma("sp", hsb[sl][:], hs[i * 128:(i + 1) * 128, :], hsb_ld[sl], reads=[B_hs[i // 4]], writes=[B_hsb[sl]])
        for i in range(min(NS3 - 1, NT)):
            m3_load(i)
        for i in range(NT):
            sl = i % NS3
            if i + NS3 - 1 < NT:
                m3_load(i + NS3 - 1)
            for (di, B_di) in ((d1i, B_d1i), (d2i, B_d2i)):
                k.s.add("pool", lambda e, sl=sl, di=di, i=i: e.indirect_dma_start(
                    out=xs[:, :], out_offset=bass.IndirectOffsetOnAxis(ap=di[:, i:i + 1], axis=0),
                    in_=hsb[sl][:], in_offset=None), [B_hsb[sl], B_di], [B_xs], dsem=sc_sem[sl], grp=("sc", i))
        k.s.barrier()
    if a.get("stage") == "M3":
        return []

    k.s.scope = pfx + "M4_experts"
    with contextlib.ExitStack() as c4:
        def sb4(name, shape, dt=F32):
            return c4.enter_context(k.nc.sbuf_tensor("s_" + pfx + name, list(shape), dt))

        def ps4(name, shape, dt=F32):
            return c4.enter_context(k.nc.psum_tensor("p_" + pfx + name, list(shape), dt))
        wg = [sb4(f"wg{i}", [128, 8, 512], BF16) for i in range(2)]
        wu = [sb4(f"wu{i}", [128, 8, 512], BF16) for i in range(2)]
        wd = [sb4(f"wd{i}", [128, 4, D], BF16) for i in range(2)]
        B_wg, B_wu, B_wd = [Buf(), Buf()], [Buf(), Buf()], [Buf(), Buf()]
        wsem = [[k.dsem(pfx + f"w{n}{i}", sw=True) for i in range(2)] for n in "gud"]
        xb = [sb4(f"xb{i}", [128, 4, D], BF16) for i in range(2)]
        B_xb = [Buf(), Buf()]
        xb_ld = [k.dsem(pfx + "xb_ld0"), k.dsem(pfx + "xb_ld1")]
        xsT_ = [sb4(f"xsT{i}", [128, 8, 512], BF16) for i in range(2)]
        B_xsT_ = [[Buf() for _ in range(8)] for _ in range(2)]
        pTx = [ps4(f"pTx{i}", [128, 512], BF16) for i in range(2)]
        B_pTx = [Buf(), Buf()]
        psG = [ps4(f"psG{i}", [128, 512]) for i in range(2)]
        psU = [ps4(f"psU{i}", [128, 512]) for i in range(2)]
        B_psG, B_psU = [Buf(), Buf()], [Buf(), Buf()]
        sg = [sb4(f"sg{i}", [128, 512]) for i in range(2)]
        B_sg = [Buf(), Buf()]
        hTe = sb4("hTe", [128, 4, 512], BF16)
        B_hTe = [Buf() for _ in range(4)]
        psY = [ps4(f"psY{i}", [128, 512]) for i in range(2)]
        B_psY = [Buf(), Buf()]
        yb = [sb4(f"yb{i}", [128, 4, D]) for i in range(2)]
        B_yb = [Buf(), Buf()]
        yb_st = [k.dsem(pfx + "yb_st0"), k.dsem(pfx + "yb_st1")]
        B_ys = Buf()
        gw, uw, dw = a["gw"], a["uw"], a["dw"]
        gw2 = gw.rearrange("r (h f) -> (r h) f", h=2)
        uw2 = uw.rearrange("r (h f) -> (r h) f", h=2)
        dw2 = dw.rearrange("r (h f) -> (r h) f", h=2)

        breg = k.bound_reg()
        k.s.add("pool", lambda e: e.reg_mov(breg, 2 * 2 * 32 * 128 - 1), [], [])

        def wload(j, sl):
            for (dst, B_d, tab, ws, nh) in ((wg[sl], B_wg[sl], gw2, wsem[0][sl], 4), (wu[sl], B_wu[sl], uw2, wsem[1][sl], 4),
                                            (wd[sl], B_wd[sl], dw2, wsem[2][sl], 2)):
                for h in range(2):
                    k.s.add("pool", lambda e, dst=dst, tab=tab, j=j, h=h, nh=nh: e.indirect_dma_start(
                        out=dst[:, h * nh:(h + 1) * nh, :].rearrange("p a f -> p (a f)"), out_offset=None, in_=tab,
                        in_offset=bass.IndirectOffsetOnAxis(ap=bei[:, h, j:j + 1], axis=0),
                        bounds_check=breg, oob_is_err=False), [B_bei], [B_d], dsem=ws,
                        grp=("w", j))

        def xload(j, sl):
            k.dma("sp", xb[sl][:], xs[j * 512:(j + 1) * 512, :].rearrange("(t p) d -> p t d", p=128), xb_ld[sl],
                  reads=[B_xs], writes=[B_xb[sl]])
        def xpose(j, cs):
            sl = j % 2
            for c in cs:
                p = c % 2
                for t in range(4):
                    k.tr(pTx[p][:, t * 128:(t + 1) * 128], xb[sl][:, t, c * 128:(c + 1) * 128], ident_b[:],
                         reads=[B_xb[sl], B_ident_b], writes=[B_pTx[p]])
                if c % 2 == 0:
                    k.op("act", lambda e, c=c, p=p, sl=sl: e.copy(out=xsT_[sl][:, c, :], in_=pTx[p][:]),
                         [B_pTx[p]], [B_xsT_[sl][c]])
                else:
                    k.op("dve", lambda e, c=c, p=p, sl=sl: e.tensor_copy(out=xsT_[sl][:, c, :], in_=pTx[p][:]),
                         [B_pTx[p]], [B_xsT_[sl][c]])
        regs = k.nused_regs()
        for eng_ in ENGS:
            k.s.add(eng_, lambda e, eng_=eng_: e.reg_load(regs[eng_], nusedi[0:1, 0:1]), [B_nusedi], [])
        JS = (2 * T) // 512
        wload(0, 0)
        xload(0, 0)
        if NBLK > 1:
            wload(1, 1)
            xload(1, 1)
        xpose(0, range(8))
        for j in range(NBLK):
            sl = j % 2
            k.s.guard = (regs, (j if not os.environ.get("DYN_NEVER") else -1)) if (j >= max(JS, int(os.environ.get("DYN_FROM", "0"))) and DYN_SKIP) else None
            xsT, B_xsT = xsT_[sl], B_xsT_[sl]
            for fc in range(4):
                q = fc % 2
                for kc in range(8):
                    k.mm(psG[q][:], wg[sl][:, kc, fc * 128:(fc + 1) * 128], xsT[:, kc, :], kc == 0, kc == 7,
                         reads=[B_wg[sl], B_xsT[kc]], writes=[B_psG[q]])
                for kc in range(8):
                    k.mm(psU[q][:], wu[sl][:, kc, fc * 128:(fc + 1) * 128], xsT[:, kc, :], kc == 0, kc == 7,
                         reads=[B_wu[sl], B_xsT[kc]], writes=[B_psU[q]])
                k.op("act", lambda e, q=q: e.activation(out=sg[q][:], in_=psG[q][:], func=AF.Silu),
                     [B_psG[q]], [B_sg[q]])
                k.op("dve", lambda e, q=q, fc=fc: e.tensor_tensor(out=hTe[:, fc, :], in0=sg[q][:], in1=psU[q][:],
                                                                 op=ALU.mult), [B_sg[q], B_psU[q]], [B_hTe[fc]])
                if j + 1 < NBLK:
                    xpose(j + 1, [2 * fc, 2 * fc + 1])
            for t in range(4):
                for dh in range(2):
                    yi = (t * 2 + dh) % 2
                    for fc in range(4):
                        k.mm(psY[yi][:], hTe[:, fc, t * 128:(t + 1) * 128], wd[sl][:, fc, dh * 512:(dh + 1) * 512],
                             fc == 0, fc == 3, reads=[B_hTe[fc], B_wd[sl]], writes=[B_psY[yi]])
                    if yi == 0:
                        k.op("act", lambda e, t=t, dh=dh, sl=sl: e.copy(out=yb[sl][:, t, dh * 512:(dh + 1) * 512],
                                                                       in_=psY[0][:]), [B_psY[0]], [B_yb[sl]])
                    else:
                        k.op("dve", lambda e, t=t, dh=dh, sl=sl: e.tensor_copy(
                            out=yb[sl][:, t, dh * 512:(dh + 1) * 512], in_=psY[1][:]), [B_psY[1]], [B_yb[sl]])
            if j + 2 < NBLK:
                wload(j + 2, sl)
                xload(j + 2, sl)
            k.dma("sp", ys[j * 512:(j + 1) * 512, :].rearrange("(t p) d -> p t d", p=128), yb[sl][:], yb_st[sl],
                  reads=[B_yb[sl]], writes=[B_ys])
        k.s.guard = None
        k.s.barrier()
    if a.get("stage") == "M4":
        return []

    k.s.scope = pfx + "M5_combine"
    fin = a.get("final")
    with contextlib.ExitStack() as c5:
        def sb5(name, shape, dt=F32):
            return c5.enter_context(k.nc.sbuf_tensor("s_" + pfx + name, list(shape), dt))
        NS = 6
        y1 = [sb5(f"y1_{i}", [128, D]) for i in range(NS)]
        y2 = [sb5(f"y2_{i}", [128, D]) for i in range(NS)]
        xc = [sb5(f"xc{i}", [128, D]) for i in range(NS)]
        B_y1, B_y2, B_xc = [Buf() for _ in range(NS)], [Buf() for _ in range(NS)], [Buf() for _ in range(NS)]
        g_sem = [[k.dsem(pfx + f"g{n}{i}", sw=True) for i in range(NS)] for n in "12"]
        xc_ld = [k.dsem(pfx + f"xc_ld{i}") for i in range(NS)]
        xc_st = [k.dsem(pfx + f"xc_st{i}") for i in range(NS)]
        if fin is not None:
            fg = sb5("fg", [128, D])
            fjunk = sb5("fjunk", [128, D], BF16)
            fss = [sb5(f"fss{i}", [128, 1]) for i in range(NS)]
            B_fg, B_fjunk = Buf(), Buf()
            B_fss = [Buf() for _ in range(NS)]
            k.dma("sp", fg[:], fin["fing"][0:1, :].partition_broadcast(128), vl, writes=[B_fg])
        def m5_load(i):
            sl = i % NS
            G = i // 4
            k.s.add("pool", lambda e, sl=sl, i=i: e.indirect_dma_start(
                out=y1[sl][:], out_offset=None, in_=ys[:, :],
                in_offset=bass.IndirectOffsetOnAxis(ap=d1i[:, i:i + 1], axis=0)), [B_ys, B_d1i], [B_y1[sl]],
                dsem=g_sem[0][sl])
            k.s.add("pool", lambda e, sl=sl, i=i: e.indirect_dma_start(
                out=y2[sl][:], out_offset=None, in_=ys[:, :],
                in_offset=bass.IndirectOffsetOnAxis(ap=d2i[:, i:i + 1], axis=0)), [B_ys, B_d2i], [B_y2[sl]],
                dsem=g_sem[1][sl])
            k.dma("sp", xc[sl][:], xr[i * 128:(i + 1) * 128, :], xc_ld[sl], reads=[B_xr[G]], writes=[B_xc[sl]])
        for i in range(min(NS - 1, NT)):
            m5_load(i)
        for i in range(NT):
            sl = i % NS
            b = (i * 128) // S
            G = i // 4
            if i + NS - 1 < NT:
                m5_load(i + NS - 1)
            k.op("act", lambda e, sl=sl, i=i: e.activation(out=y1[sl][:], in_=y1[sl][:], func=AF.Copy,
                                                          scale=Wall[:, i, 0:1]), [B_y1[sl], B_W[i]], [B_y1[sl]])
            k.op("dve", lambda e, sl=sl, i=i: e.scalar_tensor_tensor(out=y1[sl][:], in0=y2[sl][:],
                                                                    scalar=Wall[:, i, 1:2], in1=y1[sl][:],
                                                                    op0=ALU.mult, op1=ALU.add),
                 [B_y1[sl], B_y2[sl], B_W[i]], [B_y1[sl]])
            k.op("dve", lambda e, sl=sl, b=b: e.tensor_tensor(out=y1[sl][:], in0=y1[sl][:], in1=g2bc[b][:],
                                                             op=ALU.mult), [B_y1[sl], B_g2bc[b]], [B_y1[sl]])
            k.op("dve", lambda e, sl=sl: e.tensor_tensor(out=xc[sl][:], in0=xc[sl][:], in1=y1[sl][:], op=ALU.add),
                 [B_xc[sl], B_y1[sl]], [B_xc[sl]])
            if fin is None:
                k.dma("sp", xr[i * 128:(i + 1) * 128, :], xc[sl][:], xc_st[sl], reads=[B_xc[sl]], writes=[B_xr[G]])
            else:
                k.op("act", lambda e, sl=sl: e.activation(out=fjunk[:], in_=xc[sl][:], func=AF.Square,
                                                          accum_out=fss[sl][:, 0:1]), [B_xc[sl]], [B_fjunk, B_fss[sl]])
                k.op("dve", lambda e, sl=sl: e.tensor_scalar(out=fss[sl][:], in0=fss[sl][:], scalar1=1.0 / D,
                                                             scalar2=1e-6, op0=ALU.mult, op1=ALU.add),
                     [B_fss[sl]], [B_fss[sl]])
                k.op("act", lambda e, sl=sl: e.activation(out=fss[sl][:], in_=fss[sl][:], func=AF.Sqrt),
                     [B_fss[sl]], [B_fss[sl]])
                k.op("dve", lambda e, sl=sl: e.reciprocal(out=fss[sl][:], in_=fss[sl][:]), [B_fss[sl]], [B_fss[sl]])
                k.op("dve", lambda e, sl=sl: e.scalar_tensor_tensor(out=xc[sl][:], in0=xc[sl][:], scalar=fss[sl][:, 0:1],
                                                                    in1=fg[:], op0=ALU.mult, op1=ALU.mult),
                     [B_xc[sl], B_fss[sl], B_fg], [B_xc[sl]])
                k.dma("sp", fin["out"][i * 128:(i + 1) * 128, :], xc[sl][:], xc_st[sl], reads=[B_xc[sl]],
                      writes=[fin["B_out"][G]])
        k.s.barrier()


def finish(k, out_bufs):
    k.s.add("sp", None, reads=out_bufs)
    esem = {e: k.sem("e_" + e) for e in ENGS}
    stuck = k.s.simulate()
    assert not stuck, f"schedule deadlock: {stuck}"
    k.s.emit(k.nc, esem)
    k.pctx.close()
    k.ctx.close()
    return k.nc


_RL = {}


def _relayout(w, key, nch):
    if key not in _RL or _RL[key][0] is not w:
        Lr, E, R, N = w.shape
        _RL[key] = (w, np.ascontiguousarray(w.reshape(Lr, E, nch, 128, N).transpose(0, 1, 3, 2, 4)).reshape(
            Lr * E * 128, nch * N))
    return _RL[key][1]


def host_inputs(inp, core, NB, S):
    b0 = core * NB
    f = np.float32
    m = {}
    m["x"] = np.ascontiguousarray(inp["x"][b0:b0 + NB, :S].reshape(NB * S, D))
    c = inp["c"][b0:b0 + NB]
    m["cT"] = np.ascontiguousarray(c.reshape(NB, 8, 128).transpose(2, 1, 0).reshape(128, 8 * NB))
    m["ada_w"] = inp["ada_w"]
    m["ada_b"] = inp["ada_b"]
    m["gmixF"] = np.ascontiguousarray(inp["norm_mix_g"].reshape(2, 8, 128).transpose(0, 2, 1))
    m["gffn"] = inp["norm_ffn_g"]
    m["conv_in_w"] = inp["conv_in_w"][0]
    m["convwF"] = np.ascontiguousarray(inp["conv_w"][0].reshape(3, 8, 128).transpose(2, 1, 0).reshape(128, 24))
    m["conv_out_w"] = inp["conv_out_w"][0]
    m["ident"] = np.eye(128, dtype=f)
    m["tri"] = np.triu(np.ones((128, 128), dtype=f), 1)
    m["Wr"] = np.ascontiguousarray(np.concatenate(
        [inp["router_grp_w"], inp["router_exp_w"].transpose(0, 2, 1, 3).reshape(2, D, 32)], axis=2))
    m["br"] = np.ascontiguousarray(np.concatenate([inp["router_grp_b"], inp["router_exp_b"].reshape(2, 32)], axis=1))
    m["exp_gate_w"] = _relayout(inp["exp_gate_w"], "g", 8)
    m["exp_up_w"] = _relayout(inp["exp_up_w"], "u", 8)
    m["exp_down_w"] = _relayout(inp["exp_down_w"], "d", 4)
    m["pcol"] = np.arange(128, dtype=f).reshape(128, 1)
    m["attn_in_w"] = inp["attn_in_w"][0]
    m["attn_out_w"] = inp["attn_out_w"][0]
    pos = np.arange(S, dtype=f)
    inv = np.power(f(500000.0), -np.arange(0, 16, 2, dtype=f) / f(16)).astype(f)
    ang = pos[None, :] * inv[:, None]
    C = np.ones((128, S), f)
    Sg = np.zeros((128, S), f)
    Rm = np.zeros((128, 128), f)
    for h in range(2):
        for e in range(16):
            C[h * 64 + e] = np.cos(ang[e % 8])
            Sg[h * 64 + e] = (-np.sin(ang[e % 8])) if e < 8 else np.sin(ang[e % 8])
            Rm[h * 64 + (e + 8 if e < 8 else e - 8), h * 64 + e] = 1.0
    m["ropeC"], m["ropeS"] = C, Sg
    kk = np.arange(128)[:, None]
    qq = np.arange(128)[None, :]
    NEG = f(-30000.0)
    Mprev = np.where(kk >= qq, f(0), NEG).astype(f)
    Mcur = np.where(kk <= qq, f(0), NEG).astype(f)
    o0 = np.zeros((128, 128), f)
    o0[:, :64] = 1
    o1 = np.zeros((128, 128), f)
    o1[:, 64:] = 1
    m["aconst"] = np.ascontiguousarray(np.concatenate([Rm, Mprev, Mcur, o0, o1], axis=1))
    m["fing"] = inp["final_norm_g"].reshape(1, D)
    return m


def kernel(**inputs):
    NB, S = 2, 4096
    nc = build(NB, S)
    in_maps = [host_inputs(inputs, c, NB, S) for c in range(NCORES)]
    res = run_bass_kernel_spmd(nc, in_maps, core_ids=list(range(NCORES)))
    out = np.stack([r["out"].reshape(NB, S, D) for r in res.results]).reshape(NCORES * NB, S, D)
    return out.astype(np.float32)
```

```python
import contextlib
import os
import numpy as np
import concourse.bass as bass
import concourse.mybir as mybir
from concourse.bass_utils import run_bass_kernel_spmd

F32 = mybir.dt.float32
BF16 = mybir.dt.bfloat16
I32 = mybir.dt.int32
AF = mybir.ActivationFunctionType
ALU = mybir.AluOpType
AX = mybir.AxisListType

D = 1024
NCORES = 8
ENGS = ("pe", "act", "dve", "pool", "sp")
SAME_ENG_SYNC = True
OOB_BIG = 1000000
DYN_SKIP = False


class Buf:
    __slots__ = ("name", "lw", "rd")

    def __init__(self, name=""):
        self.name = name
        self.lw = None
        self.rd = {}


class DSem:
    def __init__(self, h):
        self.h = h
        self.groups = []


class Ins:
    __slots__ = ("eng", "fn", "dsem", "signal", "sig", "target", "deps", "gk", "scope", "guard")


class Sched:
    def __init__(self):
        self.L = {e: [] for e in ENGS}
        self.n = 0
        self.dsems = []
        self.scope = None
        self.trace_scopes = False
        self.guard = None


    def dsem(self, h):
        d = DSem(h)
        self.dsems.append(d)
        return d

    def add(self, eng, fn, reads=(), writes=(), dsem=None, grp=None):
        ins = Ins()
        ins.eng, ins.fn, ins.dsem = eng, fn, dsem
        ins.signal, ins.sig, ins.target = False, 0, 0
        ins.gk = (id(dsem), grp) if (dsem is not None and grp is not None) else None
        ins.scope = self.scope
        ins.guard = self.guard
        deps = {}

        def need(d, kind):
            if d.dsem is not None:
                return not (ins.gk is not None and d.gk == ins.gk)
            if d.eng == eng:
                if dsem is not None:
                    return True
                if eng == "pe":
                    return False
                return kind == "raw" and SAME_ENG_SYNC
            return True

        for b in reads:
            if b.lw is not None and need(b.lw, "raw"):
                deps[id(b.lw)] = b.lw
        for b in writes:
            if b.lw is not None and need(b.lw, "waw"):
                deps[id(b.lw)] = b.lw
            for r in b.rd.values():
                if need(r, "war"):
                    deps[id(r)] = r
        if dsem is not None:
            assert getattr(dsem, "sw", False) == (eng == "pool"), f"DMA semaphore class mismatch on {eng}"
            g = dsem.groups
            if g and grp is not None and g[-1][0] == grp:
                g[-1][1].append(ins)
            else:
                if g:
                    prev = g[-1][1][-1]
                    deps[id(prev)] = prev
                g.append((grp if grp is not None else object(), [ins]))
        ins.deps = list(deps.values())
        for d in ins.deps:
            d.signal = True
        for b in reads:
            key = eng if dsem is None else ("dma", self.n)
            b.rd[key] = ins
        for b in writes:
            b.lw = ins
            b.rd = {}
        self.L[eng].append(ins)
        self.n += 1
        return ins

    def barrier(self):
        lasts = []
        for e in ENGS:
            for ins in reversed(self.L[e]):
                if ins.fn is not None and ins.dsem is None:
                    lasts.append(ins)
                    break
        for ds in self.dsems:
            if ds.groups:
                lasts.append(ds.groups[-1][1][-1])
        for e in ENGS:
            ins = Ins()
            ins.eng, ins.fn, ins.dsem = e, None, None
            ins.signal, ins.sig, ins.target = False, 0, 0
            ins.gk = None
            ins.scope = self.scope
            ins.guard = None
            ins.deps = [d for d in lasts if not (d.dsem is None and d.eng == e)]
            for d in ins.deps:
                d.signal = True
            self.L[e].append(ins)

    def simulate(self):
        for e in ENGS:
            c = 0
            for ins in self.L[e]:
                if ins.dsem is None and ins.signal:
                    c += 1
                    ins.sig = c
        for ds in self.dsems:
            tot = 0
            for _, lst in ds.groups:
                tot += 16 * len(lst)
                for i in lst:
                    i.target = tot
        pc = {e: 0 for e in ENGS}
        ev = {e: 0 for e in ENGS}
        dv = {id(ds): 0 for ds in self.dsems}
        prog = True
        while prog:
            prog = False
            for e in ENGS:
                while pc[e] < len(self.L[e]):
                    ins = self.L[e][pc[e]]
                    ok = True
                    for d in ins.deps:
                        if d.dsem is not None:
                            if dv[id(d.dsem)] < d.target:
                                ok = False
                        elif ev[d.eng] < d.sig:
                            ok = False
                    if not ok:
                        break
                    if ins.fn is not None:
                        if ins.dsem is not None:
                            dv[id(ins.dsem)] += 16
                        elif ins.signal:
                            ev[e] += 1
                    pc[e] += 1
                    prog = True
        stuck = {e: (pc[e], len(self.L[e])) for e in ENGS if pc[e] < len(self.L[e])}
        return stuck

    def emit(self, nc, esem):
        for e in ENGS:
            c = 0
            for ins in self.L[e]:
                if ins.dsem is None and ins.signal:
                    c += 1
                    ins.sig = c
        for ds in self.dsems:
            tot = 0
            for _, lst in ds.groups:
                tot += 16 * len(lst)
                for i in lst:
                    i.target = tot

        def emit_one(e, eo, ins, seen):
            for d in ins.deps:
                if d.dsem is not None:
                    key, val, h = ("d", id(d.dsem)), d.target, d.dsem.h
                else:
                    key, val, h = ("e", d.eng), d.sig, esem[d.eng]
                if seen.get(key, 0) < val:
                    eo.wait_ge(h, val)
                    seen[key] = val
            if ins.fn is None:
                return
            r = ins.fn(eo)
            if ins.dsem is not None:
                r.then_inc(ins.dsem.h, 16)
            elif ins.signal:
                r.then_inc(esem[e], 1)

        def run(e, eo):
            seen = {}
            cur = [None, None]
            L_ = self.L[e]
            i = 0
            while i < len(L_):
                ins = L_[i]
                if self.trace_scopes and ins.scope != cur[0]:
                    if cur[1] is not None:
                        cur[1].__exit__(None, None, None)
                    cur[0] = ins.scope
                    cur[1] = nc.named_scope(f"{ins.scope}") if ins.scope else None
                    if cur[1] is not None:
                        cur[1].__enter__()
                if ins.guard is None:
                    emit_one(e, eo, ins, seen)
                    i += 1
                    continue
                g = ins.guard
                j = i
                while j < len(L_) and L_[j].guard is g and L_[j].scope == ins.scope:
                    j += 1
                region = L_[i:j]
                nsig = sum(1 for x in region if x.dsem is None and x.signal and x.fn is not None)
                dcount = {}
                for x in region:
                    if x.dsem is not None:
                        dcount[id(x.dsem)] = (x.dsem, dcount.get(id(x.dsem), (x.dsem, 0))[1] + 16)
                regs, thresh = g
                snap = dict(seen)
                with eo.If_lt(regs[e], thresh + 1):
                    for _ in range(nsig):
                        eo.nop(nofuse=True).then_inc(esem[e], 1)
                    for ds, n in dcount.values():
                        for _ in range(n // 16):
                            eo.nop(nofuse=True).then_inc(ds.h, 16)
                with eo.Else():
                    for x in region:
                        emit_one(e, eo, x, seen)
                seen = snap
                i = j
            if cur[1] is not None:
                cur[1].__exit__(None, None, None)

        with nc.Block() as block:
            @block.sync
            def _(eo):
                run("sp", eo)

            @block.tensor
            def _(eo):
                run("pe", eo)

            @block.scalar
            def _(eo):
                run("act", eo)

            @block.vector
            def _(eo):
                run("dve", eo)

            @block.gpsimd
            def _(eo):
                run("pool", eo)


class K:
    def __init__(self, NB, S, dbg=None):
        self.NB, self.S, self.T = NB, S, NB * S
        self.dbg = dbg
        self.nc = bass.Bass("TRN2", target_bir_lowering=False)
        self.ctx = contextlib.ExitStack()
        self.s = Sched()
        self.pctx = contextlib.ExitStack()
        self.nsem = 0
        self.dpool = []
        self.dcur = 0
        self.dpool_sw = []
        self.dcur_sw = 0
        self._regs = None
        self._breg = None

    def din(self, name, shape, dt=F32):
        return self.nc.dram_tensor(name, list(shape), dt, kind="ExternalInput").ap()

    def dout(self, name, shape, dt=F32):
        return self.nc.dram_tensor(name, list(shape), dt, kind="ExternalOutput").ap()

    def dscr(self, name, shape, dt=F32):
        return self.nc.dram_tensor(name, list(shape), dt, kind="Internal").ap()

    def phase(self):
        self.s.barrier()
        self.pctx.close()
        self.pctx = contextlib.ExitStack()
        self.dcur = 0
        self.dcur_sw = 0

    def sbp(self, name, shape, dt=F32):
        return self.pctx.enter_context(self.nc.sbuf_tensor("s_" + name, list(shape), dt))

    def psp(self, name, shape, dt=F32):
        return self.pctx.enter_context(self.nc.psum_tensor("p_" + name, list(shape), dt))

    def sb(self, name, shape, dt=F32):
        return self.ctx.enter_context(self.nc.sbuf_tensor("s_" + name, list(shape), dt))

    def ps(self, name, shape, dt=F32):
        return self.ctx.enter_context(self.nc.psum_tensor("p_" + name, list(shape), dt))

    def sem(self, name):
        self.nsem += 1
        return self.ctx.enter_context(self.nc.semaphore(name))

    def bound_reg(self):
        if self._breg is None:
            self._breg = self.ctx.enter_context(self.nc.gpsimd.register("oob_bound"))
        return self._breg

    def nused_regs(self):
        if self._regs is None:
            nc = self.nc
            eng = {"pe": nc.tensor, "act": nc.scalar, "dve": nc.vector, "pool": nc.gpsimd, "sp": nc.sync}
            self._regs = {e: self.ctx.enter_context(eng[e].register("nused_" + e)) for e in ENGS}
        return self._regs

    def dsem(self, name, sw=False):
        pool, cur = (self.dpool_sw, self.dcur_sw) if sw else (self.dpool, self.dcur)
        if cur < len(pool):
            d = pool[cur]
        else:
            d = self.s.dsem(self.sem(f"dma{'s' if sw else 'h'}{len(pool)}"))
            d.sw = sw
            pool.append(d)
        if sw:
            self.dcur_sw += 1
        else:
            self.dcur += 1
        return d

    def dma(self, eng, out, in_, dsem, reads=(), writes=(), grp=None, **kw):
        return self.s.add(eng, lambda e: e.dma_start(out=out, in_=in_, **kw), reads, writes, dsem=dsem, grp=grp)

    def mm(self, out, lhsT, rhs, start, stop, reads=(), writes=()):
        return self.s.add("pe", lambda e: e.matmul(out, lhsT=lhsT, rhs=rhs, start=start, stop=stop), reads, writes)

    def tr(self, out, in_, ident, reads=(), writes=()):
        return self.s.add("pe", lambda e: e.transpose(out=out, in_=in_, identity=ident), reads, writes)

    def op(self, eng, fn, reads=(), writes=()):
        return self.s.add(eng, fn, reads, writes)


def build(NB, S, stage="all", trace_scopes=False):
    k = K(NB, S)
    k.s.trace_scopes = trace_scopes
    nc, s = k.nc, k.s
    T = NB * S
    NT = T // 128
    NG = T // 512
    GPB = S // 512

    x_in = k.din("x", [T, D])
    cT_in = k.din("cT", [128, 8 * NB])
    ada_w = k.din("ada_w", [2, D, 6 * D])
    ada_b = k.din("ada_b", [2, 6 * D])
    gmixF = k.din("gmixF", [2, 128, 8])
    gffn = k.din("gffn", [2, D])
    conv_in_w = k.din("conv_in_w", [D, 3 * D])
    convwF = k.din("convwF", [128, 24])
    conv_out_w = k.din("conv_out_w", [D, D])
    ident_in = k.din("ident", [128, 128])
    tri_in = k.din("tri", [128, 128])
    Wr_in = k.din("Wr", [2, D, 36])
    br_in = k.din("br", [2, 36])
    exp_gate_w = k.din("exp_gate_w", [2 * 32 * 128, 8 * 512])
    exp_up_w = k.din("exp_up_w", [2 * 32 * 128, 8 * 512])
    exp_down_w = k.din("exp_down_w", [2 * 32 * 128, 4 * D])
    pcol_in = k.din("pcol", [128, 1])
    attn_in_w = k.din("attn_in_w", [D, 4608])
    attn_out_w = k.din("attn_out_w", [512, D])
    ropeC_in = k.din("ropeC", [128, S])
    ropeS_in = k.din("ropeS", [128, S])
    aconst_in = k.din("aconst", [128, 640])
    fing_in = k.din("fing", [1, D])
    out_ap = k.dout("out", [T, D])
    NBLK = (2 * T) // 512 + 32
    hs = k.dscr("hs", [T, D], BF16)
    xs = k.dscr("xs", [NBLK * 512, D], BF16)
    ys = k.dscr("ys", [NBLK * 512, D], F32)
    xr = k.dout("xr", [T, D])
    modrow = k.dout("modrow", [2, NB, 6 * D])

    ident_f = k.sb("ident_f", [128, 128], F32)
    ident_b = k.sb("ident_b", [128, 128], BF16)
    cT = k.sb("cT", [128, 8 * NB], F32)
    B_ident_f, B_ident_b, B_cT = Buf(), Buf(), Buf()
    B_modrow = [Buf() for _ in range(2)]

    ld0 = k.dsem("ld0")
    k.dma("sp", ident_f[:], ident_in[:, :], ld0, writes=[B_ident_f], grp="init")
    k.dma("sp", cT[:], cT_in[:, :], ld0, writes=[B_cT], grp="init")
    k.op("dve", lambda e: e.tensor_copy(out=ident_b[:], in_=ident_f[:]), [B_ident_f], [B_ident_b])
    k.op("act", lambda e: e.activation(out=cT[:], in_=cT[:], func=AF.Silu), [B_cT], [B_cT])

    s.scope = "A_mod"
    NSA = 3
    aw = [k.sbp(f"aw{i}", [128, 8, 512], F32) for i in range(NSA)]
    adab = [k.sbp(f"adab{i}", [NB, 512], F32) for i in range(NSA)]
    modc = [k.sbp(f"modc{i}", [NB, 512], F32) for i in range(NSA)]
    B_aw, B_adab, B_modc = [Buf() for _ in range(NSA)], [Buf() for _ in range(NSA)], [Buf() for _ in range(NSA)]
    aw_sem = [k.dsem(f"aw{i}") for i in range(NSA)]
    adab_sem = [k.dsem(f"adab{i}") for i in range(NSA)]
    mod_st = [k.dsem(f"mod_st{i}", sw=True) for i in range(NSA)]
    ps_mod = [k.psp(f"ps_mod{i}", [128, 512], F32) for i in range(2)]
    B_psmod = [Buf(), Buf()]
    chunks = [(l, cc) for l in range(2) for cc in range(12)]

    def a_load(ci):
        l, cc = chunks[ci]
        sl = ci % NSA
        src_b = ada_b[l:l + 1, cc * 512:(cc + 1) * 512]
        k.dma("sp", adab[sl][:], src_b.partition_broadcast(NB) if NB > 1 else src_b, adab_sem[sl],
              writes=[B_adab[sl]])
        k.dma("sp", aw[sl][:], ada_w[l, :, cc * 512:(cc + 1) * 512].rearrange("(kc p) f -> p kc f", p=128),
              aw_sem[sl], writes=[B_aw[sl]])
    for ci in range(NSA - 1):
        a_load(ci)
    for ci, (l, cc) in enumerate(chunks):
        sl = ci % NSA
        pq = ci % 2
        if ci + NSA - 1 < len(chunks):
            a_load(ci + NSA - 1)
        for kc in range(8):
            k.mm(ps_mod[pq][0:NB, :], cT[:, kc * NB:(kc + 1) * NB], aw[sl][:, kc, :], kc == 0, kc == 7,
                 reads=[B_cT, B_aw[sl]], writes=[B_psmod[pq]])
        k.op("dve", lambda e, sl=sl, pq=pq: e.tensor_tensor(out=modc[sl][:], in0=ps_mod[pq][0:NB, :], in1=adab[sl][:],
                                                           op=ALU.add),
             [B_psmod[pq], B_adab[sl]], [B_modc[sl]])
        k.dma("pool", modrow[l, :, cc * 512:(cc + 1) * 512], modc[sl][:], mod_st[sl], reads=[B_modc[sl]],
              writes=[B_modrow[l]])
    k.phase()
    if stage == "A":
        return finish(k, [B_modrow[0], B_modrow[1]])

    s.scope = "L0_conv"
    L = 0
    w_in = k.sbp("w_in", [128, 8, 3 * D], BF16)
    w_stage = [k.sbp(f"w_stage{i}", [128, D], F32) for i in range(2)]
    w_out_b1 = k.sbp("w_out_b", [128, 8, D], BF16)
    w_out_b = [w_out_b1 for b in range(NB)]
    g1bc = [k.sbp(f"g1bc{b}", [128, D], F32) for b in range(NB)]
    A1 = [k.sbp(f"A1_{b}", [128, 8], F32) for b in range(NB)]
    sh1 = [k.sbp(f"sh1_{b}", [128, 8], F32) for b in range(NB)]
    sc1t = k.sbp("sc1t", [128, 8], F32)
    gmix = k.sbp("gmix", [128, 8], F32)
    convw = k.sbp("convw", [128, 24], F32)
    B_w_in, B_sc1t, B_gmix, B_convw = Buf(), Buf(), Buf(), Buf()
    B_w_stage = [Buf(), Buf()]
    B_g1bc = [Buf() for _ in range(NB)]
    B_w_out_b1 = Buf()
    B_w_out_b = [B_w_out_b1 for _ in range(NB)]
    B_A1 = [Buf() for _ in range(NB)]
    B_sh1 = [Buf() for _ in range(NB)]
    wl = k.dsem("wl", sw=True)
    wst = [k.dsem("wst0"), k.dsem("wst1")]
    vl = k.dsem("vl")
    for kc in range(8):
        k.dma("pool", w_in[:, kc, :], conv_in_w[kc * 128:(kc + 1) * 128, :], wl, writes=[B_w_in], grp="w_in",
              max_dma_last_dim=4096)
    k.dma("sp", gmix[:], gmixF[L], vl, writes=[B_gmix], grp="v0")
    k.dma("sp", convw[:], convwF[:, :], vl, writes=[B_convw], grp="v0")
    for b in range(NB):
        k.dma("sp", sh1[b][:], modrow[L, b, 0:D].rearrange("(c p) -> p c", p=128), vl,
              reads=[B_modrow[L]], writes=[B_sh1[b]], allow_slow_non_contiguous=True)
        k.dma("sp", sc1t[:], modrow[L, b, D:2 * D].rearrange("(c p) -> p c", p=128), vl,
              reads=[B_modrow[L]], writes=[B_sc1t], allow_slow_non_contiguous=True)
        k.op("dve", lambda e, b=b: e.scalar_tensor_tensor(out=A1[b][:], in0=sc1t[:], scalar=1.0, in1=gmix[:],
                                                         op0=ALU.add, op1=ALU.mult),
             [B_sc1t, B_gmix], [B_A1[b]])
        k.dma("sp", g1bc[b][:], modrow[L, b:b + 1, 2 * D:3 * D].partition_broadcast(128), vl,
              reads=[B_modrow[L]], writes=[B_g1bc[b]])
    def build_w_out(b):
        for kc in range(8):
            sl = kc % 2
            k.dma("sp", w_stage[sl][:], conv_out_w[kc * 128:(kc + 1) * 128, :], wst[sl], writes=[B_w_stage[sl]])
            k.op("dve", lambda e, b=b, kc=kc, sl=sl: e.tensor_tensor(out=w_out_b[b][:, kc, :], in0=w_stage[sl][:],
                                                                    in1=g1bc[b][:], op=ALU.mult),
                 [B_w_stage[sl], B_g1bc[b]], [B_w_out_b[b]])
    build_w_out(0)

    if stage == "L0setup":
        return finish(k, [B_modrow[0], B_modrow[1]] + B_w_out_b + B_A1 + B_sh1 + [B_w_in, B_convw])
    NXS = 4
    xt = [k.sbp(f"xt{i}", [128, 4, D], F32) for i in range(NXS)]
    B_xt = [Buf() for _ in range(NXS)]
    xt_ld = [k.dsem(f"xt_ld{i}") for i in range(NXS)]
    xt_st = [k.dsem(f"xt_st{i}") for i in range(NXS)]
    junk = k.sbp("junk", [128, D], BF16)
    B_junk = Buf()
    ss = k.sbp("ss", [128, 4], F32)
    rstd = k.sbp("rstd", [128, 4], F32)
    B_ss, B_rstd = Buf(), Buf()
    xn = [k.sbp(f"xn{i}", [128, 4, D], BF16) for i in range(2)]
    B_xn = [[Buf() for _ in range(4)] for _ in range(2)]
    pT = k.psp("pT", [128, 512], BF16)
    B_pT = Buf()
    hT = [k.sbp(f"hT{i}", [128, 8, 512], BF16) for i in range(2)]
    B_hT = [[Buf() for _ in range(8)] for _ in range(2)]
    psBCU = [[k.psp(f"ps{n}{i}", [128, 512]) for n in "BCU"] for i in range(2)]
    B_psBCU = [[Buf() for _ in range(3)] for _ in range(2)]
    Csb = [k.sbp(f"Csb{i}", [128, 512], F32) for i in range(2)]
    B_Csb = [Buf(), Buf()]
    zb = [k.sbp(f"zb{i}", [128, 514], F32) for i in range(2)]
    B_zb = [Buf(), Buf()]
    zh = k.sbp("zh", [128, 8, 2], F32)
    B_zh = [Buf() for _ in range(8)]
    zc = [k.sbp(f"zc{i}", [128, 512], F32) for i in range(2)]
    B_zc = [Buf(), Buf()]
    gT = [k.sbp(f"gT{i}", [128, 8, 512], BF16) for i in range(2)]
    B_gT = [[Buf() for _ in range(8)] for _ in range(2)]
    psY = k.psp("psY", [128, 512])
    B_psY = Buf()
    B_xr = [Buf() for _ in range(NG)]

    def load_x(G):
        sl = G % NXS
        k.dma("sp", xt[sl][:], x_in[G * 512:(G + 1) * 512, :].rearrange("(j p) d -> p j d", p=128),
              xt_ld[sl], writes=[B_xt[sl]])

    def norm_pre(G):
        sl = G % 2
        xt_t, B_x = xt[G % NXS], B_xt[G % NXS]
        for j in range(4):
            k.op("act", lambda e, j=j: e.activation(out=junk[:], in_=xt_t[:, j, :], func=AF.Square,
                                                    accum_out=ss[:, j:j + 1]),
                 [B_x], [B_junk, B_ss])
        k.op("dve", lambda e: e.tensor_scalar(out=rstd[:], in0=ss[:], scalar1=1.0 / D, scalar2=1e-6,
                                              op0=ALU.mult, op1=ALU.add), [B_ss], [B_rstd])
        k.op("act", lambda e: e.activation(out=rstd[:], in_=rstd[:], func=AF.Sqrt), [B_rstd], [B_rstd])
        k.op("dve", lambda e: e.reciprocal(out=rstd[:], in_=rstd[:]), [B_rstd], [B_rstd])
        for j in range(4):
            k.op("act", lambda e, j=j: e.activation(out=xn[sl][:, j, :], in_=xt_t[:, j, :], func=AF.Copy,
                                                    scale=rstd[:, j:j + 1]),
                 [B_x, B_rstd], [B_xn[sl][j]])

    def norm_tr(G, c):
        b = G // GPB
        sl = G % 2
        A, B_A, sh, B_sh = A1[b], B_A1[b], sh1[b], B_sh1[b]
        for j in range(4):
            k.tr(pT[:, j * 128:(j + 1) * 128], xn[sl][:, j, c * 128:(c + 1) * 128], ident_b[:],
                 reads=[B_xn[sl][j], B_ident_b], writes=[B_pT])
        k.op("act", lambda e, c=c: e.activation(out=hT[sl][:, c, :], in_=pT[:], func=AF.Identity,
                                                scale=A[:, c:c + 1], bias=sh[:, c:c + 1]),
             [B_pT, B_A, B_sh], [B_hT[sl][c]])

    def inproj(G, fcs):
        sl = G % 2
        first = (G % GPB == 0)
        for fc in fcs:
            q = fc % 2
            (psB, psC, psU), (B_psB, B_psC, B_psU) = psBCU[q], B_psBCU[q]
            for (pst, B_p, off) in ((psB, B_psB, 0), (psC, B_psC, D), (psU, B_psU, 2 * D)):
                for kc in range(8):
                    k.mm(pst[:], w_in[:, kc, off + fc * 128: off + (fc + 1) * 128], hT[sl][:, kc, :], kc == 0, kc == 7,
                         reads=[B_w_in, B_hT[sl][kc]], writes=[B_p])
            zt, Bz, zcb, Bzc, Cs, BCs = zb[q], B_zb[q], zc[q], B_zc[q], Csb[q], B_Csb[q]
            if first:
                k.op("pool", lambda e, zt=zt: e.memset(zt[:, 0:2], 0.0), [], [Bz])
            else:
                k.op("pool", lambda e, zt=zt, fc=fc: e.tensor_copy(out=zt[:, 0:2], in_=zh[:, fc, :]),
                     [B_zh[fc]], [Bz])
            k.op("act", lambda e, Cs=Cs, psC=psC: e.copy(out=Cs[:], in_=psC[:]), [B_psC], [BCs])
            k.op("dve", lambda e, zt=zt, Cs=Cs, psU=psU: e.tensor_tensor(out=zt[:, 2:514], in0=Cs[:], in1=psU[:],
                                                                        op=ALU.mult),
                 [BCs, B_psU, Bz], [Bz])
            k.op("pool", lambda e, zt=zt, fc=fc: e.tensor_copy(out=zh[:, fc, :], in_=zt[:, 512:514]),
                 [Bz], [B_zh[fc]])
            k.op("pool", lambda e, fc=fc, zt=zt, zcb=zcb: e.tensor_scalar(
                out=zcb[:], in0=zt[:, 2:514], scalar1=convw[:, fc * 3 + 2: fc * 3 + 3], scalar2=1.0,
                op0=ALU.mult, op1=ALU.mult), [Bz, B_convw], [Bzc])
            k.op("dve", lambda e, fc=fc, zt=zt, zcb=zcb: e.scalar_tensor_tensor(
                out=zcb[:], in0=zt[:, 1:513], scalar=convw[:, fc * 3 + 1: fc * 3 + 2], in1=zcb[:],
                op0=ALU.mult, op1=ALU.add), [Bz, B_convw, Bzc], [Bzc])
            k.op("dve", lambda e, fc=fc, zt=zt, zcb=zcb: e.scalar_tensor_tensor(
                out=zcb[:], in0=zt[:, 0:512], scalar=convw[:, fc * 3: fc * 3 + 1], in1=zcb[:],
                op0=ALU.mult, op1=ALU.add), [Bz, B_convw, Bzc], [Bzc])
            k.op("dve", lambda e, fc=fc, zcb=zcb, psB=psB, sl=sl: e.tensor_tensor(out=gT[sl][:, fc, :], in0=zcb[:],
                                                                                 in1=psB[:], op=ALU.mult),
                 [Bzc, B_psB], [B_gT[sl][fc]])

    def outproj_unit(G, u):
        b = G // GPB
        sl = G % 2
        xs_ = G % NXS
        j, dh = u // 2, u % 2
        for kc in range(8):
            k.mm(psY[:], gT[sl][:, kc, j * 128:(j + 1) * 128], w_out_b[b][:, kc, dh * 512:(dh + 1) * 512],
                 kc == 0, kc == 7, reads=[B_gT[sl][kc], B_w_out_b[b]], writes=[B_psY])
        k.op("dve", lambda e: e.tensor_tensor(
            out=xt[xs_][:, j, dh * 512:(dh + 1) * 512], in0=psY[:], in1=xt[xs_][:, j, dh * 512:(dh + 1) * 512],
            op=ALU.add), [B_psY, B_xt[xs_]], [B_xt[xs_]])
        if u == 7:
            k.dma("sp", xr[G * 512:(G + 1) * 512, :].rearrange("(j p) d -> p j d", p=128), xt[xs_][:], xt_st[xs_],
                  reads=[B_xt[xs_]], writes=[B_xr[G]])

    load_x(0)
    if NG > 1:
        load_x(1)
    norm_pre(0)
    for c in range(8):
        norm_tr(0, c)
    for G in range(NG):
        if G + 1 < NG:
            norm_pre(G + 1)
        if G + 2 < NG:
            load_x(G + 2)
        for fc in range(8):
            inproj(G, [fc])
            if G + 1 < NG and fc > 0:
                norm_tr(G + 1, fc - 1)
            if G > 0:
                outproj_unit(G - 1, fc)
        if G + 1 < NG:
            norm_tr(G + 1, 7)
        if G > 0 and (G % GPB) == 0 and G // GPB < NB:
            build_w_out(G // GPB)
    for u in range(8):
        outproj_unit(NG - 1, u)

    if stage == "mix0":
        return finish(k, [B_xr[G] for G in range(NG)])
    k.phase()
    dbg = k.dout("dbg", [128, 4096]) if stage.startswith("M") else None
    r = moe_layer(k, 0, dict(stage=stage, dbg=dbg, xr=xr, B_xr=B_xr, modrow=modrow, B_modrow=B_modrow, gffn=gffn, Wr_in=Wr_in, br_in=br_in,
                         tri_in=tri_in, ident_f=ident_f, B_ident_f=B_ident_f, ident_b=ident_b, B_ident_b=B_ident_b,
                         hs=hs, xs=xs, ys=ys, gw=exp_gate_w, uw=exp_up_w, dw=exp_down_w, NBLK=NBLK, pcol_in=pcol_in))
    if r is not None:
        return finish(k, [B_xr[G] for G in range(NG)] + r)
    if stage == "ffn0":
        return finish(k, [B_xr[G] for G in range(NG)])
    k.phase()
    attn_layer(k, dict(xr=xr, B_xr=B_xr, modrow=modrow, B_modrow=B_modrow, gmixF=gmixF, ident_b=ident_b,
                       B_ident_b=B_ident_b, w_in=attn_in_w, w_out=attn_out_w, ropeC=ropeC_in, ropeS=ropeS_in,
                       aconst=aconst_in, pcol_in=pcol_in))
    if stage == "mix1":
        return finish(k, [B_xr[G] for G in range(NG)])
    k.phase()
    B_out = [Buf() for _ in range(NG)]
    moe_layer(k, 1, dict(stage=stage, dbg=None, final=dict(fing=fing_in, out=out_ap, B_out=B_out), xr=xr, B_xr=B_xr, modrow=modrow, B_modrow=B_modrow, gffn=gffn,
                         Wr_in=Wr_in, br_in=br_in, tri_in=tri_in, ident_f=ident_f, B_ident_f=B_ident_f,
                         ident_b=ident_b, B_ident_b=B_ident_b, hs=hs, xs=xs, ys=ys, gw=exp_gate_w, uw=exp_up_w,
                         dw=exp_down_w, NBLK=NBLK, pcol_in=pcol_in))
    return finish(k, [B_out[G] for G in range(NG)])


DIL = (1, 4, 16)
import os
SKIP = os.environ.get("ATT_SKIP", "").split(",")


def attn_layer(k, a):
    L = 1
    NB, S, T = k.NB, k.S, k.T
    GPB = S // 512
    xr, B_xr, modrow, B_modrow = a["xr"], a["B_xr"], a["modrow"], a["B_modrow"]
    ident_b, B_ident_b = a["ident_b"], a["B_ident_b"]
    w_in, w_out = a["w_in"], a["w_out"]
    hT = k.sbp("a_hT", [128, 8, S], BF16)
    oT = k.sbp("a_oT", [128, 4, S], BF16)
    B_hT = [[Buf() for _ in range(GPB)] for _ in range(8)]
    B_oT = [Buf() for _ in range(4)]
    vl = k.dsem("a_vl")
    for b in range(NB):
        k.s.scope = f"att{b}_1_hT"
        with contextlib.ExitStack() as c1:
            def sb1(name, shape, dt=F32):
                return c1.enter_context(k.nc.sbuf_tensor(f"s_a1_{b}_{name}", list(shape), dt))
            A1, sh1, sc1t, gmix = sb1("A1", [128, 8]), sb1("sh1", [128, 8]), sb1("sc1t", [128, 8]), sb1("gmix", [128, 8])
            B_A1, B_sh1, B_sc1t, B_gmix = Buf(), Buf(), Buf(), Buf()
            k.dma("sp", gmix[:], a["gmixF"][L], vl, writes=[B_gmix])
            k.dma("sp", sh1[:], modrow[L, b, 0:D].rearrange("(c p) -> p c", p=128), vl,
                  reads=[B_modrow[L]], writes=[B_sh1], allow_slow_non_contiguous=True)
            k.dma("sp", sc1t[:], modrow[L, b, D:2 * D].rearrange("(c p) -> p c", p=128), vl,
                  reads=[B_modrow[L]], writes=[B_sc1t], allow_slow_non_contiguous=True)
            k.op("dve", lambda e: e.scalar_tensor_tensor(out=A1[:], in0=sc1t[:], scalar=1.0, in1=gmix[:],
                                                         op0=ALU.add, op1=ALU.mult), [B_sc1t, B_gmix], [B_A1])
            xt = [sb1(f"xt{i}", [128, 4, D]) for i in range(2)]
            B_xt = [Buf(), Buf()]
            xt_ld = [k.dsem(f"a1_{b}_xt_ld0"), k.dsem(f"a1_{b}_xt_ld1")]
            junk = sb1("junk", [128, D], BF16)
            ss, rstd = sb1("ss", [128, 4]), sb1("rstd", [128, 4])
            xn = sb1("xn", [128, 4, D], BF16)
            B_junk, B_ss, B_rstd = Buf(), Buf(), Buf()
            B_xn = [Buf() for _ in range(4)]
            pT = [c1.enter_context(k.nc.psum_tensor(f"p_a1_{b}_pT{i}", [128, 512], BF16)) for i in range(2)]
            B_pT = [Buf(), Buf()]
            def a1_load(gi):
                G = b * GPB + gi
                sl = gi % 2
                k.dma("sp", xt[sl][:], xr[G * 512:(G + 1) * 512, :].rearrange("(j p) d -> p j d", p=128), xt_ld[sl],
                      reads=[B_xr[G]], writes=[B_xt[sl]])
            a1_load(0)
            for gi in range(GPB):
                G = b * GPB + gi
                sl = gi % 2
                if gi + 1 < GPB:
                    a1_load(gi + 1)
                xt_t, B_x = xt[sl], B_xt[sl]
                for j in range(4):
                    k.op("act", lambda e, j=j, xt_t=xt_t: e.activation(out=junk[:], in_=xt_t[:, j, :], func=AF.Square,
                                                                      accum_out=ss[:, j:j + 1]), [B_x], [B_junk, B_ss])
                k.op("dve", lambda e: e.tensor_scalar(out=rstd[:], in0=ss[:], scalar1=1.0 / D, scalar2=1e-6,
                                                      op0=ALU.mult, op1=ALU.add), [B_ss], [B_rstd])
                k.op("act", lambda e: e.activation(out=rstd[:], in_=rstd[:], func=AF.Sqrt), [B_rstd], [B_rstd])
                k.op("dve", lambda e: e.reciprocal(out=rstd[:], in_=rstd[:]), [B_rstd], [B_rstd])
                for j in range(4):
                    k.op("act" if j % 2 == 0 else "pool", (lambda e, j=j, xt_t=xt_t: e.activation(
                        out=xn[:, j, :], in_=xt_t[:, j, :], func=AF.Copy, scale=rstd[:, j:j + 1])) if j % 2 == 0 else (
                        lambda e, j=j, xt_t=xt_t: e.tensor_scalar(out=xn[:, j, :], in0=xt_t[:, j, :],
                                                                  scalar1=rstd[:, j:j + 1], scalar2=1.0,
                                                                  op0=ALU.mult, op1=ALU.mult)),
                         [B_x, B_rstd], [B_xn[j]])
                for c in range(8):
                    p = c % 2
                    for j in range(4):
                        k.tr(pT[p][:, j * 128:(j + 1) * 128], xn[:, j, c * 128:(c + 1) * 128], ident_b[:],
                             reads=[B_xn[j], B_ident_b], writes=[B_pT[p]])
                    if c % 2 == 0:
                        k.op("act", lambda e, c=c, p=p, gi=gi: e.activation(
                            out=hT[:, c, gi * 512:(gi + 1) * 512], in_=pT[p][:], func=AF.Identity,
                            scale=A1[:, c:c + 1], bias=sh1[:, c:c + 1]), [B_pT[p], B_A1, B_sh1], [B_hT[c][gi]])
                    else:
                        k.op("dve", lambda e, c=c, p=p, gi=gi: e.tensor_scalar(
                            out=hT[:, c, gi * 512:(gi + 1) * 512], in0=pT[p][:], scalar1=A1[:, c:c + 1],
                            scalar2=sh1[:, c:c + 1], op0=ALU.mult, op1=ALU.add), [B_pT[p], B_A1, B_sh1], [B_hT[c][gi]])
            k.s.barrier()
        k.s.scope = f"att{b}_2_core"
        with contextlib.ExitStack() as c2:
            def sb2(name, shape, dt=F32):
                return c2.enter_context(k.nc.sbuf_tensor(f"s_a2_{b}_{name}", list(shape), dt))

            def ps2(name, shape, dt=F32):
                return c2.enter_context(k.nc.psum_tensor(f"p_a2_{b}_{name}", list(shape), dt))
            Ct, St = sb2("Ct", [128, S], BF16), sb2("St", [128, S], BF16)
            acb = sb2("acb", [128, 640], BF16)
            B_Ct, B_St, B_acb = Buf(), Buf(), Buf()
            vlp = k.dsem(f"a2_{b}_vlp", sw=True)
            k.dma("pool", Ct[:], a["ropeC"][:, :], vlp, writes=[B_Ct], max_dma_last_dim=4096)
            k.dma("pool", St[:], a["ropeS"][:, :], vlp, writes=[B_St], max_dma_last_dim=4096)
            k.dma("pool", acb[:], a["aconst"][:, :], vlp, writes=[B_acb])
            Rm, Mprev, Mcur = acb[:, 0:128], acb[:, 128:256], acb[:, 256:384]
            onesp = [acb[:, 384:512], acb[:, 512:640]]
            qz = [sb2(f"qz{h}", [128, S], BF16) for h in range(2)]
            kT = sb2("kT", [128, S], BF16)
            B_qz = [[Buf() for _ in range(GPB)] for _ in range(2)]
            B_kT = [Buf() for _ in range(GPB)]
            pc = sb2("pc", [128, 1])
            hm = sb2("hm", [128, 2])
            hmb = sb2("hmb", [128, 2], BF16)
            B_pc, B_hm = Buf(), Buf()
            k.dma("sp", pc[:], a["pcol_in"][:, :], vl, writes=[B_pc])
            k.op("dve", lambda e: e.tensor_scalar(out=hm[:, 0:1], in0=pc[:], scalar1=64.0, scalar2=None, op0=ALU.is_lt),
                 [B_pc], [B_hm])
            k.op("dve", lambda e: e.tensor_scalar(out=hm[:, 1:2], in0=pc[:], scalar1=64.0, scalar2=None, op0=ALU.is_ge),
                 [B_pc], [B_hm])
            k.op("dve", lambda e: e.tensor_copy(out=hmb[:], in_=hm[:]), [B_hm], [B_hm])
            NBK = S // 128
            Vp = sb2("Vp", [128, NBK, 2, 128], BF16)
            B_Vp = [Buf() for _ in range(NBK // 4)]
            k.op("pool", lambda e: e.memset(Vp[:].rearrange("p a h f -> p (a h f)"), 0.0), [], B_Vp)
            Nacc, Dacc = sb2("Nacc", [128, S]), sb2("Dacc", [128, S])
            B_Nacc, B_Dacc = Buf(), Buf()
            wq = [sb2(f"wq{i}", [128, 8, 384], BF16) for i in range(2)]
            B_wq = [Buf(), Buf()]
            wq_sem = [k.dsem(f"a2_{b}_wq0", sw=True), k.dsem(f"a2_{b}_wq1", sw=True)]
            qsb_ = [sb2(f"qsb{i}", [128, 512], BF16) for i in range(2)]
            t1s, t2s = sb2("t1s", [128, 512]), sb2("t2s", [128, 512])
            t1_, t2_ = [t1s, t1s], [t2s, t2s]
            Bt1, Bt2 = Buf(), Buf()
            B_qsb_, B_t1_, B_t2_ = [Buf(), Buf()], [Bt1, Bt1], [Bt2, Bt2]
            PT = [sb2(f"PT{i}", [128, 2, 2, 128], BF16) for i in range(2)]
            B_PT = [Buf(), Buf()]
            psQ_ = [ps2(f"psQ{i}", [128, 512]) for i in range(2)]
            psR0 = ps2("psR0", [128, 512])
            psR_ = [psR0, psR0]
            psV = ps2("psV", [128, 4, 128])
            psS = [ps2(f"psS{i}", [128, 2, 2, 128]) for i in range(2)]
            psND = [ps2(f"psND{i}", [128, 2, 128]) for i in range(2)]
            BpsR = Buf()
            B_psQ_, B_psR_, B_psV = [Buf(), Buf()], [BpsR, BpsR], Buf()
            B_psS, B_psND = [Buf(), Buf()], [Buf(), Buf()]
            pit = 0
            allhT = [B_hT[c][gi] for c in range(8) for gi in range(GPB)]
            it = 0

            def wq_load(hp, g):
                wsl = (hp * 3 + g) % 2
                for qi in range(3):
                    col = g * 1536 + qi * 512 + hp * 128
                    for kc in range(8):
                        k.dma("pool", wq[wsl][:, kc, qi * 128:(qi + 1) * 128],
                              w_in[kc * 128:(kc + 1) * 128, col:col + 128], wq_sem[wsl], writes=[B_wq[wsl]],
                              grp=("wq", hp, g))
            wq_load(0, 0)
            for hp in range(4):
                for g in range(3):
                    dil = DIL[g]
                    nb = S // dil // 128
                    wsl = (hp * 3 + g) % 2
                    nxt = hp * 3 + g + 1
                    if nxt < 12:
                        wq_load(nxt // 3, nxt % 3)
                    def tok(r, n, dil=dil):
                        st = r + n * 128 * dil
                        return slice(st, st + 127 * dil + 1, dil)
                    blks = [(r, n) for r in range(dil) for n in range(nb)]
                    vnext = [0]

                    def emit_v(wsl=wsl, blks=blks, tok=tok, vnext=vnext):
                        bi = vnext[0]
                        vnext[0] += 1
                        r, n = blks[bi]
                        for kc in range(8):
                            k.mm(psV[:, bi % 4, :], hT[:, kc, tok(r, n)], wq[wsl][:, kc, 256:384], kc == 0, kc == 7,
                                 reads=[B_wq[wsl]] + [B_hT[kc][gi] for gi in range(GPB)], writes=[B_psV])
                        if bi % 4 == 3:
                            b4 = bi // 4
                            k.op("dve", lambda e, b4=b4: e.tensor_copy(out=Vp[:, b4 * 4:(b4 + 1) * 4, 0, 0:64],
                                                                      in_=psV[:, :, 0:64]), [B_psV], [B_Vp[b4]])
                            k.op("dve", lambda e, b4=b4: e.tensor_copy(out=Vp[:, b4 * 4:(b4 + 1) * 4, 1, 64:128],
                                                                      in_=psV[:, :, 64:128]), [B_psV], [B_Vp[b4]])
                    vper = -(-len(blks) // (2 * GPB))
                    for qi in range(2 if "qk" not in SKIP else 0):
                        for tg in range(GPB):
                            for _ in range(vper):
                                if vnext[0] < len(blks):
                                    emit_v()
                            pi = pit % 2
                            pit += 1
                            psQ, psR, qsb, t1, t2 = psQ_[pi], psR_[pi], qsb_[pi], t1_[pi], t2_[pi]
                            B_psQ, B_psR, B_qsb, B_t1, B_t2 = B_psQ_[pi], B_psR_[pi], B_qsb_[pi], B_t1_[pi], B_t2_[pi]
                            for kc in range(8):
                                k.mm(psQ[:], wq[wsl][:, kc, qi * 128:(qi + 1) * 128], hT[:, kc, tg * 512:(tg + 1) * 512],
                                     kc == 0, kc == 7, reads=[B_wq[wsl], B_hT[kc][tg]], writes=[B_psQ])
                            k.op("act", lambda e, qsb=qsb, psQ=psQ: e.copy(out=qsb[:], in_=psQ[:]), [B_psQ], [B_qsb])
                            k.mm(psR[:], Rm, qsb[:], True, True, reads=[B_acb, B_qsb], writes=[B_psR])
                            k.op("dve", lambda e, tg=tg, t1=t1, psQ=psQ: e.tensor_tensor(
                                out=t1[:], in0=psQ[:], in1=Ct[:, tg * 512:(tg + 1) * 512], op=ALU.mult),
                                 [B_psQ, B_Ct, B_qsb], [B_t1])
                            k.op("dve", lambda e, tg=tg, t2=t2, psR=psR: e.tensor_tensor(
                                out=t2[:], in0=psR[:], in1=St[:, tg * 512:(tg + 1) * 512], op=ALU.mult),
                                 [B_psR, B_St], [B_t2])
                            if qi == 0:
                                k.op("pool", lambda e, t1=t1, t2=t2, qsb=qsb: e.tensor_tensor(
                                    out=qsb[:], in0=t1[:], in1=t2[:], op=ALU.add), [B_t1, B_t2, B_qsb], [B_qsb])
                                for h in range(2):
                                    k.op("dve", lambda e, h=h, tg=tg, qsb=qsb: e.tensor_scalar(
                                        out=qz[h][:, tg * 512:(tg + 1) * 512], in0=qsb[:], scalar1=hmb[:, h:h + 1],
                                        scalar2=None, op0=ALU.mult), [B_qsb, B_hm], [B_qz[h][tg]])
                            else:
                                k.op("pool", lambda e, tg=tg, t1=t1, t2=t2: e.tensor_tensor(
                                    out=kT[:, tg * 512:(tg + 1) * 512], in0=t1[:], in1=t2[:], op=ALU.add),
                                     [B_t1, B_t2], [B_kT[tg]])
                    while vnext[0] < len(blks):
                        emit_v()
                    ablks = blks if "blocks" not in SKIP else []

                    def chunks_of(bi):
                        r, n = ablks[bi]
                        ch = [(1, tok(r, n), Mcur, bi)]
                        if n > 0:
                            ch.append((0, tok(r, n - 1), Mprev, bi - 1))
                        return ch

                    def emit_scores(bi, si):
                        r, n = ablks[bi]
                        cur = tok(r, n)
                        for h in range(2):
                            for (ci, ktok, Mk, vb) in chunks_of(bi):
                                k.mm(psS[si][:, h, ci, :], kT[:, ktok], qz[h][:, cur], True, False,
                                     reads=B_kT + B_qz[h], writes=[B_psS[si]])
                                k.mm(psS[si][:, h, ci, :], ident_b[:], Mk, False, True,
                                     reads=[B_ident_b, B_acb], writes=[B_psS[si]])

                    if ablks:
                        emit_scores(0, it % 2)
                    for bi, (r, n) in enumerate(ablks):
                        si = it % 2
                        it += 1
                        cur = tok(r, n)
                        chunks = chunks_of(bi)
                        if bi + 1 < len(ablks):
                            emit_scores(bi + 1, it % 2)
                        k.op("act", lambda e, si=si: e.activation(
                            out=PT[si][:].rearrange("p h c q -> p (h c q)"),
                            in_=psS[si][:].rearrange("p h c q -> p (h c q)"), func=AF.Exp, scale=0.125),
                            [B_psS[si]], [B_PT[si]])
                        nmm = 2 * len(chunks)
                        for which in range(2):
                            ii = 0
                            for h in range(2):
                                for (ci, ktok, Mk, vb) in chunks:
                                    lhs = Vp[:, vb, h, :] if which == 0 else onesp[h]
                                    k.mm(psND[si][:, which, :], lhs, PT[si][:, h, ci, :], ii == 0, ii == nmm - 1,
                                         reads=[B_Vp[vb // 4], B_PT[si], B_acb], writes=[B_psND[si]])
                                    ii += 1
                        if g == 0:
                            k.op("dve", lambda e, si=si, cur=cur: e.tensor_copy(out=Nacc[:, cur], in_=psND[si][:, 0, :]),
                                 [B_psND[si]], [B_Nacc])
                            k.op("dve", lambda e, si=si, cur=cur: e.tensor_copy(out=Dacc[:, cur], in_=psND[si][:, 1, :]),
                                 [B_psND[si]], [B_Dacc])
                        else:
                            k.op("dve", lambda e, si=si, cur=cur: e.tensor_tensor(out=Nacc[:, cur], in0=psND[si][:, 0, :],
                                                                                 in1=Nacc[:, cur], op=ALU.add),
                                 [B_psND[si], B_Nacc], [B_Nacc])
                            k.op("dve", lambda e, si=si, cur=cur: e.tensor_tensor(out=Dacc[:, cur], in0=psND[si][:, 1, :],
                                                                                 in1=Dacc[:, cur], op=ALU.add),
                                 [B_psND[si], B_Dacc], [B_Dacc])
                if "norm" in SKIP:
                    continue
                k.op("act", lambda e: e.activation(out=Dacc[:], in_=Dacc[:], func=AF.Ln), [B_Dacc], [B_Dacc])
                k.op("act", lambda e: e.activation(out=Dacc[:], in_=Dacc[:], func=AF.Exp, scale=-1.0),
                     [B_Dacc], [B_Dacc])
                k.op("dve", lambda e, hp=hp: e.tensor_tensor(out=oT[:, hp, :], in0=Nacc[:], in1=Dacc[:], op=ALU.mult),
                     [B_Nacc, B_Dacc], [B_oT[hp]])
            k.s.barrier()
        k.s.scope = f"att{b}_3_out"
        with contextlib.ExitStack() as c3:
            def sb3(name, shape, dt=F32):
                return c3.enter_context(k.nc.sbuf_tensor(f"s_a3_{b}_{name}", list(shape), dt))
            wof = sb3("wof", [128, 4, D])
            wob = sb3("wob", [128, 4, D], BF16)
            g1bc = sb3("g1bc", [128, D])
            B_wof, B_wob, B_g1bc = Buf(), Buf(), Buf()
            k.dma("sp", wof[:], w_out.rearrange("(c p) d -> p c d", p=128), vl, writes=[B_wof])
            k.dma("sp", g1bc[:], modrow[L, b:b + 1, 2 * D:3 * D].partition_broadcast(128), vl,
                  reads=[B_modrow[L]], writes=[B_g1bc])
            for c in range(4):
                k.op("dve", lambda e, c=c: e.tensor_tensor(out=wob[:, c, :], in0=wof[:, c, :], in1=g1bc[:], op=ALU.mult),
                     [B_wof, B_g1bc], [B_wob])
            xt = [sb3(f"xt{i}", [128, 4, D]) for i in range(3)]
            B_xt = [Buf(), Buf(), Buf()]
            xt_ld = [k.dsem(f"a3_{b}_xt_ld{i}") for i in range(3)]
            xt_st = [k.dsem(f"a3_{b}_xt_st{i}") for i in range(3)]
            psY = [c3.enter_context(k.nc.psum_tensor(f"p_a3_{b}_psY{i}", [128, 512], F32)) for i in range(2)]
            B_psY = [Buf(), Buf()]
            def a3_load(gi):
                G = b * GPB + gi
                sl = gi % 3
                k.dma("sp", xt[sl][:], xr[G * 512:(G + 1) * 512, :].rearrange("(j p) d -> p j d", p=128), xt_ld[sl],
                      reads=[B_xr[G]], writes=[B_xt[sl]])
            a3_load(0)
            for gi in range(GPB if "p3" not in SKIP else 0):
                G = b * GPB + gi
                sl = gi % 3
                if gi + 1 < GPB:
                    a3_load(gi + 1)
                for j in range(4):
                    for dh in range(2):
                        yi = (j * 2 + dh) % 2
                        t0 = gi * 512 + j * 128
                        for c in range(4):
                            k.mm(psY[yi][:], oT[:, c, t0:t0 + 128], wob[:, c, dh * 512:(dh + 1) * 512], c == 0, c == 3,
                                 reads=[B_oT[c], B_wob], writes=[B_psY[yi]])
                        k.op("dve", lambda e, j=j, dh=dh, yi=yi, sl=sl: e.tensor_tensor(
                            out=xt[sl][:, j, dh * 512:(dh + 1) * 512], in0=psY[yi][:],
                            in1=xt[sl][:, j, dh * 512:(dh + 1) * 512], op=ALU.add), [B_psY[yi], B_xt[sl]], [B_xt[sl]])
                k.dma("sp", xr[G * 512:(G + 1) * 512, :].rearrange("(j p) d -> p j d", p=128), xt[sl][:], xt_st[sl],
                      reads=[B_xt[sl]], writes=[B_xr[G]])
            k.s.barrier()


def moe_layer(k, L, a):
    NB, S, T = k.NB, k.S, k.T
    NT, NG, GPB = T // 128, T // 512, S // 512
    NBLK = a["NBLK"]
    xr, B_xr, modrow, B_modrow = a["xr"], a["B_xr"], a["modrow"], a["B_modrow"]
    ident_f, B_ident_f, ident_b, B_ident_b = a["ident_f"], a["B_ident_f"], a["ident_b"], a["B_ident_b"]
    hs, xs, ys = a["hs"], a["xs"], a["ys"]
    B_hs = [Buf() for _ in range(NG)]
    pfx = f"m{L}_"

    E1all = k.sbp(pfx + "E1all", [128, NT, 32], F32)
    E2all = k.sbp(pfx + "E2all", [128, NT, 32], F32)
    Oall = k.sbp(pfx + "Oall", [128, NT, 32], BF16)
    Wall = k.sbp(pfx + "Wall", [128, NT, 2], F32)
    B_E1 = [Buf() for _ in range(NT)]
    B_E2 = [Buf() for _ in range(NT)]
    B_O = [Buf() for _ in range(NT)]
    B_W = [Buf() for _ in range(NT)]
    g2bc = [k.sbp(pfx + f"g2bc{b}", [128, D], F32) for b in range(NB)]
    B_g2bc = [Buf() for _ in range(NB)]
    d1i = k.sbp(pfx + "d1i", [128, NT], I32)
    d2i = k.sbp(pfx + "d2i", [128, NT], I32)
    bei = k.sbp(pfx + "bei", [128, 2, NBLK], I32)
    B_d1i, B_d2i, B_bei = Buf(), Buf(), Buf()
    nusedi = k.sbp(pfx + "nusedi", [128, 1], I32)
    B_nusedi = Buf()
    vl = k.dsem(pfx + "vl")
    for b in range(NB):
        k.dma("sp", g2bc[b][:], modrow[L, b:b + 1, 5 * D:6 * D].partition_broadcast(128), vl,
              reads=[B_modrow[L]], writes=[B_g2bc[b]], grp="g2")

    k.s.scope = pfx + "M1_router"
    with contextlib.ExitStack() as c1:
        def sb1(name, shape, dt=F32):
            return c1.enter_context(k.nc.sbuf_tensor("s_" + pfx + name, list(shape), dt))

        def ps1(name, shape, dt=F32):
            return c1.enter_context(k.nc.psum_tensor("p_" + pfx + name, list(shape), dt))
        A2bc = [sb1(f"A2bc{b}", [128, D]) for b in range(NB)]
        sh2bc = [sb1(f"sh2bc{b}", [128, D]) for b in range(NB)]
        gfbc = sb1("gfbc", [128, D])
        B_A2bc = [Buf() for _ in range(NB)]
        B_sh2bc = [Buf() for _ in range(NB)]
        B_gfbc = Buf()
        Wr = sb1("Wr", [128, 8, 36])
        brbc = sb1("brbc", [128, 36])
        B_Wr, B_brbc = Buf(), Buf()
        k.dma("sp", gfbc[:], a["gffn"][L:L + 1, :].partition_broadcast(128), vl, writes=[B_gfbc], grp="g2")
        k.dma("sp", Wr[:], a["Wr_in"][L].rearrange("(kc p) f -> p kc f", p=128), vl, writes=[B_Wr], grp="g2")
        k.dma("sp", brbc[:], a["br_in"][L:L + 1, :].partition_broadcast(128), vl, writes=[B_brbc], grp="g2")
        for b in range(NB):
            k.dma("sp", sh2bc[b][:], modrow[L, b:b + 1, 3 * D:4 * D].partition_broadcast(128), vl,
                  reads=[B_modrow[L]], writes=[B_sh2bc[b]], grp="g2")
            k.dma("sp", A2bc[b][:], modrow[L, b:b + 1, 4 * D:5 * D].partition_broadcast(128), vl,
                  reads=[B_modrow[L]], writes=[B_A2bc[b]], grp="g2")
            k.op("dve", lambda e, b=b: e.scalar_tensor_tensor(out=A2bc[b][:], in0=A2bc[b][:], scalar=1.0, in1=gfbc[:],
                                                             op0=ALU.add, op1=ALU.mult),
                 [B_A2bc[b], B_gfbc], [B_A2bc[b]])
        xt = [sb1(f"xt{i}", [128, 4, D]) for i in range(2)]
        B_xt = [Buf(), Buf()]
        xt_ld = [k.dsem(pfx + "xt_ld0"), k.dsem(pfx + "xt_ld1")]
        junk = sb1("junk", [128, D], BF16)
        B_junk = Buf()
        ss, rstd = sb1("ss", [128, 4]), sb1("rstd", [128, 4])
        B_ss, B_rstd = Buf(), Buf()
        h2_ = [sb1(f"h2_{i}", [128, 4, D]) for i in range(2)]
        B_h2_ = [[Buf() for _ in range(4)] for _ in range(2)]
        h2b = [sb1(f"h2b{i}", [128, 4, D], BF16) for i in range(2)]
        B_h2b = [Buf(), Buf()]
        h2b_st = [k.dsem(pfx + "h2b_st0"), k.dsem(pfx + "h2b_st1")]
        pTf = [ps1(f"pTf{i}", [128, 512]) for i in range(2)]
        B_pTf = [Buf(), Buf()]
        hT2 = sb1("hT2", [128, 8, 512])
        B_hT2 = [Buf() for _ in range(8)]
        pslg = ps1("pslg", [128, 4, 36])
        B_pslg = Buf()
        lg4 = sb1("lg4", [128, 4, 36])
        gmax, gsum = sb1("gmax", [128, 4]), sb1("gsum", [128, 4])
        gsh = sb1("gsh", [128, 4, 4])
        ohg4 = sb1("ohg4", [128, 4, 4])
        tmp4 = sb1("tmp4", [128, 4, 4, 8])
        sel4, sel4b = sb1("sel4", [128, 4, 8]), sb1("sel4b", [128, 4, 8])
        oh1_4, oh2_4 = sb1("oh1_4", [128, 4, 8]), sb1("oh2_4", [128, 4, 8])
        m1, m2, dlt = sb1("m1", [128, 4]), sb1("m2", [128, 4]), sb1("dlt", [128, 4])
        B_lg, B_sm, B_sm2, B_ohg, B_ge, B_sel, B_selb, B_oh1, B_oh2, B_tmp4, B_m1, B_m2, B_dlt = (Buf() for _ in range(13))

        def load_x(G):
            sl = G % 2
            k.dma("sp", xt[sl][:], xr[G * 512:(G + 1) * 512, :].rearrange("(j p) d -> p j d", p=128),
                  xt_ld[sl], reads=[B_xr[G]], writes=[B_xt[sl]])
        def stage1(G):
            b = G // GPB
            sl = G % 2
            xt_t, B_x = xt[sl], B_xt[sl]
            h2, B_h2 = h2_[sl], B_h2_[sl]
            for j in range(4):
                k.op("act", lambda e, j=j, xt_t=xt_t: e.activation(out=junk[:], in_=xt_t[:, j, :], func=AF.Square,
                                                                  accum_out=ss[:, j:j + 1]), [B_x], [B_junk, B_ss])
            k.op("dve", lambda e: e.tensor_scalar(out=rstd[:], in0=ss[:], scalar1=1.0 / D, scalar2=1e-6,
                                                  op0=ALU.mult, op1=ALU.add), [B_ss], [B_rstd])
            k.op("act", lambda e: e.activation(out=rstd[:], in_=rstd[:], func=AF.Sqrt), [B_rstd], [B_rstd])
            k.op("dve", lambda e: e.reciprocal(out=rstd[:], in_=rstd[:]), [B_rstd], [B_rstd])
            for j in range(4):
                k.op("dve", lambda e, j=j, xt_t=xt_t, b=b, h2=h2: e.scalar_tensor_tensor(
                    out=h2[:, j, :], in0=xt_t[:, j, :], scalar=rstd[:, j:j + 1], in1=A2bc[b][:],
                    op0=ALU.mult, op1=ALU.mult), [B_x, B_rstd, B_A2bc[b]], [B_h2[j]])
                k.op("pool" if j > 0 else "dve", lambda e, j=j, b=b, h2=h2: e.tensor_tensor(
                    out=h2[:, j, :], in0=h2[:, j, :], in1=sh2bc[b][:], op=ALU.add),
                     [B_h2[j], B_sh2bc[b]], [B_h2[j]])

        def stage1b(G):
            sl = G % 2
            h2, B_h2 = h2_[sl], B_h2_[sl]
            for j in range(4):
                k.op("act", lambda e, j=j, sl=sl, h2=h2: e.copy(out=h2b[sl][:, j, :], in_=h2[:, j, :]),
                     [B_h2[j]], [B_h2b[sl]])
            k.dma("sp", hs[G * 512:(G + 1) * 512, :].rearrange("(j p) d -> p j d", p=128), h2b[sl][:], h2b_st[sl],
                  reads=[B_h2b[sl]], writes=[B_hs[G]])

        load_x(0)
        if NG > 1:
            load_x(1)
        stage1(0)
        stage1b(0)
        for G in range(NG):
            b = G // GPB
            sl = G % 2
            h2, B_h2 = h2_[sl], B_h2_[sl]
            for c in range(8):
                p = c % 2
                for j in range(4):
                    k.tr(pTf[p][:, j * 128:(j + 1) * 128], h2[:, j, c * 128:(c + 1) * 128], ident_f[:],
                         reads=[B_h2[j], B_ident_f], writes=[B_pTf[p]])
                if c % 4 != 3:
                    k.op("act", lambda e, c=c, p=p: e.copy(out=hT2[:, c, :], in_=pTf[p][:]), [B_pTf[p]], [B_hT2[c]])
                else:
                    k.op("dve", lambda e, c=c, p=p: e.tensor_copy(out=hT2[:, c, :], in_=pTf[p][:]),
                         [B_pTf[p]], [B_hT2[c]])
            for j in range(4):
                for kc in range(8):
                    k.mm(pslg[:, j, :], hT2[:, kc, j * 128:(j + 1) * 128], Wr[:, kc, :], kc == 0, kc == 7,
                         reads=[B_hT2[kc], B_Wr], writes=[B_pslg])
            if G + 1 < NG:
                stage1(G + 1)
            if G + 2 < NG:
                load_x(G + 2)
            V = "dve"
            i0_ = G * 4
            J = 4

            def bc(ap, shape):
                return ap.to_broadcast(list(shape))
            k.op(V, lambda e: e.tensor_tensor(out=lg4[:], in0=pslg[:], in1=bc(brbc[:].unsqueeze(1), [128, J, 36]),
                                              op=ALU.add), [B_pslg, B_brbc], [B_lg])
            k.op(V, lambda e: e.tensor_reduce(out=gmax[:], in_=lg4[:, :, 0:4], axis=AX.X, op=ALU.max), [B_lg], [B_sm])
            k.op(V, lambda e: e.tensor_tensor(out=gsh[:], in0=lg4[:, :, 0:4], in1=bc(gmax[:].unsqueeze(2), [128, J, 4]),
                                              op=ALU.subtract), [B_lg, B_sm], [B_ge])
            k.op(V, lambda e: e.tensor_tensor(out=ohg4[:], in0=lg4[:, :, 0:4], in1=bc(gmax[:].unsqueeze(2), [128, J, 4]),
                                              op=ALU.is_equal), [B_lg, B_sm], [B_ohg])
            k.op("act", lambda e: e.activation(out=gsh[:], in_=gsh[:], func=AF.Exp), [B_ge], [B_ge])
            k.op(V, lambda e: e.tensor_reduce(out=gsum[:], in_=gsh[:], axis=AX.X, op=ALU.add), [B_ge], [B_sm2])
            k.op(V, lambda e: e.reciprocal(out=gsum[:], in_=gsum[:]), [B_sm2], [B_sm2])
            k.op(V, lambda e: e.tensor_tensor(out=tmp4[:], in0=lg4[:, :, 4:36].rearrange("p j (g e) -> p j g e", g=4),
                                              in1=bc(ohg4[:].unsqueeze(3), [128, J, 4, 8]), op=ALU.mult),
                 [B_lg, B_ohg], [B_tmp4])
            k.op(V, lambda e: e.tensor_reduce(out=sel4[:], in_=tmp4[:].rearrange("p j g e -> p j e g"), axis=AX.X,
                                              op=ALU.add), [B_tmp4], [B_sel])
            k.op(V, lambda e: e.tensor_reduce(out=m1[:], in_=sel4[:], axis=AX.X, op=ALU.max), [B_sel], [B_m1])
            k.op(V, lambda e: e.tensor_tensor(out=oh1_4[:], in0=sel4[:], in1=bc(m1[:].unsqueeze(2), [128, J, 8]),
                                              op=ALU.is_equal), [B_sel, B_m1], [B_oh1])
            k.op(V, lambda e: e.scalar_tensor_tensor(out=sel4b[:], in0=oh1_4[:], scalar=-1.0e30, in1=sel4[:],
                                                     op0=ALU.mult, op1=ALU.add), [B_oh1, B_sel], [B_selb])
            k.op(V, lambda e: e.tensor_reduce(out=m2[:], in_=sel4b[:], axis=AX.X, op=ALU.max), [B_selb], [B_m2])
            k.op(V, lambda e: e.tensor_tensor(out=oh2_4[:], in0=sel4b[:], in1=bc(m2[:].unsqueeze(2), [128, J, 8]),
                                              op=ALU.is_equal), [B_selb, B_m2], [B_oh2])
            k.op(V, lambda e: e.tensor_tensor(out=dlt[:], in0=m2[:], in1=m1[:], op=ALU.subtract), [B_m1, B_m2], [B_dlt])
            k.op("act", lambda e: e.activation(out=dlt[:], in_=dlt[:], func=AF.Exp), [B_dlt], [B_dlt])
            k.op(V, lambda e: e.tensor_scalar(out=dlt[:], in0=dlt[:], scalar1=1.0, scalar2=None, op0=ALU.add),
                 [B_dlt], [B_dlt])
            k.op(V, lambda e: e.reciprocal(out=dlt[:], in_=dlt[:]), [B_dlt], [B_dlt])
            Bws = B_W[i0_:i0_ + J]
            k.op(V, lambda e, i0_=i0_: e.tensor_tensor(out=Wall[:, i0_:i0_ + J, 0], in0=gsum[:], in1=dlt[:], op=ALU.mult),
                 [B_sm2, B_dlt], Bws)
            k.op(V, lambda e, i0_=i0_: e.tensor_tensor(out=Wall[:, i0_:i0_ + J, 1], in0=gsum[:],
                                                      in1=Wall[:, i0_:i0_ + J, 0], op=ALU.subtract),
                 [B_sm2] + Bws, Bws)
            for (Eall, oh, B_oh, B_E) in ((E1all, oh1_4, B_oh1, B_E1), (E2all, oh2_4, B_oh2, B_E2)):
                k.op(V, lambda e, Eall=Eall, oh=oh, i0_=i0_: e.tensor_tensor(
                    out=Eall[:, i0_:i0_ + J, :].rearrange("p j (g e) -> p j g e", g=4),
                    in0=bc(ohg4[:].unsqueeze(3), [128, J, 4, 8]), in1=bc(oh[:].unsqueeze(2), [128, J, 4, 8]),
                    op=ALU.mult), [B_ohg, B_oh], B_E[i0_:i0_ + J])
            k.op("pool", lambda e, i0_=i0_: e.tensor_tensor(out=Oall[:, i0_:i0_ + J, :], in0=E1all[:, i0_:i0_ + J, :],
                                                           in1=E2all[:, i0_:i0_ + J, :], op=ALU.add),
                 B_E1[i0_:i0_ + J] + B_E2[i0_:i0_ + J], B_O[i0_:i0_ + J])
            if G + 1 < NG:
                stage1b(G + 1)
        k.s.barrier()
    dbgsem = k.dsem(pfx + "dbg")
    B_dbg = Buf()
    if a.get("stage") == "M1":
        dbg = a["dbg"]
        k.dma("sp", dbg[:, 0:NT * 2], Wall[:].rearrange("p i w -> p (i w)"), dbgsem, reads=B_W, writes=[B_dbg])
        k.dma("sp", dbg[:, 1024:1024 + NT * 32], E1all[:].rearrange("p i e -> p (i e)"), dbgsem, reads=B_E1, writes=[B_dbg])
        k.dma("sp", dbg[:, 2048:2048 + NT * 32], E2all[:].rearrange("p i e -> p (i e)"), dbgsem, reads=B_E2, writes=[B_dbg])
        return [B_dbg]

    k.s.scope = pfx + "M2_index"
    with contextlib.ExitStack() as c2:
        def sb2(name, shape, dt=F32):
            return c2.enter_context(k.nc.sbuf_tensor("s_" + pfx + name, list(shape), dt))

        def ps2(name, shape, dt=F32):
            return c2.enter_context(k.nc.psum_tensor("p_" + pfx + name, list(shape), dt))
        trif, trib, onesb = sb2("trif", [128, 128]), sb2("trib", [128, 128], BF16), sb2("onesb", [128, 128], BF16)
        B_trif, B_trib, B_onesb = Buf(), Buf(), Buf()
        k.dma("sp", trif[:], a["tri_in"][:, :], vl, writes=[B_trif])
        k.op("dve", lambda e: e.tensor_copy(out=trib[:], in_=trif[:]), [B_trif], [B_trib])
        k.op("dve", lambda e: e.memset(onesb[:], 1.0), [], [B_onesb])
        base = sb2("base", [128, NT, 32])
        tot = sb2("tot", [128, NT, 32])
        B_base, B_tot = Buf(), Buf()
        psw = [ps2(f"psw{i}", [128, 512]) for i in range(2)]
        B_psw = [Buf(), Buf()]
        TPC = 16
        nch = (NT + TPC - 1) // TPC
        for ci in range(nch):
            t0, t1 = ci * TPC, min(NT, (ci + 1) * TPC)
            w = (t1 - t0) * 32
            for which, (lhs, B_l, dst, B_d) in enumerate(((trib, B_trib, base, B_base), (onesb, B_onesb, tot, B_tot))):
                k.mm(psw[which][:, 0:w], lhs[:], Oall[:, t0:t1, :], True, True,
                     reads=[B_l] + B_O[t0:t1], writes=[B_psw[which]])
                k.op("dve", lambda e, which=which, dst=dst, t0=t0, t1=t1, w=w: e.tensor_copy(
                    out=dst[:, t0:t1, :], in_=psw[which][:, 0:w]), [B_psw[which]], [B_d])
        cnt = sb2("cnt", [128, 32])
        nblk = sb2("nblk", [128, 32])
        cum = sb2("cum", [128, 32])
        carry = sb2("carry", [128, 32])
        tmp32 = sb2("tmp32", [128, 32])
        B_cnt, B_nblk, B_cum, B_carry, B_tmp32 = Buf(), Buf(), Buf(), Buf(), Buf()
        V = "dve"
        k.op(V, lambda e: e.tensor_reduce(out=cnt[:], in_=tot[:].rearrange("p i e -> p e i"), axis=AX.X, op=ALU.add),
             [B_tot], [B_cnt])
        k.op(V, lambda e: e.tensor_scalar(out=nblk[:], in0=cnt[:], scalar1=0.0, scalar2=None, op0=ALU.is_gt),
             [B_cnt], [B_nblk])
        for j in range(1, (2 * T) // 512 + 1):
            k.op(V, lambda e, j=j: e.scalar_tensor_tensor(out=nblk[:], in0=cnt[:], scalar=512.0 * j, in1=nblk[:],
                                                         op0=ALU.is_gt, op1=ALU.add), [B_cnt, B_nblk], [B_nblk])
        k.op(V, lambda e: e.tensor_copy(out=cum[:], in_=nblk[:]), [B_nblk], [B_cum])
        for ee in range(1, 32):
            k.op(V, lambda e, ee=ee: e.tensor_tensor(out=cum[:, ee:ee + 1], in0=cum[:, ee - 1:ee], in1=nblk[:, ee:ee + 1],
                                                    op=ALU.add), [B_cum, B_nblk], [B_cum])
        k.op(V, lambda e: e.tensor_tensor(out=carry[:], in0=cum[:], in1=nblk[:], op=ALU.subtract),
             [B_cum, B_nblk], [B_carry])
        k.op(V, lambda e: e.tensor_scalar(out=carry[:], in0=carry[:], scalar1=512.0, scalar2=None, op0=ALU.mult),
             [B_carry], [B_carry])
        for i in range(NT):
            k.op(V, lambda e, i=i: e.tensor_tensor(out=base[:, i, :], in0=base[:, i, :], in1=carry[:], op=ALU.add),
                 [B_base, B_carry], [B_base])
            if i + 1 < NT:
                k.op(V, lambda e, i=i: e.tensor_tensor(out=carry[:], in0=carry[:], in1=tot[:, i, :], op=ALU.add),
                     [B_carry, B_tot], [B_carry])
        dtmp = sb2("dtmp", [128, NT, 32])
        dfl = sb2("dfl", [128, NT])
        B_dtmp, B_dfl = Buf(), Buf()
        for (Eall, B_E, di, B_di) in ((E1all, B_E1, d1i, B_d1i), (E2all, B_E2, d2i, B_d2i)):
            k.op(V, lambda e, Eall=Eall: e.tensor_tensor(out=dtmp[:], in0=Eall[:], in1=base[:], op=ALU.mult),
                 list(B_E) + [B_base], [B_dtmp])
            k.op(V, lambda e: e.tensor_reduce(out=dfl[:], in_=dtmp[:], axis=AX.X, op=ALU.add), [B_dtmp], [B_dfl])
            k.op(V, lambda e, di=di: e.tensor_copy(out=di[:], in_=dfl[:]), [B_dfl], [B_di])
        bef = sb2("bef", [128, NBLK])
        B_bef = Buf()
        for j in range(NBLK):
            k.op(V, lambda e, j=j: e.tensor_scalar(out=tmp32[:], in0=cum[:], scalar1=float(j), scalar2=None,
                                                  op0=ALU.is_le, op1=ALU.add, accum_out=bef[:, j:j + 1]),
                 [B_cum], [B_tmp32, B_bef])
        oob = sb2("oob", [128, NBLK])
        B_oob = Buf()
        k.op(V, lambda e: e.tensor_scalar(out=oob[:], in0=bef[:], scalar1=32.0, scalar2=float(OOB_BIG),
                                          op0=ALU.is_ge, op1=ALU.mult), [B_bef], [B_oob])
        k.op(V, lambda e: e.tensor_scalar(out=bef[:], in0=bef[:], scalar1=31.0, scalar2=None, op0=ALU.min),
             [B_bef], [B_bef])
        k.op(V, lambda e: e.tensor_copy(out=nusedi[:], in_=cum[:, 31:32]), [B_cum], [B_nusedi])
        pcol = sb2("pcol", [128, 1])
        B_pcol = Buf()
        k.dma("sp", pcol[:], a["pcol_in"][:, :], vl, writes=[B_pcol])
        k.op(V, lambda e: e.tensor_scalar(out=bef[:], in0=bef[:], scalar1=float(L * 32), scalar2=128.0,
                                          op0=ALU.add, op1=ALU.mult), [B_bef], [B_bef])
        k.op(V, lambda e: e.tensor_scalar(out=bef[:], in0=bef[:], scalar1=pcol[:, 0:1], scalar2=None, op0=ALU.add),
             [B_bef, B_pcol], [B_bef])
        k.op(V, lambda e: e.tensor_scalar(out=bef[:], in0=bef[:], scalar1=2.0, scalar2=None, op0=ALU.mult),
             [B_bef], [B_bef])
        k.op(V, lambda e: e.tensor_tensor(out=bef[:], in0=bef[:], in1=oob[:], op=ALU.add), [B_bef, B_oob], [B_bef])
        k.op(V, lambda e: e.tensor_copy(out=bei[:, 0, :], in_=bef[:]), [B_bef], [B_bei])
        k.op(V, lambda e: e.tensor_scalar(out=bef[:], in0=bef[:], scalar1=1.0, scalar2=None, op0=ALU.add),
             [B_bef], [B_bef])
        k.op(V, lambda e: e.tensor_copy(out=bei[:, 1, :], in_=bef[:]), [B_bef], [B_bei])
        k.s.barrier()
        if a.get("stage") == "M2":
            dbg = a["dbg"].bitcast(I32)
            k.dma("sp", dbg[:, 0:NT], d1i[:], dbgsem, reads=[B_d1i], writes=[B_dbg])
            k.dma("sp", dbg[:, 1024:1024 + NT], d2i[:], dbgsem, reads=[B_d2i], writes=[B_dbg])
            k.dma("sp", dbg[:, 2048:2048 + NBLK], bei[:, 0, :], dbgsem, reads=[B_bei], writes=[B_dbg])
            k.s.barrier()
    if a.get("stage") == "M2":
        return [B_dbg]

    k.s.scope = pfx + "M3_scatter"
    with contextlib.ExitStack() as c3:
        NS3 = 6
        hsb = [c3.enter_context(k.nc.sbuf_tensor(f"s_{pfx}hsb{i}", [128, D], BF16)) for i in range(NS3)]
        B_hsb = [Buf() for _ in range(NS3)]
        hsb_ld = [k.dsem(pfx + f"hsb_ld{i}") for i in range(NS3)]
        sc_sem = [k.dsem(pfx + f"sc{i}", sw=True) for i in range(NS3)]
        B_xs = Buf()

        def m3_load(i):
            sl = i % NS3
            k.dma("sp", hsb[sl][:], hs[i * 128:(i + 1) * 128, :], hsb_ld[sl], reads=[B_hs[i // 4]], writes=[B_hsb[sl]])
        for i in range(min(NS3 - 1, NT)):
            m3_load(i)
        for i in range(NT):
            sl = i % NS3
            if i + NS3 - 1 < NT:
                m3_load(i + NS3 - 1)
            for (di, B_di) in ((d1i, B_d1i), (d2i, B_d2i)):
                k.s.add("pool", lambda e, sl=sl, di=di, i=i: e.indirect_dma_start(
                    out=xs[:, :], out_offset=bass.IndirectOffsetOnAxis(ap=di[:, i:i + 1], axis=0),
                    in_=hsb[sl][:], in_offset=None), [B_hsb[sl], B_di], [B_xs], dsem=sc_sem[sl], grp=("sc", i))
        k.s.barrier()
    if a.get("stage") == "M3":
        return []

    k.s.scope = pfx + "M4_experts"
    with contextlib.ExitStack() as c4:
        def sb4(name, shape, dt=F32):
            return c4.enter_context(k.nc.sbuf_tensor("s_" + pfx + name, list(shape), dt))

        def ps4(name, shape, dt=F32):
            return c4.enter_context(k.nc.psum_tensor("p_" + pfx + name, list(shape), dt))
        wg = [sb4(f"wg{i}", [128, 8, 512], BF16) for i in range(2)]
        wu = [sb4(f"wu{i}", [128, 8, 512], BF16) for i in range(2)]
        wd = [sb4(f"wd{i}", [128, 4, D], BF16) for i in range(2)]
        B_wg, B_wu, B_wd = [Buf(), Buf()], [Buf(), Buf()], [Buf(), Buf()]
        wsem = [[k.dsem(pfx + f"w{n}{i}", sw=True) for i in range(2)] for n in "gud"]
        xb = [sb4(f"xb{i}", [128, 4, D], BF16) for i in range(2)]
        B_xb = [Buf(), Buf()]
        xb_ld = [k.dsem(pfx + "xb_ld0"), k.dsem(pfx + "xb_ld1")]
        xsT_ = [sb4(f"xsT{i}", [128, 8, 512], BF16) for i in range(2)]
        B_xsT_ = [[Buf() for _ in range(8)] for _ in range(2)]
        pTx = [ps4(f"pTx{i}", [128, 512], BF16) for i in range(2)]
        B_pTx = [Buf(), Buf()]
        psG = [ps4(f"psG{i}", [128, 512]) for i in range(2)]
        psU = [ps4(f"psU{i}", [128, 512]) for i in range(2)]
        B_psG, B_psU = [Buf(), Buf()], [Buf(), Buf()]
        sg = [sb4(f"sg{i}", [128, 512]) for i in range(2)]
        B_sg = [Buf(), Buf()]
        hTe = sb4("hTe", [128, 4, 512], BF16)
        B_hTe = [Buf() for _ in range(4)]
        psY = [ps4(f"psY{i}", [128, 512]) for i in range(2)]
        B_psY = [Buf(), Buf()]
        yb = [sb4(f"yb{i}", [128, 4, D]) for i in range(2)]
        B_yb = [Buf(), Buf()]
        yb_st = [k.dsem(pfx + "yb_st0"), k.dsem(pfx + "yb_st1")]
        B_ys = Buf()
        gw, uw, dw = a["gw"], a["uw"], a["dw"]
        gw2 = gw.rearrange("r (h f) -> (r h) f", h=2)
        uw2 = uw.rearrange("r (h f) -> (r h) f", h=2)
        dw2 = dw.rearrange("r (h f) -> (r h) f", h=2)

        breg = k.bound_reg()
        k.s.add("pool", lambda e: e.reg_mov(breg, 2 * 2 * 32 * 128 - 1), [], [])

        def wload(j, sl):
            for (dst, B_d, tab, ws, nh) in ((wg[sl], B_wg[sl], gw2, wsem[0][sl], 4), (wu[sl], B_wu[sl], uw2, wsem[1][sl], 4),
                                            (wd[sl], B_wd[sl], dw2, wsem[2][sl], 2)):
                for h in range(2):
                    k.s.add("pool", lambda e, dst=dst, tab=tab, j=j, h=h, nh=nh: e.indirect_dma_start(
                        out=dst[:, h * nh:(h + 1) * nh, :].rearrange("p a f -> p (a f)"), out_offset=None, in_=tab,
                        in_offset=bass.IndirectOffsetOnAxis(ap=bei[:, h, j:j + 1], axis=0),
                        bounds_check=breg, oob_is_err=False), [B_bei], [B_d], dsem=ws,
                        grp=("w", j))

        def xload(j, sl):
            k.dma("sp", xb[sl][:], xs[j * 512:(j + 1) * 512, :].rearrange("(t p) d -> p t d", p=128), xb_ld[sl],
                  reads=[B_xs], writes=[B_xb[sl]])
        def xpose(j, cs):
            sl = j % 2
            for c in cs:
                p = c % 2
                for t in range(4):
                    k.tr(pTx[p][:, t * 128:(t + 1) * 128], xb[sl][:, t, c * 128:(c + 1) * 128], ident_b[:],
                         reads=[B_xb[sl], B_ident_b], writes=[B_pTx[p]])
                if c % 2 == 0:
                    k.op("act", lambda e, c=c, p=p, sl=sl: e.copy(out=xsT_[sl][:, c, :], in_=pTx[p][:]),
                         [B_pTx[p]], [B_xsT_[sl][c]])
                else:
                    k.op("dve", lambda e, c=c, p=p, sl=sl: e.tensor_copy(out=xsT_[sl][:, c, :], in_=pTx[p][:]),
                         [B_pTx[p]], [B_xsT_[sl][c]])
        regs = k.nused_regs()
        for eng_ in ENGS:
            k.s.add(eng_, lambda e, eng_=eng_: e.reg_load(regs[eng_], nusedi[0:1, 0:1]), [B_nusedi], [])
        JS = (2 * T) // 512
        wload(0, 0)
        xload(0, 0)
        if NBLK > 1:
            wload(1, 1)
            xload(1, 1)
        xpose(0, range(8))
        for j in range(NBLK):
            sl = j % 2
            k.s.guard = (regs, (j if not os.environ.get("DYN_NEVER") else -1)) if (j >= max(JS, int(os.environ.get("DYN_FROM", "0"))) and DYN_SKIP) else None
            xsT, B_xsT = xsT_[sl], B_xsT_[sl]
            for fc in range(4):
                q = fc % 2
                for kc in range(8):
                    k.mm(psG[q][:], wg[sl][:, kc, fc * 128:(fc + 1) * 128], xsT[:, kc, :], kc == 0, kc == 7,
                         reads=[B_wg[sl], B_xsT[kc]], writes=[B_psG[q]])
                for kc in range(8):
                    k.mm(psU[q][:], wu[sl][:, kc, fc * 128:(fc + 1) * 128], xsT[:, kc, :], kc == 0, kc == 7,
                         reads=[B_wu[sl], B_xsT[kc]], writes=[B_psU[q]])
                k.op("act", lambda e, q=q: e.activation(out=sg[q][:], in_=psG[q][:], func=AF.Silu),
                     [B_psG[q]], [B_sg[q]])
                k.op("dve", lambda e, q=q, fc=fc: e.tensor_tensor(out=hTe[:, fc, :], in0=sg[q][:], in1=psU[q][:],
                                                                 op=ALU.mult), [B_sg[q], B_psU[q]], [B_hTe[fc]])
                if j + 1 < NBLK:
                    xpose(j + 1, [2 * fc, 2 * fc + 1])
            for t in range(4):
                for dh in range(2):
                    yi = (t * 2 + dh) % 2
                    for fc in range(4):
                        k.mm(psY[yi][:], hTe[:, fc, t * 128:(t + 1) * 128], wd[sl][:, fc, dh * 512:(dh + 1) * 512],
                             fc == 0, fc == 3, reads=[B_hTe[fc], B_wd[sl]], writes=[B_psY[yi]])
                    if yi == 0:
                        k.op("act", lambda e, t=t, dh=dh, sl=sl: e.copy(out=yb[sl][:, t, dh * 512:(dh + 1) * 512],
                                                                       in_=psY[0][:]), [B_psY[0]], [B_yb[sl]])
                    else:
                        k.op("dve", lambda e, t=t, dh=dh, sl=sl: e.tensor_copy(
                            out=yb[sl][:, t, dh * 512:(dh + 1) * 512], in_=psY[1][:]), [B_psY[1]], [B_yb[sl]])
            if j + 2 < NBLK:
                wload(j + 2, sl)
                xload(j + 2, sl)
            k.dma("sp", ys[j * 512:(j + 1) * 512, :].rearrange("(t p) d -> p t d", p=128), yb[sl][:], yb_st[sl],
                  reads=[B_yb[sl]], writes=[B_ys])
        k.s.guard = None
        k.s.barrier()
    if a.get("stage") == "M4":
        return []

    k.s.scope = pfx + "M5_combine"
    fin = a.get("final")
    with contextlib.ExitStack() as c5:
        def sb5(name, shape, dt=F32):
            return c5.enter_context(k.nc.sbuf_tensor("s_" + pfx + name, list(shape), dt))
        NS = 6
        y1 = [sb5(f"y1_{i}", [128, D]) for i in range(NS)]
        y2 = [sb5(f"y2_{i}", [128, D]) for i in range(NS)]
        xc = [sb5(f"xc{i}", [128, D]) for i in range(NS)]
        B_y1, B_y2, B_xc = [Buf() for _ in range(NS)], [Buf() for _ in range(NS)], [Buf() for _ in range(NS)]
        g_sem = [[k.dsem(pfx + f"g{n}{i}", sw=True) for i in range(NS)] for n in "12"]
        xc_ld = [k.dsem(pfx + f"xc_ld{i}") for i in range(NS)]
        xc_st = [k.dsem(pfx + f"xc_st{i}") for i in range(NS)]
        if fin is not None:
            fg = sb5("fg", [128, D])
            fjunk = sb5("fjunk", [128, D], BF16)
            fss = [sb5(f"fss{i}", [128, 1]) for i in range(NS)]
            B_fg, B_fjunk = Buf(), Buf()
            B_fss = [Buf() for _ in range(NS)]
            k.dma("sp", fg[:], fin["fing"][0:1, :].partition_broadcast(128), vl, writes=[B_fg])
        def m5_load(i):
            sl = i % NS
            G = i // 4
            k.s.add("pool", lambda e, sl=sl, i=i: e.indirect_dma_start(
                out=y1[sl][:], out_offset=None, in_=ys[:, :],
                in_offset=bass.IndirectOffsetOnAxis(ap=d1i[:, i:i + 1], axis=0)), [B_ys, B_d1i], [B_y1[sl]],
                dsem=g_sem[0][sl])
            k.s.add("pool", lambda e, sl=sl, i=i: e.indirect_dma_start(
                out=y2[sl][:], out_offset=None, in_=ys[:, :],
                in_offset=bass.IndirectOffsetOnAxis(ap=d2i[:, i:i + 1], axis=0)), [B_ys, B_d2i], [B_y2[sl]],
                dsem=g_sem[1][sl])
            k.dma("sp", xc[sl][:], xr[i * 128:(i + 1) * 128, :], xc_ld[sl], reads=[B_xr[G]], writes=[B_xc[sl]])
        for i in range(min(NS - 1, NT)):
            m5_load(i)
        for i in range(NT):
            sl = i % NS
            b = (i * 128) // S
            G = i // 4
            if i + NS - 1 < NT:
                m5_load(i + NS - 1)
            k.op("act", lambda e, sl=sl, i=i: e.activation(out=y1[sl][:], in_=y1[sl][:], func=AF.Copy,
                                                          scale=Wall[:, i, 0:1]), [B_y1[sl], B_W[i]], [B_y1[sl]])
            k.op("dve", lambda e, sl=sl, i=i: e.scalar_tensor_tensor(out=y1[sl][:], in0=y2[sl][:],
                                                                    scalar=Wall[:, i, 1:2], in1=y1[sl][:],
                                                                    op0=ALU.mult, op1=ALU.add),
                 [B_y1[sl], B_y2[sl], B_W[i]], [B_y1[sl]])
            k.op("dve", lambda e, sl=sl, b=b: e.tensor_tensor(out=y1[sl][:], in0=y1[sl][:], in1=g2bc[b][:],
                                                             op=ALU.mult), [B_y1[sl], B_g2bc[b]], [B_y1[sl]])
            k.op("dve", lambda e, sl=sl: e.tensor_tensor(out=xc[sl][:], in0=xc[sl][:], in1=y1[sl][:], op=ALU.add),
                 [B_xc[sl], B_y1[sl]], [B_xc[sl]])
            if fin is None:
                k.dma("sp", xr[i * 128:(i + 1) * 128, :], xc[sl][:], xc_st[sl], reads=[B_xc[sl]], writes=[B_xr[G]])
            else:
                k.op("act", lambda e, sl=sl: e.activation(out=fjunk[:], in_=xc[sl][:], func=AF.Square,
                                                          accum_out=fss[sl][:, 0:1]), [B_xc[sl]], [B_fjunk, B_fss[sl]])
                k.op("dve", lambda e, sl=sl: e.tensor_scalar(out=fss[sl][:], in0=fss[sl][:], scalar1=1.0 / D,
                                                             scalar2=1e-6, op0=ALU.mult, op1=ALU.add),
                     [B_fss[sl]], [B_fss[sl]])
                k.op("act", lambda e, sl=sl: e.activation(out=fss[sl][:], in_=fss[sl][:], func=AF.Sqrt),
                     [B_fss[sl]], [B_fss[sl]])
                k.op("dve", lambda e, sl=sl: e.reciprocal(out=fss[sl][:], in_=fss[sl][:]), [B_fss[sl]], [B_fss[sl]])
                k.op("dve", lambda e, sl=sl: e.scalar_tensor_tensor(out=xc[sl][:], in0=xc[sl][:], scalar=fss[sl][:, 0:1],
                                                                    in1=fg[:], op0=ALU.mult, op1=ALU.mult),
                     [B_xc[sl], B_fss[sl], B_fg], [B_xc[sl]])
                k.dma("sp", fin["out"][i * 128:(i + 1) * 128, :], xc[sl][:], xc_st[sl], reads=[B_xc[sl]],
                      writes=[fin["B_out"][G]])
        k.s.barrier()


def finish(k, out_bufs):
    k.s.add("sp", None, reads=out_bufs)
    esem = {e: k.sem("e_" + e) for e in ENGS}
    stuck = k.s.simulate()
    assert not stuck, f"schedule deadlock: {stuck}"
    k.s.emit(k.nc, esem)
    k.pctx.close()
    k.ctx.close()
    return k.nc


_RL = {}


def _relayout(w, key, nch):
    if key not in _RL or _RL[key][0] is not w:
        Lr, E, R, N = w.shape
        _RL[key] = (w, np.ascontiguousarray(w.reshape(Lr, E, nch, 128, N).transpose(0, 1, 3, 2, 4)).reshape(
            Lr * E * 128, nch * N))
    return _RL[key][1]


def host_inputs(inp, core, NB, S):
    b0 = core * NB
    f = np.float32
    m = {}
    m["x"] = np.ascontiguousarray(inp["x"][b0:b0 + NB, :S].reshape(NB * S, D))
    c = inp["c"][b0:b0 + NB]
    m["cT"] = np.ascontiguousarray(c.reshape(NB, 8, 128).transpose(2, 1, 0).reshape(128, 8 * NB))
    m["ada_w"] = inp["ada_w"]
    m["ada_b"] = inp["ada_b"]
    m["gmixF"] = np.ascontiguousarray(inp["norm_mix_g"].reshape(2, 8, 128).transpose(0, 2, 1))
    m["gffn"] = inp["norm_ffn_g"]
    m["conv_in_w"] = inp["conv_in_w"][0]
    m["convwF"] = np.ascontiguousarray(inp["conv_w"][0].reshape(3, 8, 128).transpose(2, 1, 0).reshape(128, 24))
    m["conv_out_w"] = inp["conv_out_w"][0]
    m["ident"] = np.eye(128, dtype=f)
    m["tri"] = np.triu(np.ones((128, 128), dtype=f), 1)
    m["Wr"] = np.ascontiguousarray(np.concatenate(
        [inp["router_grp_w"], inp["router_exp_w"].transpose(0, 2, 1, 3).reshape(2, D, 32)], axis=2))
    m["br"] = np.ascontiguousarray(np.concatenate([inp["router_grp_b"], inp["router_exp_b"].reshape(2, 32)], axis=1))
    m["exp_gate_w"] = _relayout(inp["exp_gate_w"], "g", 8)
    m["exp_up_w"] = _relayout(inp["exp_up_w"], "u", 8)
    m["exp_down_w"] = _relayout(inp["exp_down_w"], "d", 4)
    m["pcol"] = np.arange(128, dtype=f).reshape(128, 1)
    m["attn_in_w"] = inp["attn_in_w"][0]
    m["attn_out_w"] = inp["attn_out_w"][0]
    pos = np.arange(S, dtype=f)
    inv = np.power(f(500000.0), -np.arange(0, 16, 2, dtype=f) / f(16)).astype(f)
    ang = pos[None, :] * inv[:, None]
    C = np.ones((128, S), f)
    Sg = np.zeros((128, S), f)
    Rm = np.zeros((128, 128), f)
    for h in range(2):
        for e in range(16):
            C[h * 64 + e] = np.cos(ang[e % 8])
            Sg[h * 64 + e] = (-np.sin(ang[e % 8])) if e < 8 else np.sin(ang[e % 8])
            Rm[h * 64 + (e + 8 if e < 8 else e - 8), h * 64 + e] = 1.0
    m["ropeC"], m["ropeS"] = C, Sg
    kk = np.arange(128)[:, None]
    qq = np.arange(128)[None, :]
    NEG = f(-30000.0)
    Mprev = np.where(kk >= qq, f(0), NEG).astype(f)
    Mcur = np.where(kk <= qq, f(0), NEG).astype(f)
    o0 = np.zeros((128, 128), f)
    o0[:, :64] = 1
    o1 = np.zeros((128, 128), f)
    o1[:, 64:] = 1
    m["aconst"] = np.ascontiguousarray(np.concatenate([Rm, Mprev, Mcur, o0, o1], axis=1))
    m["fing"] = inp["final_norm_g"].reshape(1, D)
    return m


def kernel(**inputs):
    NB, S = 2, 4096
    nc = build(NB, S)
    in_maps = [host_inputs(inputs, c, NB, S) for c in range(NCORES)]
    res = run_bass_kernel_spmd(nc, in_maps, core_ids=list(range(NCORES)))
    out = np.stack([r["out"].reshape(NB, S, D) for r in res.results]).reshape(NCORES * NB, S, D)
    return out.astype(np.float32)
```
